# Optimizing a Trainium2 kernel written in Bass

```python
import math
import jax, jax.numpy as jnp
from jax import lax
import numpy as np

D_MODEL = 1024
BATCH = 4
SEQ = 4096
DEPTH = 1

MEM_TOKENS = 256
EPS = 1e-6
MLA_HEADS = 8
MLA_NOPE = 64
MLA_ROPE = 32
MLA_V = 64
MLA_Q_RANK = 256
MLA_KV_RANK = 128
ROPE_BASE = 10000.0
Q_BLOCK = 128
HY_WIDTH = D_MODEL - MLA_HEADS * MLA_V
HY_ORDER = 2
HY_DIRS = 2
HY_BANDS = 16
HY_EMB = 1 + 2 * HY_BANDS
HY_FFN = 64
HY_TARGET = 1e-2
HY_FAST_PCT = 0.3
HY_SLOW_PCT = 1.5
OFF_Q = 0
OFF_KV = OFF_Q + MLA_Q_RANK
OFF_KR = OFF_KV + MLA_KV_RANK
OFF_HY = OFF_KR + MLA_ROPE
IN_COLS = OFF_HY + 3 * HY_WIDTH
MEM_HEADS = 4
MEM_HEAD_DIM = D_MODEL // MEM_HEADS
N_GROUPS = 4
EXPERTS_PER_GROUP = 8
TOP_K_IN_GROUP = 2
D_EXPERT = 256

kernel_name = "hybrid_mla_hyena_hmoe_encoder"


def _rms(x, g):
    xf = x.astype(jnp.float32)
    y = xf * lax.rsqrt(jnp.mean(xf * xf, axis=-1, keepdims=True) + EPS)
    return (y * g.astype(jnp.float32)).astype(x.dtype)


def _rope(x, cos, sin):
    x1, x2 = jnp.split(x.astype(jnp.float32), 2, axis=-1)
    return jnp.concatenate([x1 * cos - x2 * sin, x1 * sin + x2 * cos], axis=-1).astype(x.dtype)


def _mla(h_q, h_kv, h_kr, q_norm_g, kv_norm_g, w_uq, w_ukv):
    B, S, _ = h_q.shape
    q = (_rms(h_q, q_norm_g) @ w_uq).reshape(B, S, MLA_HEADS, MLA_NOPE + MLA_ROPE)
    q_nope, q_rot = q[..., :MLA_NOPE], q[..., MLA_NOPE:]
    kv = (_rms(h_kv, kv_norm_g) @ w_ukv).reshape(B, S, MLA_HEADS, MLA_NOPE + MLA_V)
    k_nope, v = kv[..., :MLA_NOPE], kv[..., MLA_NOPE:]
    pos = jnp.arange(S, dtype=jnp.float32)
    half = MLA_ROPE // 2
    inv = ROPE_BASE ** (-jnp.arange(half, dtype=jnp.float32) / half)
    ang = pos[:, None] * inv[None, :]
    cos, sin = jnp.cos(ang), jnp.sin(ang)
    q_rot = _rope(q_rot, cos[:, None, :], sin[:, None, :])
    k_rot = _rope(h_kr, cos, sin)
    scale = (MLA_NOPE + MLA_ROPE) ** -0.5
    nb = S // Q_BLOCK

    def to_blocks(t):
        return jnp.moveaxis(t.reshape(B, nb, Q_BLOCK, *t.shape[2:]), 1, 0)

    def attend(blk):
        qn, qr = blk
        s = (jnp.einsum('bqhd,bkhd->bhqk', qn, k_nope)
             + jnp.einsum('bqhr,bkr->bhqk', qr, k_rot)).astype(jnp.float32) * scale
        p = jax.nn.softmax(s, axis=-1).astype(v.dtype)
        return jnp.einsum('bhqk,bkhd->bqhd', p, v)

    o = lax.map(attend, (to_blocks(q_nope), to_blocks(q_rot)))
    return jnp.moveaxis(o, 0, 1).reshape(B, S, MLA_HEADS * MLA_V)


def _hyena_filters(L, w1, b1, freq, w2, b2, w3, b3, decay):
    f32 = jnp.float32
    t = jnp.arange(L, dtype=f32)
    t01 = t / L
    bands = jnp.linspace(1e-4, HY_BANDS - 1, HY_BANDS, dtype=f32)
    ang = 2.0 * math.pi * t[:, None] * bands[None, :] / L
    z = jnp.concatenate([t01[:, None], jnp.cos(ang), -jnp.sin(ang)], axis=-1)
    fr = freq.astype(f32)
    h = jnp.sin(fr[0] * (z @ w1.astype(f32) + b1.astype(f32)))
    h = jnp.sin(fr[1] * (h @ w2.astype(f32) + b2.astype(f32)))
    h = h @ w3.astype(f32) + b3.astype(f32)
    h = h * jnp.exp(-t01[:, None] * jnp.abs(decay.astype(f32))[None, :])
    h = h.reshape(L, HY_ORDER, HY_DIRS, HY_WIDTH)
    fwd, bwd = h[:, :, 0], h[:, :, 1]
    k = jnp.concatenate([fwd, jnp.zeros((1, HY_ORDER, HY_WIDTH), f32), bwd[:0:-1]], axis=0)
    return jnp.fft.rfft(k, axis=0)


def _hyena(u, conv_w, conv_b, kf, skip):
    B, S, _ = u.shape
    up = jnp.pad(u, ((0, 0), (1, 1), (0, 0)))
    u = up[:, :-2] * conv_w[0] + up[:, 1:-1] * conv_w[1] + up[:, 2:] * conv_w[2] + conv_b
    x1, x2, v = jnp.split(u, 3, axis=-1)
    gates = (x1, x2)
    z = v.astype(jnp.float32)
    for n in range(HY_ORDER):
        zf = jnp.fft.rfft(z, n=2 * S, axis=1)
        conv = jnp.fft.irfft(zf * kf[None, :, n], n=2 * S, axis=1)[:, :S]
        z = gates[n].astype(jnp.float32) * (conv + skip[n].astype(jnp.float32) * z)
    return z.astype(u.dtype)


def _mem_xattn(hx, hm, w_mq, w_mkv, w_mo):
    B, S, _ = hx.shape
    M = hm.shape[1]
    q = (hx @ w_mq).reshape(B, S, MEM_HEADS, MEM_HEAD_DIM)
    k, v = jnp.split(hm @ w_mkv, 2, axis=-1)
    k = k.reshape(B, M, MEM_HEADS, MEM_HEAD_DIM)
    v = v.reshape(B, M, MEM_HEADS, MEM_HEAD_DIM)
    s = jnp.einsum('bshd,bmhd->bhsm', q, k).astype(jnp.float32) * MEM_HEAD_DIM ** -0.5
    p = jax.nn.softmax(s, axis=-1).astype(v.dtype)
    o = jnp.einsum('bhsm,bmhd->bshd', p, v).reshape(B, S, MEM_HEADS * MEM_HEAD_DIM)
    return o @ w_mo


def _hier_moe(h, w_rg, b_rg, w_re, b_re, w_gate, w_up, w_down):
    B, S, D = h.shape
    t = h.reshape(B * S, D)
    g_logits = (t @ w_rg).astype(jnp.float32) + b_rg.astype(jnp.float32)
    g_prob = jax.nn.softmax(g_logits, axis=-1)
    _, g_idx = lax.top_k(g_logits, 1)
    p_group = jnp.take_along_axis(g_prob, g_idx, axis=1)
    e_logits = ((t @ w_re).astype(jnp.float32) + b_re.astype(jnp.float32)).reshape(-1, N_GROUPS, EXPERTS_PER_GROUP)
    e_in = jnp.take_along_axis(e_logits, g_idx[:, :, None], axis=1)[:, 0]
    top_v, top_i = lax.top_k(e_in, TOP_K_IN_GROUP)
    p_exp = jax.nn.softmax(top_v, axis=-1)
    w_e = jnp.sum(jax.nn.one_hot(top_i, EXPERTS_PER_GROUP, dtype=jnp.float32) * p_exp[..., None], axis=1)
    combine = ((p_group * w_e)[:, None, :]
               * jax.nn.one_hot(g_idx[:, 0], N_GROUPS, dtype=jnp.float32)[:, :, None]).astype(t.dtype)
    y = jnp.zeros_like(t)
    for g in range(N_GROUPS):
        a = jnp.einsum('td,edf->tef', t, w_gate[g])
        b = jnp.einsum('td,edf->tef', t, w_up[g])
        m = jax.nn.silu(a) * b * combine[:, g, :, None]
        y = y + jnp.einsum('tef,efd->td', m, w_down[g])
    return y.reshape(B, S, D)


def setup_inputs(seed: int = 0) -> dict:
    key = jax.random.key(seed)
    ks = iter(jax.random.split(key, 40))
    f32 = jnp.float32
    L_ = DEPTH

    def nrm(shape, fan_in, scale=1.0):
        return jax.random.normal(next(ks), shape, f32) * (scale * fan_in ** -0.5)

    def gain(shape):
        return 1.0 + 0.02 * jax.random.normal(next(ks), shape, f32)

    def small(shape, s=0.02):
        return s * jax.random.normal(next(ks), shape, f32)

    G, E, F = N_GROUPS, EXPERTS_PER_GROUP, D_EXPERT
    d_min = math.log(1.0 / HY_TARGET) / HY_SLOW_PCT
    d_max = math.log(1.0 / HY_TARGET) / HY_FAST_PCT
    return {
        "x": jax.random.normal(next(ks), (BATCH, SEQ, D_MODEL), f32),
        "mem": jax.random.normal(next(ks), (BATCH, MEM_TOKENS, D_MODEL), f32),
        "mix_norm_g": gain((L_, D_MODEL)),
        "w_in": nrm((L_, D_MODEL, IN_COLS), D_MODEL),
        "q_norm_g": gain((L_, MLA_Q_RANK)),
        "kv_norm_g": gain((L_, MLA_KV_RANK)),
        "w_uq": nrm((L_, MLA_Q_RANK, MLA_HEADS * (MLA_NOPE + MLA_ROPE)), MLA_Q_RANK),
        "w_ukv": nrm((L_, MLA_KV_RANK, MLA_HEADS * (MLA_NOPE + MLA_V)), MLA_KV_RANK),
        "hy_conv_w": nrm((L_, 3, 3 * HY_WIDTH), 3),
        "hy_conv_b": small((L_, 3 * HY_WIDTH)),
        "hy_w1": nrm((L_, HY_EMB, HY_FFN), HY_EMB),
        "hy_b1": small((L_, HY_FFN), 0.1),
        "hy_freq": gain((L_, 2, HY_FFN)),
        "hy_w2": nrm((L_, HY_FFN, HY_FFN), HY_FFN),
        "hy_b2": small((L_, HY_FFN), 0.1),
        "hy_w3": nrm((L_, HY_FFN, HY_ORDER * HY_DIRS * HY_WIDTH), HY_FFN, 0.1),
        "hy_b3": small((L_, HY_ORDER * HY_DIRS * HY_WIDTH), 0.01),
        "hy_decay": jax.random.uniform(next(ks), (L_, HY_ORDER * HY_DIRS * HY_WIDTH), f32, d_min, d_max),
        "hy_skip": jax.random.normal(next(ks), (L_, HY_ORDER, HY_WIDTH), f32),
        "attn_out_g": gain((L_, MLA_HEADS * MLA_V)),
        "hy_out_g": gain((L_, HY_WIDTH)),
        "w_out": nrm((L_, D_MODEL, D_MODEL), D_MODEL),
        "cross_norm_g": gain((L_, D_MODEL)),
        "mem_norm_g": gain((L_, D_MODEL)),
        "w_mq": nrm((L_, D_MODEL, MEM_HEADS * MEM_HEAD_DIM), D_MODEL),
        "w_mkv": nrm((L_, D_MODEL, 2 * MEM_HEADS * MEM_HEAD_DIM), D_MODEL),
        "w_mo": nrm((L_, MEM_HEADS * MEM_HEAD_DIM, D_MODEL), MEM_HEADS * MEM_HEAD_DIM),
        "ffn_norm_g": gain((L_, D_MODEL)),
        "w_route_group": nrm((L_, D_MODEL, G), D_MODEL),
        "b_route_group": small((L_, G), 0.01),
        "w_route_expert": nrm((L_, D_MODEL, G * E), D_MODEL),
        "b_route_expert": small((L_, G * E), 0.01),
        "w_gate": nrm((L_, G, E, D_MODEL, F), D_MODEL),
        "w_up": nrm((L_, G, E, D_MODEL, F), D_MODEL),
        "w_down": nrm((L_, G, E, F, D_MODEL), F),
        "final_norm_g": gain((D_MODEL,)),
    }


def reference(x, mem, mix_norm_g, w_in, q_norm_g, kv_norm_g, w_uq, w_ukv, hy_conv_w, hy_conv_b,
              hy_w1, hy_b1, hy_freq, hy_w2, hy_b2, hy_w3, hy_b3, hy_decay, hy_skip, attn_out_g, hy_out_g,
              w_out, cross_norm_g, mem_norm_g, w_mq, w_mkv, w_mo, ffn_norm_g, w_route_group, b_route_group,
              w_route_expert, b_route_expert, w_gate, w_up, w_down, final_norm_g):
    S = x.shape[1]
    for l in range(DEPTH):
        h = _rms(x, mix_norm_g[l])
        p = h @ w_in[l]
        a_out = _mla(p[..., OFF_Q:OFF_KV], p[..., OFF_KV:OFF_KR], p[..., OFF_KR:OFF_HY],
                     q_norm_g[l], kv_norm_g[l], w_uq[l], w_ukv[l])
        kf = _hyena_filters(S, hy_w1[l], hy_b1[l], hy_freq[l], hy_w2[l], hy_b2[l], hy_w3[l], hy_b3[l], hy_decay[l])
        h_out = _hyena(p[..., OFF_HY:], hy_conv_w[l], hy_conv_b[l], kf, hy_skip[l])
        mixed = jnp.concatenate([_rms(a_out, attn_out_g[l]), _rms(h_out, hy_out_g[l])], axis=-1)
        x = x + mixed @ w_out[l]
        x = x + _mem_xattn(_rms(x, cross_norm_g[l]), _rms(mem, mem_norm_g[l]), w_mq[l], w_mkv[l], w_mo[l])
        x = x + _hier_moe(_rms(x, ffn_norm_g[l]), w_route_group[l], b_route_group[l], w_route_expert[l],
                          b_route_expert[l], w_gate[l], w_up[l], w_down[l])
    return _rms(x, final_norm_g)
```

```python
import math
import os
import contextlib
import numpy as np
import concourse.bass as bass
import concourse.mybir as mybir
from concourse.bass_utils import run_bass_kernel_spmd

F32 = mybir.dt.float32
BF16 = mybir.dt.bfloat16
AF = mybir.ActivationFunctionType
ALU = mybir.AluOpType
AX = mybir.AxisListType
ENGS = ("pe", "act", "dve", "pool", "sp")

D = 1024
S = 4096
OWN = 2048
NT = 32
NTO = 16
EPS = 1e-6
HD = 96
NH = 8


class Op:
    __slots__ = ("eng", "fn", "deps", "dma", "flag", "seq", "idx", "dmaval")

    def __init__(self, eng, fn, dma):
        self.eng = eng
        self.fn = fn
        self.deps = set()
        self.dma = dma
        self.flag = False
        self.seq = 0
        self.dmaval = 0


class Prog:
    ARENA_WORDS = 51200

    def __init__(self, nc):
        self.nc = nc
        self.ops = []
        self.lastw = {}
        self.readers = {}
        self.dma_count = {}
        self.sb_off = 0
        self.sb_marks = []
        self.arena = None

    def sb(self, shape, dtype, name=None):
        if self.arena is None:
            self.arena = self.nc.alloc_sbuf_tensor("arena", [128, self.ARENA_WORDS], F32)
        esz = 4 if dtype == F32 else 2
        nel = int(np.prod(shape[1:]))
        nwords = (nel * esz + 3) // 4
        nwords = (nwords + 15) // 16 * 16
        o = self.sb_off
        self.sb_off += nwords
        assert self.sb_off <= self.ARENA_WORDS, ("SBUF overflow", self.sb_off * 4, name)
        v = self.arena[0:shape[0], o:o + nwords]
        if esz == 2:
            v = v.bitcast(dtype)[:, 0:nel]
        else:
            v = v[:, 0:nel]
        if len(shape) > 2:
            names = " ".join("a%d" % i for i in range(len(shape) - 1))
            kw = {"a%d" % i: int(shape[i + 1]) for i in range(len(shape) - 1)}
            v = v.rearrange("p (%s) -> p %s" % (names, names), **kw)
        return v

    def mark(self):
        self.sb_marks.append(self.sb_off)

    def release(self):
        self.sb_off = self.sb_marks.pop()

    def add(self, eng, fn, R=(), W=(), dma=None):
        op = Op(eng, fn, dma)
        op.idx = len(self.ops)
        if eng != "pe":
            psr = [r for r in R if isinstance(r, str) and r.startswith("ps") and r[2:].isdigit()]
            if psr:
                R = [r for r in R if r not in psr]
                W = list(W) + psr
        deps = set()
        for r in R:
            lw = self.lastw.get(r)
            if lw is not None:
                deps.add(lw)
        for w in W:
            lw = self.lastw.get(w)
            if lw is not None:
                deps.add(lw)
            for rd in self.readers.get(w, ()):
                deps.add(rd)
        if dma is not None:
            k = ("__dmasem", dma)
            lw = self.lastw.get(k)
            if lw is not None:
                deps.add(lw)
            self.lastw[k] = op
            self.dma_count[dma] = self.dma_count.get(dma, 0) + 1
            op.dmaval = 16 * self.dma_count[dma]
        deps.discard(op)
        for d in deps:
            if d.dma is None and d.eng == "pe" and eng == "pe" and dma is None:
                continue
            op.deps.add(d)
            d.flag = True
        for r in R:
            self.readers.setdefault(r, []).append(op)
        for w in W:
            self.lastw[w] = op
            self.readers[w] = []
        self.ops.append(op)
        return op

    def barrier(self):
        fr = {}
        dmas = set()
        allops = set(self.lastw.values())
        for v in self.readers.values():
            allops.update(v)
        for o in allops:
            if o.dma is not None:
                dmas.add(o)
            elif o.eng not in fr or fr[o.eng].idx < o.idx:
                fr[o.eng] = o
        for e in ENGS:
            op = Op(e, None, None)
            op.idx = len(self.ops)
            for d in list(fr.values()) + list(dmas):
                op.deps.add(d)
                d.flag = True
            self.ops.append(op)
        newlw = {}
        for k, v in self.lastw.items():
            if isinstance(k, tuple) and k and k[0] == "__dmasem":
                newlw[k] = v
        self.lastw = newlw
        self.readers = {}

    def emit(self):
        nc = self.nc
        with contextlib.ExitStack() as st:
            esem = {e: st.enter_context(nc.semaphore("s_" + e)) for e in ENGS}
            dsem = {}
            for k in self.dma_count:
                dsem[k] = st.enter_context(nc.semaphore("d_%d" % len(dsem)))
            cnt = {e: 0 for e in ENGS}
            for op in self.ops:
                if op.dma is None and op.flag:
                    cnt[op.eng] += 1
                    op.seq = cnt[op.eng]
            byeng = {e: [o for o in self.ops if o.eng == e] for e in ENGS}
            if os.environ.get("KDEBUG"):
                print("sem counts", cnt, "ndma sems", len(dsem), "nops", {e: len(v) for e, v in byeng.items()})
            block = st.enter_context(nc.Block())

            def run(e, eng):
                waited = {}
                for op in byeng[e]:
                    need = {}
                    for d in op.deps:
                        if d.dma is not None:
                            s, v = dsem[d.dma], d.dmaval
                        else:
                            s, v = esem[d.eng], d.seq
                        key = id(s)
                        if waited.get(key, 0) >= v:
                            continue
                        if key not in need or need[key][1] < v:
                            need[key] = (s, v)
                    for key, (s, v) in need.items():
                        eng.wait_ge(s, v)
                        waited[key] = v
                    if op.fn is None:
                        continue
                    ins = op.fn(eng)
                    if op.dma is not None:
                        ins.then_inc(dsem[op.dma], 16)
                    elif op.flag:
                        ins.then_inc(esem[e], 1)

            @block.tensor
            def _(eng):
                run("pe", eng)

            @block.scalar
            def _(eng):
                run("act", eng)

            @block.vector
            def _(eng):
                run("dve", eng)

            @block.gpsimd
            def _(eng):
                run("pool", eng)

            @block.sync
            def _(eng):
                run("sp", eng)


INPUT_SHAPES = {
    "x_rot": [S, D], "mem_b": [256, D],
    "mix_norm_g": [1, D], "w_in": [D, 1952], "q_norm_g": [1, 256], "kv_norm_g": [1, 128],
    "w_uq": [256, 768], "w_ukv": [128, 1024], "hy_conv_w": [3, 1536], "hy_conv_b": [1, 1536],
    "attn_out_g": [1, 512], "hy_out_g": [1, 512], "w_out": [D, D],
    "cross_norm_g": [1, D], "mem_norm_g": [1, D], "w_mq": [D, D], "w_mkv": [D, 2 * D], "w_mo": [D, D],
    "ffn_norm_g": [1, D], "w_route": [D, 36], "b_route": [1, 36],
    "w_gate": [32, D, 256], "w_up": [32, D, 256], "w_down": [32, 256, D], "final_norm_g": [1, D],
    "ident": [128, 128], "rope_cs": [128, NT * 32], "halfmask": [128, 2],
    "hy_D0": [64, 64 * 128], "hy_Dc": [64, 64 * 128], "hy_F2a": [128, 128], "hy_F2b": [128, 128], "hy_G": [128, 128],
    "hy_Dinv": [128, 64 * 64], "hy_sgn": [128, 1], "hy_zT": [33, S], "hy_nt01": [128, NT],
    "hy_cols": [64, 4], "hy_w1": [33, 64], "hy_w2": [64, 64], "hy_w3": [64, 2048], "hy_b3": [1, 2048], "hy_decay": [1, 2048],
    "hy_skip": [1, 1024],
}


def build(stop=None, dbg=()):
    nc = bass.Bass("TRN2", target_bir_lowering=False)
    I = {k: nc.dram_tensor(k, v, F32, kind="ExternalInput").ap() for k, v in INPUT_SHAPES.items()}
    out_d = nc.dram_tensor("out", [OWN, D], F32, kind="ExternalOutput").ap()
    dbg_d = {}
    U_d = nc.dram_tensor("U_scr", [S, 1536], F32, kind="Internal").ap()
    hout_d = nc.dram_tensor("hout_scr", [OWN, 512], F32, kind="Internal").ap()
    combT_d = nc.dram_tensor("combT_scr", [32, OWN], F32, kind="Internal").ap()

    P = Prog(nc)
    ps = [nc.alloc_psum_tensor("ps%d" % i, [128, 512], F32) for i in range(8)]
    psk = ["ps%d" % i for i in range(8)]

    def psb(i):
        return ps[i][:].bitcast(BF16)

    def dbg_out(name, shape):
        t = nc.dram_tensor("dbg_" + name, shape, F32, kind="ExternalOutput").ap()
        dbg_d[name] = t
        return t

    cnt = [0]

    def uid(s):
        cnt[0] += 1
        return "%s_%d" % (s, cnt[0])

    identf = P.sb([128, 128], F32, "identf")
    identb = P.sb([128, 128], BF16, "identb")
    halfm = P.sb([128, 2], F32, "halfm")
    st = P.sb([128, 8], F32, "st")
    junk = P.sb([128, 1024], F32, "junk")
    gb = P.sb([128, 1024], F32, "gb")
    onesb = P.sb([128, 128], BF16, "onesb")
    onesf = P.sb([128, 128], F32, "onesf")
    P.add("sp", lambda e: e.dma_start(out=identf, in_=I["ident"]), W=["identf"], dma="c0")
    P.add("sp", lambda e: e.dma_start(out=halfm, in_=I["halfmask"]), W=["halfm"], dma="c1")
    P.add("dve", lambda e: e.tensor_copy(out=identb, in_=identf), R=["identf"], W=["identb"])
    P.add("pool", lambda e: e.memset(onesb, 1.0), W=["onesb"])
    P.add("pool", lambda e: e.memset(onesf, 1.0), W=["onesf"])

    def load_gain(name, n=D, key="gb"):
        P.add("sp", lambda e: e.dma_start(out=gb[:, 0:n], in_=I[name].partition_broadcast(128)), W=[key], dma="gain")

    st_tiles = {}
    for _n in ("st", "stq", "stk", "sta", "sth", "stm", "stx", "stf", "stg"):
        st_tiles[_n] = P.sb([128, 4], F32, "st_" + _n)

    def rms_norm(src, n, gview, out_bf, Rk, Wk, stk="st"):
        if stk not in st_tiles:
            st_tiles[stk] = P.sb([128, 4], F32, "st_" + stk)
        st = st_tiles[stk]
        P.add("act", lambda e: e.activation(out=junk[:, 0:n], in_=src, func=AF.Square, accum_out=st[:, 0:1]),
              R=Rk, W=["junk", stk + "0"])
        P.add("dve", lambda e: e.tensor_scalar(out=st[:, 1:2], in0=st[:, 0:1], scalar1=1.0 / n, scalar2=EPS,
                                               op0=ALU.mult, op1=ALU.add), R=[stk + "0"], W=[stk + "1"])
        P.add("act", lambda e: e.sqrt(out=st[:, 2:3], in_=st[:, 1:2]), R=[stk + "1"], W=[stk + "2"])
        P.add("dve", lambda e: e.reciprocal(out=st[:, 3:4], in_=st[:, 2:3]), R=[stk + "2"], W=[stk + "3"])
        P.add("dve", lambda e: e.scalar_tensor_tensor(out=out_bf, in0=src, scalar=st[:, 3:4], in1=gview,
                                                      op0=ALU.mult, op1=ALU.mult),
              R=list(Rk) + [stk + "3", "gb"], W=Wk)

    P.mark()
    hqT = P.sb([128, 2, OWN], BF16, "hqT")
    hkvT = P.sb([128, S], BF16, "hkvT")
    krot = P.sb([128, NT, 32], F32, "krot")
    ropecs = P.sb([128, NT, 32], F32, "ropecs")
    P.add("sp", lambda e: e.dma_start(out=ropecs.rearrange("p a b -> p (a b)"), in_=I["rope_cs"]), W=["ropecs"], dma="c2")

    P.mark()
    hT = P.sb([128, 8, 2, OWN + 2], BF16, "hT")
    xt = [P.sb([128, D], F32, "xt%d" % i) for i in range(2)]
    xn = [P.sb([128, D], BF16, "xn%d" % i) for i in range(2)]
    load_gain("mix_norm_g")
    for i in range(NT):
        b = i % 2
        seg, j = divmod(i, NTO)
        P.add("sp", lambda e, i=i, b=b: e.dma_start(out=xt[b], in_=I["x_rot"][i * 128:(i + 1) * 128, :]),
              W=["xt%d" % b], dma="xt%d" % b)
        rms_norm(xt[b], D, gb, xn[b], ["xt%d" % b], ["xn%d" % b])
        pb = 0 + b
        for k in range(8):
            P.add("pe", lambda e, k=k, b=b, pb=pb: e.transpose(out=psb(pb)[:, k * 128:(k + 1) * 128],
                                                                 in_=xn[b][:, k * 128:(k + 1) * 128], identity=identb),
                  R=["xn%d" % b, "identb"], W=[psk[pb]])
        eng = "act" if b == 0 else "dve"
        dst = hT[:, :, seg, 1 + j * 128:1 + (j + 1) * 128]
        src = psb(pb).rearrange("p (k t) -> p k t", k=8)
        if eng == "act":
            P.add("act", lambda e, dst=dst, src=src: e.copy(out=dst, in_=src), R=[psk[pb]], W=["hT"])
        else:
            P.add("dve", lambda e, dst=dst, src=src: e.tensor_copy(out=dst, in_=src), R=[psk[pb]], W=["hT"])
    for (ds, dc, ss_, sc, m) in ((0, 0, 1, OWN, 0), (0, OWN + 1, 1, 1, 1), (1, 0, 0, OWN, 1), (1, OWN + 1, 0, 1, 0)):
        P.add("dve", lambda e, ds=ds, dc=dc, ss_=ss_, sc=sc, m=m: e.tensor_scalar_mul(
            out=hT[:, :, ds, dc:dc + 1], in0=hT[:, :, ss_, sc:sc + 1], scalar1=halfm[:, m:m + 1]),
            R=["hT", "halfm"], W=["hT"])

    w_mla = P.sb([128, 8, 416], BF16, "w_mla")
    P.add("pool", lambda e: e.dma_start(out=w_mla, in_=I["w_in"][:, 0:416].rearrange("(k p) n -> p k n", p=128)),
          W=["w_mla"], dma="w0")
    gq = P.sb([128, 256], F32, "gq")
    gkv = P.sb([128, 128], F32, "gkv")
    P.add("sp", lambda e: e.dma_start(out=gq, in_=I["q_norm_g"].partition_broadcast(128)), W=["gq"], dma="c3")
    P.add("sp", lambda e: e.dma_start(out=gkv, in_=I["kv_norm_g"].partition_broadcast(128)), W=["gkv"], dma="c4")
    hqn = [P.sb([128, 256], BF16, "hqn%d" % i) for i in range(2)]
    hkvn = [P.sb([128, 128], BF16, "hkvn%d" % i) for i in range(2)]
    tmp16 = P.sb([128, 4, 16], F32, "tmp16")
    for i in range(NT):
        b = i % 2
        seg, j = divmod(i, NTO)
        pb = 2 + b
        for k in range(8):
            P.add("pe", lambda e, k=k, pb=pb, seg=seg, j=j: e.matmul(
                ps[pb][:, 0:416], lhsT=hT[:, k, seg, 1 + j * 128:1 + (j + 1) * 128], rhs=w_mla[:, k, :],
                start=(k == 0), stop=(k == 7)), R=["hT", "w_mla"], W=[psk[pb]])
        if seg == 0:
            rms_norm(ps[pb][:, 0:256], 256, gq, hqn[b], [psk[pb], "gq"], ["hqn%d" % b], stk="stq")
            pt = 4 + b
            for k in range(2):
                P.add("pe", lambda e, k=k, b=b, pt=pt: e.transpose(out=psb(pt)[:, k * 128:(k + 1) * 128],
                                                                     in_=hqn[b][:, k * 128:(k + 1) * 128], identity=identb),
                      R=["hqn%d" % b, "identb"], W=[psk[pt]])
            P.add("act", lambda e, pt=pt, j=j: e.copy(out=hqT[:, :, j * 128:(j + 1) * 128],
                                                       in_=psb(pt)[:, 0:256].rearrange("p (k t) -> p k t", k=2)),
                  R=[psk[pt]], W=["hqT"])
        rms_norm(ps[pb][:, 256:384], 128, gkv, hkvn[b], [psk[pb], "gkv"], ["hkvn%d" % b], stk="stk")
        pt = 6 + b
        P.add("pe", lambda e, b=b, pt=pt: e.transpose(out=psb(pt)[:, 0:128], in_=hkvn[b], identity=identb),
              R=["hkvn%d" % b, "identb"], W=[psk[pt]])
        P.add("act", lambda e, pt=pt, i=i: e.copy(out=hkvT[:, i * 128:(i + 1) * 128], in_=psb(pt)[:, 0:128]),
              R=[psk[pt]], W=["hkvT"])
        x1 = ps[pb][:, 384:400]
        x2 = ps[pb][:, 400:416]
        c = ropecs[:, i, 0:16]
        s_ = ropecs[:, i, 16:32]
        P.add("dve", lambda e, x1=x1, c=c: e.tensor_tensor(out=tmp16[:, 0, :], in0=x1, in1=c, op=ALU.mult), R=[psk[pb], "ropecs"], W=["t16a"])
        P.add("dve", lambda e, x2=x2, s_=s_: e.tensor_tensor(out=tmp16[:, 1, :], in0=x2, in1=s_, op=ALU.mult), R=[psk[pb], "ropecs"], W=["t16b"])
        P.add("dve", lambda e, x1=x1, s_=s_: e.tensor_tensor(out=tmp16[:, 2, :], in0=x1, in1=s_, op=ALU.mult), R=[psk[pb], "ropecs"], W=["t16c"])
        P.add("dve", lambda e, x2=x2, c=c: e.tensor_tensor(out=tmp16[:, 3, :], in0=x2, in1=c, op=ALU.mult), R=[psk[pb], "ropecs"], W=["t16d"])
        P.add("dve", lambda e, i=i: e.tensor_tensor(out=krot[:, i, 0:16], in0=tmp16[:, 0, :], in1=tmp16[:, 1, :], op=ALU.subtract),
              R=["t16a", "t16b"], W=["krot"])
        P.add("dve", lambda e, i=i: e.tensor_tensor(out=krot[:, i, 16:32], in0=tmp16[:, 2, :], in1=tmp16[:, 3, :], op=ALU.add),
              R=["t16c", "t16d"], W=["krot"])

    w_hy = P.sb([128, 8, 512], BF16, "w_hy")
    w_k3 = P.sb([128, 3, 8, 512], BF16, "w_k3")
    cw = P.sb([128, 3, 512], F32, "cw")
    brow = P.sb([1, 512], BF16, "brow")
    uo = [P.sb([128, 512], F32, "uo%d" % i) for i in range(2)]
    for c3 in range(3):
        c0 = 416 + c3 * 512
        P.add("pool", lambda e, c0=c0: e.dma_start(out=w_hy, in_=I["w_in"][:, c0:c0 + 512].rearrange("(k p) n -> p k n", p=128)),
              W=["w_hy"], dma="w1")
        for k3 in range(3):
            P.add("sp", lambda e, k3=k3, c3=c3: e.dma_start(
                out=cw[:, k3, :], in_=I["hy_conv_w"][k3:k3 + 1, c3 * 512:(c3 + 1) * 512].partition_broadcast(128)),
                W=["cw%d" % k3], dma="cw%d" % k3)
        P.add("pool", lambda e, c3=c3: e.dma_start(out=brow, in_=I["hy_conv_b"][0:1, c3 * 512:(c3 + 1) * 512]),
              W=["brow"], dma="w2")
        for k3 in range(3):
            for k in range(8):
                P.add("dve" if k % 2 == 0 else "pool", lambda e, k3=k3, k=k: e.tensor_tensor(
                    out=w_k3[:, k3, k, :], in0=w_hy[:, k, :], in1=cw[:, k3, :], op=ALU.mult),
                    R=["w_hy", "cw%d" % k3], W=["w_k3_%d_%d" % (k3, k)])
        for i in range(NT):
            b = i % 2
            seg, j = divmod(i, NTO)
            pb = 2 + b
            n = 0
            for k3 in range(3):
                for k in range(8):
                    P.add("pe", lambda e, k3=k3, k=k, pb=pb, seg=seg, j=j, n=n: e.matmul(
                        ps[pb][:, :], lhsT=hT[:, k, seg, k3 + j * 128:k3 + (j + 1) * 128], rhs=w_k3[:, k3, k, :],
                        start=(n == 0), stop=False), R=["hT", "w_k3_%d_%d" % (k3, k)], W=[psk[pb]])
                    n += 1
            P.add("pe", lambda e, pb=pb: e.matmul(ps[pb][:, :], lhsT=onesb[0:1, 0:128], rhs=brow[0:1, :], start=False, stop=True),
                  R=["onesb", "brow"], W=[psk[pb]])
            if b == 0:
                P.add("act", lambda e, pb=pb, b=b: e.copy(out=uo[b], in_=ps[pb][:, :]), R=[psk[pb]], W=["uo%d" % b])
            else:
                P.add("dve", lambda e, pb=pb, b=b: e.tensor_copy(out=uo[b], in_=ps[pb][:, :]), R=[psk[pb]], W=["uo%d" % b])
            P.add("sp", lambda e, i=i, b=b, c3=c3: e.dma_start(out=U_d[i * 128:(i + 1) * 128, c3 * 512:(c3 + 1) * 512], in_=uo[b]),
                  R=["uo%d" % b], W=["U_d"], dma="uo%d" % b)
    P.barrier()
    P.release()

    if stop == "A0":
        P.add("sp", None, R=[])
        P.emit()
        return nc, dbg_d
    if "uc" in dbg:
        tu = dbg_out("uc", [S, 1536])
        P.mark()
        tb = P.sb([128, 1536], F32, "dbgt")
        for i in range(NT):
            P.add("sp", lambda e, i=i: e.dma_start(out=tb, in_=U_d[i * 128:(i + 1) * 128, :]), R=["U_d"], W=["dbgt"], dma="dbg0")
            P.add("sp", lambda e, i=i: e.dma_start(out=tu[i * 128:(i + 1) * 128, :], in_=tb), R=["dbgt"], W=["dbgo"], dma="dbg1")
        P.barrier()
        P.release()

    if stop == "A":
        P.add("sp", None, R=["dbgo"])
        P.emit()
        return nc, dbg_d
    aout_d = nc.dram_tensor("aout_scr", [OWN, 512], F32, kind="Internal").ap()
    P.mark()
    G4 = 4
    KT = P.sb([128, G4, S], BF16, "KT")
    QT = P.sb([128, G4, OWN], BF16, "QT")
    Vaug = P.sb([128, NT, G4, 68], BF16, "Vaug")
    w_ukv = P.sb([128, 1024], BF16, "w_ukv")
    w_uq = P.sb([128, 2, 768], BF16, "w_uq")
    P.add("pool", lambda e: e.dma_start(out=w_ukv, in_=I["w_ukv"]), W=["w_ukv"], dma="w0")
    P.add("pool", lambda e: e.dma_start(out=w_uq, in_=I["w_uq"].rearrange("(k p) n -> p k n", p=128)), W=["w_uq"], dma="w1")
    Kaug = [P.sb([128, G4, 100], BF16, "Kaug%d" % i) for i in range(2)]
    Qaug = [P.sb([128, G4, 100], BF16, "Qaug%d" % i) for i in range(2)]
    ksq = P.sb([128, G4, 96], F32, "ksq")
    kn2 = P.sb([128, G4], F32, "kn2")
    kmax = P.sb([128, G4], F32, "kmax")
    kb = P.sb([128, 4], F32, "kb")
    qs = P.sb([128, G4, 96], F32, "qs")
    qsq = P.sb([128, G4, 96], F32, "qsq")
    qn = P.sb([128, G4], F32, "qn")
    qt4 = P.sb([128, 4, G4, 16], F32, "qt4")
    PT = [P.sb([128, 512], BF16, "PT%d" % i) for i in range(3)]
    oTs = P.sb([65, 512], F32, "oTs")
    rden = P.sb([128, 4], F32, "rden")
    astage = [P.sb([128, 4, 256], F32, "astage%d" % i) for i in range(2)]
    scale = HD ** -0.5
    it = 0
    for g in range(2):
        P.add("pool", lambda e: e.memset(Vaug.rearrange("p a b c -> p (a b c)"), 1.0), W=["Vaug"])
        for b in range(2):
            P.add("pool", lambda e, b=b: e.memset(Kaug[b].rearrange("p a b -> p (a b)"), 1.0), W=["Kaug%d" % b])
        P.add("pool", lambda e: e.memset(kmax, 0.0), W=["kmax"])
        for i in range(NT):
            b = i % 2
            pbk = 0 + b
            P.add("pe", lambda e, g=g, pbk=pbk, i=i: e.matmul(ps[pbk][:, :], lhsT=hkvT[:, i * 128:(i + 1) * 128],
                                                               rhs=w_ukv[:, g * 512:(g + 1) * 512], start=True, stop=True),
                  R=["hkvT", "w_ukv"], W=[psk[pbk]])
            v = ps[pbk][:, :].rearrange("p (h c) -> p h c", h=4)
            KCUT = int(os.environ.get("KCUT", "9"))
            if KCUT < 1:
                continue
            P.add("act", lambda e, v=v, i=i: e.copy(out=Vaug[:, i, :, 0:64], in_=v[:, :, 64:128]), R=[psk[pbk]], W=["Vaug", "ser%d" % pbk])
            P.add("dve", lambda e, v=v, b=b: e.tensor_copy(out=Kaug[b][:, :, 0:64], in_=v[:, :, 0:64]), R=[psk[pbk], "ser%d" % pbk], W=["Kaug%d" % b])
            if KCUT < 2:
                continue
            for h in range(G4):
                P.add("pool", lambda e, h=h, b=b, i=i: e.tensor_copy(out=Kaug[b][:, h, 64:96], in_=krot[:, i, :]),
                      R=["krot"], W=["Kaug%d" % b])
            if KCUT < 3:
                continue
            P.add("dve", lambda e, b=b: e.tensor_tensor(out=ksq, in0=Kaug[b][:, :, 0:96], in1=Kaug[b][:, :, 0:96], op=ALU.mult),
                  R=["Kaug%d" % b], W=["ksq"])
            P.add("dve", lambda e: e.tensor_reduce(out=kn2, in_=ksq, axis=AX.X, op=ALU.add), R=["ksq"], W=["kn2"])
            P.add("dve", lambda e: e.tensor_tensor(out=kmax, in0=kmax, in1=kn2, op=ALU.max), R=["kn2", "kmax"], W=["kmax"])
            if KCUT < 4:
                continue
            pt = 4 + b
            for h in range(G4):
                P.add("pe", lambda e, h=h, b=b, pt=pt: e.transpose(out=psb(pt)[0:97, h * 128:(h + 1) * 128], in_=Kaug[b][:, h, 0:97], identity=identb),
                      R=["Kaug%d" % b, "identb"], W=[psk[pt]])
            P.add("act", lambda e, pt=pt, i=i: e.copy(out=KT[0:97, :, i * 128:(i + 1) * 128],
                                                       in_=psb(pt)[0:97, 0:G4 * 128].rearrange("p (h t) -> p h t", h=G4)),
                  R=[psk[pt]], W=["KT"])
        if KCUT < 5:
            P.add("sp", None, R=[])
            P.barrier()
            P.emit()
            return nc, dbg_d
        P.add("dve", lambda e: e.tensor_reduce(out=kb[:, 1:2], in_=kmax, axis=AX.X, op=ALU.max), R=["kmax"], W=["kb1"])
        P.add("pe", lambda e: e.transpose(out=ps[6][0:1, 0:128], in_=kb[:, 1:2], identity=identf), R=["kb1", "identf"], W=[psk[6]])
        P.add("dve", lambda e: e.tensor_reduce(out=kb[0:1, 2:3], in_=ps[6][0:1, 0:128], axis=AX.X, op=ALU.max), R=[psk[6]], W=["kb2"])
        P.add("pe", lambda e: e.matmul(ps[7][:, 0:1], lhsT=onesf[0:1, 0:128], rhs=kb[0:1, 2:3], start=True, stop=True),
              R=["kb2", "onesf"], W=[psk[7]])
        P.add("act", lambda e: e.sqrt(out=kb[:, 0:1], in_=ps[7][:, 0:1]), R=[psk[7]], W=["kb0"])
        if stop == "K":
            P.add("sp", None, R=[])
            P.barrier()
            P.emit()
            return nc, dbg_d
        for j in range(NTO):
            b = j % 2
            pa = 0 + b
            for k in range(2):
                P.add("pe", lambda e, pa=pa, k=k, j=j, g=g: e.matmul(
                    ps[pa][:, 0:384], lhsT=hqT[:, k, j * 128:(j + 1) * 128], rhs=w_uq[:, k, g * 384:(g + 1) * 384],
                    start=(k == 0), stop=(k == 1)), R=["hqT", "w_uq"], W=[psk[pa]])
            P.add("act", lambda e, pa=pa: e.mul(out=qs, in_=ps[pa][:, 0:384].rearrange("p (h c) -> p h c", h=G4), mul=scale),
                  R=[psk[pa]], W=["qs"])
            c = ropecs[:, j:j + 1, 0:16].broadcast_to([128, G4, 16])
            s_ = ropecs[:, j:j + 1, 16:32].broadcast_to([128, G4, 16])
            x1 = qs[:, :, 64:80]
            x2 = qs[:, :, 80:96]
            P.add("dve", lambda e, x1=x1, c=c: e.tensor_tensor(out=qt4[:, 0], in0=x1, in1=c, op=ALU.mult), R=["qs", "ropecs"], W=["qt4a"])
            P.add("dve", lambda e, x2=x2, s_=s_: e.tensor_tensor(out=qt4[:, 1], in0=x2, in1=s_, op=ALU.mult), R=["qs", "ropecs"], W=["qt4b"])
            P.add("pool", lambda e, x1=x1, s_=s_: e.tensor_tensor(out=qt4[:, 2], in0=x1, in1=s_, op=ALU.mult), R=["qs", "ropecs"], W=["qt4c"])
            P.add("pool", lambda e, x2=x2, c=c: e.tensor_tensor(out=qt4[:, 3], in0=x2, in1=c, op=ALU.mult), R=["qs", "ropecs"], W=["qt4d"])
            P.add("dve", lambda e: e.tensor_tensor(out=qs[:, :, 64:80], in0=qt4[:, 0], in1=qt4[:, 1], op=ALU.subtract),
                  R=["qt4a", "qt4b"], W=["qs"])
            P.add("dve", lambda e: e.tensor_tensor(out=qs[:, :, 80:96], in0=qt4[:, 2], in1=qt4[:, 3], op=ALU.add),
                  R=["qt4c", "qt4d"], W=["qs"])
            P.add("dve", lambda e: e.tensor_tensor(out=qsq, in0=qs, in1=qs, op=ALU.mult), R=["qs"], W=["qsq"])
            P.add("dve", lambda e: e.tensor_reduce(out=qn, in_=qsq, axis=AX.X, op=ALU.add), R=["qsq"], W=["qn"])
            P.add("act", lambda e: e.sqrt(out=qn, in_=qn), R=["qn"], W=["qn"])
            P.add("dve", lambda e, b=b: e.tensor_scalar(out=Qaug[b][:, :, 96:97], in0=qn.rearrange("p (h o) -> p h o", o=1),
                                                        scalar1=kb[:, 0:1], scalar2=-1.0, op0=ALU.mult, op1=ALU.mult),
                  R=["qn", "kb0"], W=["Qaug%d" % b])
            P.add("act", lambda e, b=b: e.copy(out=Qaug[b][:, :, 0:96], in_=qs), R=["qs"], W=["Qaug%d" % b])
            pt = 4 + b
            for h in range(G4):
                P.add("pe", lambda e, h=h, b=b, pt=pt: e.transpose(out=psb(pt)[0:97, h * 128:(h + 1) * 128], in_=Qaug[b][:, h, 0:97], identity=identb),
                      R=["Qaug%d" % b, "identb"], W=[psk[pt]])
            P.add("dve", lambda e, pt=pt, j=j: e.tensor_copy(out=QT[0:97, :, j * 128:(j + 1) * 128],
                                                             in_=psb(pt)[0:97, 0:G4 * 128].rearrange("p (h t) -> p h t", h=G4)),
                  R=[psk[pt]], W=["QT"])
        if stop == "Q":
            P.add("sp", None, R=[])
            P.barrier()
            P.emit()
            return nc, dbg_d
        for qc in range(4):
            sb_ = qc % 2
            for h in range(G4):
                po = 6 + h % 2
                for kt in range(NT):
                    pb_ = it % 3
                    P.add("pe", lambda e, h=h, qc=qc, kt=kt, pb_=pb_: e.matmul(
                        ps[pb_][:, :], lhsT=KT[0:97, h, kt * 128:(kt + 1) * 128], rhs=QT[0:97, h, qc * 512:(qc + 1) * 512],
                        start=True, stop=True), R=["KT", "QT"], W=[psk[pb_]])
                    P.add("act", lambda e, pb_=pb_: e.activation(out=PT[pb_], in_=ps[pb_][:, :], func=AF.Exp),
                          R=[psk[pb_]], W=["PT%d" % pb_])
                    P.add("pe", lambda e, h=h, kt=kt, pb_=pb_, po=po: e.matmul(
                        ps[po][0:65, :], lhsT=Vaug[:, kt, h, 0:65], rhs=PT[pb_], start=(kt == 0), stop=(kt == NT - 1)),
                        R=["Vaug", "PT%d" % pb_], W=[psk[po]])
                    it += 1
                P.add("dve", lambda e, po=po: e.tensor_copy(out=oTs, in_=ps[po][0:65, :]), R=[psk[po]], W=["oTs"])
                for t4 in range(4):
                    pt = 3 + (t4 % 2)
                    P.add("pe", lambda e, t4=t4, pt=pt: e.transpose(out=ps[pt][:, 0:65], in_=oTs[:, t4 * 128:(t4 + 1) * 128], identity=identf[0:65, 0:65]),
                          R=["oTs", "identf"], W=[psk[pt]])
                    P.add("dve", lambda e, pt=pt, t4=t4: e.reciprocal(out=rden[:, t4:t4 + 1], in_=ps[pt][:, 64:65]), R=[psk[pt]], W=["rden%d" % t4])
                    P.add("dve", lambda e, pt=pt, t4=t4, h=h, sb_=sb_: e.tensor_scalar_mul(
                        out=astage[sb_][:, t4, h * 64:(h + 1) * 64], in0=ps[pt][:, 0:64], scalar1=rden[:, t4:t4 + 1]),
                        R=[psk[pt], "rden%d" % t4], W=["astage%d" % sb_])
            P.add("sp", lambda e, qc=qc, g=g, sb_=sb_: e.dma_start(
                out=aout_d[qc * 512:(qc + 1) * 512, g * 256:(g + 1) * 256].rearrange("(t p) c -> p t c", p=128), in_=astage[sb_]),
                R=["astage%d" % sb_], W=["aout_d"], dma="ast%d" % sb_)
    P.barrier()
    P.release()
    P.release()

    if "a_out" in dbg:
        ta = dbg_out("a_out", [OWN, 512])
        P.mark()
        tba = P.sb([128, NTO, 512], F32, "dbgt2")
        P.add("sp", lambda e: e.dma_start(out=tba, in_=aout_d.rearrange("(j p) c -> p j c", p=128)), R=["aout_d"], W=["dbgt2"], dma="dbg0")
        P.add("sp", lambda e: e.dma_start(out=ta.rearrange("(j p) c -> p j c", p=128), in_=tba), R=["dbgt2"], W=["dbgo"], dma="dbg1")
        P.barrier()
        P.release()

    if stop == "attn":
        P.add("sp", None, R=["dbgo"])
        P.emit()
        return nc, dbg_d

    if "hout_in" in dbg:
        hin = nc.dram_tensor("dbg_hout_in", [OWN, 512], F32, kind="ExternalInput").ap()
        P.mark()
        tbh = P.sb([128, NTO, 512], F32, "tbh")
        P.add("sp", lambda e: e.dma_start(out=tbh, in_=hin.rearrange("(j p) c -> p j c", p=128)), W=["tbh"], dma="dbg0")
        P.add("sp", lambda e: e.dma_start(out=hout_d.rearrange("(j p) c -> p j c", p=128), in_=tbh), R=["tbh"], W=["hout_d"], dma="dbg1")
        P.barrier()
        P.release()
    else:
        hyena_phase(nc, P, I, ps, psk, psb, U_d, hout_d, identf, identb, onesb, onesf, halfm, dbg, dbg_out)

    xres = P.sb([128, NTO, D], F32, "xres")
    P.add("sp", lambda e: e.dma_start(out=xres, in_=I["x_rot"][0:OWN, :].rearrange("(j p) c -> p j c", p=128)), W=["xres"], dma="xres")
    P.mark()
    w_out = P.sb([128, 8, D], BF16, "w_out")
    P.add("pool", lambda e: e.dma_start(out=w_out, in_=I["w_out"].rearrange("(k p) n -> p k n", p=128)), W=["w_out"], dma="w0")
    P.add("sp", lambda e: e.dma_start(out=gb[:, 0:512], in_=I["attn_out_g"].partition_broadcast(128)), W=["gb"], dma="gain")
    P.add("sp", lambda e: e.dma_start(out=gb[:, 512:1024], in_=I["hy_out_g"].partition_broadcast(128)), W=["gb"], dma="gain")
    mixin = [P.sb([128, D], F32, "mixin%d" % i) for i in range(2)]
    mixbf = [P.sb([128, D], BF16, "mixbf%d" % i) for i in range(2)]
    mT = [P.sb([128, 8, 128], BF16, "mT%d" % i) for i in range(2)]
    for j in range(NTO):
        b = j % 2
        P.add("sp", lambda e, j=j, b=b: e.dma_start(out=mixin[b][:, 0:512], in_=aout_d[j * 128:(j + 1) * 128, :]),
              R=["aout_d"], W=["mixin%d" % b], dma="mixa%d" % b)
        P.add("sp", lambda e, j=j, b=b: e.dma_start(out=mixin[b][:, 512:1024], in_=hout_d[j * 128:(j + 1) * 128, :]),
              R=["hout_d"], W=["mixin%d" % b], dma="mixh%d" % b)
        rms_norm(mixin[b][:, 0:512], 512, gb[:, 0:512], mixbf[b][:, 0:512], ["mixin%d" % b], ["mixbfa%d" % b], stk="sta")
        rms_norm(mixin[b][:, 512:1024], 512, gb[:, 512:1024], mixbf[b][:, 512:1024], ["mixin%d" % b], ["mixbfh%d" % b], stk="sth")
        pt = 0 + b
        for k in range(8):
            P.add("pe", lambda e, k=k, b=b, pt=pt: e.transpose(out=psb(pt)[:, k * 128:(k + 1) * 128], in_=mixbf[b][:, k * 128:(k + 1) * 128], identity=identb),
                  R=["mixbfa%d" % b, "mixbfh%d" % b, "identb"], W=[psk[pt]])
        P.add("act", lambda e, b=b, pt=pt: e.copy(out=mT[b].rearrange("p k t -> p (k t)"), in_=psb(pt)), R=[psk[pt]], W=["mT%d" % b])
        for n in range(2):
            py = 2 + 2 * b + n
            for k in range(8):
                P.add("pe", lambda e, k=k, b=b, n=n, py=py: e.matmul(ps[py][:, :], lhsT=mT[b][:, k, :], rhs=w_out[:, k, n * 512:(n + 1) * 512],
                                                                      start=(k == 0), stop=(k == 7)), R=["mT%d" % b, "w_out"], W=[psk[py]])
            P.add("dve", lambda e, j=j, n=n, py=py: e.tensor_tensor(out=xres[:, j, n * 512:(n + 1) * 512], in0=ps[py][:, :],
                                                                     in1=xres[:, j, n * 512:(n + 1) * 512], op=ALU.add),
                  R=[psk[py], "xres"], W=["xres"])
    P.barrier()
    P.release()
    if "x1" in dbg:
        tx1 = dbg_out("x1", [OWN, D])
        P.add("sp", lambda e: e.dma_start(out=tx1.rearrange("(j p) c -> p j c", p=128), in_=xres), R=["xres"], W=["dbgo"], dma="dbg1")
        P.barrier()

    P.mark()
    hmT = P.sb([128, 8, 256], BF16, "hmT")
    KmT = P.sb([128, 8, 256], BF16, "KmT")
    Vm = P.sb([128, 2, 4, 260], BF16, "Vm")
    ksqm = P.sb([128, 8, 256], BF16, "ksqm")
    kbx = P.sb([1, 8], F32, "kbx")
    P.mark()
    w_mkv = P.sb([128, 8, 2 * D], BF16, "w_mkv")
    P.add("pool", lambda e: e.dma_start(out=w_mkv, in_=I["w_mkv"].rearrange("(k p) n -> p k n", p=128)), W=["w_mkv"], dma="w0")
    load_gain("mem_norm_g")
    P.add("pool", lambda e: e.memset(Vm.rearrange("p a b c -> p (a b c)"), 1.0), W=["Vm"])
    memt = [P.sb([128, D], F32, "memt%d" % i) for i in range(2)]
    membf = [P.sb([128, D], BF16, "membf%d" % i) for i in range(2)]
    for mt in range(2):
        P.add("sp", lambda e, mt=mt: e.dma_start(out=memt[mt], in_=I["mem_b"][mt * 128:(mt + 1) * 128, :]), W=["memt%d" % mt], dma="memt%d" % mt)
        rms_norm(memt[mt], D, gb, membf[mt], ["memt%d" % mt], ["membf%d" % mt], stk="stm")
        for k in range(8):
            P.add("pe", lambda e, k=k, mt=mt: e.transpose(out=psb(mt)[:, k * 128:(k + 1) * 128], in_=membf[mt][:, k * 128:(k + 1) * 128], identity=identb),
                  R=["membf%d" % mt, "identb"], W=[psk[mt]])
        P.add("act", lambda e, mt=mt: e.copy(out=hmT[:, :, mt * 128:(mt + 1) * 128], in_=psb(mt).rearrange("p (k t) -> p k t", k=8)),
              R=[psk[mt]], W=["hmT"])
    for dt in range(8):
        pk = 2 + dt % 2
        for k in range(8):
            P.add("pe", lambda e, k=k, dt=dt, pk=pk: e.matmul(ps[pk][:, 0:256], lhsT=w_mkv[:, k, dt * 128:(dt + 1) * 128], rhs=hmT[:, k, :],
                                                               start=(k == 0), stop=(k == 7)), R=["w_mkv", "hmT"], W=[psk[pk]])
        P.add("act", lambda e, dt=dt, pk=pk: e.copy(out=KmT[:, dt, :], in_=ps[pk][:, 0:256]), R=[psk[pk]], W=["KmT"])
    for mt in range(2):
        for n in range(2):
            pv = 4 + n
            for k in range(8):
                P.add("pe", lambda e, k=k, mt=mt, n=n, pv=pv: e.matmul(ps[pv][:, :], lhsT=hmT[:, k, mt * 128:(mt + 1) * 128],
                                                                        rhs=w_mkv[:, k, D + n * 512:D + (n + 1) * 512],
                                                                        start=(k == 0), stop=(k == 7)), R=["w_mkv", "hmT"], W=[psk[pv]])
            P.add("dve", lambda e, mt=mt, n=n, pv=pv: e.tensor_copy(out=Vm[:, mt, 2 * n:2 * n + 2, 0:256],
                                                                    in_=ps[pv][:, :].rearrange("p (h c) -> p h c", h=2)),
                  R=[psk[pv]], W=["Vm"])
    P.add("dve", lambda e: e.tensor_tensor(out=ksqm, in0=KmT, in1=KmT, op=ALU.mult), R=["KmT"], W=["ksqm"])
    for hh in range(4):
        for dt in range(2):
            P.add("pe", lambda e, hh=hh, dt=dt: e.matmul(ps[6][0:1, 0:256], lhsT=onesb[:, 0:1], rhs=ksqm[:, 2 * hh + dt, :],
                                                          start=(dt == 0), stop=(dt == 1)), R=["ksqm", "onesb"], W=[psk[6]])
        P.add("dve", lambda e, hh=hh: e.tensor_reduce(out=kbx[0:1, hh:hh + 1], in_=ps[6][0:1, 0:256], axis=AX.X, op=ALU.max),
              R=[psk[6]], W=["kbx%d" % hh])
    P.add("dve", lambda e: e.tensor_reduce(out=kbx[0:1, 4:5], in_=kbx[0:1, 0:4], axis=AX.X, op=ALU.max),
          R=["kbx0", "kbx1", "kbx2", "kbx3"], W=["kbx4"])
    P.add("act", lambda e: e.sqrt(out=kbx[0:1, 5:6], in_=kbx[0:1, 4:5]), R=["kbx4"], W=["kbx5"])
    P.add("dve", lambda e: e.tensor_scalar_mul(out=kbx[0:1, 6:7], in0=kbx[0:1, 5:6], scalar1=-1.04), R=["kbx5"], W=["kbx6"])
    P.barrier()
    P.release()
    w_mq = P.sb([128, 8, D], BF16, "w_mq")
    w_mo = P.sb([128, 8, D], BF16, "w_mo")
    P.add("pool", lambda e: e.dma_start(out=w_mq, in_=I["w_mq"].rearrange("(k p) n -> p k n", p=128)), W=["w_mq"], dma="w0")
    P.add("pool", lambda e: e.dma_start(out=w_mo, in_=I["w_mo"].rearrange("(k p) n -> p k n", p=128)), W=["w_mo"], dma="w1")
    load_gain("cross_norm_g")
    hxbf = [P.sb([128, D], BF16, "hxbf%d" % i) for i in range(2)]
    hxT = P.sb([128, 8, 512], BF16, "hxT")
    qT = P.sb([128, 8, 512], BF16, "qT")
    qsqx = P.sb([128, 8, 512], BF16, "qsqx")
    negm = P.sb([1, 4, 512], BF16, "negm")
    qn1 = P.sb([1, 512], F32, "qn1")
    PTm = [P.sb([128, 512], BF16, "PTm%d" % i) for i in range(2)]
    rdn = P.sb([1, 512], F32, "rdn")
    rdb = P.sb([128, 512], F32, "rdb")
    oTx = P.sb([128, 8, 512], BF16, "oTx")
    for qc in range(4):
        for t4 in range(4):
            j = qc * 4 + t4
            b = t4 % 2
            rms_norm(xres[:, j, :], D, gb, hxbf[b], ["xres"], ["hxbf%d" % b], stk="stx")
            for k in range(8):
                P.add("pe", lambda e, k=k, b=b: e.transpose(out=psb(b)[:, k * 128:(k + 1) * 128], in_=hxbf[b][:, k * 128:(k + 1) * 128], identity=identb),
                      R=["hxbf%d" % b, "identb"], W=[psk[b]])
            P.add("act", lambda e, b=b, t4=t4: e.copy(out=hxT[:, :, t4 * 128:(t4 + 1) * 128], in_=psb(b).rearrange("p (k t) -> p k t", k=8)),
                  R=[psk[b]], W=["hxT"])
        for dt in range(8):
            pq = 6 + dt % 2
            for k in range(8):
                P.add("pe", lambda e, k=k, dt=dt, pq=pq: e.matmul(ps[pq][:, :], lhsT=w_mq[:, k, dt * 128:(dt + 1) * 128], rhs=hxT[:, k, :],
                                                                   start=(k == 0), stop=(k == 7)), R=["w_mq", "hxT"], W=[psk[pq]])
            P.add("act", lambda e, dt=dt, pq=pq: e.mul(out=qT[:, dt, :], in_=ps[pq][:, :], mul=1.0 / 16.0), R=[psk[pq]], W=["qT"])
        P.add("dve", lambda e: e.tensor_tensor(out=qsqx, in0=qT, in1=qT, op=ALU.mult), R=["qT"], W=["qsqx"])
        for hh in range(4):
            for dt in range(2):
                P.add("pe", lambda e, hh=hh, dt=dt: e.matmul(ps[4][0:1, :], lhsT=onesb[:, 0:1], rhs=qsqx[:, 2 * hh + dt, :],
                                                              start=(dt == 0), stop=(dt == 1)), R=["qsqx", "onesb"], W=[psk[4]])
            P.add("act", lambda e: e.sqrt(out=qn1, in_=ps[4][0:1, :]), R=[psk[4]], W=["qn1"])
            P.add("dve", lambda e, hh=hh: e.tensor_scalar_mul(out=negm[0:1, hh, :], in0=qn1, scalar1=kbx[0:1, 6:7]), R=["qn1", "kbx6"], W=["negm"])
        for hh in range(4):
            for mt in range(2):
                for dt in range(2):
                    P.add("pe", lambda e, hh=hh, mt=mt, dt=dt: e.matmul(ps[mt][:, :], lhsT=KmT[:, 2 * hh + dt, mt * 128:(mt + 1) * 128],
                                                                         rhs=qT[:, 2 * hh + dt, :], start=(dt == 0), stop=False),
                          R=["KmT", "qT"], W=[psk[mt]])
                P.add("pe", lambda e, hh=hh, mt=mt: e.matmul(ps[mt][:, :], lhsT=onesb[0:1, 0:128], rhs=negm[0:1, hh, :], start=False, stop=True),
                      R=["negm", "onesb"], W=[psk[mt]])
                P.add("act", lambda e, mt=mt: e.activation(out=PTm[mt], in_=ps[mt][:, :], func=AF.Exp), R=[psk[mt]], W=["PTm%d" % mt])
            for dv in range(2):
                for mt in range(2):
                    P.add("pe", lambda e, hh=hh, mt=mt, dv=dv: e.matmul(ps[2 + dv][:, :], lhsT=Vm[:, mt, hh, dv * 128:(dv + 1) * 128], rhs=PTm[mt],
                                                                         start=(mt == 0), stop=(mt == 1)), R=["Vm", "PTm%d" % mt], W=[psk[2 + dv]])
            for mt in range(2):
                P.add("pe", lambda e, hh=hh, mt=mt: e.matmul(ps[4][0:1, :], lhsT=Vm[:, mt, hh, 256:257], rhs=PTm[mt], start=(mt == 0), stop=(mt == 1)),
                      R=["Vm", "PTm%d" % mt], W=[psk[4]])
            P.add("dve", lambda e: e.reciprocal(out=rdn, in_=ps[4][0:1, :]), R=[psk[4]], W=["rdn"])
            P.add("pe", lambda e: e.matmul(ps[5][:, :], lhsT=onesf[0:1, 0:128], rhs=rdn, start=True, stop=True), R=["rdn", "onesf"], W=[psk[5]])
            P.add("act", lambda e: e.copy(out=rdb, in_=ps[5][:, :]), R=[psk[5]], W=["rdb"])
            for dv in range(2):
                P.add("dve", lambda e, hh=hh, dv=dv: e.tensor_tensor(out=oTx[:, 2 * hh + dv, :], in0=ps[2 + dv][:, :], in1=rdb, op=ALU.mult),
                      R=[psk[2 + dv], "rdb"], W=["oTx"])
        for t4 in range(4):
            j = qc * 4 + t4
            for n in range(2):
                py = 6 + n
                for dt in range(8):
                    P.add("pe", lambda e, dt=dt, t4=t4, n=n, py=py: e.matmul(ps[py][:, :], lhsT=oTx[:, dt, t4 * 128:(t4 + 1) * 128],
                                                                              rhs=w_mo[:, dt, n * 512:(n + 1) * 512], start=(dt == 0), stop=(dt == 7)),
                          R=["oTx", "w_mo"], W=[psk[py]])
                P.add("dve", lambda e, j=j, n=n, py=py: e.tensor_tensor(out=xres[:, j, n * 512:(n + 1) * 512], in0=ps[py][:, :],
                                                                         in1=xres[:, j, n * 512:(n + 1) * 512], op=ALU.add),
                      R=[psk[py], "xres"], W=["xres"])
    P.barrier()
    P.release()
    if "x2" in dbg:
        tx2 = dbg_out("x2", [OWN, D])
        P.add("sp", lambda e: e.dma_start(out=tx2.rearrange("(j p) c -> p j c", p=128), in_=xres), R=["xres"], W=["dbgo"], dma="dbg1")
        P.barrier()
    if stop == "E":
        P.add("sp", None, R=["dbgo"])
        P.emit()
        return nc, dbg_d

    P.mark()
    tT = P.sb([128, 8, OWN], BF16, "tT")
    combT = P.sb([32, OWN], F32, "combT")
    P.mark()
    load_gain("ffn_norm_g")
    w_r = P.sb([128, 8, 36], F32, "w_r")
    b_r = P.sb([128, 36], F32, "b_r")
    P.add("sp", lambda e: e.dma_start(out=w_r, in_=I["w_route"].rearrange("(k p) n -> p k n", p=128)), W=["w_r"], dma="c3")
    P.add("sp", lambda e: e.dma_start(out=b_r, in_=I["b_route"].partition_broadcast(128)), W=["b_r"], dma="c4")
    tnf = [P.sb([128, D], F32, "tnf%d" % i) for i in range(2)]
    tnb = [P.sb([128, D], BF16, "tnb%d" % i) for i in range(2)]
    tTf = P.sb([128, 8, 128], F32, "tTf")
    lg = P.sb([128, 36], F32, "lg")
    r8 = P.sb([128, 16], F32, "r8")
    oh = P.sb([128, 4], F32, "oh")
    ge = P.sb([128, 4], F32, "ge")
    ein = P.sb([128, 8], F32, "ein")
    e2 = P.sb([128, 8], F32, "e2")
    mk1 = P.sb([128, 8], F32, "mk1")
    mk2 = P.sb([128, 8], F32, "mk2")
    we = P.sb([128, 8], F32, "we")
    comb = P.sb([128, 32], F32, "comb")
    seq = [0]

    def dv(fn, R, W):
        P.add("dve", fn, R=R, W=W)

    for j in range(NTO):
        b = j % 2
        rms_norm(xres[:, j, :], D, gb, tnf[b], ["xres"], ["tnf%d" % b], stk="stf")
        P.add("act", lambda e, b=b: e.copy(out=tnb[b], in_=tnf[b]), R=["tnf%d" % b], W=["tnb%d" % b])
        for k in range(8):
            P.add("pe", lambda e, k=k, b=b: e.transpose(out=psb(b)[:, k * 128:(k + 1) * 128], in_=tnb[b][:, k * 128:(k + 1) * 128], identity=identb),
                  R=["tnb%d" % b, "identb"], W=[psk[b]])
        P.add("act", lambda e, b=b, j=j: e.copy(out=tT[:, :, j * 128:(j + 1) * 128], in_=psb(b).rearrange("p (k t) -> p k t", k=8)),
              R=[psk[b]], W=["tT"])
        for k in range(8):
            pf = 2 + (k // 4)
            P.add("pe", lambda e, k=k, b=b, pf=pf: e.transpose(out=ps[pf][:, (k % 4) * 128:(k % 4 + 1) * 128], in_=tnf[b][:, k * 128:(k + 1) * 128], identity=identf),
                  R=["tnf%d" % b, "identf"], W=[psk[pf]])
        for hf in range(2):
            P.add("dve" if hf == 0 else "act", (lambda e, hf=hf: e.tensor_copy(out=tTf[:, hf * 4:(hf + 1) * 4, :], in_=ps[2 + hf][:, :].rearrange("p (k t) -> p k t", k=4)))
                  if hf == 0 else (lambda e, hf=hf: e.copy(out=tTf[:, hf * 4:(hf + 1) * 4, :], in_=ps[2 + hf][:, :].rearrange("p (k t) -> p k t", k=4))),
                  R=[psk[2 + hf]], W=["tTf%d" % hf])
        for k in range(8):
            P.add("pe", lambda e, k=k: e.matmul(ps[4][:, 0:36], lhsT=tTf[:, k, :], rhs=w_r[:, k, :], start=(k == 0), stop=(k == 7)),
                  R=["tTf0", "tTf1", "w_r"], W=[psk[4]])
        dv(lambda e: e.tensor_tensor(out=lg, in0=ps[4][:, 0:36], in1=b_r, op=ALU.add), [psk[4], "b_r"], ["lg"])
        dv(lambda e: e.tensor_reduce(out=r8[:, 0:1], in_=lg[:, 0:4], axis=AX.X, op=ALU.max), ["lg"], ["r8_0"])
        dv(lambda e: e.tensor_scalar(out=oh, in0=lg[:, 0:4], scalar1=r8[:, 0:1], scalar2=None, op0=ALU.is_equal), ["lg", "r8_0"], ["oh"])
        dv(lambda e: e.tensor_scalar(out=ge, in0=lg[:, 0:4], scalar1=r8[:, 0:1], scalar2=None, op0=ALU.subtract), ["lg", "r8_0"], ["ge"])
        P.add("act", lambda e: e.activation(out=ge, in_=ge, func=AF.Exp), R=["ge"], W=["ge"])
        dv(lambda e: e.tensor_reduce(out=r8[:, 1:2], in_=ge, axis=AX.X, op=ALU.add), ["ge"], ["r8_1"])
        dv(lambda e: e.reciprocal(out=r8[:, 2:3], in_=r8[:, 1:2]), ["r8_1"], ["r8_2"])
        dv(lambda e: e.tensor_scalar_mul(out=ein, in0=lg[:, 4:12], scalar1=oh[:, 0:1]), ["lg", "oh"], ["ein"])
        for g in range(1, 4):
            dv(lambda e, g=g: e.scalar_tensor_tensor(out=ein, in0=lg[:, 4 + 8 * g:12 + 8 * g], scalar=oh[:, g:g + 1], in1=ein,
                                                     op0=ALU.mult, op1=ALU.add), ["lg", "oh", "ein"], ["ein"])
        dv(lambda e: e.tensor_reduce(out=r8[:, 3:4], in_=ein, axis=AX.X, op=ALU.max), ["ein"], ["r8_3"])
        dv(lambda e: e.tensor_scalar(out=mk1, in0=ein, scalar1=r8[:, 3:4], scalar2=None, op0=ALU.is_equal), ["ein", "r8_3"], ["mk1"])
        dv(lambda e: e.scalar_tensor_tensor(out=e2, in0=mk1, scalar=-1e30, in1=ein, op0=ALU.mult, op1=ALU.add), ["mk1", "ein"], ["e2"])
        dv(lambda e: e.tensor_reduce(out=r8[:, 4:5], in_=e2, axis=AX.X, op=ALU.max), ["e2"], ["r8_4"])
        dv(lambda e: e.tensor_scalar(out=mk2, in0=e2, scalar1=r8[:, 4:5], scalar2=None, op0=ALU.is_equal), ["e2", "r8_4"], ["mk2"])
        dv(lambda e: e.tensor_tensor(out=r8[:, 5:6], in0=r8[:, 4:5], in1=r8[:, 3:4], op=ALU.subtract), ["r8_3", "r8_4"], ["r8_5"])
        P.add("act", lambda e: e.activation(out=r8[:, 6:7], in_=r8[:, 5:6], func=AF.Exp), R=["r8_5"], W=["r8_6"])
        dv(lambda e: e.tensor_scalar_add(out=r8[:, 7:8], in0=r8[:, 6:7], scalar1=1.0), ["r8_6"], ["r8_7"])
        dv(lambda e: e.reciprocal(out=r8[:, 8:9], in_=r8[:, 7:8]), ["r8_7"], ["r8_8"])
        dv(lambda e: e.tensor_tensor(out=r8[:, 9:10], in0=r8[:, 6:7], in1=r8[:, 8:9], op=ALU.mult), ["r8_6", "r8_8"], ["r8_9"])
        dv(lambda e: e.tensor_tensor(out=r8[:, 10:11], in0=r8[:, 8:9], in1=r8[:, 2:3], op=ALU.mult), ["r8_8", "r8_2"], ["r8_10"])
        dv(lambda e: e.tensor_tensor(out=r8[:, 11:12], in0=r8[:, 9:10], in1=r8[:, 2:3], op=ALU.mult), ["r8_9", "r8_2"], ["r8_11"])
        dv(lambda e: e.tensor_scalar_mul(out=we, in0=mk1, scalar1=r8[:, 10:11]), ["mk1", "r8_10"], ["we"])
        dv(lambda e: e.scalar_tensor_tensor(out=we, in0=mk2, scalar=r8[:, 11:12], in1=we, op0=ALU.mult, op1=ALU.add), ["mk2", "r8_11", "we"], ["we"])
        for g in range(4):
            dv(lambda e, g=g: e.tensor_scalar_mul(out=comb[:, 8 * g:8 * g + 8], in0=we, scalar1=oh[:, g:g + 1]), ["we", "oh"], ["comb"])
        P.add("pe", lambda e: e.transpose(out=ps[5][0:32, 0:128], in_=comb, identity=identf), R=["comb", "identf"], W=[psk[5]])
        dv(lambda e, j=j: e.tensor_copy(out=combT[:, j * 128:(j + 1) * 128], in_=ps[5][0:32, 0:128]), [psk[5]], ["combT"])
    P.add("sp", lambda e: e.dma_start(out=combT_d, in_=combT), R=["combT"], W=["combT_d"], dma="combT")
    if "comb" in dbg:
        tcb = dbg_out("comb", [32, OWN])
        P.add("sp", lambda e: e.dma_start(out=tcb, in_=combT), R=["combT"], W=["dbgo"], dma="dbg1")
    P.barrier()
    P.release()
    NSLOT = 4
    wg = [P.sb([128, 8, 256], BF16, "wg%d" % i) for i in range(NSLOT)]
    wu = [P.sb([128, 8, 256], BF16, "wu%d" % i) for i in range(NSLOT)]
    wd = [P.sb([128, 2, D], BF16, "wd%d" % i) for i in range(NSLOT)]
    CB = [P.sb([128, OWN], F32, "CB%d" % i) for i in range(2)]
    sa = [P.sb([128, 2, 512], F32, "sa%d" % i) for i in range(2)]
    sc = [P.sb([128, 2, 512], F32, "sc%d" % i) for i in range(2)]
    mTe = [P.sb([128, 2, 512], BF16, "mTe%d" % i) for i in range(2)]
    def load_expert(e_):
        sl = e_ % NSLOT
        P.add("pool", lambda e, e_=e_, sl=sl: e.dma_start(out=wg[sl], in_=I["w_gate"][e_].rearrange("(k p) n -> p k n", p=128)), W=["wg%d" % sl], dma="wg%d" % sl)
        P.add("pool", lambda e, e_=e_, sl=sl: e.dma_start(out=wu[sl], in_=I["w_up"][e_].rearrange("(k p) n -> p k n", p=128)), W=["wu%d" % sl], dma="wu%d" % sl)
        P.add("pool", lambda e, e_=e_, sl=sl: e.dma_start(out=wd[sl], in_=I["w_down"][e_].rearrange("(k p) n -> p k n", p=128)), W=["wd%d" % sl], dma="wd%d" % sl)

    for e_ in range(2):
        load_expert(e_)
    yb = 0
    for pr in range(16):
        for ee in range(2):
            if 2 * pr + 2 + ee < 32:
                load_expert(2 * pr + 2 + ee)
        for ee in range(2):
            e_ = 2 * pr + ee
            P.add("sp", lambda e, e_=e_, ee=ee: e.dma_start(out=CB[ee], in_=combT_d[e_:e_ + 1, :].partition_broadcast(128)),
                  R=["combT_d"], W=["CB%d" % ee], dma="CB%d" % ee)
        for c in range(4):
            for ee in range(2):
                e_ = 2 * pr + ee
                sl = e_ % NSLOT
                for f in range(2):
                    for k in range(8):
                        P.add("pe", lambda e, f=f, k=k, sl=sl, c=c: e.matmul(ps[f][:, :], lhsT=wg[sl][:, k, f * 128:(f + 1) * 128],
                                                                            rhs=tT[:, k, c * 512:(c + 1) * 512], start=(k == 0), stop=(k == 7)),
                              R=["wg%d" % sl, "tT"], W=[psk[f]])
                for f in range(2):
                    for k in range(8):
                        P.add("pe", lambda e, f=f, k=k, sl=sl, c=c: e.matmul(ps[2 + f][:, :], lhsT=wu[sl][:, k, f * 128:(f + 1) * 128],
                                                                            rhs=tT[:, k, c * 512:(c + 1) * 512], start=(k == 0), stop=(k == 7)),
                              R=["wu%d" % sl, "tT"], W=[psk[2 + f]])
                for f in range(2):
                    P.add("act", lambda e, f=f, ee=ee: e.activation(out=sa[ee][:, f, :], in_=ps[f][:, :], func=AF.Silu),
                          R=[psk[f]], W=["sa%d_%d" % (ee, f)])
                    P.add("pool", lambda e, f=f, ee=ee, c=c: e.tensor_tensor(out=sc[ee][:, f, :], in0=sa[ee][:, f, :],
                                                                              in1=CB[ee][:, c * 512:(c + 1) * 512], op=ALU.mult),
                          R=["sa%d_%d" % (ee, f), "CB%d" % ee], W=["sc%d_%d" % (ee, f)])
                    P.add("dve", lambda e, f=f, ee=ee: e.tensor_tensor(out=mTe[ee][:, f, :], in0=ps[2 + f][:, :], in1=sc[ee][:, f, :], op=ALU.mult),
                          R=[psk[2 + f], "sc%d_%d" % (ee, f)], W=["mTe%d" % ee])
            for t4 in range(4):
                j = c * 4 + t4
                for n in range(2):
                    py = 4 + (yb % 4)
                    yb += 1
                    cnt_mm = 0
                    for ee in range(2):
                        sl = (2 * pr + ee) % NSLOT
                        for f in range(2):
                            P.add("pe", lambda e, ee=ee, f=f, sl=sl, t4=t4, n=n, py=py, cnt_mm=cnt_mm: e.matmul(
                                ps[py][:, :], lhsT=mTe[ee][:, f, t4 * 128:(t4 + 1) * 128], rhs=wd[sl][:, f, n * 512:(n + 1) * 512],
                                start=(cnt_mm == 0), stop=(cnt_mm == 3)), R=["mTe%d" % ee, "wd%d" % sl], W=[psk[py]])
                            cnt_mm += 1
                    P.add("dve", lambda e, j=j, n=n, py=py: e.tensor_tensor(out=xres[:, j, n * 512:(n + 1) * 512], in0=ps[py][:, :],
                                                                             in1=xres[:, j, n * 512:(n + 1) * 512], op=ALU.add),
                          R=[psk[py], "xres"], W=["xres"])
    P.barrier()
    P.release()
    if "x3" in dbg:
        tx3 = dbg_out("x3", [OWN, D])
        P.add("sp", lambda e: e.dma_start(out=tx3.rearrange("(j p) c -> p j c", p=128), in_=xres), R=["xres"], W=["dbgo"], dma="dbg1")
        P.barrier()

    load_gain("final_norm_g")
    xo = [P.sb([128, D], F32, "xo%d" % i) for i in range(2)]
    for j in range(NTO):
        b = j % 2
        rms_norm(xres[:, j, :], D, gb, xo[b], ["xres"], ["xo%d" % b], stk="stg")
        P.add("sp", lambda e, j=j, b=b: e.dma_start(out=out_d[j * 128:(j + 1) * 128, :], in_=xo[b]),
              R=["xo%d" % b], W=["out_d%d" % b], dma="xo%d" % b)
    P.add("sp", None, R=["out_d0", "out_d1", "dbgo"])
    P.emit()
    return nc, dbg_d


def hyena_phase(nc, P, I, ps, psk, psb, U_d, hout_d, identf, identb, onesb, onesf, halfm, dbg, dbg_out):
    NF = 64
    Hd = nc.dram_tensor("hy_H", [S, 2048], BF16, kind="Internal").ap()
    Bd = [nc.dram_tensor("hy_B%d" % i, [128, NF, 512], BF16, kind="Internal").ap() for i in range(2)]
    Kd = [nc.dram_tensor("hy_K%d" % i, [128, NF, 512], BF16, kind="Internal").ap() for i in range(2)]
    Btd = nc.dram_tensor("hy_Bt", [128, NF, 512], BF16, kind="Internal").ap()
    z1_d = nc.dram_tensor("hy_z1", [S, 512], F32, kind="Internal").ap()
    P.mark()
    Dt = P.sb([64, 64, 128], BF16, "Dt")
    Dinv = P.sb([128, 64, 64], BF16, "Dinv")
    F2a = P.sb([128, 128], BF16, "F2a")
    F2b = P.sb([128, 128], BF16, "F2b")
    Gm = P.sb([128, 128], BF16, "Gm")
    sgn = P.sb([128, 1], F32, "sgn")
    skipb = P.sb([128, 2, 512], F32, "skipb")
    P.add("pool", lambda e: e.dma_start(out=Dt, in_=I["hy_D0"].rearrange("a (b c) -> a b c", b=64)), W=["Dt"], dma="ht0")
    P.add("pool", lambda e: e.dma_start(out=F2a, in_=I["hy_F2a"]), W=["F2a"], dma="ht1")
    P.add("pool", lambda e: e.dma_start(out=F2b, in_=I["hy_F2b"]), W=["F2b"], dma="ht2")
    P.add("pool", lambda e: e.dma_start(out=Gm, in_=I["hy_G"]), W=["Gm"], dma="ht3")
    P.add("pool", lambda e: e.dma_start(out=Dinv, in_=I["hy_Dinv"].rearrange("a (b c) -> a b c", b=64)), W=["Dinv"], dma="ht4")
    P.add("sp", lambda e: e.dma_start(out=sgn, in_=I["hy_sgn"]), W=["sgn"], dma="c3")
    P.add("sp", lambda e: e.dma_start(out=skipb.rearrange("p a b -> p (a b)"), in_=I["hy_skip"].partition_broadcast(128)), W=["skipb"], dma="c4")

    P.mark()
    zT = P.sb([33, S], F32, "zT")
    g1T = P.sb([64, S], F32, "g1T")
    g2T = P.sb([64, S], F32, "g2T")
    hcols = P.sb([64, 4], F32, "hcols")
    w1 = P.sb([33, 64], F32, "w1")
    w2 = P.sb([64, 64], F32, "w2")
    w3 = P.sb([64, 2048], F32, "w3")
    b3r = P.sb([1, 2048], F32, "b3r")
    adec = P.sb([128, 2048], F32, "adec")
    nt01 = P.sb([128, NT], F32, "nt01")
    mpi = P.sb([128, 1], F32, "mpi")
    argt = P.sb([64, 512], F32, "argt")
    argm = P.sb([64, 512], F32, "argm")
    Et = [P.sb([128, 512], F32, "Et%d" % i) for i in range(2)]
    hfo = [P.sb([128, 2048], BF16, "hfo%d" % i) for i in range(2)]
    P.add("sp", lambda e: e.dma_start(out=zT, in_=I["hy_zT"]), W=["zT"], dma="hf0")
    P.add("sp", lambda e: e.dma_start(out=hcols, in_=I["hy_cols"]), W=["hcols"], dma="hf1")
    P.add("sp", lambda e: e.dma_start(out=w1, in_=I["hy_w1"]), W=["w1"], dma="hf2")
    P.add("sp", lambda e: e.dma_start(out=w2, in_=I["hy_w2"]), W=["w2"], dma="hf3")
    P.add("sp", lambda e: e.dma_start(out=w3, in_=I["hy_w3"]), W=["w3"], dma="hf4")
    P.add("sp", lambda e: e.dma_start(out=b3r, in_=I["hy_b3"]), W=["b3r"], dma="hf5")
    P.add("sp", lambda e: e.dma_start(out=adec, in_=I["hy_decay"].partition_broadcast(128)), W=["adec"], dma="hf6")
    P.add("sp", lambda e: e.dma_start(out=nt01, in_=I["hy_nt01"]), W=["nt01"], dma="hf7")
    P.add("pool", lambda e: e.memset(mpi, -math.pi), W=["mpi"])
    P.add("act", lambda e: e.activation(out=adec, in_=adec, func=AF.Abs), R=["adec"], W=["adec"])
    OFFS = math.pi + 16.0 * math.pi
    for (src, wt, kk, bcol, fcol, dst, nm) in ((zT, w1, 33, 0, 2, g1T, "g1T"), (g1T, w2, 64, 1, 3, g2T, "g2T")):
        for ch in range(8):
            pb_ = ch % 2
            P.add("pe", lambda e, src=src, wt=wt, kk=kk, ch=ch, pb_=pb_: e.matmul(ps[pb_][0:64, :], lhsT=wt[0:kk, :], rhs=src[0:kk, ch * 512:(ch + 1) * 512],
                                                                               start=True, stop=True), R=["zT", "g1T", "w1", "w2"], W=[psk[pb_]])
            P.add("dve", lambda e, pb_=pb_, bcol=bcol, fcol=fcol: e.tensor_scalar(out=argt, in0=ps[pb_][0:64, :], scalar1=hcols[:, bcol:bcol + 1],
                                                                                  scalar2=hcols[:, fcol:fcol + 1], op0=ALU.add, op1=ALU.mult),
                  R=[psk[pb_], "hcols"], W=["argt"])
            for _rep in range(2):
                P.add("dve", lambda e: e.tensor_scalar(out=argm, in0=argt, scalar1=math.pi, scalar2=None, op0=ALU.is_gt), R=["argt"], W=["argm"])
                P.add("dve", lambda e: e.scalar_tensor_tensor(out=argt, in0=argm, scalar=-2.0 * math.pi, in1=argt, op0=ALU.mult, op1=ALU.add),
                      R=["argm", "argt"], W=["argt"])
                P.add("dve", lambda e: e.tensor_scalar(out=argm, in0=argt, scalar1=-math.pi, scalar2=None, op0=ALU.is_lt), R=["argt"], W=["argm"])
                P.add("dve", lambda e: e.scalar_tensor_tensor(out=argt, in0=argm, scalar=2.0 * math.pi, in1=argt, op0=ALU.mult, op1=ALU.add),
                      R=["argm", "argt"], W=["argt"])
            P.add("act", lambda e, dst=dst, ch=ch: e.activation(out=dst[:, ch * 512:(ch + 1) * 512], in_=argt, func=AF.Sin),
                  R=["argt"], W=[nm])
    for i in range(NT):
        b = i % 2
        for cg in range(4):
            pb_ = 2 + cg
            P.add("pe", lambda e, i=i, cg=cg, pb_=pb_: e.matmul(ps[pb_][:, :], lhsT=g2T[:, i * 128:(i + 1) * 128], rhs=w3[:, cg * 512:(cg + 1) * 512],
                                                                 start=True, stop=False), R=["g2T", "w3"], W=[psk[pb_]])
            P.add("pe", lambda e, cg=cg, pb_=pb_: e.matmul(ps[pb_][:, :], lhsT=onesf[0:1, 0:128], rhs=b3r[0:1, cg * 512:(cg + 1) * 512],
                                                            start=False, stop=True), R=["onesf", "b3r"], W=[psk[pb_]])
            eb = cg % 2
            P.add("act", lambda e, i=i, cg=cg, eb=eb: e.activation(out=Et[eb], in_=adec[:, cg * 512:(cg + 1) * 512], func=AF.Exp, scale=nt01[:, i:i + 1]),
                  R=["adec", "nt01"], W=["Et%d" % eb])
            P.add("dve", lambda e, cg=cg, pb_=pb_, eb=eb, b=b: e.tensor_tensor(out=hfo[b][:, cg * 512:(cg + 1) * 512], in0=ps[pb_][:, :], in1=Et[eb], op=ALU.mult),
                  R=[psk[pb_], "Et%d" % eb], W=["hfo%d" % b])
        if i == 0:
            for o in range(2):
                P.add("pool", lambda e, o=o: e.memset(hfo[0][0:1, o * 1024 + 512:o * 1024 + 1024], 0.0), R=[], W=["hfo0"])
        P.add("sp", lambda e, i=i, b=b: e.dma_start(out=Hd[i * 128:(i + 1) * 128, :], in_=hfo[b]), R=["hfo%d" % b], W=["Hd"], dma="hfo%d" % b)
    P.barrier()
    P.release()
    if "hf" in dbg:
        thf = dbg_out("hf", [S, 2048])
        P.mark()
        tbf = P.sb([128, 2048], BF16, "tbf")
        tbf32 = P.sb([128, 2048], F32, "tbf32")
        for i in range(NT):
            P.add("sp", lambda e, i=i: e.dma_start(out=tbf, in_=Hd[i * 128:(i + 1) * 128, :]), R=["Hd"], W=["tbf"], dma="dbg0")
            P.add("dve", lambda e: e.tensor_copy(out=tbf32, in_=tbf), R=["tbf"], W=["tbf32"])
            P.add("sp", lambda e, i=i: e.dma_start(out=thf[i * 128:(i + 1) * 128, :], in_=tbf32), R=["tbf32"], W=["dbgo"], dma="dbg1")
        P.barrier()
        P.release()

    ev = [0]

    def evac(out, in_, Rk, Wk):
        ev[0] += 1
        if ev[0] % 2:
            P.add("act", lambda e: e.copy(out=out, in_=in_), R=Rk, W=Wk)
        else:
            P.add("dve", lambda e: e.tensor_copy(out=out, in_=in_), R=Rk, W=Wk)

    def stage1(src_view, cast, Bdst, bkey, srckey):
        P.barrier()
        P.mark()
        xsb = [P.sb([64, 16, 512], BF16, "xsb%d" % i) for i in range(2)]
        Bsb = [P.sb([128, 16, 512], BF16, "Bsb%d" % i) for i in range(2)]
        for ch in range(4):
            xb_ = ch % 2
            q = "pool" if cast else "sp"
            P.add(q, lambda e, ch=ch, xb_=xb_: e.dma_start(out=xsb[xb_], in_=src_view[:, ch * 16:(ch + 1) * 16, :]),
                  R=[srckey], W=["xsb%d" % xb_], dma="xsb%d" % xb_)
            for s2l in range(16):
                s2 = ch * 16 + s2l
                pb_ = s2 % 4
                P.add("pe", lambda e, s2=s2, s2l=s2l, xb_=xb_, pb_=pb_: e.matmul(ps[pb_][:, :], lhsT=Dt[0:64, s2, :], rhs=xsb[xb_][0:64, s2l, :],
                                                                                  start=True, stop=True), R=["Dt", "xsb%d" % xb_], W=[psk[pb_]])
                evac(Bsb[xb_][:, s2l, :], ps[pb_][:, :], [psk[pb_]], ["Bsb%d_%d" % (xb_, s2l)])
            P.add("sp", lambda e, ch=ch, xb_=xb_: e.dma_start(out=Bdst[:, ch * 16:(ch + 1) * 16, :], in_=Bsb[xb_]),
                  R=["Bsb%d_%d" % (xb_, k) for k in range(16)], W=[bkey], dma="Bsb%d" % xb_)
        P.barrier()
        P.release()

    def blocked(ap2d):
        return ap2d.rearrange("(s1 s2) c -> s1 s2 c", s2=64)

    for o in range(2):
        stage1(blocked(Hd[:, o * 1024:o * 1024 + 512]), False, Bd[0], "Bd0", "Hd")
        stage1(blocked(Hd[:, o * 1024 + 512:o * 1024 + 1024]), False, Bd[1], "Bd1", "Hd")
        P.mark()
        BTf = [P.sb([128, 8, 512], BF16, "BTf%d" % i) for i in range(2)]
        BTb = [P.sb([128, 8, 512], BF16, "BTb%d" % i) for i in range(2)]
        xfs = [P.sb([128, 512], F32, "xfs%d" % i) for i in range(2)]
        kst = [P.sb([128, 512], F32, "kst%d" % i) for i in range(2)]
        Kc = [P.sb([128, 8, 512], BF16, "Kc%d" % i) for i in range(2)]
        for fc in range(8):
            cb_ = fc % 2
            for r in range(2):
                P.add("sp", lambda e, r=r, fc=fc, cb_=cb_: e.dma_start(
                    out=BTf[cb_][r * 64:(r + 1) * 64, :, :], in_=Bd[0][r * 64 + fc * 8:r * 64 + fc * 8 + 8, :, :].rearrange("f s c -> s f c")),
                    R=["Bd0"], W=["BTf%d" % cb_], dma="BTf%d_%d" % (cb_, r))
                P.add("sp", lambda e, r=r, fc=fc, cb_=cb_: e.dma_start(
                    out=BTb[cb_][r * 64:(r + 1) * 64, :, :], in_=Bd[1][r * 64 + fc * 8:r * 64 + fc * 8 + 8, :, :].rearrange("f s c -> s f c")),
                    R=["Bd1"], W=["BTb%d" % cb_], dma="BTb%d_%d" % (cb_, r))
            for f1l in range(8):
                t_ = f1l % 2
                pa, pb2 = 0 + 2 * t_, 1 + 2 * t_
                P.add("pe", lambda e, f1l=f1l, cb_=cb_, pa=pa: e.matmul(ps[pa][:, :], lhsT=F2a, rhs=BTf[cb_][:, f1l, :], start=True, stop=True),
                      R=["F2a", "BTf%d" % cb_], W=[psk[pa]])
                P.add("pe", lambda e, f1l=f1l, cb_=cb_, pb2=pb2: e.matmul(ps[pb2][:, :], lhsT=F2a, rhs=BTb[cb_][:, f1l, :], start=True, stop=True),
                      R=["F2a", "BTb%d" % cb_], W=[psk[pb2]])
                P.add("act", lambda e, pa=pa, t_=t_: e.copy(out=xfs[t_], in_=ps[pa][:, :]), R=[psk[pa]], W=["xfs%d" % t_])
                P.add("dve", lambda e, pb2=pb2, t_=t_: e.scalar_tensor_tensor(out=kst[t_], in0=ps[pb2][:, :], scalar=sgn[:, 0:1], in1=xfs[t_],
                                                                             op0=ALU.mult, op1=ALU.add),
                      R=[psk[pb2], "xfs%d" % t_, "sgn"], W=["kst%d" % t_])
                P.add("pool", lambda e, t_=t_, cb_=cb_, f1l=f1l, o=o: e.tensor_tensor(out=Kc[cb_][0:64, f1l, :], in0=kst[t_][0:64, :], in1=skipb[0:64, o, :], op=ALU.add),
                      R=["kst%d" % t_, "skipb"], W=["Kc%d_a%d" % (cb_, f1l)])
                P.add("pool", lambda e, t_=t_, cb_=cb_, f1l=f1l: e.tensor_copy(out=Kc[cb_][64:128, f1l, :], in_=kst[t_][64:128, :]),
                      R=["kst%d" % t_], W=["Kc%d_b%d" % (cb_, f1l)])
            P.add("sp", lambda e, fc=fc, cb_=cb_, o=o: e.dma_start(out=Kd[o][:, fc * 8:(fc + 1) * 8, :], in_=Kc[cb_]),
                  R=["Kc%d_a%d" % (cb_, k) for k in range(8)] + ["Kc%d_b%d" % (cb_, k) for k in range(8)], W=["Kd%d" % o], dma="Kc%d" % cb_)
        P.barrier()
        P.release()
    if "kf" in dbg:
        tkf = dbg_out("kf", [2, 128, NF * 512])
        P.mark()
        tk16 = P.sb([128, 8, 512], BF16, "tk16")
        tk32 = P.sb([128, 8, 512], F32, "tk32")
        for o in range(2):
            for fc in range(8):
                P.add("sp", lambda e, o=o, fc=fc: e.dma_start(out=tk16, in_=Kd[o][:, fc * 8:(fc + 1) * 8, :]), R=["Kd%d" % o], W=["tk16"], dma="dbg0")
                P.add("dve", lambda e: e.tensor_copy(out=tk32, in_=tk16), R=["tk16"], W=["tk32"])
                P.add("sp", lambda e, o=o, fc=fc: e.dma_start(out=tkf[o][:, fc * 4096:(fc + 1) * 4096], in_=tk32.rearrange("p a b -> p (a b)")),
                      R=["tk32"], W=["dbgo"], dma="dbg1")
        P.barrier()
        P.release()

    P.add("pool", lambda e: e.dma_start(out=Dt, in_=I["hy_Dc"].rearrange("a (b c) -> a b c", b=64)), W=["Dt"], dma="ht0")
    for o in range(2):
        if o == 0:
            stage1(blocked(U_d[:, 1024:1536]), True, Bd[0], "Bd0", "U_d")
        else:
            stage1(blocked(z1_d), True, Bd[0], "Bd0", "z1_d")
        P.mark()
        BT = [P.sb([128, 8, 512], BF16, "BT%d" % i) for i in range(2)]
        KA = [P.sb([128, 8, 512], BF16, "KA%d" % i) for i in range(2)]
        KB = [P.sb([128, 8, 512], BF16, "KB%d" % i) for i in range(2)]
        ta = [P.sb([128, 512], F32, "ta%d" % i) for i in range(2)]
        tb2 = [P.sb([128, 512], F32, "tb2%d" % i) for i in range(2)]
        Yc = [P.sb([128, 512], BF16, "Yc%d" % i) for i in range(2)]
        Btsb = [P.sb([128, 8, 512], BF16, "Btsb%d" % i) for i in range(2)]
        for fc in range(8):
            cb_ = fc % 2
            for r in range(2):
                P.add("sp", lambda e, r=r, fc=fc, cb_=cb_: e.dma_start(
                    out=BT[cb_][r * 64:(r + 1) * 64, :, :], in_=Bd[0][r * 64 + fc * 8:r * 64 + fc * 8 + 8, :, :].rearrange("f s c -> s f c")),
                    R=["Bd0"], W=["BT%d" % cb_], dma="BT%d_%d" % (cb_, r))
                P.add("sp", lambda e, r=r, fc=fc, cb_=cb_, o=o: e.dma_start(out=KA[cb_][r * 64:(r + 1) * 64, :, :], in_=Kd[o][0:64, fc * 8:(fc + 1) * 8, :]),
                      R=["Kd%d" % o], W=["KA%d" % cb_], dma="KA%d_%d" % (cb_, r))
                P.add("sp", lambda e, r=r, fc=fc, cb_=cb_, o=o: e.dma_start(out=KB[cb_][r * 64:(r + 1) * 64, :, :], in_=Kd[o][64:128, fc * 8:(fc + 1) * 8, :]),
                      R=["Kd%d" % o], W=["KB%d" % cb_], dma="KB%d_%d" % (cb_, r))
            for f1l in range(8):
                t_ = f1l % 2
                pa, pb2, pc = 0 + 3 * t_, 1 + 3 * t_, 2 + 3 * t_
                P.add("pe", lambda e, f1l=f1l, cb_=cb_, pa=pa: e.matmul(ps[pa][:, :], lhsT=F2a, rhs=BT[cb_][:, f1l, :], start=True, stop=True),
                      R=["F2a", "BT%d" % cb_], W=[psk[pa]])
                P.add("pe", lambda e, f1l=f1l, cb_=cb_, pb2=pb2: e.matmul(ps[pb2][:, :], lhsT=F2b, rhs=BT[cb_][:, f1l, :], start=True, stop=True),
                      R=["F2b", "BT%d" % cb_], W=[psk[pb2]])
                P.add("dve", lambda e, pa=pa, t_=t_, cb_=cb_, f1l=f1l: e.tensor_tensor(out=ta[t_], in0=ps[pa][:, :], in1=KA[cb_][:, f1l, :], op=ALU.mult),
                      R=[psk[pa], "KA%d" % cb_], W=["ta%d" % t_])
                P.add("dve", lambda e, pb2=pb2, t_=t_, cb_=cb_, f1l=f1l: e.tensor_tensor(out=tb2[t_], in0=ps[pb2][:, :], in1=KB[cb_][:, f1l, :], op=ALU.mult),
                      R=[psk[pb2], "KB%d" % cb_], W=["tb2%d" % t_])
                P.add("pool", lambda e, t_=t_: e.tensor_tensor(out=Yc[t_], in0=ta[t_], in1=tb2[t_], op=ALU.add),
                      R=["ta%d" % t_, "tb2%d" % t_], W=["Yc%d" % t_])
                P.add("pe", lambda e, t_=t_, pc=pc: e.matmul(ps[pc][:, :], lhsT=Gm, rhs=Yc[t_], start=True, stop=True),
                      R=["Gm", "Yc%d" % t_], W=[psk[pc]])
                P.add("act", lambda e, pc=pc, cb_=cb_, f1l=f1l: e.copy(out=Btsb[cb_][:, f1l, :], in_=ps[pc][:, :]),
                      R=[psk[pc]], W=["Btsb%d_%d" % (cb_, f1l)])
            P.add("sp", lambda e, fc=fc, cb_=cb_: e.dma_start(out=Btd[:, fc * 8:(fc + 1) * 8, :], in_=Btsb[cb_]),
                  R=["Btsb%d_%d" % (cb_, k) for k in range(8)], W=["Btd"], dma="Btsb%d" % cb_)
        P.barrier()
        P.release()
        P.mark()
        BtT = [P.sb([128, 8, 512], BF16, "BtT%d" % i) for i in range(2)]
        gch = [P.sb([64, 8, 512], F32, "gch%d" % i) for i in range(2)]
        zo = [P.sb([64, 8, 512], F32, "zo%d" % i) for i in range(2)]
        M = 64 if o == 0 else 32
        gcol = 0 if o == 0 else 512
        dst = blocked(z1_d) if o == 0 else blocked(hout_d)
        dkey = "z1_d" if o == 0 else "hout_d"
        for tc in range(8):
            cb_ = tc % 2
            for r in range(2):
                P.add("sp", lambda e, r=r, tc=tc, cb_=cb_: e.dma_start(
                    out=BtT[cb_][r * 64:(r + 1) * 64, :, :], in_=Btd[r * 64 + tc * 8:r * 64 + tc * 8 + 8, :, :].rearrange("t f c -> f t c")),
                    R=["Btd"], W=["BtT%d" % cb_], dma="BtT%d_%d" % (cb_, r))
            P.add("sp", lambda e, tc=tc, cb_=cb_, M=M, gcol=gcol: e.dma_start(
                out=gch[cb_][0:M, :, :], in_=blocked(U_d[0:M * 64, gcol:gcol + 512])[:, tc * 8:(tc + 1) * 8, :]),
                R=["U_d"], W=["gch%d" % cb_], dma="gch%d" % cb_)
            for t2l in range(8):
                t2 = tc * 8 + t2l
                pb_ = 6 + t2l % 2
                P.add("pe", lambda e, t2=t2, t2l=t2l, cb_=cb_, pb_=pb_, M=M: e.matmul(ps[pb_][0:M, :], lhsT=Dinv[:, t2, 0:M], rhs=BtT[cb_][:, t2l, :],
                                                                                     start=True, stop=True), R=["Dinv", "BtT%d" % cb_], W=[psk[pb_]])
                P.add("dve", lambda e, t2l=t2l, cb_=cb_, pb_=pb_, M=M: e.tensor_tensor(out=zo[cb_][0:M, t2l, :], in0=ps[pb_][0:M, :], in1=gch[cb_][0:M, t2l, :], op=ALU.mult),
                      R=[psk[pb_], "gch%d" % cb_], W=["zo%d_%d" % (cb_, t2l)])
            P.add("sp", lambda e, tc=tc, cb_=cb_, M=M, dst=dst: e.dma_start(out=dst[0:M, tc * 8:(tc + 1) * 8, :], in_=zo[cb_][0:M, :, :]),
                  R=["zo%d_%d" % (cb_, k) for k in range(8)], W=[dkey], dma="zo%d" % cb_)
        P.barrier()
        P.release()
    P.barrier()
    P.release()
    if "z1" in dbg:
        tz1 = dbg_out("z1", [S, 512])
        P.mark()
        tz = P.sb([128, 512], F32, "tz")
        for i in range(NT):
            P.add("sp", lambda e, i=i: e.dma_start(out=tz, in_=z1_d[i * 128:(i + 1) * 128, :]), R=["z1_d"], W=["tz"], dma="dbg0")
            P.add("sp", lambda e, i=i: e.dma_start(out=tz1[i * 128:(i + 1) * 128, :], in_=tz), R=["tz"], W=["dbgo"], dma="dbg1")
        P.barrier()
        P.release()
    if "h_out" in dbg:
        tho = dbg_out("h_out", [OWN, 512])
        P.mark()
        tz_ = P.sb([128, 512], F32, "tz_")
        for i in range(NTO):
            P.add("sp", lambda e, i=i: e.dma_start(out=tz_, in_=hout_d[i * 128:(i + 1) * 128, :]), R=["hout_d"], W=["tz_"], dma="dbg0")
            P.add("sp", lambda e, i=i: e.dma_start(out=tho[i * 128:(i + 1) * 128, :], in_=tz_), R=["tz_"], W=["dbgo"], dma="dbg1")
        P.barrier()
        P.release()


def hyena_tables(half):
    N = 8192
    f1 = np.arange(64, dtype=np.float64)
    s1 = np.arange(64, dtype=np.float64)
    s2 = np.arange(64, dtype=np.float64)
    th = 2 * np.pi * (f1[None, None, :] + 0.5) * (64 * s1[:, None, None] + s2[None, :, None]) / N
    D0 = np.concatenate([np.cos(th), -np.sin(th)], axis=2)
    perm = (np.arange(64) + 32 * half) % 64
    Dc = D0[perm]
    f2 = np.arange(64, dtype=np.float64)
    ph = 2 * np.pi * np.outer(s2, f2) / 64
    c, s_ = np.cos(ph), np.sin(ph)
    F2a = np.block([[c, -s_], [s_, c]])
    F2b = np.block([[s_, c], [-c, s_]])
    G = np.block([[c, s_], [-s_, c]])
    thi = 2 * np.pi * (f1[:, None, None] + 0.5) * (64 * s1[None, None, :] + s2[None, :, None]) / N
    Dinv0 = np.concatenate([np.cos(thi), -np.sin(thi)], axis=0) * (2.0 / N)
    Dinv = Dinv0[:, :, perm]
    sgn = np.ones((128, 1)); sgn[64:] = -1
    L = S
    t = np.arange(L, dtype=np.float32)
    t01 = t / np.float32(L)
    bands = np.linspace(1e-4, 15, 16, dtype=np.float32)
    ang = (np.float32(2.0 * math.pi) * t[:, None] * bands[None, :] / np.float32(L)).astype(np.float32)
    z = np.concatenate([t01[:, None], np.cos(ang), -np.sin(ang)], axis=-1).astype(np.float32)
    nt01 = (-t01).reshape(NT, 128).T
    f = np.float32
    return {
        "hy_D0": np.ascontiguousarray(D0.reshape(64, 64 * 128).astype(f)), "hy_Dc": np.ascontiguousarray(Dc.reshape(64, 64 * 128).astype(f)),
        "hy_F2a": np.ascontiguousarray(F2a.astype(f)), "hy_F2b": np.ascontiguousarray(F2b.astype(f)), "hy_G": np.ascontiguousarray(G.astype(f)),
        "hy_Dinv": np.ascontiguousarray(Dinv.reshape(128, 64 * 64).astype(f)), "hy_sgn": sgn.astype(f),
        "hy_zT": np.ascontiguousarray(z.T), "hy_nt01": np.ascontiguousarray(nt01.astype(f)),
    }


def host_inputs(inputs, core):
    b, half = divmod(core, 2)
    f32 = np.float32
    x = np.asarray(inputs["x"], dtype=f32)[b]
    own = slice(half * OWN, (half + 1) * OWN)
    oth = slice((1 - half) * OWN, (2 - half) * OWN)
    pos = np.concatenate([np.arange(S)[own], np.arange(S)[oth]])
    m = {}
    m["x_rot"] = np.ascontiguousarray(np.concatenate([x[own], x[oth]], axis=0))
    m["mem_b"] = np.ascontiguousarray(np.asarray(inputs["mem"], dtype=f32)[b])
    for k in ("mix_norm_g", "q_norm_g", "kv_norm_g", "hy_conv_b", "attn_out_g", "hy_out_g", "cross_norm_g",
              "mem_norm_g", "ffn_norm_g"):
        m[k] = np.ascontiguousarray(np.asarray(inputs[k], dtype=f32).reshape(1, -1))
    m["final_norm_g"] = np.ascontiguousarray(np.asarray(inputs["final_norm_g"], dtype=f32).reshape(1, -1))
    for k in ("w_in", "w_uq", "w_ukv", "hy_conv_w", "w_out", "w_mq", "w_mkv", "w_mo"):
        m[k] = np.ascontiguousarray(np.asarray(inputs[k], dtype=f32)[0])
    m["w_route"] = np.ascontiguousarray(np.concatenate([np.asarray(inputs["w_route_group"], f32)[0],
                                                        np.asarray(inputs["w_route_expert"], f32)[0]], axis=1))
    m["b_route"] = np.ascontiguousarray(np.concatenate([np.asarray(inputs["b_route_group"], f32)[0],
                                                        np.asarray(inputs["b_route_expert"], f32)[0]], axis=0).reshape(1, 36))
    m["w_gate"] = np.ascontiguousarray(np.asarray(inputs["w_gate"], f32)[0].reshape(32, D, 256))
    m["w_up"] = np.ascontiguousarray(np.asarray(inputs["w_up"], f32)[0].reshape(32, D, 256))
    m["w_down"] = np.ascontiguousarray(np.asarray(inputs["w_down"], f32)[0].reshape(32, 256, D))
    m["ident"] = np.eye(128, dtype=f32)
    inv = (10000.0 ** (-np.arange(16, dtype=np.float64) / 16)).astype(f32)
    ang = pos.astype(f32)[:, None] * inv[None, :]
    cs = np.concatenate([np.cos(ang), np.sin(ang)], axis=1).astype(f32)
    m["rope_cs"] = np.ascontiguousarray(cs.reshape(NT, 128, 32).transpose(1, 0, 2).reshape(128, NT * 32))
    m.update(hyena_tables(half))
    m["hy_cols"] = np.ascontiguousarray(np.stack([np.asarray(inputs["hy_b1"], f32)[0], np.asarray(inputs["hy_b2"], f32)[0],
                                                  np.asarray(inputs["hy_freq"], f32)[0, 0], np.asarray(inputs["hy_freq"], f32)[0, 1]], axis=1))
    for k in ("hy_w1", "hy_w2", "hy_w3"):
        m[k] = np.ascontiguousarray(np.asarray(inputs[k], f32)[0])
    m["hy_b3"] = np.ascontiguousarray(np.asarray(inputs["hy_b3"], f32).reshape(1, 2048))
    m["hy_decay"] = np.ascontiguousarray(np.asarray(inputs["hy_decay"], f32).reshape(1, 2048))
    m["hy_skip"] = np.ascontiguousarray(np.asarray(inputs["hy_skip"], f32).reshape(1, 1024))
    hm = np.zeros((128, 2), f32)
    hm[:, 0] = half
    hm[:, 1] = 1 - half
    m["halfmask"] = hm
    return m


def kernel(**inputs):
    n = 8
    nc, _ = build()
    in_maps = [host_inputs(inputs, c) for c in range(n)]
    res = run_bass_kernel_spmd(nc, in_maps, core_ids=list(range(n)))
    out = np.zeros((4, S, D), np.float32)
    for c in range(n):
        b, half = divmod(c, 2)
        out[b, half * OWN:(half + 1) * OWN] = res.results[c]["out"]
    return out
```

```python
import math
import os
import contextlib
import numpy as np
import concourse.bass as bass
import concourse.mybir as mybir
from concourse.bass_utils import run_bass_kernel_spmd

F32 = mybir.dt.float32
BF16 = mybir.dt.bfloat16
AF = mybir.ActivationFunctionType
ALU = mybir.AluOpType
AX = mybir.AxisListType
ENGS = ("pe", "act", "dve", "pool", "sp")

D = 1024
S = 4096
OWN = 2048
NT = 32
NTO = 16
EPS = 1e-6
HD = 96
NH = 8


class Op:
    __slots__ = ("eng", "fn", "deps", "dma", "flag", "seq", "idx", "dmaval")

    def __init__(self, eng, fn, dma):
        self.eng = eng
        self.fn = fn
        self.deps = set()
        self.dma = dma
        self.flag = False
        self.seq = 0
        self.dmaval = 0


class Prog:
    ARENA_WORDS = 51200

    def __init__(self, nc):
        self.nc = nc
        self.ops = []
        self.lastw = {}
        self.readers = {}
        self.dma_count = {}
        self.sb_off = 0
        self.sb_marks = []
        self.arena = None

    def sb(self, shape, dtype, name=None):
        if self.arena is None:
            self.arena = self.nc.alloc_sbuf_tensor("arena", [128, self.ARENA_WORDS], F32)
        esz = 4 if dtype == F32 else 2
        nel = int(np.prod(shape[1:]))
        nwords = (nel * esz + 3) // 4
        nwords = (nwords + 15) // 16 * 16
        o = self.sb_off
        self.sb_off += nwords
        assert self.sb_off <= self.ARENA_WORDS, ("SBUF overflow", self.sb_off * 4, name)
        v = self.arena[0:shape[0], o:o + nwords]
        if esz == 2:
            v = v.bitcast(dtype)[:, 0:nel]
        else:
            v = v[:, 0:nel]
        if len(shape) > 2:
            names = " ".join("a%d" % i for i in range(len(shape) - 1))
            kw = {"a%d" % i: int(shape[i + 1]) for i in range(len(shape) - 1)}
            v = v.rearrange("p (%s) -> p %s" % (names, names), **kw)
        return v

    def mark(self):
        self.sb_marks.append(self.sb_off)

    def release(self):
        self.sb_off = self.sb_marks.pop()

    def add(self, eng, fn, R=(), W=(), dma=None):
        op = Op(eng, fn, dma)
        op.idx = len(self.ops)
        if eng != "pe":
            psr = [r for r in R if isinstance(r, str) and r.startswith("ps") and r[2:].isdigit()]
            if psr:
                R = [r for r in R if r not in psr]
                W = list(W) + psr
        deps = set()
        for r in R:
            lw = self.lastw.get(r)
            if lw is not None:
                deps.add(lw)
        for w in W:
            lw = self.lastw.get(w)
            if lw is not None:
                deps.add(lw)
            for rd in self.readers.get(w, ()):
                deps.add(rd)
        if dma is not None:
            k = ("__dmasem", dma)
            lw = self.lastw.get(k)
            if lw is not None:
                deps.add(lw)
            self.lastw[k] = op
            self.dma_count[dma] = self.dma_count.get(dma, 0) + 1
            op.dmaval = 16 * self.dma_count[dma]
        deps.discard(op)
        for d in deps:
            if d.dma is None and d.eng == "pe" and eng == "pe" and dma is None:
                continue
            op.deps.add(d)
            d.flag = True
        for r in R:
            self.readers.setdefault(r, []).append(op)
        for w in W:
            self.lastw[w] = op
            self.readers[w] = []
        self.ops.append(op)
        return op

    def barrier(self):
        fr = {}
        dmas = set()
        allops = set(self.lastw.values())
        for v in self.readers.values():
            allops.update(v)
        for o in allops:
            if o.dma is not None:
                dmas.add(o)
            elif o.eng not in fr or fr[o.eng].idx < o.idx:
                fr[o.eng] = o
        for e in ENGS:
            op = Op(e, None, None)
            op.idx = len(self.ops)
            for d in list(fr.values()) + list(dmas):
                op.deps.add(d)
                d.flag = True
            self.ops.append(op)
        newlw = {}
        for k, v in self.lastw.items():
            if isinstance(k, tuple) and k and k[0] == "__dmasem":
                newlw[k] = v
        self.lastw = newlw
        self.readers = {}

    def emit(self):
        nc = self.nc
        with contextlib.ExitStack() as st:
            esem = {e: st.enter_context(nc.semaphore("s_" + e)) for e in ENGS}
            dsem = {}
            for k in self.dma_count:
                dsem[k] = st.enter_context(nc.semaphore("d_%d" % len(dsem)))
            cnt = {e: 0 for e in ENGS}
            for op in self.ops:
                if op.dma is None and op.flag:
                    cnt[op.eng] += 1
                    op.seq = cnt[op.eng]
            byeng = {e: [o for o in self.ops if o.eng == e] for e in ENGS}
            if os.environ.get("KDEBUG"):
                print("sem counts", cnt, "ndma sems", len(dsem), "nops", {e: len(v) for e, v in byeng.items()})
            block = st.enter_context(nc.Block())

            def run(e, eng):
                waited = {}
                for op in byeng[e]:
                    need = {}
                    for d in op.deps:
                        if d.dma is not None:
                            s, v = dsem[d.dma], d.dmaval
                        else:
                            s, v = esem[d.eng], d.seq
                        key = id(s)
                        if waited.get(key, 0) >= v:
                            continue
                        if key not in need or need[key][1] < v:
                            need[key] = (s, v)
                    for key, (s, v) in need.items():
                        eng.wait_ge(s, v)
                        waited[key] = v
                    if op.fn is None:
                        continue
                    ins = op.fn(eng)
                    if op.dma is not None:
                        ins.then_inc(dsem[op.dma], 16)
                    elif op.flag:
                        ins.then_inc(esem[e], 1)

            @block.tensor
            def _(eng):
                run("pe", eng)

            @block.scalar
            def _(eng):
                run("act", eng)

            @block.vector
            def _(eng):
                run("dve", eng)

            @block.gpsimd
            def _(eng):
                run("pool", eng)

            @block.sync
            def _(eng):
                run("sp", eng)


INPUT_SHAPES = {
    "x_rot": [S, D], "mem_b": [256, D],
    "mix_norm_g": [1, D], "w_in": [D, 1952], "q_norm_g": [1, 256], "kv_norm_g": [1, 128],
    "w_uq": [256, 768], "w_ukv": [128, 1024], "hy_conv_w": [3, 1536], "hy_conv_b": [1, 1536],
    "attn_out_g": [1, 512], "hy_out_g": [1, 512], "w_out": [D, D],
    "cross_norm_g": [1, D], "mem_norm_g": [1, D], "w_mq": [D, D], "w_mkv": [D, 2 * D], "w_mo": [D, D],
    "ffn_norm_g": [1, D], "w_route": [D, 36], "b_route": [1, 36],
    "w_gate": [32, D, 256], "w_up": [32, D, 256], "w_down": [32, 256, D], "final_norm_g": [1, D],
    "ident": [128, 128], "rope_cs": [128, NT * 32], "halfmask": [128, 2],
    "hy_D0": [64, 64 * 128], "hy_Dc": [64, 64 * 128], "hy_F2a": [128, 128], "hy_F2b": [128, 128], "hy_G": [128, 128],
    "hy_Dinv": [128, 64 * 64], "hy_sgn": [128, 1], "hy_zT": [33, S], "hy_nt01": [128, NT],
    "hy_cols": [64, 4], "hy_w1": [33, 64], "hy_w2": [64, 64], "hy_w3": [64, 2048], "hy_b3": [1, 2048], "hy_decay": [1, 2048],
    "hy_skip": [1, 1024],
}


def build(stop=None, dbg=()):
    nc = bass.Bass("TRN2", target_bir_lowering=False)
    I = {k: nc.dram_tensor(k, v, F32, kind="ExternalInput").ap() for k, v in INPUT_SHAPES.items()}
    out_d = nc.dram_tensor("out", [OWN, D], F32, kind="ExternalOutput").ap()
    dbg_d = {}
    U_d = nc.dram_tensor("U_scr", [S, 1536], F32, kind="Internal").ap()
    hout_d = nc.dram_tensor("hout_scr", [OWN, 512], F32, kind="Internal").ap()
    combT_d = nc.dram_tensor("combT_scr", [32, OWN], F32, kind="Internal").ap()

    P = Prog(nc)
    ps = [nc.alloc_psum_tensor("ps%d" % i, [128, 512], F32) for i in range(8)]
    psk = ["ps%d" % i for i in range(8)]

    def psb(i):
        return ps[i][:].bitcast(BF16)

    def dbg_out(name, shape):
        t = nc.dram_tensor("dbg_" + name, shape, F32, kind="ExternalOutput").ap()
        dbg_d[name] = t
        return t

    cnt = [0]

    def uid(s):
        cnt[0] += 1
        return "%s_%d" % (s, cnt[0])

    identf = P.sb([128, 128], F32, "identf")
    identb = P.sb([128, 128], BF16, "identb")
    halfm = P.sb([128, 2], F32, "halfm")
    st = P.sb([128, 8], F32, "st")
    junk = P.sb([128, 1024], F32, "junk")
    gb = P.sb([128, 1024], F32, "gb")
    onesb = P.sb([128, 128], BF16, "onesb")
    onesf = P.sb([128, 128], F32, "onesf")
    P.add("sp", lambda e: e.dma_start(out=identf, in_=I["ident"]), W=["identf"], dma="c0")
    P.add("sp", lambda e: e.dma_start(out=halfm, in_=I["halfmask"]), W=["halfm"], dma="c1")
    P.add("dve", lambda e: e.tensor_copy(out=identb, in_=identf), R=["identf"], W=["identb"])
    P.add("pool", lambda e: e.memset(onesb, 1.0), W=["onesb"])
    P.add("pool", lambda e: e.memset(onesf, 1.0), W=["onesf"])

    def load_gain(name, n=D, key="gb"):
        P.add("sp", lambda e: e.dma_start(out=gb[:, 0:n], in_=I[name].partition_broadcast(128)), W=[key], dma="gain")

    st_tiles = {}
    for _n in ("st", "stq", "stk", "sta", "sth", "stm", "stx", "stf", "stg"):
        st_tiles[_n] = P.sb([128, 4], F32, "st_" + _n)

    def rms_norm(src, n, gview, out_bf, Rk, Wk, stk="st"):
        if stk not in st_tiles:
            st_tiles[stk] = P.sb([128, 4], F32, "st_" + stk)
        st = st_tiles[stk]
        P.add("act", lambda e: e.activation(out=junk[:, 0:n], in_=src, func=AF.Square, accum_out=st[:, 0:1]),
              R=Rk, W=["junk", stk + "0"])
        P.add("dve", lambda e: e.tensor_scalar(out=st[:, 1:2], in0=st[:, 0:1], scalar1=1.0 / n, scalar2=EPS,
                                               op0=ALU.mult, op1=ALU.add), R=[stk + "0"], W=[stk + "1"])
        P.add("act", lambda e: e.sqrt(out=st[:, 2:3], in_=st[:, 1:2]), R=[stk + "1"], W=[stk + "2"])
        P.add("dve", lambda e: e.reciprocal(out=st[:, 3:4], in_=st[:, 2:3]), R=[stk + "2"], W=[stk + "3"])
        P.add("dve", lambda e: e.scalar_tensor_tensor(out=out_bf, in0=src, scalar=st[:, 3:4], in1=gview,
                                                      op0=ALU.mult, op1=ALU.mult),
              R=list(Rk) + [stk + "3", "gb"], W=Wk)

    P.mark()
    hqT = P.sb([128, 2, OWN], BF16, "hqT")
    hkvT = P.sb([128, S], BF16, "hkvT")
    krot = P.sb([128, NT, 32], F32, "krot")
    ropecs = P.sb([128, NT, 32], F32, "ropecs")
    P.add("sp", lambda e: e.dma_start(out=ropecs.rearrange("p a b -> p (a b)"), in_=I["rope_cs"]), W=["ropecs"], dma="c2")

    P.mark()
    hT = P.sb([128, 8, 2, OWN + 2], BF16, "hT")
    xt = [P.sb([128, D], F32, "xt%d" % i) for i in range(2)]
    xn = [P.sb([128, D], BF16, "xn%d" % i) for i in range(2)]
    load_gain("mix_norm_g")
    for i in range(NT):
        b = i % 2
        seg, j = divmod(i, NTO)
        P.add("sp", lambda e, i=i, b=b: e.dma_start(out=xt[b], in_=I["x_rot"][i * 128:(i + 1) * 128, :]),
              W=["xt%d" % b], dma="xt%d" % b)
        rms_norm(xt[b], D, gb, xn[b], ["xt%d" % b], ["xn%d" % b])
        pb = 0 + b
        for k in range(8):
            P.add("pe", lambda e, k=k, b=b, pb=pb: e.transpose(out=psb(pb)[:, k * 128:(k + 1) * 128],
                                                                 in_=xn[b][:, k * 128:(k + 1) * 128], identity=identb),
                  R=["xn%d" % b, "identb"], W=[psk[pb]])
        eng = "act" if b == 0 else "dve"
        dst = hT[:, :, seg, 1 + j * 128:1 + (j + 1) * 128]
        src = psb(pb).rearrange("p (k t) -> p k t", k=8)
        if eng == "act":
            P.add("act", lambda e, dst=dst, src=src: e.copy(out=dst, in_=src), R=[psk[pb]], W=["hT"])
        else:
            P.add("dve", lambda e, dst=dst, src=src: e.tensor_copy(out=dst, in_=src), R=[psk[pb]], W=["hT"])
    for (ds, dc, ss_, sc, m) in ((0, 0, 1, OWN, 0), (0, OWN + 1, 1, 1, 1), (1, 0, 0, OWN, 1), (1, OWN + 1, 0, 1, 0)):
        P.add("dve", lambda e, ds=ds, dc=dc, ss_=ss_, sc=sc, m=m: e.tensor_scalar_mul(
            out=hT[:, :, ds, dc:dc + 1], in0=hT[:, :, ss_, sc:sc + 1], scalar1=halfm[:, m:m + 1]),
            R=["hT", "halfm"], W=["hT"])

    w_mla = P.sb([128, 8, 416], BF16, "w_mla")
    P.add("pool", lambda e: e.dma_start(out=w_mla, in_=I["w_in"][:, 0:416].rearrange("(k p) n -> p k n", p=128)),
          W=["w_mla"], dma="w0")
    gq = P.sb([128, 256], F32, "gq")
    gkv = P.sb([128, 128], F32, "gkv")
    P.add("sp", lambda e: e.dma_start(out=gq, in_=I["q_norm_g"].partition_broadcast(128)), W=["gq"], dma="c3")
    P.add("sp", lambda e: e.dma_start(out=gkv, in_=I["kv_norm_g"].partition_broadcast(128)), W=["gkv"], dma="c4")
    hqn = [P.sb([128, 256], BF16, "hqn%d" % i) for i in range(2)]
    hkvn = [P.sb([128, 128], BF16, "hkvn%d" % i) for i in range(2)]
    tmp16 = P.sb([128, 4, 16], F32, "tmp16")
    for i in range(NT):
        b = i % 2
        seg, j = divmod(i, NTO)
        pb = 2 + b
        for k in range(8):
            P.add("pe", lambda e, k=k, pb=pb, seg=seg, j=j: e.matmul(
                ps[pb][:, 0:416], lhsT=hT[:, k, seg, 1 + j * 128:1 + (j + 1) * 128], rhs=w_mla[:, k, :],
                start=(k == 0), stop=(k == 7)), R=["hT", "w_mla"], W=[psk[pb]])
        if seg == 0:
            rms_norm(ps[pb][:, 0:256], 256, gq, hqn[b], [psk[pb], "gq"], ["hqn%d" % b], stk="stq")
            pt = 4 + b
            for k in range(2):
                P.add("pe", lambda e, k=k, b=b, pt=pt: e.transpose(out=psb(pt)[:, k * 128:(k + 1) * 128],
                                                                     in_=hqn[b][:, k * 128:(k + 1) * 128], identity=identb),
                      R=["hqn%d" % b, "identb"], W=[psk[pt]])
            P.add("act", lambda e, pt=pt, j=j: e.copy(out=hqT[:, :, j * 128:(j + 1) * 128],
                                                       in_=psb(pt)[:, 0:256].rearrange("p (k t) -> p k t", k=2)),
                  R=[psk[pt]], W=["hqT"])
        rms_norm(ps[pb][:, 256:384], 128, gkv, hkvn[b], [psk[pb], "gkv"], ["hkvn%d" % b], stk="stk")
        pt = 6 + b
        P.add("pe", lambda e, b=b, pt=pt: e.transpose(out=psb(pt)[:, 0:128], in_=hkvn[b], identity=identb),
              R=["hkvn%d" % b, "identb"], W=[psk[pt]])
        P.add("act", lambda e, pt=pt, i=i: e.copy(out=hkvT[:, i * 128:(i + 1) * 128], in_=psb(pt)[:, 0:128]),
              R=[psk[pt]], W=["hkvT"])
        x1 = ps[pb][:, 384:400]
        x2 = ps[pb][:, 400:416]
        c = ropecs[:, i, 0:16]
        s_ = ropecs[:, i, 16:32]
        P.add("dve", lambda e, x1=x1, c=c: e.tensor_tensor(out=tmp16[:, 0, :], in0=x1, in1=c, op=ALU.mult), R=[psk[pb], "ropecs"], W=["t16a"])
        P.add("dve", lambda e, x2=x2, s_=s_: e.tensor_tensor(out=tmp16[:, 1, :], in0=x2, in1=s_, op=ALU.mult), R=[psk[pb], "ropecs"], W=["t16b"])
        P.add("dve", lambda e, x1=x1, s_=s_: e.tensor_tensor(out=tmp16[:, 2, :], in0=x1, in1=s_, op=ALU.mult), R=[psk[pb], "ropecs"], W=["t16c"])
        P.add("dve", lambda e, x2=x2, c=c: e.tensor_tensor(out=tmp16[:, 3, :], in0=x2, in1=c, op=ALU.mult), R=[psk[pb], "ropecs"], W=["t16d"])
        P.add("dve", lambda e, i=i: e.tensor_tensor(out=krot[:, i, 0:16], in0=tmp16[:, 0, :], in1=tmp16[:, 1, :], op=ALU.subtract),
              R=["t16a", "t16b"], W=["krot"])
        P.add("dve", lambda e, i=i: e.tensor_tensor(out=krot[:, i, 16:32], in0=tmp16[:, 2, :], in1=tmp16[:, 3, :], op=ALU.add),
              R=["t16c", "t16d"], W=["krot"])

    w_hy = P.sb([128, 8, 512], BF16, "w_hy")
    w_k3 = P.sb([128, 3, 8, 512], BF16, "w_k3")
    cw = P.sb([128, 3, 512], F32, "cw")
    brow = P.sb([1, 512], BF16, "brow")
    uo = [P.sb([128, 512], F32, "uo%d" % i) for i in range(2)]
    for c3 in range(3):
        c0 = 416 + c3 * 512
        P.add("pool", lambda e, c0=c0: e.dma_start(out=w_hy, in_=I["w_in"][:, c0:c0 + 512].rearrange("(k p) n -> p k n", p=128)),
              W=["w_hy"], dma="w1")
        for k3 in range(3):
            P.add("sp", lambda e, k3=k3, c3=c3: e.dma_start(
                out=cw[:, k3, :], in_=I["hy_conv_w"][k3:k3 + 1, c3 * 512:(c3 + 1) * 512].partition_broadcast(128)),
                W=["cw%d" % k3], dma="cw%d" % k3)
        P.add("pool", lambda e, c3=c3: e.dma_start(out=brow, in_=I["hy_conv_b"][0:1, c3 * 512:(c3 + 1) * 512]),
              W=["brow"], dma="w2")
        for k3 in range(3):
            for k in range(8):
                P.add("dve" if k % 2 == 0 else "pool", lambda e, k3=k3, k=k: e.tensor_tensor(
                    out=w_k3[:, k3, k, :], in0=w_hy[:, k, :], in1=cw[:, k3, :], op=ALU.mult),
                    R=["w_hy", "cw%d" % k3], W=["w_k3_%d_%d" % (k3, k)])
        for i in range(NT):
            b = i % 2
            seg, j = divmod(i, NTO)
            pb = 2 + b
            n = 0
            for k3 in range(3):
                for k in range(8):
                    P.add("pe", lambda e, k3=k3, k=k, pb=pb, seg=seg, j=j, n=n: e.matmul(
                        ps[pb][:, :], lhsT=hT[:, k, seg, k3 + j * 128:k3 + (j + 1) * 128], rhs=w_k3[:, k3, k, :],
                        start=(n == 0), stop=False), R=["hT", "w_k3_%d_%d" % (k3, k)], W=[psk[pb]])
                    n += 1
            P.add("pe", lambda e, pb=pb: e.matmul(ps[pb][:, :], lhsT=onesb[0:1, 0:128], rhs=brow[0:1, :], start=False, stop=True),
                  R=["onesb", "brow"], W=[psk[pb]])
            if b == 0:
                P.add("act", lambda e, pb=pb, b=b: e.copy(out=uo[b], in_=ps[pb][:, :]), R=[psk[pb]], W=["uo%d" % b])
            else:
                P.add("dve", lambda e, pb=pb, b=b: e.tensor_copy(out=uo[b], in_=ps[pb][:, :]), R=[psk[pb]], W=["uo%d" % b])
            P.add("sp", lambda e, i=i, b=b, c3=c3: e.dma_start(out=U_d[i * 128:(i + 1) * 128, c3 * 512:(c3 + 1) * 512], in_=uo[b]),
                  R=["uo%d" % b], W=["U_d"], dma="uo%d" % b)
    P.barrier()
    P.release()

    if stop == "A0":
        P.add("sp", None, R=[])
        P.emit()
        return nc, dbg_d
    if "uc" in dbg:
        tu = dbg_out("uc", [S, 1536])
        P.mark()
        tb = P.sb([128, 1536], F32, "dbgt")
        for i in range(NT):
            P.add("sp", lambda e, i=i: e.dma_start(out=tb, in_=U_d[i * 128:(i + 1) * 128, :]), R=["U_d"], W=["dbgt"], dma="dbg0")
            P.add("sp", lambda e, i=i: e.dma_start(out=tu[i * 128:(i + 1) * 128, :], in_=tb), R=["dbgt"], W=["dbgo"], dma="dbg1")
        P.barrier()
        P.release()

    if stop == "A":
        P.add("sp", None, R=["dbgo"])
        P.emit()
        return nc, dbg_d
    aout_d = nc.dram_tensor("aout_scr", [OWN, 512], F32, kind="Internal").ap()
    P.mark()
    G4 = 4
    KT = P.sb([128, G4, S], BF16, "KT")
    QT = P.sb([128, G4, OWN], BF16, "QT")
    Vaug = P.sb([128, NT, G4, 68], BF16, "Vaug")
    w_ukv = P.sb([128, 1024], BF16, "w_ukv")
    w_uq = P.sb([128, 2, 768], BF16, "w_uq")
    P.add("pool", lambda e: e.dma_start(out=w_ukv, in_=I["w_ukv"]), W=["w_ukv"], dma="w0")
    P.add("pool", lambda e: e.dma_start(out=w_uq, in_=I["w_uq"].rearrange("(k p) n -> p k n", p=128)), W=["w_uq"], dma="w1")
    Kaug = [P.sb([128, G4, 100], BF16, "Kaug%d" % i) for i in range(2)]
    Qaug = [P.sb([128, G4, 100], BF16, "Qaug%d" % i) for i in range(2)]
    ksq = P.sb([128, G4, 96], F32, "ksq")
    kn2 = P.sb([128, G4], F32, "kn2")
    kmax = P.sb([128, G4], F32, "kmax")
    kb = P.sb([128, 4], F32, "kb")
    qs = P.sb([128, G4, 96], F32, "qs")
    qsq = P.sb([128, G4, 96], F32, "qsq")
    qn = P.sb([128, G4], F32, "qn")
    qt4 = P.sb([128, 4, G4, 16], F32, "qt4")
    PT = [P.sb([128, 512], BF16, "PT%d" % i) for i in range(3)]
    oTs = P.sb([65, 512], F32, "oTs")
    rden = P.sb([128, 4], F32, "rden")
    astage = [P.sb([128, 4, 256], F32, "astage%d" % i) for i in range(2)]
    scale = HD ** -0.5
    it = 0
    for g in range(2):
        P.add("pool", lambda e: e.memset(Vaug.rearrange("p a b c -> p (a b c)"), 1.0), W=["Vaug"])
        for b in range(2):
            P.add("pool", lambda e, b=b: e.memset(Kaug[b].rearrange("p a b -> p (a b)"), 1.0), W=["Kaug%d" % b])
        P.add("pool", lambda e: e.memset(kmax, 0.0), W=["kmax"])
        for i in range(NT):
            b = i % 2
            pbk = 0 + b
            P.add("pe", lambda e, g=g, pbk=pbk, i=i: e.matmul(ps[pbk][:, :], lhsT=hkvT[:, i * 128:(i + 1) * 128],
                                                               rhs=w_ukv[:, g * 512:(g + 1) * 512], start=True, stop=True),
                  R=["hkvT", "w_ukv"], W=[psk[pbk]])
            v = ps[pbk][:, :].rearrange("p (h c) -> p h c", h=4)
            KCUT = int(os.environ.get("KCUT", "9"))
            if KCUT < 1:
                continue
            P.add("act", lambda e, v=v, i=i: e.copy(out=Vaug[:, i, :, 0:64], in_=v[:, :, 64:128]), R=[psk[pbk]], W=["Vaug", "ser%d" % pbk])
            P.add("dve", lambda e, v=v, b=b: e.tensor_copy(out=Kaug[b][:, :, 0:64], in_=v[:, :, 0:64]), R=[psk[pbk], "ser%d" % pbk], W=["Kaug%d" % b])
            if KCUT < 2:
                continue
            for h in range(G4):
                P.add("pool", lambda e, h=h, b=b, i=i: e.tensor_copy(out=Kaug[b][:, h, 64:96], in_=krot[:, i, :]),
                      R=["krot"], W=["Kaug%d" % b])
            if KCUT < 3:
                continue
            P.add("dve", lambda e, b=b: e.tensor_tensor(out=ksq, in0=Kaug[b][:, :, 0:96], in1=Kaug[b][:, :, 0:96], op=ALU.mult),
                  R=["Kaug%d" % b], W=["ksq"])
            P.add("dve", lambda e: e.tensor_reduce(out=kn2, in_=ksq, axis=AX.X, op=ALU.add), R=["ksq"], W=["kn2"])
            P.add("dve", lambda e: e.tensor_tensor(out=kmax, in0=kmax, in1=kn2, op=ALU.max), R=["kn2", "kmax"], W=["kmax"])
            if KCUT < 4:
                continue
            pt = 4 + b
            for h in range(G4):
                P.add("pe", lambda e, h=h, b=b, pt=pt: e.transpose(out=psb(pt)[0:97, h * 128:(h + 1) * 128], in_=Kaug[b][:, h, 0:97], identity=identb),
                      R=["Kaug%d" % b, "identb"], W=[psk[pt]])
            P.add("act", lambda e, pt=pt, i=i: e.copy(out=KT[0:97, :, i * 128:(i + 1) * 128],
                                                       in_=psb(pt)[0:97, 0:G4 * 128].rearrange("p (h t) -> p h t", h=G4)),
                  R=[psk[pt]], W=["KT"])
        if KCUT < 5:
            P.add("sp", None, R=[])
            P.barrier()
            P.emit()
            return nc, dbg_d
        P.add("dve", lambda e: e.tensor_reduce(out=kb[:, 1:2], in_=kmax, axis=AX.X, op=ALU.max), R=["kmax"], W=["kb1"])
        P.add("pe", lambda e: e.transpose(out=ps[6][0:1, 0:128], in_=kb[:, 1:2], identity=identf), R=["kb1", "identf"], W=[psk[6]])
        P.add("dve", lambda e: e.tensor_reduce(out=kb[0:1, 2:3], in_=ps[6][0:1, 0:128], axis=AX.X, op=ALU.max), R=[psk[6]], W=["kb2"])
        P.add("pe", lambda e: e.matmul(ps[7][:, 0:1], lhsT=onesf[0:1, 0:128], rhs=kb[0:1, 2:3], start=True, stop=True),
              R=["kb2", "onesf"], W=[psk[7]])
        P.add("act", lambda e: e.sqrt(out=kb[:, 0:1], in_=ps[7][:, 0:1]), R=[psk[7]], W=["kb0"])
        if stop == "K":
            P.add("sp", None, R=[])
            P.barrier()
            P.emit()
            return nc, dbg_d
        for j in range(NTO):
            b = j % 2
            pa = 0 + b
            for k in range(2):
                P.add("pe", lambda e, pa=pa, k=k, j=j, g=g: e.matmul(
                    ps[pa][:, 0:384], lhsT=hqT[:, k, j * 128:(j + 1) * 128], rhs=w_uq[:, k, g * 384:(g + 1) * 384],
                    start=(k == 0), stop=(k == 1)), R=["hqT", "w_uq"], W=[psk[pa]])
            P.add("act", lambda e, pa=pa: e.mul(out=qs, in_=ps[pa][:, 0:384].rearrange("p (h c) -> p h c", h=G4), mul=scale),
                  R=[psk[pa]], W=["qs"])
            c = ropecs[:, j:j + 1, 0:16].broadcast_to([128, G4, 16])
            s_ = ropecs[:, j:j + 1, 16:32].broadcast_to([128, G4, 16])
            x1 = qs[:, :, 64:80]
            x2 = qs[:, :, 80:96]
            P.add("dve", lambda e, x1=x1, c=c: e.tensor_tensor(out=qt4[:, 0], in0=x1, in1=c, op=ALU.mult), R=["qs", "ropecs"], W=["qt4a"])
            P.add("dve", lambda e, x2=x2, s_=s_: e.tensor_tensor(out=qt4[:, 1], in0=x2, in1=s_, op=ALU.mult), R=["qs", "ropecs"], W=["qt4b"])
            P.add("pool", lambda e, x1=x1, s_=s_: e.tensor_tensor(out=qt4[:, 2], in0=x1, in1=s_, op=ALU.mult), R=["qs", "ropecs"], W=["qt4c"])
            P.add("pool", lambda e, x2=x2, c=c: e.tensor_tensor(out=qt4[:, 3], in0=x2, in1=c, op=ALU.mult), R=["qs", "ropecs"], W=["qt4d"])
            P.add("dve", lambda e: e.tensor_tensor(out=qs[:, :, 64:80], in0=qt4[:, 0], in1=qt4[:, 1], op=ALU.subtract),
                  R=["qt4a", "qt4b"], W=["qs"])
            P.add("dve", lambda e: e.tensor_tensor(out=qs[:, :, 80:96], in0=qt4[:, 2], in1=qt4[:, 3], op=ALU.add),
                  R=["qt4c", "qt4d"], W=["qs"])
            P.add("dve", lambda e: e.tensor_tensor(out=qsq, in0=qs, in1=qs, op=ALU.mult), R=["qs"], W=["qsq"])
            P.add("dve", lambda e: e.tensor_reduce(out=qn, in_=qsq, axis=AX.X, op=ALU.add), R=["qsq"], W=["qn"])
            P.add("act", lambda e: e.sqrt(out=qn, in_=qn), R=["qn"], W=["qn"])
            P.add("dve", lambda e, b=b: e.tensor_scalar(out=Qaug[b][:, :, 96:97], in0=qn.rearrange("p (h o) -> p h o", o=1),
                                                        scalar1=kb[:, 0:1], scalar2=-1.0, op0=ALU.mult, op1=ALU.mult),
                  R=["qn", "kb0"], W=["Qaug%d" % b])
            P.add("act", lambda e, b=b: e.copy(out=Qaug[b][:, :, 0:96], in_=qs), R=["qs"], W=["Qaug%d" % b])
            pt = 4 + b
            for h in range(G4):
                P.add("pe", lambda e, h=h, b=b, pt=pt: e.transpose(out=psb(pt)[0:97, h * 128:(h + 1) * 128], in_=Qaug[b][:, h, 0:97], identity=identb),
                      R=["Qaug%d" % b, "identb"], W=[psk[pt]])
            P.add("dve", lambda e, pt=pt, j=j: e.tensor_copy(out=QT[0:97, :, j * 128:(j + 1) * 128],
                                                             in_=psb(pt)[0:97, 0:G4 * 128].rearrange("p (h t) -> p h t", h=G4)),
                  R=[psk[pt]], W=["QT"])
        if stop == "Q":
            P.add("sp", None, R=[])
            P.barrier()
            P.emit()
            return nc, dbg_d
        items = [(qc, h, kt) for qc in range(4) for h in range(G4) for kt in range(NT)]
        LA = 2

        def emit_scores(idx):
            qc, h, kt = items[idx]
            pb_ = idx % 3
            P.add("pe", lambda e, h=h, qc=qc, kt=kt, pb_=pb_: e.matmul(
                ps[pb_][:, :], lhsT=KT[0:97, h, kt * 128:(kt + 1) * 128], rhs=QT[0:97, h, qc * 512:(qc + 1) * 512],
                start=True, stop=True), R=["KT", "QT"], W=[psk[pb_]])

        def emit_epilogue(qc, h):
            po = 6 + h % 2
            sb_ = qc % 2
            P.add("dve", lambda e, po=po: e.tensor_copy(out=oTs, in_=ps[po][0:65, :]), R=[psk[po]], W=["oTs"])
            for t4 in range(4):
                pt = 3 + (t4 % 2)
                P.add("pe", lambda e, t4=t4, pt=pt: e.transpose(out=ps[pt][:, 0:65], in_=oTs[:, t4 * 128:(t4 + 1) * 128], identity=identf[0:65, 0:65]),
                      R=["oTs", "identf"], W=[psk[pt]])
                P.add("dve", lambda e, pt=pt, t4=t4: e.reciprocal(out=rden[:, t4:t4 + 1], in_=ps[pt][:, 64:65]), R=[psk[pt]], W=["rden%d" % t4])
                P.add("dve", lambda e, pt=pt, t4=t4, h=h, sb_=sb_: e.tensor_scalar_mul(
                    out=astage[sb_][:, t4, h * 64:(h + 1) * 64], in0=ps[pt][:, 0:64], scalar1=rden[:, t4:t4 + 1]),
                    R=[psk[pt], "rden%d" % t4], W=["astage%d" % sb_])
            if h == G4 - 1:
                P.add("sp", lambda e, qc=qc, g=g, sb_=sb_: e.dma_start(
                    out=aout_d[qc * 512:(qc + 1) * 512, g * 256:(g + 1) * 256].rearrange("(t p) c -> p t c", p=128), in_=astage[sb_]),
                    R=["astage%d" % sb_], W=["aout_d"], dma="ast%d" % sb_)

        for idx in range(min(LA, len(items))):
            emit_scores(idx)
        pending = None
        for idx, (qc, h, kt) in enumerate(items):
            pb_ = idx % 3
            po = 6 + h % 2
            P.add("act", lambda e, pb_=pb_: e.activation(out=PT[pb_], in_=ps[pb_][:, :], func=AF.Exp),
                  R=[psk[pb_]], W=["PT%d" % pb_])
            if idx + LA < len(items):
                emit_scores(idx + LA)
            P.add("pe", lambda e, h=h, kt=kt, pb_=pb_, po=po: e.matmul(
                ps[po][0:65, :], lhsT=Vaug[:, kt, h, 0:65], rhs=PT[pb_], start=(kt == 0), stop=(kt == NT - 1)),
                R=["Vaug", "PT%d" % pb_], W=[psk[po]])
            if pending is not None and kt == 3:
                emit_epilogue(*pending)
                pending = None
            if kt == NT - 1:
                pending = (qc, h)
        if pending is not None:
            emit_epilogue(*pending)
    P.barrier()
    P.release()
    P.release()

    if "a_out" in dbg:
        ta = dbg_out("a_out", [OWN, 512])
        P.mark()
        tba = P.sb([128, NTO, 512], F32, "dbgt2")
        P.add("sp", lambda e: e.dma_start(out=tba, in_=aout_d.rearrange("(j p) c -> p j c", p=128)), R=["aout_d"], W=["dbgt2"], dma="dbg0")
        P.add("sp", lambda e: e.dma_start(out=ta.rearrange("(j p) c -> p j c", p=128), in_=tba), R=["dbgt2"], W=["dbgo"], dma="dbg1")
        P.barrier()
        P.release()

    if stop == "attn":
        P.add("sp", None, R=[])
        P.emit()
        return nc, dbg_d

    if "hout_in" in dbg:
        hin = nc.dram_tensor("dbg_hout_in", [OWN, 512], F32, kind="ExternalInput").ap()
        P.mark()
        tbh = P.sb([128, NTO, 512], F32, "tbh")
        P.add("sp", lambda e: e.dma_start(out=tbh, in_=hin.rearrange("(j p) c -> p j c", p=128)), W=["tbh"], dma="dbg0")
        P.add("sp", lambda e: e.dma_start(out=hout_d.rearrange("(j p) c -> p j c", p=128), in_=tbh), R=["tbh"], W=["hout_d"], dma="dbg1")
        P.barrier()
        P.release()
    else:
        hyena_phase(nc, P, I, ps, psk, psb, U_d, hout_d, identf, identb, onesb, onesf, halfm, dbg, dbg_out)

    if stop == "C":
        P.add("sp", None, R=[])
        P.emit()
        return nc, dbg_d
    xres = P.sb([128, NTO, D], F32, "xres")
    P.add("sp", lambda e: e.dma_start(out=xres, in_=I["x_rot"][0:OWN, :].rearrange("(j p) c -> p j c", p=128)), W=["xres"], dma="xres")
    P.mark()
    w_out = P.sb([128, 8, D], BF16, "w_out")
    P.add("pool", lambda e: e.dma_start(out=w_out, in_=I["w_out"].rearrange("(k p) n -> p k n", p=128)), W=["w_out"], dma="w0")
    P.add("sp", lambda e: e.dma_start(out=gb[:, 0:512], in_=I["attn_out_g"].partition_broadcast(128)), W=["gb"], dma="gain")
    P.add("sp", lambda e: e.dma_start(out=gb[:, 512:1024], in_=I["hy_out_g"].partition_broadcast(128)), W=["gb"], dma="gain")
    mixin = [P.sb([128, D], F32, "mixin%d" % i) for i in range(2)]
    mixbf = [P.sb([128, D], BF16, "mixbf%d" % i) for i in range(2)]
    mT = [P.sb([128, 8, 128], BF16, "mT%d" % i) for i in range(2)]
    for j in range(NTO):
        b = j % 2
        P.add("sp", lambda e, j=j, b=b: e.dma_start(out=mixin[b][:, 0:512], in_=aout_d[j * 128:(j + 1) * 128, :]),
              R=["aout_d"], W=["mixin%d" % b], dma="mixa%d" % b)
        P.add("sp", lambda e, j=j, b=b: e.dma_start(out=mixin[b][:, 512:1024], in_=hout_d[j * 128:(j + 1) * 128, :]),
              R=["hout_d"] + ["hout_d_%d" % k for k in range(8)], W=["mixin%d" % b], dma="mixh%d" % b)
        rms_norm(mixin[b][:, 0:512], 512, gb[:, 0:512], mixbf[b][:, 0:512], ["mixin%d" % b], ["mixbfa%d" % b], stk="sta")
        rms_norm(mixin[b][:, 512:1024], 512, gb[:, 512:1024], mixbf[b][:, 512:1024], ["mixin%d" % b], ["mixbfh%d" % b], stk="sth")
        pt = 0 + b
        for k in range(8):
            P.add("pe", lambda e, k=k, b=b, pt=pt: e.transpose(out=psb(pt)[:, k * 128:(k + 1) * 128], in_=mixbf[b][:, k * 128:(k + 1) * 128], identity=identb),
                  R=["mixbfa%d" % b, "mixbfh%d" % b, "identb"], W=[psk[pt]])
        P.add("act", lambda e, b=b, pt=pt: e.copy(out=mT[b].rearrange("p k t -> p (k t)"), in_=psb(pt)), R=[psk[pt]], W=["mT%d" % b])
        for n in range(2):
            py = 2 + 2 * b + n
            for k in range(8):
                P.add("pe", lambda e, k=k, b=b, n=n, py=py: e.matmul(ps[py][:, :], lhsT=mT[b][:, k, :], rhs=w_out[:, k, n * 512:(n + 1) * 512],
                                                                      start=(k == 0), stop=(k == 7)), R=["mT%d" % b, "w_out"], W=[psk[py]])
            P.add("dve", lambda e, j=j, n=n, py=py: e.tensor_tensor(out=xres[:, j, n * 512:(n + 1) * 512], in0=ps[py][:, :],
                                                                     in1=xres[:, j, n * 512:(n + 1) * 512], op=ALU.add),
                  R=[psk[py], "xres"], W=["xres"])
    P.barrier()
    P.release()
    if stop == "D":
        P.add("sp", None, R=[])
        P.emit()
        return nc, dbg_d
    if "x1" in dbg:
        tx1 = dbg_out("x1", [OWN, D])
        P.add("sp", lambda e: e.dma_start(out=tx1.rearrange("(j p) c -> p j c", p=128), in_=xres), R=["xres"], W=["dbgo"], dma="dbg1")
        P.barrier()

    P.mark()
    hmT = P.sb([128, 8, 256], BF16, "hmT")
    KmT = P.sb([128, 8, 256], BF16, "KmT")
    Vm = P.sb([128, 2, 4, 260], BF16, "Vm")
    ksqm = P.sb([128, 8, 256], BF16, "ksqm")
    kbx = P.sb([1, 8], F32, "kbx")
    P.mark()
    w_mkv = P.sb([128, 8, 2 * D], BF16, "w_mkv")
    P.add("pool", lambda e: e.dma_start(out=w_mkv, in_=I["w_mkv"].rearrange("(k p) n -> p k n", p=128)), W=["w_mkv"], dma="w0")
    load_gain("mem_norm_g")
    P.add("pool", lambda e: e.memset(Vm.rearrange("p a b c -> p (a b c)"), 1.0), W=["Vm"])
    memt = [P.sb([128, D], F32, "memt%d" % i) for i in range(2)]
    membf = [P.sb([128, D], BF16, "membf%d" % i) for i in range(2)]
    for mt in range(2):
        P.add("sp", lambda e, mt=mt: e.dma_start(out=memt[mt], in_=I["mem_b"][mt * 128:(mt + 1) * 128, :]), W=["memt%d" % mt], dma="memt%d" % mt)
        rms_norm(memt[mt], D, gb, membf[mt], ["memt%d" % mt], ["membf%d" % mt], stk="stm")
        for k in range(8):
            P.add("pe", lambda e, k=k, mt=mt: e.transpose(out=psb(mt)[:, k * 128:(k + 1) * 128], in_=membf[mt][:, k * 128:(k + 1) * 128], identity=identb),
                  R=["membf%d" % mt, "identb"], W=[psk[mt]])
        P.add("act", lambda e, mt=mt: e.copy(out=hmT[:, :, mt * 128:(mt + 1) * 128], in_=psb(mt).rearrange("p (k t) -> p k t", k=8)),
              R=[psk[mt]], W=["hmT"])
    for dt in range(8):
        pk = 2 + dt % 2
        for k in range(8):
            P.add("pe", lambda e, k=k, dt=dt, pk=pk: e.matmul(ps[pk][:, 0:256], lhsT=w_mkv[:, k, dt * 128:(dt + 1) * 128], rhs=hmT[:, k, :],
                                                               start=(k == 0), stop=(k == 7)), R=["w_mkv", "hmT"], W=[psk[pk]])
        P.add("act", lambda e, dt=dt, pk=pk: e.copy(out=KmT[:, dt, :], in_=ps[pk][:, 0:256]), R=[psk[pk]], W=["KmT"])
    for mt in range(2):
        for n in range(2):
            pv = 4 + n
            for k in range(8):
                P.add("pe", lambda e, k=k, mt=mt, n=n, pv=pv: e.matmul(ps[pv][:, :], lhsT=hmT[:, k, mt * 128:(mt + 1) * 128],
                                                                        rhs=w_mkv[:, k, D + n * 512:D + (n + 1) * 512],
                                                                        start=(k == 0), stop=(k == 7)), R=["w_mkv", "hmT"], W=[psk[pv]])
            P.add("dve", lambda e, mt=mt, n=n, pv=pv: e.tensor_copy(out=Vm[:, mt, 2 * n:2 * n + 2, 0:256],
                                                                    in_=ps[pv][:, :].rearrange("p (h c) -> p h c", h=2)),
                  R=[psk[pv]], W=["Vm"])
    P.add("dve", lambda e: e.tensor_tensor(out=ksqm, in0=KmT, in1=KmT, op=ALU.mult), R=["KmT"], W=["ksqm"])
    for hh in range(4):
        for dt in range(2):
            P.add("pe", lambda e, hh=hh, dt=dt: e.matmul(ps[6][0:1, 0:256], lhsT=onesb[:, 0:1], rhs=ksqm[:, 2 * hh + dt, :],
                                                          start=(dt == 0), stop=(dt == 1)), R=["ksqm", "onesb"], W=[psk[6]])
        P.add("dve", lambda e, hh=hh: e.tensor_reduce(out=kbx[0:1, hh:hh + 1], in_=ps[6][0:1, 0:256], axis=AX.X, op=ALU.max),
              R=[psk[6]], W=["kbx%d" % hh])
    P.add("dve", lambda e: e.tensor_reduce(out=kbx[0:1, 4:5], in_=kbx[0:1, 0:4], axis=AX.X, op=ALU.max),
          R=["kbx0", "kbx1", "kbx2", "kbx3"], W=["kbx4"])
    P.add("act", lambda e: e.sqrt(out=kbx[0:1, 5:6], in_=kbx[0:1, 4:5]), R=["kbx4"], W=["kbx5"])
    P.add("dve", lambda e: e.tensor_scalar_mul(out=kbx[0:1, 6:7], in0=kbx[0:1, 5:6], scalar1=-1.04), R=["kbx5"], W=["kbx6"])
    P.barrier()
    P.release()
    w_mq = P.sb([128, 8, D], BF16, "w_mq")
    w_mo = P.sb([128, 8, D], BF16, "w_mo")
    P.add("pool", lambda e: e.dma_start(out=w_mq, in_=I["w_mq"].rearrange("(k p) n -> p k n", p=128)), W=["w_mq"], dma="w0")
    P.add("pool", lambda e: e.dma_start(out=w_mo, in_=I["w_mo"].rearrange("(k p) n -> p k n", p=128)), W=["w_mo"], dma="w1")
    load_gain("cross_norm_g")
    hxbf = [P.sb([128, D], BF16, "hxbf%d" % i) for i in range(2)]
    hxT = P.sb([128, 8, 512], BF16, "hxT")
    qT = P.sb([128, 8, 512], BF16, "qT")
    qsqx = P.sb([128, 8, 512], BF16, "qsqx")
    negm = P.sb([1, 4, 512], BF16, "negm")
    qn1 = P.sb([1, 512], F32, "qn1")
    PTm = [P.sb([128, 512], BF16, "PTm%d" % i) for i in range(2)]
    rdn = P.sb([1, 512], F32, "rdn")
    rdb = P.sb([128, 512], F32, "rdb")
    oTx = P.sb([128, 8, 512], BF16, "oTx")
    for qc in range(4):
        for t4 in range(4):
            j = qc * 4 + t4
            b = t4 % 2
            rms_norm(xres[:, j, :], D, gb, hxbf[b], ["xres"], ["hxbf%d" % b], stk="stx")
            for k in range(8):
                P.add("pe", lambda e, k=k, b=b: e.transpose(out=psb(b)[:, k * 128:(k + 1) * 128], in_=hxbf[b][:, k * 128:(k + 1) * 128], identity=identb),
                      R=["hxbf%d" % b, "identb"], W=[psk[b]])
            P.add("act", lambda e, b=b, t4=t4: e.copy(out=hxT[:, :, t4 * 128:(t4 + 1) * 128], in_=psb(b).rearrange("p (k t) -> p k t", k=8)),
                  R=[psk[b]], W=["hxT"])
        for dt in range(8):
            pq = 6 + dt % 2
            for k in range(8):
                P.add("pe", lambda e, k=k, dt=dt, pq=pq: e.matmul(ps[pq][:, :], lhsT=w_mq[:, k, dt * 128:(dt + 1) * 128], rhs=hxT[:, k, :],
                                                                   start=(k == 0), stop=(k == 7)), R=["w_mq", "hxT"], W=[psk[pq]])
            P.add("act", lambda e, dt=dt, pq=pq: e.mul(out=qT[:, dt, :], in_=ps[pq][:, :], mul=1.0 / 16.0), R=[psk[pq]], W=["qT"])
        P.add("dve", lambda e: e.tensor_tensor(out=qsqx, in0=qT, in1=qT, op=ALU.mult), R=["qT"], W=["qsqx"])
        for hh in range(4):
            for dt in range(2):
                P.add("pe", lambda e, hh=hh, dt=dt: e.matmul(ps[4][0:1, :], lhsT=onesb[:, 0:1], rhs=qsqx[:, 2 * hh + dt, :],
                                                              start=(dt == 0), stop=(dt == 1)), R=["qsqx", "onesb"], W=[psk[4]])
            P.add("act", lambda e: e.sqrt(out=qn1, in_=ps[4][0:1, :]), R=[psk[4]], W=["qn1"])
            P.add("dve", lambda e, hh=hh: e.tensor_scalar_mul(out=negm[0:1, hh, :], in0=qn1, scalar1=kbx[0:1, 6:7]), R=["qn1", "kbx6"], W=["negm"])
        for hh in range(4):
            for mt in range(2):
                for dt in range(2):
                    P.add("pe", lambda e, hh=hh, mt=mt, dt=dt: e.matmul(ps[mt][:, :], lhsT=KmT[:, 2 * hh + dt, mt * 128:(mt + 1) * 128],
                                                                         rhs=qT[:, 2 * hh + dt, :], start=(dt == 0), stop=False),
                          R=["KmT", "qT"], W=[psk[mt]])
                P.add("pe", lambda e, hh=hh, mt=mt: e.matmul(ps[mt][:, :], lhsT=onesb[0:1, 0:128], rhs=negm[0:1, hh, :], start=False, stop=True),
                      R=["negm", "onesb"], W=[psk[mt]])
                P.add("act", lambda e, mt=mt: e.activation(out=PTm[mt], in_=ps[mt][:, :], func=AF.Exp), R=[psk[mt]], W=["PTm%d" % mt])
            for dv in range(2):
                for mt in range(2):
                    P.add("pe", lambda e, hh=hh, mt=mt, dv=dv: e.matmul(ps[2 + dv][:, :], lhsT=Vm[:, mt, hh, dv * 128:(dv + 1) * 128], rhs=PTm[mt],
                                                                         start=(mt == 0), stop=(mt == 1)), R=["Vm", "PTm%d" % mt], W=[psk[2 + dv]])
            for mt in range(2):
                P.add("pe", lambda e, hh=hh, mt=mt: e.matmul(ps[4][0:1, :], lhsT=Vm[:, mt, hh, 256:257], rhs=PTm[mt], start=(mt == 0), stop=(mt == 1)),
                      R=["Vm", "PTm%d" % mt], W=[psk[4]])
            P.add("dve", lambda e: e.reciprocal(out=rdn, in_=ps[4][0:1, :]), R=[psk[4]], W=["rdn"])
            P.add("pe", lambda e: e.matmul(ps[5][:, :], lhsT=onesf[0:1, 0:128], rhs=rdn, start=True, stop=True), R=["rdn", "onesf"], W=[psk[5]])
            P.add("act", lambda e: e.copy(out=rdb, in_=ps[5][:, :]), R=[psk[5]], W=["rdb"])
            for dv in range(2):
                P.add("dve", lambda e, hh=hh, dv=dv: e.tensor_tensor(out=oTx[:, 2 * hh + dv, :], in0=ps[2 + dv][:, :], in1=rdb, op=ALU.mult),
                      R=[psk[2 + dv], "rdb"], W=["oTx"])
        for t4 in range(4):
            j = qc * 4 + t4
            for n in range(2):
                py = 6 + n
                for dt in range(8):
                    P.add("pe", lambda e, dt=dt, t4=t4, n=n, py=py: e.matmul(ps[py][:, :], lhsT=oTx[:, dt, t4 * 128:(t4 + 1) * 128],
                                                                              rhs=w_mo[:, dt, n * 512:(n + 1) * 512], start=(dt == 0), stop=(dt == 7)),
                          R=["oTx", "w_mo"], W=[psk[py]])
                P.add("dve", lambda e, j=j, n=n, py=py: e.tensor_tensor(out=xres[:, j, n * 512:(n + 1) * 512], in0=ps[py][:, :],
                                                                         in1=xres[:, j, n * 512:(n + 1) * 512], op=ALU.add),
                      R=[psk[py], "xres"], W=["xres"])
    P.barrier()
    P.release()
    if "x2" in dbg:
        tx2 = dbg_out("x2", [OWN, D])
        P.add("sp", lambda e: e.dma_start(out=tx2.rearrange("(j p) c -> p j c", p=128), in_=xres), R=["xres"], W=["dbgo"], dma="dbg1")
        P.barrier()
    if stop == "E":
        P.add("sp", None, R=[])
        P.emit()
        return nc, dbg_d

    P.mark()
    tT = P.sb([128, 8, OWN], BF16, "tT")
    combT = P.sb([32, OWN], F32, "combT")
    P.mark()
    load_gain("ffn_norm_g")
    w_r = P.sb([128, 8, 36], F32, "w_r")
    b_r = P.sb([128, 36], F32, "b_r")
    P.add("sp", lambda e: e.dma_start(out=w_r, in_=I["w_route"].rearrange("(k p) n -> p k n", p=128)), W=["w_r"], dma="c3")
    P.add("sp", lambda e: e.dma_start(out=b_r, in_=I["b_route"].partition_broadcast(128)), W=["b_r"], dma="c4")
    tnf = [P.sb([128, D], F32, "tnf%d" % i) for i in range(2)]
    tnb = [P.sb([128, D], BF16, "tnb%d" % i) for i in range(2)]
    tTf = P.sb([128, 8, 128], F32, "tTf")
    lg = P.sb([128, 36], F32, "lg")
    r8 = P.sb([128, 16], F32, "r8")
    oh = P.sb([128, 4], F32, "oh")
    ge = P.sb([128, 4], F32, "ge")
    ein = P.sb([128, 8], F32, "ein")
    e2 = P.sb([128, 8], F32, "e2")
    mk1 = P.sb([128, 8], F32, "mk1")
    mk2 = P.sb([128, 8], F32, "mk2")
    we = P.sb([128, 8], F32, "we")
    comb = P.sb([128, 32], F32, "comb")
    seq = [0]

    def dv(fn, R, W):
        P.add("dve", fn, R=R, W=W)

    for j in range(NTO):
        b = j % 2
        rms_norm(xres[:, j, :], D, gb, tnf[b], ["xres"], ["tnf%d" % b], stk="stf")
        P.add("act", lambda e, b=b: e.copy(out=tnb[b], in_=tnf[b]), R=["tnf%d" % b], W=["tnb%d" % b])
        for k in range(8):
            P.add("pe", lambda e, k=k, b=b: e.transpose(out=psb(b)[:, k * 128:(k + 1) * 128], in_=tnb[b][:, k * 128:(k + 1) * 128], identity=identb),
                  R=["tnb%d" % b, "identb"], W=[psk[b]])
        P.add("act", lambda e, b=b, j=j: e.copy(out=tT[:, :, j * 128:(j + 1) * 128], in_=psb(b).rearrange("p (k t) -> p k t", k=8)),
              R=[psk[b]], W=["tT"])
        for k in range(8):
            pf = 2 + (k // 4)
            P.add("pe", lambda e, k=k, b=b, pf=pf: e.transpose(out=ps[pf][:, (k % 4) * 128:(k % 4 + 1) * 128], in_=tnf[b][:, k * 128:(k + 1) * 128], identity=identf),
                  R=["tnf%d" % b, "identf"], W=[psk[pf]])
        for hf in range(2):
            P.add("dve" if hf == 0 else "act", (lambda e, hf=hf: e.tensor_copy(out=tTf[:, hf * 4:(hf + 1) * 4, :], in_=ps[2 + hf][:, :].rearrange("p (k t) -> p k t", k=4)))
                  if hf == 0 else (lambda e, hf=hf: e.copy(out=tTf[:, hf * 4:(hf + 1) * 4, :], in_=ps[2 + hf][:, :].rearrange("p (k t) -> p k t", k=4))),
                  R=[psk[2 + hf]], W=["tTf%d" % hf])
        for k in range(8):
            P.add("pe", lambda e, k=k: e.matmul(ps[4][:, 0:36], lhsT=tTf[:, k, :], rhs=w_r[:, k, :], start=(k == 0), stop=(k == 7)),
                  R=["tTf0", "tTf1", "w_r"], W=[psk[4]])
        dv(lambda e: e.tensor_tensor(out=lg, in0=ps[4][:, 0:36], in1=b_r, op=ALU.add), [psk[4], "b_r"], ["lg"])
        dv(lambda e: e.tensor_reduce(out=r8[:, 0:1], in_=lg[:, 0:4], axis=AX.X, op=ALU.max), ["lg"], ["r8_0"])
        dv(lambda e: e.tensor_scalar(out=oh, in0=lg[:, 0:4], scalar1=r8[:, 0:1], scalar2=None, op0=ALU.is_equal), ["lg", "r8_0"], ["oh"])
        dv(lambda e: e.tensor_scalar(out=ge, in0=lg[:, 0:4], scalar1=r8[:, 0:1], scalar2=None, op0=ALU.subtract), ["lg", "r8_0"], ["ge"])
        P.add("act", lambda e: e.activation(out=ge, in_=ge, func=AF.Exp), R=["ge"], W=["ge"])
        dv(lambda e: e.tensor_reduce(out=r8[:, 1:2], in_=ge, axis=AX.X, op=ALU.add), ["ge"], ["r8_1"])
        dv(lambda e: e.reciprocal(out=r8[:, 2:3], in_=r8[:, 1:2]), ["r8_1"], ["r8_2"])
        dv(lambda e: e.tensor_scalar_mul(out=ein, in0=lg[:, 4:12], scalar1=oh[:, 0:1]), ["lg", "oh"], ["ein"])
        for g in range(1, 4):
            dv(lambda e, g=g: e.scalar_tensor_tensor(out=ein, in0=lg[:, 4 + 8 * g:12 + 8 * g], scalar=oh[:, g:g + 1], in1=ein,
                                                     op0=ALU.mult, op1=ALU.add), ["lg", "oh", "ein"], ["ein"])
        dv(lambda e: e.tensor_reduce(out=r8[:, 3:4], in_=ein, axis=AX.X, op=ALU.max), ["ein"], ["r8_3"])
        dv(lambda e: e.tensor_scalar(out=mk1, in0=ein, scalar1=r8[:, 3:4], scalar2=None, op0=ALU.is_equal), ["ein", "r8_3"], ["mk1"])
        dv(lambda e: e.scalar_tensor_tensor(out=e2, in0=mk1, scalar=-1e30, in1=ein, op0=ALU.mult, op1=ALU.add), ["mk1", "ein"], ["e2"])
        dv(lambda e: e.tensor_reduce(out=r8[:, 4:5], in_=e2, axis=AX.X, op=ALU.max), ["e2"], ["r8_4"])
        dv(lambda e: e.tensor_scalar(out=mk2, in0=e2, scalar1=r8[:, 4:5], scalar2=None, op0=ALU.is_equal), ["e2", "r8_4"], ["mk2"])
        dv(lambda e: e.tensor_tensor(out=r8[:, 5:6], in0=r8[:, 4:5], in1=r8[:, 3:4], op=ALU.subtract), ["r8_3", "r8_4"], ["r8_5"])
        P.add("act", lambda e: e.activation(out=r8[:, 6:7], in_=r8[:, 5:6], func=AF.Exp), R=["r8_5"], W=["r8_6"])
        dv(lambda e: e.tensor_scalar_add(out=r8[:, 7:8], in0=r8[:, 6:7], scalar1=1.0), ["r8_6"], ["r8_7"])
        dv(lambda e: e.reciprocal(out=r8[:, 8:9], in_=r8[:, 7:8]), ["r8_7"], ["r8_8"])
        dv(lambda e: e.tensor_tensor(out=r8[:, 9:10], in0=r8[:, 6:7], in1=r8[:, 8:9], op=ALU.mult), ["r8_6", "r8_8"], ["r8_9"])
        dv(lambda e: e.tensor_tensor(out=r8[:, 10:11], in0=r8[:, 8:9], in1=r8[:, 2:3], op=ALU.mult), ["r8_8", "r8_2"], ["r8_10"])
        dv(lambda e: e.tensor_tensor(out=r8[:, 11:12], in0=r8[:, 9:10], in1=r8[:, 2:3], op=ALU.mult), ["r8_9", "r8_2"], ["r8_11"])
        dv(lambda e: e.tensor_scalar_mul(out=we, in0=mk1, scalar1=r8[:, 10:11]), ["mk1", "r8_10"], ["we"])
        dv(lambda e: e.scalar_tensor_tensor(out=we, in0=mk2, scalar=r8[:, 11:12], in1=we, op0=ALU.mult, op1=ALU.add), ["mk2", "r8_11", "we"], ["we"])
        for g in range(4):
            dv(lambda e, g=g: e.tensor_scalar_mul(out=comb[:, 8 * g:8 * g + 8], in0=we, scalar1=oh[:, g:g + 1]), ["we", "oh"], ["comb"])
        P.add("pe", lambda e: e.transpose(out=ps[5][0:32, 0:128], in_=comb, identity=identf), R=["comb", "identf"], W=[psk[5]])
        dv(lambda e, j=j: e.tensor_copy(out=combT[:, j * 128:(j + 1) * 128], in_=ps[5][0:32, 0:128]), [psk[5]], ["combT"])
    P.add("sp", lambda e: e.dma_start(out=combT_d, in_=combT), R=["combT"], W=["combT_d"], dma="combT")
    if "comb" in dbg:
        tcb = dbg_out("comb", [32, OWN])
        P.add("sp", lambda e: e.dma_start(out=tcb, in_=combT), R=["combT"], W=["dbgo"], dma="dbg1")
    P.barrier()
    P.release()
    NSLOT = 4
    wg = [P.sb([128, 8, 256], BF16, "wg%d" % i) for i in range(NSLOT)]
    wu = [P.sb([128, 8, 256], BF16, "wu%d" % i) for i in range(NSLOT)]
    wd = [P.sb([128, 2, D], BF16, "wd%d" % i) for i in range(NSLOT)]
    CB = [P.sb([128, OWN], F32, "CB%d" % i) for i in range(2)]
    sa = [P.sb([128, 2, 512], F32, "sa%d" % i) for i in range(2)]
    sc = [P.sb([128, 2, 512], F32, "sc%d" % i) for i in range(2)]
    mTe = [P.sb([128, 2, 512], BF16, "mTe%d" % i) for i in range(2)]
    def load_expert(e_):
        sl = e_ % NSLOT
        P.add("pool", lambda e, e_=e_, sl=sl: e.dma_start(out=wg[sl], in_=I["w_gate"][e_].rearrange("(k p) n -> p k n", p=128)), W=["wg%d" % sl], dma="wg%d" % sl)
        P.add("pool", lambda e, e_=e_, sl=sl: e.dma_start(out=wu[sl], in_=I["w_up"][e_].rearrange("(k p) n -> p k n", p=128)), W=["wu%d" % sl], dma="wu%d" % sl)
        P.add("pool", lambda e, e_=e_, sl=sl: e.dma_start(out=wd[sl], in_=I["w_down"][e_].rearrange("(k p) n -> p k n", p=128)), W=["wd%d" % sl], dma="wd%d" % sl)

    for e_ in range(2):
        load_expert(e_)
    yb = 0
    for pr in range(16):
        for ee in range(2):
            if 2 * pr + 2 + ee < 32:
                load_expert(2 * pr + 2 + ee)
        for ee in range(2):
            e_ = 2 * pr + ee
            P.add("sp", lambda e, e_=e_, ee=ee: e.dma_start(out=CB[ee], in_=combT_d[e_:e_ + 1, :].partition_broadcast(128)),
                  R=["combT_d"], W=["CB%d" % ee], dma="CB%d" % ee)
        for c in range(4):
            for ee in range(2):
                e_ = 2 * pr + ee
                sl = e_ % NSLOT
                for f in range(2):
                    for k in range(8):
                        P.add("pe", lambda e, f=f, k=k, sl=sl, c=c: e.matmul(ps[f][:, :], lhsT=wg[sl][:, k, f * 128:(f + 1) * 128],
                                                                            rhs=tT[:, k, c * 512:(c + 1) * 512], start=(k == 0), stop=(k == 7)),
                              R=["wg%d" % sl, "tT"], W=[psk[f]])
                for f in range(2):
                    for k in range(8):
                        P.add("pe", lambda e, f=f, k=k, sl=sl, c=c: e.matmul(ps[2 + f][:, :], lhsT=wu[sl][:, k, f * 128:(f + 1) * 128],
                                                                            rhs=tT[:, k, c * 512:(c + 1) * 512], start=(k == 0), stop=(k == 7)),
                              R=["wu%d" % sl, "tT"], W=[psk[2 + f]])
                for f in range(2):
                    P.add("act", lambda e, f=f, ee=ee: e.activation(out=sa[ee][:, f, :], in_=ps[f][:, :], func=AF.Silu),
                          R=[psk[f]], W=["sa%d_%d" % (ee, f)])
                    P.add("pool", lambda e, f=f, ee=ee, c=c: e.tensor_tensor(out=sc[ee][:, f, :], in0=sa[ee][:, f, :],
                                                                              in1=CB[ee][:, c * 512:(c + 1) * 512], op=ALU.mult),
                          R=["sa%d_%d" % (ee, f), "CB%d" % ee], W=["sc%d_%d" % (ee, f)])
                    P.add("dve", lambda e, f=f, ee=ee: e.tensor_tensor(out=mTe[ee][:, f, :], in0=ps[2 + f][:, :], in1=sc[ee][:, f, :], op=ALU.mult),
                          R=[psk[2 + f], "sc%d_%d" % (ee, f)], W=["mTe%d" % ee])
            for t4 in range(4):
                j = c * 4 + t4
                for n in range(2):
                    py = 4 + (yb % 4)
                    yb += 1
                    cnt_mm = 0
                    for ee in range(2):
                        sl = (2 * pr + ee) % NSLOT
                        for f in range(2):
                            P.add("pe", lambda e, ee=ee, f=f, sl=sl, t4=t4, n=n, py=py, cnt_mm=cnt_mm: e.matmul(
                                ps[py][:, :], lhsT=mTe[ee][:, f, t4 * 128:(t4 + 1) * 128], rhs=wd[sl][:, f, n * 512:(n + 1) * 512],
                                start=(cnt_mm == 0), stop=(cnt_mm == 3)), R=["mTe%d" % ee, "wd%d" % sl], W=[psk[py]])
                            cnt_mm += 1
                    P.add("dve", lambda e, j=j, n=n, py=py: e.tensor_tensor(out=xres[:, j, n * 512:(n + 1) * 512], in0=ps[py][:, :],
                                                                             in1=xres[:, j, n * 512:(n + 1) * 512], op=ALU.add),
                          R=[psk[py], "xres"], W=["xres"])
    P.barrier()
    P.release()
    if "x3" in dbg:
        tx3 = dbg_out("x3", [OWN, D])
        P.add("sp", lambda e: e.dma_start(out=tx3.rearrange("(j p) c -> p j c", p=128), in_=xres), R=["xres"], W=["dbgo"], dma="dbg1")
        P.barrier()

    load_gain("final_norm_g")
    xo = [P.sb([128, D], F32, "xo%d" % i) for i in range(2)]
    for j in range(NTO):
        b = j % 2
        rms_norm(xres[:, j, :], D, gb, xo[b], ["xres"], ["xo%d" % b], stk="stg")
        P.add("sp", lambda e, j=j, b=b: e.dma_start(out=out_d[j * 128:(j + 1) * 128, :], in_=xo[b]),
              R=["xo%d" % b], W=["out_d%d" % b], dma="xo%d" % b)
    P.add("sp", None, R=["out_d0", "out_d1", "dbgo"])
    P.emit()
    return nc, dbg_d


def hyena_phase(nc, P, I, ps, psk, psb, U_d, hout_d, identf, identb, onesb, onesf, halfm, dbg, dbg_out):
    NF = 64
    Hd = nc.dram_tensor("hy_H", [S, 2048], BF16, kind="Internal").ap()
    Bd = [nc.dram_tensor("hy_B%d" % i, [128, NF, 512], BF16, kind="Internal").ap() for i in range(2)]
    Kd = [nc.dram_tensor("hy_K%d" % i, [128, NF, 512], BF16, kind="Internal").ap() for i in range(2)]
    Btd = nc.dram_tensor("hy_Bt", [128, NF, 512], BF16, kind="Internal").ap()
    z1_d = nc.dram_tensor("hy_z1", [S, 512], F32, kind="Internal").ap()
    P.mark()
    Dt = P.sb([64, 64, 128], BF16, "Dt")
    Dinv = P.sb([128, 64, 64], BF16, "Dinv")
    F2a = P.sb([128, 128], BF16, "F2a")
    F2b = P.sb([128, 128], BF16, "F2b")
    Gm = P.sb([128, 128], BF16, "Gm")
    sgn = P.sb([128, 1], F32, "sgn")
    skipb = P.sb([128, 2, 512], F32, "skipb")
    P.add("pool", lambda e: e.dma_start(out=Dt, in_=I["hy_D0"].rearrange("a (b c) -> a b c", b=64)), W=["Dt"], dma="ht0")
    P.add("pool", lambda e: e.dma_start(out=F2a, in_=I["hy_F2a"]), W=["F2a"], dma="ht1")
    P.add("pool", lambda e: e.dma_start(out=F2b, in_=I["hy_F2b"]), W=["F2b"], dma="ht2")
    P.add("pool", lambda e: e.dma_start(out=Gm, in_=I["hy_G"]), W=["Gm"], dma="ht3")
    P.add("pool", lambda e: e.dma_start(out=Dinv, in_=I["hy_Dinv"].rearrange("a (b c) -> a b c", b=64)), W=["Dinv"], dma="ht4")
    P.add("sp", lambda e: e.dma_start(out=sgn, in_=I["hy_sgn"]), W=["sgn"], dma="c3")
    P.add("sp", lambda e: e.dma_start(out=skipb.rearrange("p a b -> p (a b)"), in_=I["hy_skip"].partition_broadcast(128)), W=["skipb"], dma="c4")

    P.mark()
    zT = P.sb([33, S], F32, "zT")
    g1T = P.sb([64, S], F32, "g1T")
    g2T = P.sb([64, S], F32, "g2T")
    hcols = P.sb([64, 4], F32, "hcols")
    w1 = P.sb([33, 64], F32, "w1")
    w2 = P.sb([64, 64], F32, "w2")
    w3 = P.sb([64, 2048], F32, "w3")
    b3r = P.sb([1, 2048], F32, "b3r")
    adec = P.sb([128, 2048], F32, "adec")
    nt01 = P.sb([128, NT], F32, "nt01")
    mpi = P.sb([128, 1], F32, "mpi")
    argt = P.sb([64, 512], F32, "argt")
    argm = P.sb([64, 512], F32, "argm")
    Et = [P.sb([128, 512], F32, "Et%d" % i) for i in range(2)]
    hfo = [P.sb([128, 2048], BF16, "hfo%d" % i) for i in range(2)]
    P.add("sp", lambda e: e.dma_start(out=zT, in_=I["hy_zT"]), W=["zT"], dma="hf0")
    P.add("sp", lambda e: e.dma_start(out=hcols, in_=I["hy_cols"]), W=["hcols"], dma="hf1")
    P.add("sp", lambda e: e.dma_start(out=w1, in_=I["hy_w1"]), W=["w1"], dma="hf2")
    P.add("sp", lambda e: e.dma_start(out=w2, in_=I["hy_w2"]), W=["w2"], dma="hf3")
    P.add("sp", lambda e: e.dma_start(out=w3, in_=I["hy_w3"]), W=["w3"], dma="hf4")
    P.add("sp", lambda e: e.dma_start(out=b3r, in_=I["hy_b3"]), W=["b3r"], dma="hf5")
    P.add("sp", lambda e: e.dma_start(out=adec, in_=I["hy_decay"].partition_broadcast(128)), W=["adec"], dma="hf6")
    P.add("sp", lambda e: e.dma_start(out=nt01, in_=I["hy_nt01"]), W=["nt01"], dma="hf7")
    P.add("pool", lambda e: e.memset(mpi, -math.pi), W=["mpi"])
    P.add("act", lambda e: e.activation(out=adec, in_=adec, func=AF.Abs), R=["adec"], W=["adec"])
    OFFS = math.pi + 16.0 * math.pi
    for (src, wt, kk, bcol, fcol, dst, nm) in ((zT, w1, 33, 0, 2, g1T, "g1T"), (g1T, w2, 64, 1, 3, g2T, "g2T")):
        for ch in range(8):
            pb_ = ch % 2
            P.add("pe", lambda e, src=src, wt=wt, kk=kk, ch=ch, pb_=pb_: e.matmul(ps[pb_][0:64, :], lhsT=wt[0:kk, :], rhs=src[0:kk, ch * 512:(ch + 1) * 512],
                                                                               start=True, stop=True), R=["zT", "g1T", "w1", "w2"], W=[psk[pb_]])
            P.add("dve", lambda e, pb_=pb_, bcol=bcol, fcol=fcol: e.tensor_scalar(out=argt, in0=ps[pb_][0:64, :], scalar1=hcols[:, bcol:bcol + 1],
                                                                                  scalar2=hcols[:, fcol:fcol + 1], op0=ALU.add, op1=ALU.mult),
                  R=[psk[pb_], "hcols"], W=["argt"])
            for _rep in range(2):
                P.add("dve", lambda e: e.tensor_scalar(out=argm, in0=argt, scalar1=math.pi, scalar2=None, op0=ALU.is_gt), R=["argt"], W=["argm"])
                P.add("dve", lambda e: e.scalar_tensor_tensor(out=argt, in0=argm, scalar=-2.0 * math.pi, in1=argt, op0=ALU.mult, op1=ALU.add),
                      R=["argm", "argt"], W=["argt"])
                P.add("dve", lambda e: e.tensor_scalar(out=argm, in0=argt, scalar1=-math.pi, scalar2=None, op0=ALU.is_lt), R=["argt"], W=["argm"])
                P.add("dve", lambda e: e.scalar_tensor_tensor(out=argt, in0=argm, scalar=2.0 * math.pi, in1=argt, op0=ALU.mult, op1=ALU.add),
                      R=["argm", "argt"], W=["argt"])
            P.add("act", lambda e, dst=dst, ch=ch: e.activation(out=dst[:, ch * 512:(ch + 1) * 512], in_=argt, func=AF.Sin),
                  R=["argt"], W=[nm])
    for i in range(NT):
        b = i % 2
        for cg in range(4):
            pb_ = 2 + cg
            P.add("pe", lambda e, i=i, cg=cg, pb_=pb_: e.matmul(ps[pb_][:, :], lhsT=g2T[:, i * 128:(i + 1) * 128], rhs=w3[:, cg * 512:(cg + 1) * 512],
                                                                 start=True, stop=False), R=["g2T", "w3"], W=[psk[pb_]])
            P.add("pe", lambda e, cg=cg, pb_=pb_: e.matmul(ps[pb_][:, :], lhsT=onesf[0:1, 0:128], rhs=b3r[0:1, cg * 512:(cg + 1) * 512],
                                                            start=False, stop=True), R=["onesf", "b3r"], W=[psk[pb_]])
            eb = cg % 2
            P.add("act", lambda e, i=i, cg=cg, eb=eb: e.activation(out=Et[eb], in_=adec[:, cg * 512:(cg + 1) * 512], func=AF.Exp, scale=nt01[:, i:i + 1]),
                  R=["adec", "nt01"], W=["Et%d" % eb])
            P.add("dve", lambda e, cg=cg, pb_=pb_, eb=eb, b=b: e.tensor_tensor(out=hfo[b][:, cg * 512:(cg + 1) * 512], in0=ps[pb_][:, :], in1=Et[eb], op=ALU.mult),
                  R=[psk[pb_], "Et%d" % eb], W=["hfo%d" % b])
        if i == 0:
            for o in range(2):
                P.add("pool", lambda e, o=o: e.memset(hfo[0][0:1, o * 1024 + 512:o * 1024 + 1024], 0.0), R=[], W=["hfo0"])
        P.add("sp", lambda e, i=i, b=b: e.dma_start(out=Hd[i * 128:(i + 1) * 128, :], in_=hfo[b]), R=["hfo%d" % b], W=["Hd_%d" % i], dma="hfo%d" % b)
    P.barrier()
    P.release()
    if "hf" in dbg:
        thf = dbg_out("hf", [S, 2048])
        P.mark()
        tbf = P.sb([128, 2048], BF16, "tbf")
        tbf32 = P.sb([128, 2048], F32, "tbf32")
        for i in range(NT):
            P.add("sp", lambda e, i=i: e.dma_start(out=tbf, in_=Hd[i * 128:(i + 1) * 128, :]), R=["Hd_%d" % i], W=["tbf"], dma="dbg0")
            P.add("dve", lambda e: e.tensor_copy(out=tbf32, in_=tbf), R=["tbf"], W=["tbf32"])
            P.add("sp", lambda e, i=i: e.dma_start(out=thf[i * 128:(i + 1) * 128, :], in_=tbf32), R=["tbf32"], W=["dbgo"], dma="dbg1")
        P.barrier()
        P.release()

    ev = [0]
    HDK = ["Hd_%d" % i for i in range(NT)]
    Z1K = ["z1_d_%d" % i for i in range(8)]
    BD0K = ["Bd0_%d" % i for i in range(4)]
    BD1K = ["Bd1_%d" % i for i in range(4)]
    BTDK = ["Btd_%d" % i for i in range(8)]

    def evac(out, in_, Rk, Wk):
        ev[0] += 1
        if ev[0] % 2:
            P.add("act", lambda e: e.copy(out=out, in_=in_), R=Rk, W=Wk)
        else:
            P.add("dve", lambda e: e.tensor_copy(out=out, in_=in_), R=Rk, W=Wk)

    def stage1(src_view, cast, Bdst, bkey, srckeys):
        P.barrier()
        P.mark()
        xsb = [P.sb([64, 16, 512], BF16, "xsb%d" % i) for i in range(2)]
        Bsb = [P.sb([128, 16, 512], BF16, "Bsb%d" % i) for i in range(2)]
        def s1_load(ch):
            xb_ = ch % 2
            q = "pool" if cast else "sp"
            P.add(q, lambda e, ch=ch, xb_=xb_: e.dma_start(out=xsb[xb_], in_=src_view[:, ch * 16:(ch + 1) * 16, :]),
                  R=srckeys, W=["xsb%d" % xb_], dma="xsb%d" % xb_)

        s1_load(0)
        for ch in range(4):
            xb_ = ch % 2
            if ch + 1 < 4:
                s1_load(ch + 1)
            for s2l in range(16):
                s2 = ch * 16 + s2l
                pb_ = s2 % 4
                P.add("pe", lambda e, s2=s2, s2l=s2l, xb_=xb_, pb_=pb_: e.matmul(ps[pb_][:, :], lhsT=Dt[0:64, s2, :], rhs=xsb[xb_][0:64, s2l, :],
                                                                                  start=True, stop=True), R=["Dt", "xsb%d" % xb_], W=[psk[pb_]])
                evac(Bsb[xb_][:, s2l, :], ps[pb_][:, :], [psk[pb_]], ["Bsb%d_%d" % (xb_, s2l)])
            P.add("sp", lambda e, ch=ch, xb_=xb_: e.dma_start(out=Bdst[:, ch * 16:(ch + 1) * 16, :], in_=Bsb[xb_]),
                  R=["Bsb%d_%d" % (xb_, k) for k in range(16)], W=["%s_%d" % (bkey, ch)], dma="Bsb%d" % xb_)
        P.barrier()
        P.release()

    def blocked(ap2d):
        return ap2d.rearrange("(s1 s2) c -> s1 s2 c", s2=64)

    for o in range(2):
        stage1(blocked(Hd[:, o * 1024:o * 1024 + 512]), False, Bd[0], "Bd0", HDK)
        stage1(blocked(Hd[:, o * 1024 + 512:o * 1024 + 1024]), False, Bd[1], "Bd1", HDK)
        P.mark()
        BTf = [P.sb([128, 8, 512], BF16, "BTf%d" % i) for i in range(2)]
        BTb = [P.sb([128, 8, 512], BF16, "BTb%d" % i) for i in range(2)]
        xfs = [P.sb([128, 512], F32, "xfs%d" % i) for i in range(2)]
        kst = [P.sb([128, 512], F32, "kst%d" % i) for i in range(2)]
        Kc = [P.sb([128, 8, 512], BF16, "Kc%d" % i) for i in range(2)]
        def f2_load(fc):
            cb_ = fc % 2
            for r in range(2):
                P.add("sp", lambda e, r=r, fc=fc, cb_=cb_: e.dma_start(
                    out=BTf[cb_][r * 64:(r + 1) * 64, :, :], in_=Bd[0][r * 64 + fc * 8:r * 64 + fc * 8 + 8, :, :].rearrange("f s c -> s f c")),
                    R=BD0K, W=["BTf%d" % cb_], dma="BTf%d_%d" % (cb_, r))
                P.add("sp", lambda e, r=r, fc=fc, cb_=cb_: e.dma_start(
                    out=BTb[cb_][r * 64:(r + 1) * 64, :, :], in_=Bd[1][r * 64 + fc * 8:r * 64 + fc * 8 + 8, :, :].rearrange("f s c -> s f c")),
                    R=BD1K, W=["BTb%d" % cb_], dma="BTb%d_%d" % (cb_, r))

        f2_load(0)
        for fc in range(8):
            cb_ = fc % 2
            if fc + 1 < 8:
                f2_load(fc + 1)
            for f1l in range(8):
                t_ = f1l % 2
                pa, pb2 = 0 + 2 * t_, 1 + 2 * t_
                P.add("pe", lambda e, f1l=f1l, cb_=cb_, pa=pa: e.matmul(ps[pa][:, :], lhsT=F2a, rhs=BTf[cb_][:, f1l, :], start=True, stop=True),
                      R=["F2a", "BTf%d" % cb_], W=[psk[pa]])
                P.add("pe", lambda e, f1l=f1l, cb_=cb_, pb2=pb2: e.matmul(ps[pb2][:, :], lhsT=F2a, rhs=BTb[cb_][:, f1l, :], start=True, stop=True),
                      R=["F2a", "BTb%d" % cb_], W=[psk[pb2]])
                P.add("act", lambda e, pa=pa, t_=t_: e.copy(out=xfs[t_], in_=ps[pa][:, :]), R=[psk[pa]], W=["xfs%d" % t_])
                P.add("dve", lambda e, pb2=pb2, t_=t_: e.scalar_tensor_tensor(out=kst[t_], in0=ps[pb2][:, :], scalar=sgn[:, 0:1], in1=xfs[t_],
                                                                             op0=ALU.mult, op1=ALU.add),
                      R=[psk[pb2], "xfs%d" % t_, "sgn"], W=["kst%d" % t_])
                P.add("pool", lambda e, t_=t_, cb_=cb_, f1l=f1l, o=o: e.tensor_tensor(out=Kc[cb_][0:64, f1l, :], in0=kst[t_][0:64, :], in1=skipb[0:64, o, :], op=ALU.add),
                      R=["kst%d" % t_, "skipb"], W=["Kc%d_a%d" % (cb_, f1l)])
                P.add("pool", lambda e, t_=t_, cb_=cb_, f1l=f1l: e.tensor_copy(out=Kc[cb_][64:128, f1l, :], in_=kst[t_][64:128, :]),
                      R=["kst%d" % t_], W=["Kc%d_b%d" % (cb_, f1l)])
            P.add("sp", lambda e, fc=fc, cb_=cb_, o=o: e.dma_start(out=Kd[o][:, fc * 8:(fc + 1) * 8, :], in_=Kc[cb_]),
                  R=["Kc%d_a%d" % (cb_, k) for k in range(8)] + ["Kc%d_b%d" % (cb_, k) for k in range(8)], W=["Kd%d_%d" % (o, fc)], dma="Kc%d" % cb_)
        P.barrier()
        P.release()
    if "kf" in dbg:
        tkf = dbg_out("kf", [2, 128, NF * 512])
        P.mark()
        tk16 = P.sb([128, 8, 512], BF16, "tk16")
        tk32 = P.sb([128, 8, 512], F32, "tk32")
        for o in range(2):
            for fc in range(8):
                P.add("sp", lambda e, o=o, fc=fc: e.dma_start(out=tk16, in_=Kd[o][:, fc * 8:(fc + 1) * 8, :]), R=["Kd%d_%d" % (o, fc)], W=["tk16"], dma="dbg0")
                P.add("dve", lambda e: e.tensor_copy(out=tk32, in_=tk16), R=["tk16"], W=["tk32"])
                P.add("sp", lambda e, o=o, fc=fc: e.dma_start(out=tkf[o][:, fc * 4096:(fc + 1) * 4096], in_=tk32.rearrange("p a b -> p (a b)")),
                      R=["tk32"], W=["dbgo"], dma="dbg1")
        P.barrier()
        P.release()

    P.add("pool", lambda e: e.dma_start(out=Dt, in_=I["hy_Dc"].rearrange("a (b c) -> a b c", b=64)), W=["Dt"], dma="ht0")
    for o in range(2):
        if o == 0:
            stage1(blocked(U_d[:, 1024:1536]), True, Bd[0], "Bd0", ["U_d"])
        else:
            stage1(blocked(z1_d), True, Bd[0], "Bd0", Z1K)
        P.mark()
        BT = [P.sb([128, 8, 512], BF16, "BT%d" % i) for i in range(2)]
        KA = [P.sb([128, 8, 512], BF16, "KA%d" % i) for i in range(2)]
        KB = [P.sb([128, 8, 512], BF16, "KB%d" % i) for i in range(2)]
        ta = [P.sb([128, 512], F32, "ta%d" % i) for i in range(2)]
        tb2 = [P.sb([128, 512], F32, "tb2%d" % i) for i in range(2)]
        Yc = [P.sb([128, 512], BF16, "Yc%d" % i) for i in range(2)]
        Btsb = [P.sb([128, 8, 512], BF16, "Btsb%d" % i) for i in range(2)]
        def c2_load(fc):
            cb_ = fc % 2
            for r in range(2):
                P.add("sp", lambda e, r=r, fc=fc, cb_=cb_: e.dma_start(
                    out=BT[cb_][r * 64:(r + 1) * 64, :, :], in_=Bd[0][r * 64 + fc * 8:r * 64 + fc * 8 + 8, :, :].rearrange("f s c -> s f c")),
                    R=BD0K, W=["BT%d" % cb_], dma="BT%d_%d" % (cb_, r))
                P.add("sp", lambda e, r=r, fc=fc, cb_=cb_, o=o: e.dma_start(out=KA[cb_][r * 64:(r + 1) * 64, :, :], in_=Kd[o][0:64, fc * 8:(fc + 1) * 8, :]),
                      R=["Kd%d_%d" % (o, fc)], W=["KA%d" % cb_], dma="KA%d_%d" % (cb_, r))
                P.add("sp", lambda e, r=r, fc=fc, cb_=cb_, o=o: e.dma_start(out=KB[cb_][r * 64:(r + 1) * 64, :, :], in_=Kd[o][64:128, fc * 8:(fc + 1) * 8, :]),
                      R=["Kd%d_%d" % (o, fc)], W=["KB%d" % cb_], dma="KB%d_%d" % (cb_, r))

        c2_load(0)
        for fc in range(8):
            cb_ = fc % 2
            if fc + 1 < 8:
                c2_load(fc + 1)
            for f1l in range(8):
                t_ = f1l % 2
                pa, pb2, pc = 0 + 3 * t_, 1 + 3 * t_, 2 + 3 * t_
                P.add("pe", lambda e, f1l=f1l, cb_=cb_, pa=pa: e.matmul(ps[pa][:, :], lhsT=F2a, rhs=BT[cb_][:, f1l, :], start=True, stop=True),
                      R=["F2a", "BT%d" % cb_], W=[psk[pa]])
                P.add("pe", lambda e, f1l=f1l, cb_=cb_, pb2=pb2: e.matmul(ps[pb2][:, :], lhsT=F2b, rhs=BT[cb_][:, f1l, :], start=True, stop=True),
                      R=["F2b", "BT%d" % cb_], W=[psk[pb2]])
                P.add("dve", lambda e, pa=pa, t_=t_, cb_=cb_, f1l=f1l: e.tensor_tensor(out=ta[t_], in0=ps[pa][:, :], in1=KA[cb_][:, f1l, :], op=ALU.mult),
                      R=[psk[pa], "KA%d" % cb_], W=["ta%d" % t_])
                P.add("dve", lambda e, pb2=pb2, t_=t_, cb_=cb_, f1l=f1l: e.tensor_tensor(out=tb2[t_], in0=ps[pb2][:, :], in1=KB[cb_][:, f1l, :], op=ALU.mult),
                      R=[psk[pb2], "KB%d" % cb_], W=["tb2%d" % t_])
                P.add("pool", lambda e, t_=t_: e.tensor_tensor(out=Yc[t_], in0=ta[t_], in1=tb2[t_], op=ALU.add),
                      R=["ta%d" % t_, "tb2%d" % t_], W=["Yc%d" % t_])
                P.add("pe", lambda e, t_=t_, pc=pc: e.matmul(ps[pc][:, :], lhsT=Gm, rhs=Yc[t_], start=True, stop=True),
                      R=["Gm", "Yc%d" % t_], W=[psk[pc]])
                P.add("act", lambda e, pc=pc, cb_=cb_, f1l=f1l: e.copy(out=Btsb[cb_][:, f1l, :], in_=ps[pc][:, :]),
                      R=[psk[pc]], W=["Btsb%d_%d" % (cb_, f1l)])
            P.add("sp", lambda e, fc=fc, cb_=cb_: e.dma_start(out=Btd[:, fc * 8:(fc + 1) * 8, :], in_=Btsb[cb_]),
                  R=["Btsb%d_%d" % (cb_, k) for k in range(8)], W=["Btd_%d" % fc], dma="Btsb%d" % cb_)
        P.barrier()
        P.release()
        P.mark()
        BtT = [P.sb([128, 8, 512], BF16, "BtT%d" % i) for i in range(2)]
        gch = [P.sb([64, 8, 512], F32, "gch%d" % i) for i in range(2)]
        zo = [P.sb([64, 8, 512], F32, "zo%d" % i) for i in range(2)]
        M = 64 if o == 0 else 32
        gcol = 0 if o == 0 else 512
        dst = blocked(z1_d) if o == 0 else blocked(hout_d)
        dkey = "z1_d" if o == 0 else "hout_d"
        def i1_load(tc):
            cb_ = tc % 2
            for r in range(2):
                P.add("sp", lambda e, r=r, tc=tc, cb_=cb_: e.dma_start(
                    out=BtT[cb_][r * 64:(r + 1) * 64, :, :], in_=Btd[r * 64 + tc * 8:r * 64 + tc * 8 + 8, :, :].rearrange("t f c -> f t c")),
                    R=BTDK, W=["BtT%d" % cb_], dma="BtT%d_%d" % (cb_, r))
            P.add("sp", lambda e, tc=tc, cb_=cb_, M=M, gcol=gcol: e.dma_start(
                out=gch[cb_][0:M, :, :], in_=blocked(U_d[0:M * 64, gcol:gcol + 512])[:, tc * 8:(tc + 1) * 8, :]),
                R=["U_d"], W=["gch%d" % cb_], dma="gch%d" % cb_)

        i1_load(0)
        for tc in range(8):
            cb_ = tc % 2
            if tc + 1 < 8:
                i1_load(tc + 1)
            for t2l in range(8):
                t2 = tc * 8 + t2l
                pb_ = 6 + t2l % 2
                P.add("pe", lambda e, t2=t2, t2l=t2l, cb_=cb_, pb_=pb_, M=M: e.matmul(ps[pb_][0:M, :], lhsT=Dinv[:, t2, 0:M], rhs=BtT[cb_][:, t2l, :],
                                                                                     start=True, stop=True), R=["Dinv", "BtT%d" % cb_], W=[psk[pb_]])
                P.add("dve", lambda e, t2l=t2l, cb_=cb_, pb_=pb_, M=M: e.tensor_tensor(out=zo[cb_][0:M, t2l, :], in0=ps[pb_][0:M, :], in1=gch[cb_][0:M, t2l, :], op=ALU.mult),
                      R=[psk[pb_], "gch%d" % cb_], W=["zo%d_%d" % (cb_, t2l)])
            P.add("sp", lambda e, tc=tc, cb_=cb_, M=M, dst=dst: e.dma_start(out=dst[0:M, tc * 8:(tc + 1) * 8, :], in_=zo[cb_][0:M, :, :]),
                  R=["zo%d_%d" % (cb_, k) for k in range(8)], W=["%s_%d" % (dkey, tc)], dma="zo%d" % cb_)
        P.barrier()
        P.release()
    P.barrier()
    P.release()
    if "z1" in dbg:
        tz1 = dbg_out("z1", [S, 512])
        P.mark()
        tz = P.sb([128, 512], F32, "tz")
        for i in range(NT):
            P.add("sp", lambda e, i=i: e.dma_start(out=tz, in_=z1_d[i * 128:(i + 1) * 128, :]), R=Z1K, W=["tz"], dma="dbg0")
            P.add("sp", lambda e, i=i: e.dma_start(out=tz1[i * 128:(i + 1) * 128, :], in_=tz), R=["tz"], W=["dbgo"], dma="dbg1")
        P.barrier()
        P.release()
    if "h_out" in dbg:
        tho = dbg_out("h_out", [OWN, 512])
        P.mark()
        tz_ = P.sb([128, 512], F32, "tz_")
        for i in range(NTO):
            P.add("sp", lambda e, i=i: e.dma_start(out=tz_, in_=hout_d[i * 128:(i + 1) * 128, :]), R=["hout_d_%d" % k for k in range(8)], W=["tz_"], dma="dbg0")
            P.add("sp", lambda e, i=i: e.dma_start(out=tho[i * 128:(i + 1) * 128, :], in_=tz_), R=["tz_"], W=["dbgo"], dma="dbg1")
        P.barrier()
        P.release()


def hyena_tables(half):
    N = 8192
    f1 = np.arange(64, dtype=np.float64)
    s1 = np.arange(64, dtype=np.float64)
    s2 = np.arange(64, dtype=np.float64)
    th = 2 * np.pi * (f1[None, None, :] + 0.5) * (64 * s1[:, None, None] + s2[None, :, None]) / N
    D0 = np.concatenate([np.cos(th), -np.sin(th)], axis=2)
    perm = (np.arange(64) + 32 * half) % 64
    Dc = D0[perm]
    f2 = np.arange(64, dtype=np.float64)
    ph = 2 * np.pi * np.outer(s2, f2) / 64
    c, s_ = np.cos(ph), np.sin(ph)
    F2a = np.block([[c, -s_], [s_, c]])
    F2b = np.block([[s_, c], [-c, s_]])
    G = np.block([[c, s_], [-s_, c]])
    thi = 2 * np.pi * (f1[:, None, None] + 0.5) * (64 * s1[None, None, :] + s2[None, :, None]) / N
    Dinv0 = np.concatenate([np.cos(thi), -np.sin(thi)], axis=0) * (2.0 / N)
    Dinv = Dinv0[:, :, perm]
    sgn = np.ones((128, 1)); sgn[64:] = -1
    L = S
    t = np.arange(L, dtype=np.float32)
    t01 = t / np.float32(L)
    bands = np.linspace(1e-4, 15, 16, dtype=np.float32)
    ang = (np.float32(2.0 * math.pi) * t[:, None] * bands[None, :] / np.float32(L)).astype(np.float32)
    z = np.concatenate([t01[:, None], np.cos(ang), -np.sin(ang)], axis=-1).astype(np.float32)
    nt01 = (-t01).reshape(NT, 128).T
    f = np.float32
    return {
        "hy_D0": np.ascontiguousarray(D0.reshape(64, 64 * 128).astype(f)), "hy_Dc": np.ascontiguousarray(Dc.reshape(64, 64 * 128).astype(f)),
        "hy_F2a": np.ascontiguousarray(F2a.astype(f)), "hy_F2b": np.ascontiguousarray(F2b.astype(f)), "hy_G": np.ascontiguousarray(G.astype(f)),
        "hy_Dinv": np.ascontiguousarray(Dinv.reshape(128, 64 * 64).astype(f)), "hy_sgn": sgn.astype(f),
        "hy_zT": np.ascontiguousarray(z.T), "hy_nt01": np.ascontiguousarray(nt01.astype(f)),
    }


def host_inputs(inputs, core):
    b, half = divmod(core, 2)
    f32 = np.float32
    x = np.asarray(inputs["x"], dtype=f32)[b]
    own = slice(half * OWN, (half + 1) * OWN)
    oth = slice((1 - half) * OWN, (2 - half) * OWN)
    pos = np.concatenate([np.arange(S)[own], np.arange(S)[oth]])
    m = {}
    m["x_rot"] = np.ascontiguousarray(np.concatenate([x[own], x[oth]], axis=0))
    m["mem_b"] = np.ascontiguousarray(np.asarray(inputs["mem"], dtype=f32)[b])
    for k in ("mix_norm_g", "q_norm_g", "kv_norm_g", "hy_conv_b", "attn_out_g", "hy_out_g", "cross_norm_g",
              "mem_norm_g", "ffn_norm_g"):
        m[k] = np.ascontiguousarray(np.asarray(inputs[k], dtype=f32).reshape(1, -1))
    m["final_norm_g"] = np.ascontiguousarray(np.asarray(inputs["final_norm_g"], dtype=f32).reshape(1, -1))
    for k in ("w_in", "w_uq", "w_ukv", "hy_conv_w", "w_out", "w_mq", "w_mkv", "w_mo"):
        m[k] = np.ascontiguousarray(np.asarray(inputs[k], dtype=f32)[0])
    m["w_route"] = np.ascontiguousarray(np.concatenate([np.asarray(inputs["w_route_group"], f32)[0],
                                                        np.asarray(inputs["w_route_expert"], f32)[0]], axis=1))
    m["b_route"] = np.ascontiguousarray(np.concatenate([np.asarray(inputs["b_route_group"], f32)[0],
                                                        np.asarray(inputs["b_route_expert"], f32)[0]], axis=0).reshape(1, 36))
    m["w_gate"] = np.ascontiguousarray(np.asarray(inputs["w_gate"], f32)[0].reshape(32, D, 256))
    m["w_up"] = np.ascontiguousarray(np.asarray(inputs["w_up"], f32)[0].reshape(32, D, 256))
    m["w_down"] = np.ascontiguousarray(np.asarray(inputs["w_down"], f32)[0].reshape(32, 256, D))
    m["ident"] = np.eye(128, dtype=f32)
    inv = (10000.0 ** (-np.arange(16, dtype=np.float64) / 16)).astype(f32)
    ang = pos.astype(f32)[:, None] * inv[None, :]
    cs = np.concatenate([np.cos(ang), np.sin(ang)], axis=1).astype(f32)
    m["rope_cs"] = np.ascontiguousarray(cs.reshape(NT, 128, 32).transpose(1, 0, 2).reshape(128, NT * 32))
    m.update(hyena_tables(half))
    m["hy_cols"] = np.ascontiguousarray(np.stack([np.asarray(inputs["hy_b1"], f32)[0], np.asarray(inputs["hy_b2"], f32)[0],
                                                  np.asarray(inputs["hy_freq"], f32)[0, 0], np.asarray(inputs["hy_freq"], f32)[0, 1]], axis=1))
    for k in ("hy_w1", "hy_w2", "hy_w3"):
        m[k] = np.ascontiguousarray(np.asarray(inputs[k], f32)[0])
    m["hy_b3"] = np.ascontiguousarray(np.asarray(inputs["hy_b3"], f32).reshape(1, 2048))
    m["hy_decay"] = np.ascontiguousarray(np.asarray(inputs["hy_decay"], f32).reshape(1, 2048))
    m["hy_skip"] = np.ascontiguousarray(np.asarray(inputs["hy_skip"], f32).reshape(1, 1024))
    hm = np.zeros((128, 2), f32)
    hm[:, 0] = half
    hm[:, 1] = 1 - half
    m["halfmask"] = hm
    return m


def kernel(**inputs):
    n = 8
    nc, _ = build()
    in_maps = [host_inputs(inputs, c) for c in range(n)]
    res = run_bass_kernel_spmd(nc, in_maps, core_ids=list(range(n)))
    out = np.zeros((4, S, D), np.float32)
    for c in range(n):
        b, half = divmod(c, 2)
        out[b, half * OWN:(half + 1) * OWN] = res.results[c]["out"]
    return out
```

```python
import math
import os
import contextlib
import numpy as np
import concourse.bass as bass
import concourse.mybir as mybir
from concourse.bass_utils import run_bass_kernel_spmd

F32 = mybir.dt.float32
BF16 = mybir.dt.bfloat16
AF = mybir.ActivationFunctionType
ALU = mybir.AluOpType
AX = mybir.AxisListType
ENGS = ("pe", "act", "dve", "pool", "sp")

D = 1024
S = 4096
OWN = 2048
NT = 32
NTO = 16
EPS = 1e-6
HD = 96
NH = 8


class Op:
    __slots__ = ("eng", "fn", "deps", "dma", "flag", "seq", "idx", "dmaval")

    def __init__(self, eng, fn, dma):
        self.eng = eng
        self.fn = fn
        self.deps = set()
        self.dma = dma
        self.flag = False
        self.seq = 0
        self.dmaval = 0


class Prog:
    ARENA_WORDS = 52000

    def __init__(self, nc):
        self.nc = nc
        self.ops = []
        self.lastw = {}
        self.readers = {}
        self.dma_count = {}
        self.sb_off = 0
        self.sb_marks = []
        self.arena = None
        self.dma_slots = {}

    def sb(self, shape, dtype, name=None):
        if self.arena is None:
            self.arena = self.nc.alloc_sbuf_tensor("arena", [128, self.ARENA_WORDS], F32)
        esz = 4 if dtype == F32 else 2
        nel = int(np.prod(shape[1:]))
        nwords = (nel * esz + 3) // 4
        nwords = (nwords + 15) // 16 * 16
        o = self.sb_off
        self.sb_off += nwords
        assert self.sb_off <= self.ARENA_WORDS, ("SBUF overflow", self.sb_off * 4, name)
        v = self.arena[0:shape[0], o:o + nwords]
        if esz == 2:
            v = v.bitcast(dtype)[:, 0:nel]
        else:
            v = v[:, 0:nel]
        if len(shape) > 2:
            names = " ".join("a%d" % i for i in range(len(shape) - 1))
            kw = {"a%d" % i: int(shape[i + 1]) for i in range(len(shape) - 1)}
            v = v.rearrange("p (%s) -> p %s" % (names, names), **kw)
        return v

    def mark(self):
        self.sb_marks.append(self.sb_off)

    def release(self):
        self.sb_off = self.sb_marks.pop()

    def add(self, eng, fn, R=(), W=(), dma=None):
        if dma is not None:
            slots = self.dma_slots.setdefault(eng, {"free": [], "n": 0, "map": {}})
            if dma not in slots["map"]:
                if slots["free"]:
                    slots["map"][dma] = slots["free"].pop()
                else:
                    slots["map"][dma] = slots["n"]
                    slots["n"] += 1
            dma = (eng, slots["map"][dma])
        op = Op(eng, fn, dma)
        op.idx = len(self.ops)
        if eng != "pe":
            psr = [r for r in R if isinstance(r, str) and r.startswith("ps") and r[2:].isdigit()]
            if psr:
                R = [r for r in R if r not in psr]
                W = list(W) + psr
        deps = set()
        for r in R:
            lw = self.lastw.get(r)
            if lw is not None:
                deps.add(lw)
        for w in W:
            lw = self.lastw.get(w)
            if lw is not None:
                deps.add(lw)
            for rd in self.readers.get(w, ()):
                deps.add(rd)
        if dma is not None:
            k = ("__dmasem", dma)
            lw = self.lastw.get(k)
            if lw is not None:
                deps.add(lw)
            self.lastw[k] = op
            self.dma_count[dma] = self.dma_count.get(dma, 0) + 1
            op.dmaval = 16 * self.dma_count[dma]
        deps.discard(op)
        for d in deps:
            if d.dma is None and d.eng == "pe" and eng == "pe" and dma is None:
                continue
            op.deps.add(d)
            d.flag = True
        for r in R:
            self.readers.setdefault(r, []).append(op)
        for w in W:
            self.lastw[w] = op
            self.readers[w] = []
        self.ops.append(op)
        return op

    def barrier(self):
        fr = {}
        dmas = set()
        allops = set(self.lastw.values())
        for v in self.readers.values():
            allops.update(v)
        for o in allops:
            if o.dma is not None:
                dmas.add(o)
            elif o.eng not in fr or fr[o.eng].idx < o.idx:
                fr[o.eng] = o
        for e in ENGS:
            op = Op(e, None, None)
            op.idx = len(self.ops)
            for d in list(fr.values()) + list(dmas):
                op.deps.add(d)
                d.flag = True
            self.ops.append(op)
        self.lastw = {}
        self.readers = {}
        for sl in self.dma_slots.values():
            sl["free"].extend(sl["map"].values())
            sl["map"].clear()

    def emit(self):
        nc = self.nc
        with contextlib.ExitStack() as st:
            esem = {e: st.enter_context(nc.semaphore("s_" + e)) for e in ENGS}
            dsem = {}
            for k in self.dma_count:
                dsem[k] = st.enter_context(nc.semaphore("d_%d" % len(dsem)))
            cnt = {e: 0 for e in ENGS}
            for op in self.ops:
                if op.dma is None and op.flag:
                    cnt[op.eng] += 1
                    op.seq = cnt[op.eng]
            byeng = {e: [o for o in self.ops if o.eng == e] for e in ENGS}
            if os.environ.get("KDEBUG"):
                print("sem counts", cnt, "ndma sems", len(dsem), "nops", {e: len(v) for e, v in byeng.items()})
            block = st.enter_context(nc.Block())

            def run(e, eng):
                waited = {}
                for op in byeng[e]:
                    need = {}
                    for d in op.deps:
                        if d.dma is not None:
                            s, v = dsem[d.dma], d.dmaval
                        else:
                            s, v = esem[d.eng], d.seq
                        key = id(s)
                        if waited.get(key, 0) >= v:
                            continue
                        if key not in need or need[key][1] < v:
                            need[key] = (s, v)
                    for key, (s, v) in need.items():
                        eng.wait_ge(s, v)
                        waited[key] = v
                    if op.fn is None:
                        continue
                    ins = op.fn(eng)
                    if op.dma is not None:
                        ins.then_inc(dsem[op.dma], 16)
                    elif op.flag:
                        ins.then_inc(esem[e], 1)

            @block.tensor
            def _(eng):
                run("pe", eng)

            @block.scalar
            def _(eng):
                run("act", eng)

            @block.vector
            def _(eng):
                run("dve", eng)

            @block.gpsimd
            def _(eng):
                run("pool", eng)

            @block.sync
            def _(eng):
                run("sp", eng)


INPUT_SHAPES = {
    "x_rot": [S, D], "mem_b": [256, D],
    "mix_norm_g": [1, D], "w_in": [D, 1952], "q_norm_g": [1, 256], "kv_norm_g": [1, 128],
    "w_uq": [256, 768], "w_ukv": [128, 1024], "hy_conv_w": [3, 1536], "hy_conv_b": [1, 1536],
    "attn_out_g": [1, 512], "hy_out_g": [1, 512], "w_out": [D, D],
    "cross_norm_g": [1, D], "mem_norm_g": [1, D], "w_mq": [D, D], "w_mkv": [D, 2 * D], "w_mo": [D, D],
    "ffn_norm_g": [1, D], "w_route": [D, 36], "b_route": [1, 36],
    "w_gate": [32, D, 256], "w_up": [32, D, 256], "w_down": [32, 256, D], "final_norm_g": [1, D],
    "ident": [128, 128], "rope_cs": [128, NT * 32], "halfmask": [128, 2],
    "hy_D0": [64, 64 * 128], "hy_Dc": [64, 64 * 128], "hy_F2a": [128, 128], "hy_F2b": [128, 128], "hy_G": [128, 128],
    "hy_Dinv": [128, 64 * 64], "hy_sgn": [128, 1], "hy_zT": [33, S], "hy_nt01": [128, NT],
    "hy_cols": [64, 4], "hy_w1": [33, 64], "hy_w2": [64, 64], "hy_w3": [64, 2048], "hy_b3": [1, 2048], "hy_decay": [1, 2048],
    "hy_skip": [1, 1024],
}


def build(stop=None, dbg=()):
    nc = bass.Bass("TRN2", target_bir_lowering=False)
    I = {k: nc.dram_tensor(k, v, F32, kind="ExternalInput").ap() for k, v in INPUT_SHAPES.items()}
    out_d = nc.dram_tensor("out", [OWN, D], F32, kind="ExternalOutput").ap()
    dbg_d = {}
    U_d = nc.dram_tensor("U_scr", [S, 1536], F32, kind="Internal").ap()
    hout_d = nc.dram_tensor("hout_scr", [OWN, 512], F32, kind="Internal").ap()
    combT_d = nc.dram_tensor("combT_scr", [32, OWN], F32, kind="Internal").ap()

    P = Prog(nc)
    psall = nc.alloc_psum_tensor("psall", [128, 4096], F32)
    ps = [psall[:, i * 512:(i + 1) * 512] for i in range(8)]
    psk = ["ps%d" % i for i in range(8)]

    def psb(i):
        return ps[i].bitcast(BF16)

    def dbg_out(name, shape):
        t = nc.dram_tensor("dbg_" + name, shape, F32, kind="ExternalOutput").ap()
        dbg_d[name] = t
        return t

    cnt = [0]

    def uid(s):
        cnt[0] += 1
        return "%s_%d" % (s, cnt[0])

    identf = P.sb([128, 128], F32, "identf")
    identb = P.sb([128, 128], BF16, "identb")
    halfm = P.sb([128, 2], F32, "halfm")
    st = P.sb([128, 8], F32, "st")
    junk = P.sb([128, 1024], F32, "junk")
    gb = P.sb([128, 1024], F32, "gb")
    onesb = P.sb([128, 128], BF16, "onesb")
    onesf = P.sb([128, 128], F32, "onesf")
    P.add("sp", lambda e: e.dma_start(out=identf, in_=I["ident"]), W=["identf"], dma="c0")
    P.add("sp", lambda e: e.dma_start(out=halfm, in_=I["halfmask"]), W=["halfm"], dma="c1")
    P.add("dve", lambda e: e.tensor_copy(out=identb, in_=identf), R=["identf"], W=["identb"])
    epst = P.sb([128, 1], F32, "epst")
    P.add("pool", lambda e: e.memset(epst, EPS), W=["epst"])
    P.add("pool", lambda e: e.memset(onesb, 1.0), W=["onesb"])
    P.add("pool", lambda e: e.memset(onesf, 1.0), W=["onesf"])

    def load_gain(name, n=D, key="gb"):
        P.add("sp", lambda e: e.dma_start(out=gb[:, 0:n], in_=I[name].partition_broadcast(128)), W=[key], dma="gain")

    st_tiles = {}
    for _n in ("st", "stq", "stk", "sta", "sth", "stm", "stx", "stf", "stg"):
        st_tiles[_n] = P.sb([128, 4], F32, "st_" + _n)

    def rms_norm(src, n, gview, out_bf, Rk, Wk, stk="st"):
        if stk not in st_tiles:
            st_tiles[stk] = P.sb([128, 4], F32, "st_" + stk)
        st = st_tiles[stk]
        P.add("act", lambda e: e.activation(out=junk[:, 0:n], in_=src, func=AF.Square, accum_out=st[:, 0:1]),
              R=Rk, W=["junk", stk + "0"])
        P.add("act", lambda e: e.activation(out=st[:, 2:3], in_=st[:, 0:1], func=AF.Sqrt, scale=1.0 / n, bias=epst[:, 0:1]),
              R=[stk + "0", "epst"], W=[stk + "2"])
        P.add("dve", lambda e: e.reciprocal(out=st[:, 3:4], in_=st[:, 2:3]), R=[stk + "2"], W=[stk + "3"])
        P.add("dve", lambda e: e.scalar_tensor_tensor(out=out_bf, in0=src, scalar=st[:, 3:4], in1=gview,
                                                      op0=ALU.mult, op1=ALU.mult),
              R=list(Rk) + [stk + "3", "gb"], W=Wk)

    P.mark()
    hqT = P.sb([128, 2, OWN], BF16, "hqT")
    hkvT = P.sb([128, S], BF16, "hkvT")
    krot = P.sb([128, NT, 32], F32, "krot")
    ropecs = P.sb([128, NT, 32], F32, "ropecs")
    P.add("sp", lambda e: e.dma_start(out=ropecs.rearrange("p a b -> p (a b)"), in_=I["rope_cs"]), W=["ropecs"], dma="c2")

    P.mark()
    hT = P.sb([128, 8, 2, OWN + 2], BF16, "hT")
    xt = [P.sb([128, D], F32, "xt%d" % i) for i in range(2)]
    xn = [P.sb([128, D], BF16, "xn%d" % i) for i in range(2)]
    load_gain("mix_norm_g")
    for i in range(NT):
        b = i % 2
        seg, j = divmod(i, NTO)
        P.add("sp", lambda e, i=i, b=b: e.dma_start(out=xt[b], in_=I["x_rot"][i * 128:(i + 1) * 128, :]),
              W=["xt%d" % b], dma="xt%d" % b)
        rms_norm(xt[b], D, gb, xn[b], ["xt%d" % b], ["xn%d" % b])
        pb = 0 + b
        for k in range(8):
            P.add("pe", lambda e, k=k, b=b, pb=pb: e.transpose(out=psb(pb)[:, k * 128:(k + 1) * 128],
                                                                 in_=xn[b][:, k * 128:(k + 1) * 128], identity=identb),
                  R=["xn%d" % b, "identb"], W=[psk[pb]])
        eng = "act" if b == 0 else "dve"
        dst = hT[:, :, seg, 1 + j * 128:1 + (j + 1) * 128]
        src = psb(pb).rearrange("p (k t) -> p k t", k=8)
        if eng == "act":
            P.add("act", lambda e, dst=dst, src=src: e.copy(out=dst, in_=src), R=[psk[pb]], W=["hT"])
        else:
            P.add("dve", lambda e, dst=dst, src=src: e.tensor_copy(out=dst, in_=src), R=[psk[pb]], W=["hT"])
    for (ds, dc, ss_, sc, m) in ((0, 0, 1, OWN, 0), (0, OWN + 1, 1, 1, 1), (1, 0, 0, OWN, 1), (1, OWN + 1, 0, 1, 0)):
        P.add("dve", lambda e, ds=ds, dc=dc, ss_=ss_, sc=sc, m=m: e.tensor_scalar_mul(
            out=hT[:, :, ds, dc:dc + 1], in0=hT[:, :, ss_, sc:sc + 1], scalar1=halfm[:, m:m + 1]),
            R=["hT", "halfm"], W=["hT"])

    w_mla = P.sb([128, 8, 416], BF16, "w_mla")
    P.add("pool", lambda e: e.dma_start(out=w_mla, in_=I["w_in"][:, 0:416].rearrange("(k p) n -> p k n", p=128)),
          W=["w_mla"], dma="w0")
    gq = P.sb([128, 256], F32, "gq")
    gkv = P.sb([128, 128], F32, "gkv")
    P.add("sp", lambda e: e.dma_start(out=gq, in_=I["q_norm_g"].partition_broadcast(128)), W=["gq"], dma="c3")
    P.add("sp", lambda e: e.dma_start(out=gkv, in_=I["kv_norm_g"].partition_broadcast(128)), W=["gkv"], dma="c4")
    hqn = [P.sb([128, 256], BF16, "hqn%d" % i) for i in range(2)]
    hkvn = [P.sb([128, 128], BF16, "hkvn%d" % i) for i in range(2)]
    tmp16 = P.sb([128, 4, 16], F32, "tmp16")
    for i in range(NT):
        b = i % 2
        seg, j = divmod(i, NTO)
        pb = 2 + b
        for k in range(8):
            P.add("pe", lambda e, k=k, pb=pb, seg=seg, j=j: e.matmul(
                ps[pb][:, 0:416], lhsT=hT[:, k, seg, 1 + j * 128:1 + (j + 1) * 128], rhs=w_mla[:, k, :],
                start=(k == 0), stop=(k == 7)), R=["hT", "w_mla"], W=[psk[pb]])
        if seg == 0:
            rms_norm(ps[pb][:, 0:256], 256, gq, hqn[b], [psk[pb], "gq"], ["hqn%d" % b], stk="stq")
            pt = 4 + b
            for k in range(2):
                P.add("pe", lambda e, k=k, b=b, pt=pt: e.transpose(out=psb(pt)[:, k * 128:(k + 1) * 128],
                                                                     in_=hqn[b][:, k * 128:(k + 1) * 128], identity=identb),
                      R=["hqn%d" % b, "identb"], W=[psk[pt]])
            P.add("act", lambda e, pt=pt, j=j: e.copy(out=hqT[:, :, j * 128:(j + 1) * 128],
                                                       in_=psb(pt)[:, 0:256].rearrange("p (k t) -> p k t", k=2)),
                  R=[psk[pt]], W=["hqT"])
        rms_norm(ps[pb][:, 256:384], 128, gkv, hkvn[b], [psk[pb], "gkv"], ["hkvn%d" % b], stk="stk")
        pt = 6 + b
        P.add("pe", lambda e, b=b, pt=pt: e.transpose(out=psb(pt)[:, 0:128], in_=hkvn[b], identity=identb),
              R=["hkvn%d" % b, "identb"], W=[psk[pt]])
        P.add("act", lambda e, pt=pt, i=i: e.copy(out=hkvT[:, i * 128:(i + 1) * 128], in_=psb(pt)[:, 0:128]),
              R=[psk[pt]], W=["hkvT"])
        x1 = ps[pb][:, 384:400]
        x2 = ps[pb][:, 400:416]
        c = ropecs[:, i, 0:16]
        s_ = ropecs[:, i, 16:32]
        P.add("dve", lambda e, x1=x1, c=c: e.tensor_tensor(out=tmp16[:, 0, :], in0=x1, in1=c, op=ALU.mult), R=[psk[pb], "ropecs"], W=["t16a"])
        P.add("dve", lambda e, x2=x2, s_=s_: e.tensor_tensor(out=tmp16[:, 1, :], in0=x2, in1=s_, op=ALU.mult), R=[psk[pb], "ropecs"], W=["t16b"])
        P.add("dve", lambda e, x1=x1, s_=s_: e.tensor_tensor(out=tmp16[:, 2, :], in0=x1, in1=s_, op=ALU.mult), R=[psk[pb], "ropecs"], W=["t16c"])
        P.add("dve", lambda e, x2=x2, c=c: e.tensor_tensor(out=tmp16[:, 3, :], in0=x2, in1=c, op=ALU.mult), R=[psk[pb], "ropecs"], W=["t16d"])
        P.add("dve", lambda e, i=i: e.tensor_tensor(out=krot[:, i, 0:16], in0=tmp16[:, 0, :], in1=tmp16[:, 1, :], op=ALU.subtract),
              R=["t16a", "t16b"], W=["krot"])
        P.add("dve", lambda e, i=i: e.tensor_tensor(out=krot[:, i, 16:32], in0=tmp16[:, 2, :], in1=tmp16[:, 3, :], op=ALU.add),
              R=["t16c", "t16d"], W=["krot"])

    w_hy = P.sb([128, 8, 512], BF16, "w_hy")
    w_k3 = P.sb([128, 3, 8, 512], BF16, "w_k3")
    cw = P.sb([128, 3, 512], F32, "cw")
    brow = P.sb([1, 512], BF16, "brow")
    uo = [P.sb([128, 512], F32, "uo%d" % i) for i in range(2)]
    for c3 in range(3):
        c0 = 416 + c3 * 512
        P.add("pool", lambda e, c0=c0: e.dma_start(out=w_hy, in_=I["w_in"][:, c0:c0 + 512].rearrange("(k p) n -> p k n", p=128)),
              W=["w_hy"], dma="w1")
        for k3 in range(3):
            P.add("sp", lambda e, k3=k3, c3=c3: e.dma_start(
                out=cw[:, k3, :], in_=I["hy_conv_w"][k3:k3 + 1, c3 * 512:(c3 + 1) * 512].partition_broadcast(128)),
                W=["cw%d" % k3], dma="cw%d" % k3)
        P.add("pool", lambda e, c3=c3: e.dma_start(out=brow, in_=I["hy_conv_b"][0:1, c3 * 512:(c3 + 1) * 512]),
              W=["brow"], dma="w2")
        for k3 in range(3):
            for k in range(8):
                P.add("dve" if k % 2 == 0 else "pool", lambda e, k3=k3, k=k: e.tensor_tensor(
                    out=w_k3[:, k3, k, :], in0=w_hy[:, k, :], in1=cw[:, k3, :], op=ALU.mult),
                    R=["w_hy", "cw%d" % k3], W=["w_k3_%d_%d" % (k3, k)])
        for i in range(NT):
            b = i % 2
            seg, j = divmod(i, NTO)
            pb = 2 + b
            n = 0
            for k3 in range(3):
                for k in range(8):
                    P.add("pe", lambda e, k3=k3, k=k, pb=pb, seg=seg, j=j, n=n: e.matmul(
                        ps[pb][:, :], lhsT=hT[:, k, seg, k3 + j * 128:k3 + (j + 1) * 128], rhs=w_k3[:, k3, k, :],
                        start=(n == 0), stop=False), R=["hT", "w_k3_%d_%d" % (k3, k)], W=[psk[pb]])
                    n += 1
            P.add("pe", lambda e, pb=pb: e.matmul(ps[pb][:, :], lhsT=onesb[0:1, 0:128], rhs=brow[0:1, :], start=False, stop=True),
                  R=["onesb", "brow"], W=[psk[pb]])
            if b == 0:
                P.add("act", lambda e, pb=pb, b=b: e.copy(out=uo[b], in_=ps[pb][:, :]), R=[psk[pb]], W=["uo%d" % b])
            else:
                P.add("dve", lambda e, pb=pb, b=b: e.tensor_copy(out=uo[b], in_=ps[pb][:, :]), R=[psk[pb]], W=["uo%d" % b])
            P.add("sp", lambda e, i=i, b=b, c3=c3: e.dma_start(out=U_d[i * 128:(i + 1) * 128, c3 * 512:(c3 + 1) * 512], in_=uo[b]),
                  R=["uo%d" % b], W=["U_d"], dma="uo%d" % b)
    P.barrier()
    P.release()

    if stop == "A0":
        P.add("sp", None, R=[])
        P.emit()
        return nc, dbg_d
    if "uc" in dbg:
        tu = dbg_out("uc", [S, 1536])
        P.mark()
        tb = P.sb([128, 1536], F32, "dbgt")
        for i in range(NT):
            P.add("sp", lambda e, i=i: e.dma_start(out=tb, in_=U_d[i * 128:(i + 1) * 128, :]), R=["U_d"], W=["dbgt"], dma="dbg0")
            P.add("sp", lambda e, i=i: e.dma_start(out=tu[i * 128:(i + 1) * 128, :], in_=tb), R=["dbgt"], W=["dbgo"], dma="dbg1")
        P.barrier()
        P.release()

    if stop == "A":
        P.add("sp", None, R=["dbgo"])
        P.emit()
        return nc, dbg_d
    aout_d = nc.dram_tensor("aout_scr", [OWN, 512], F32, kind="Internal").ap()
    P.mark()
    G4 = 8
    KT = P.sb([128, G4, S], BF16, "KT")
    QT = P.sb([128, G4, OWN], BF16, "QT")
    Vaug = P.sb([128, NT, G4, 68], BF16, "Vaug")
    w_ukv = P.sb([128, 1024], BF16, "w_ukv")
    w_uq = P.sb([128, 2, 768], BF16, "w_uq")
    P.add("pool", lambda e: e.dma_start(out=w_ukv, in_=I["w_ukv"]), W=["w_ukv"], dma="w0")
    P.add("pool", lambda e: e.dma_start(out=w_uq, in_=I["w_uq"].rearrange("(k p) n -> p k n", p=128)), W=["w_uq"], dma="w1")
    Kaug = [P.sb([128, G4, 100], BF16, "Kaug%d" % i) for i in range(2)]
    Qaug = [P.sb([128, G4, 100], BF16, "Qaug%d" % i) for i in range(2)]
    ksq = P.sb([128, G4, 96], F32, "ksq")
    kn2 = P.sb([128, G4], F32, "kn2")
    kmax = P.sb([128, G4], F32, "kmax")
    kb = P.sb([128, 4], F32, "kb")
    qs = P.sb([128, G4, 96], F32, "qs")
    qsq = ksq
    qn = P.sb([128, G4], F32, "qn")
    qt4 = P.sb([128, 4, G4, 16], F32, "qt4")
    PT = [P.sb([128, 512], BF16, "PT%d" % i) for i in range(3)]
    oTs = P.sb([65, 512], F32, "oTs")
    rden = P.sb([128, 4], F32, "rden")
    astage = [P.sb([128, 4, 512], F32, "astage0")] * 2
    scale = HD ** -0.5
    it = 0
    for g in range(1):
        P.add("pool", lambda e: e.memset(Vaug.rearrange("p a b c -> p (a b c)"), 1.0), W=["Vaug"])
        for b in range(2):
            P.add("pool", lambda e, b=b: e.memset(Kaug[b].rearrange("p a b -> p (a b)"), 1.0), W=["Kaug%d" % b])
        P.add("pool", lambda e: e.memset(kmax, 0.0), W=["kmax"])
        for i in range(NT):
            b = i % 2
            pbk = 2 * b
            for hh in range(2):
                P.add("pe", lambda e, hh=hh, pbk=pbk, i=i: e.matmul(ps[pbk + hh][:, :], lhsT=hkvT[:, i * 128:(i + 1) * 128],
                                                                     rhs=w_ukv[:, hh * 512:(hh + 1) * 512], start=True, stop=True),
                      R=["hkvT", "w_ukv"], W=[psk[pbk + hh]])
            v = psall[:, pbk * 512:(pbk + 2) * 512].rearrange("p (h c) -> p h c", h=8)
            P.add("act", lambda e, v=v, i=i: e.copy(out=Vaug[:, i, :, 0:64], in_=v[:, :, 64:128]), R=[psk[pbk], psk[pbk + 1]], W=["Vaug"])
            P.add("dve", lambda e, v=v, b=b: e.tensor_copy(out=Kaug[b][:, :, 0:64], in_=v[:, :, 0:64]), R=[psk[pbk], psk[pbk + 1]], W=["Kaug%d" % b])
            for h in range(G4):
                P.add("pool", lambda e, h=h, b=b, i=i: e.tensor_copy(out=Kaug[b][:, h, 64:96], in_=krot[:, i, :]),
                      R=["krot"], W=["Kaug%d" % b])
            P.add("dve", lambda e, b=b: e.tensor_tensor(out=ksq, in0=Kaug[b][:, :, 0:96], in1=Kaug[b][:, :, 0:96], op=ALU.mult),
                  R=["Kaug%d" % b], W=["ksq"])
            P.add("dve", lambda e: e.tensor_reduce(out=kn2, in_=ksq, axis=AX.X, op=ALU.add), R=["ksq"], W=["kn2"])
            P.add("dve", lambda e: e.tensor_tensor(out=kmax, in0=kmax, in1=kn2, op=ALU.max), R=["kn2", "kmax"], W=["kmax"])
            pt = 4 + b
            for h in range(G4):
                P.add("pe", lambda e, h=h, b=b, pt=pt: e.transpose(out=psb(pt)[0:97, h * 128:(h + 1) * 128], in_=Kaug[b][:, h, 0:97], identity=identb),
                      R=["Kaug%d" % b, "identb"], W=[psk[pt]])
            P.add("act", lambda e, pt=pt, i=i: e.copy(out=KT[0:97, :, i * 128:(i + 1) * 128],
                                                       in_=psb(pt)[0:97, 0:G4 * 128].rearrange("p (h t) -> p h t", h=G4)),
                  R=[psk[pt]], W=["KT"])
        P.add("dve", lambda e: e.tensor_reduce(out=kb[:, 1:2], in_=kmax, axis=AX.X, op=ALU.max), R=["kmax"], W=["kb1"])
        P.add("pe", lambda e: e.transpose(out=ps[6][0:1, 0:128], in_=kb[:, 1:2], identity=identf), R=["kb1", "identf"], W=[psk[6]])
        P.add("dve", lambda e: e.tensor_reduce(out=kb[0:1, 2:3], in_=ps[6][0:1, 0:128], axis=AX.X, op=ALU.max), R=[psk[6]], W=["kb2"])
        P.add("pe", lambda e: e.matmul(ps[7][:, 0:1], lhsT=onesf[0:1, 0:128], rhs=kb[0:1, 2:3], start=True, stop=True),
              R=["kb2", "onesf"], W=[psk[7]])
        P.add("act", lambda e: e.sqrt(out=kb[:, 0:1], in_=ps[7][:, 0:1]), R=[psk[7]], W=["kb0"])
        for j in range(NTO):
            b = j % 2
            pa = 2 * b
            for (pq, c0, ncol) in ((pa, 0, 480), (pa + 1, 480, 288)):
                for k in range(2):
                    P.add("pe", lambda e, pq=pq, c0=c0, ncol=ncol, k=k, j=j: e.matmul(
                        ps[pq][:, 0:ncol], lhsT=hqT[:, k, j * 128:(j + 1) * 128], rhs=w_uq[:, k, c0:c0 + ncol],
                        start=(k == 0), stop=(k == 1)), R=["hqT", "w_uq"], W=[psk[pq]])
            P.add("act", lambda e, pa=pa: e.mul(out=qs[:, 0:5, :], in_=ps[pa][:, 0:480].rearrange("p (h c) -> p h c", h=5), mul=scale),
                  R=[psk[pa]], W=["qs"])
            P.add("act", lambda e, pa=pa: e.mul(out=qs[:, 5:8, :], in_=ps[pa + 1][:, 0:288].rearrange("p (h c) -> p h c", h=3), mul=scale),
                  R=[psk[pa + 1]], W=["qs"])
            c = ropecs[:, j:j + 1, 0:16].broadcast_to([128, G4, 16])
            s_ = ropecs[:, j:j + 1, 16:32].broadcast_to([128, G4, 16])
            x1 = qs[:, :, 64:80]
            x2 = qs[:, :, 80:96]
            P.add("dve", lambda e, x1=x1, c=c: e.tensor_tensor(out=qt4[:, 0], in0=x1, in1=c, op=ALU.mult), R=["qs", "ropecs"], W=["qt4a"])
            P.add("dve", lambda e, x2=x2, s_=s_: e.tensor_tensor(out=qt4[:, 1], in0=x2, in1=s_, op=ALU.mult), R=["qs", "ropecs"], W=["qt4b"])
            P.add("pool", lambda e, x1=x1, s_=s_: e.tensor_tensor(out=qt4[:, 2], in0=x1, in1=s_, op=ALU.mult), R=["qs", "ropecs"], W=["qt4c"])
            P.add("pool", lambda e, x2=x2, c=c: e.tensor_tensor(out=qt4[:, 3], in0=x2, in1=c, op=ALU.mult), R=["qs", "ropecs"], W=["qt4d"])
            P.add("dve", lambda e: e.tensor_tensor(out=qs[:, :, 64:80], in0=qt4[:, 0], in1=qt4[:, 1], op=ALU.subtract),
                  R=["qt4a", "qt4b"], W=["qs"])
            P.add("dve", lambda e: e.tensor_tensor(out=qs[:, :, 80:96], in0=qt4[:, 2], in1=qt4[:, 3], op=ALU.add),
                  R=["qt4c", "qt4d"], W=["qs"])
            P.add("dve", lambda e: e.tensor_tensor(out=qsq, in0=qs, in1=qs, op=ALU.mult), R=["qs"], W=["ksq"])
            P.add("dve", lambda e: e.tensor_reduce(out=qn, in_=qsq, axis=AX.X, op=ALU.add), R=["ksq"], W=["qn"])
            P.add("act", lambda e: e.sqrt(out=qn, in_=qn), R=["qn"], W=["qn"])
            P.add("dve", lambda e, b=b: e.tensor_scalar(out=Qaug[b][:, :, 96:97], in0=qn.rearrange("p (h o) -> p h o", o=1),
                                                        scalar1=kb[:, 0:1], scalar2=-1.0, op0=ALU.mult, op1=ALU.mult),
                  R=["qn", "kb0"], W=["Qaug%d" % b])
            P.add("act", lambda e, b=b: e.copy(out=Qaug[b][:, :, 0:96], in_=qs), R=["qs"], W=["Qaug%d" % b])
            pt = 4 + b
            for h in range(G4):
                P.add("pe", lambda e, h=h, b=b, pt=pt: e.transpose(out=psb(pt)[0:97, h * 128:(h + 1) * 128], in_=Qaug[b][:, h, 0:97], identity=identb),
                      R=["Qaug%d" % b, "identb"], W=[psk[pt]])
            P.add("dve", lambda e, pt=pt, j=j: e.tensor_copy(out=QT[0:97, :, j * 128:(j + 1) * 128],
                                                             in_=psb(pt)[0:97, 0:G4 * 128].rearrange("p (h t) -> p h t", h=G4)),
                  R=[psk[pt]], W=["QT"])
        items = [(qc, h, kt) for qc in range(4) for h in range(G4) for kt in range(NT)]
        LA = 2

        def emit_scores(idx):
            qc, h, kt = items[idx]
            pb_ = idx % 3
            P.add("pe", lambda e, h=h, qc=qc, kt=kt, pb_=pb_: e.matmul(
                ps[pb_][:, :], lhsT=KT[0:97, h, kt * 128:(kt + 1) * 128], rhs=QT[0:97, h, qc * 512:(qc + 1) * 512],
                start=True, stop=True), R=["KT", "QT"], W=[psk[pb_]])

        def emit_epilogue(qc, h):
            po = 6 + h % 2
            sb_ = qc % 2
            P.add("dve", lambda e, po=po: e.tensor_copy(out=oTs, in_=ps[po][0:65, :]), R=[psk[po]], W=["oTs"])
            for t4 in range(4):
                pt = 3 + (t4 % 2)
                P.add("pe", lambda e, t4=t4, pt=pt: e.transpose(out=ps[pt][:, 0:65], in_=oTs[:, t4 * 128:(t4 + 1) * 128], identity=identf[0:65, 0:65]),
                      R=["oTs", "identf"], W=[psk[pt]])
                P.add("dve", lambda e, pt=pt, t4=t4: e.reciprocal(out=rden[:, t4:t4 + 1], in_=ps[pt][:, 64:65]), R=[psk[pt]], W=["rden%d" % t4])
                P.add("dve", lambda e, pt=pt, t4=t4, h=h, sb_=sb_: e.tensor_scalar_mul(
                    out=astage[sb_][:, t4, h * 64:(h + 1) * 64], in0=ps[pt][:, 0:64], scalar1=rden[:, t4:t4 + 1]),
                    R=[psk[pt], "rden%d" % t4], W=["astage0"])
            if h == G4 - 1:
                P.add("sp", lambda e, qc=qc, g=g, sb_=sb_: e.dma_start(
                    out=aout_d[qc * 512:(qc + 1) * 512, :].rearrange("(t p) c -> p t c", p=128), in_=astage[sb_]),
                    R=["astage0"], W=["aout_d"], dma="ast0")

        for idx in range(min(LA, len(items))):
            emit_scores(idx)
        pending = None
        for idx, (qc, h, kt) in enumerate(items):
            pb_ = idx % 3
            po = 6 + h % 2
            P.add("act", lambda e, pb_=pb_: e.activation(out=PT[pb_], in_=ps[pb_][:, :], func=AF.Exp),
                  R=[psk[pb_]], W=["PT%d" % pb_])
            if idx + LA < len(items):
                emit_scores(idx + LA)
            P.add("pe", lambda e, h=h, kt=kt, pb_=pb_, po=po: e.matmul(
                ps[po][0:65, :], lhsT=Vaug[:, kt, h, 0:65], rhs=PT[pb_], start=(kt == 0), stop=(kt == NT - 1)),
                R=["Vaug", "PT%d" % pb_], W=[psk[po]])
            if pending is not None and kt == 3:
                emit_epilogue(*pending)
                pending = None
            if kt == NT - 1:
                pending = (qc, h)
        if pending is not None:
            emit_epilogue(*pending)
    P.barrier()
    P.release()
    P.release()

    if "a_out" in dbg:
        ta = dbg_out("a_out", [OWN, 512])
        P.mark()
        tba = P.sb([128, NTO, 512], F32, "dbgt2")
        P.add("sp", lambda e: e.dma_start(out=tba, in_=aout_d.rearrange("(j p) c -> p j c", p=128)), R=["aout_d"], W=["dbgt2"], dma="dbg0")
        P.add("sp", lambda e: e.dma_start(out=ta.rearrange("(j p) c -> p j c", p=128), in_=tba), R=["dbgt2"], W=["dbgo"], dma="dbg1")
        P.barrier()
        P.release()

    if stop == "attn":
        P.add("sp", None, R=[])
        P.emit()
        return nc, dbg_d

    if "hout_in" in dbg:
        hin = nc.dram_tensor("dbg_hout_in", [OWN, 512], F32, kind="ExternalInput").ap()
        P.mark()
        tbh = P.sb([128, NTO, 512], F32, "tbh")
        P.add("sp", lambda e: e.dma_start(out=tbh, in_=hin.rearrange("(j p) c -> p j c", p=128)), W=["tbh"], dma="dbg0")
        P.add("sp", lambda e: e.dma_start(out=hout_d.rearrange("(j p) c -> p j c", p=128), in_=tbh), R=["tbh"], W=["hout_d"], dma="dbg1")
        P.barrier()
        P.release()
    else:
        hyena_phase(nc, P, I, ps, psk, psb, U_d, hout_d, identf, identb, onesb, onesf, halfm, dbg, dbg_out, psall)

    if stop == "C":
        P.add("sp", None, R=[])
        P.emit()
        return nc, dbg_d
    xres = P.sb([128, NTO, D], F32, "xres")
    P.add("sp", lambda e: e.dma_start(out=xres, in_=I["x_rot"][0:OWN, :].rearrange("(j p) c -> p j c", p=128)), W=["xres"], dma="xres")
    P.mark()
    w_out = P.sb([128, 8, D], BF16, "w_out")
    P.add("pool", lambda e: e.dma_start(out=w_out, in_=I["w_out"].rearrange("(k p) n -> p k n", p=128)), W=["w_out"], dma="w0")
    P.add("sp", lambda e: e.dma_start(out=gb[:, 0:512], in_=I["attn_out_g"].partition_broadcast(128)), W=["gb"], dma="gain")
    P.add("sp", lambda e: e.dma_start(out=gb[:, 512:1024], in_=I["hy_out_g"].partition_broadcast(128)), W=["gb"], dma="gain")
    mixin = [P.sb([128, D], F32, "mixin%d" % i) for i in range(2)]
    mixbf = [P.sb([128, D], BF16, "mixbf%d" % i) for i in range(2)]
    mT = [P.sb([128, 8, 128], BF16, "mT%d" % i) for i in range(2)]
    for j in range(NTO):
        b = j % 2
        P.add("sp", lambda e, j=j, b=b: e.dma_start(out=mixin[b][:, 0:512], in_=aout_d[j * 128:(j + 1) * 128, :]),
              R=["aout_d"], W=["mixin%d" % b], dma="mixa%d" % b)
        P.add("sp", lambda e, j=j, b=b: e.dma_start(out=mixin[b][:, 512:1024], in_=hout_d[j * 128:(j + 1) * 128, :]),
              R=["hout_d"] + ["hout_d_%d" % k for k in range(8)], W=["mixin%d" % b], dma="mixh%d" % b)
        rms_norm(mixin[b][:, 0:512], 512, gb[:, 0:512], mixbf[b][:, 0:512], ["mixin%d" % b], ["mixbfa%d" % b], stk="sta")
        rms_norm(mixin[b][:, 512:1024], 512, gb[:, 512:1024], mixbf[b][:, 512:1024], ["mixin%d" % b], ["mixbfh%d" % b], stk="sth")
        pt = 0 + b
        for k in range(8):
            P.add("pe", lambda e, k=k, b=b, pt=pt: e.transpose(out=psb(pt)[:, k * 128:(k + 1) * 128], in_=mixbf[b][:, k * 128:(k + 1) * 128], identity=identb),
                  R=["mixbfa%d" % b, "mixbfh%d" % b, "identb"], W=[psk[pt]])
        P.add("act", lambda e, b=b, pt=pt: e.copy(out=mT[b].rearrange("p k t -> p (k t)"), in_=psb(pt)), R=[psk[pt]], W=["mT%d" % b])
        for n in range(2):
            py = 2 + 2 * b + n
            for k in range(8):
                P.add("pe", lambda e, k=k, b=b, n=n, py=py: e.matmul(ps[py][:, :], lhsT=mT[b][:, k, :], rhs=w_out[:, k, n * 512:(n + 1) * 512],
                                                                      start=(k == 0), stop=(k == 7)), R=["mT%d" % b, "w_out"], W=[psk[py]])
            P.add("dve", lambda e, j=j, n=n, py=py: e.tensor_tensor(out=xres[:, j, n * 512:(n + 1) * 512], in0=ps[py][:, :],
                                                                     in1=xres[:, j, n * 512:(n + 1) * 512], op=ALU.add),
                  R=[psk[py], "xres"], W=["xres"])
    P.barrier()
    P.release()
    if stop == "D":
        P.add("sp", None, R=[])
        P.emit()
        return nc, dbg_d
    if "x1" in dbg:
        tx1 = dbg_out("x1", [OWN, D])
        P.add("sp", lambda e: e.dma_start(out=tx1.rearrange("(j p) c -> p j c", p=128), in_=xres), R=["xres"], W=["dbgo"], dma="dbg1")
        P.barrier()

    P.mark()
    hmT = P.sb([128, 8, 256], BF16, "hmT")
    KmT = P.sb([128, 8, 256], BF16, "KmT")
    Vm = P.sb([128, 2, 4, 260], BF16, "Vm")
    ksqm = P.sb([128, 8, 256], BF16, "ksqm")
    kbx = P.sb([1, 8], F32, "kbx")
    P.mark()
    w_mkv = P.sb([128, 8, 2 * D], BF16, "w_mkv")
    P.add("pool", lambda e: e.dma_start(out=w_mkv, in_=I["w_mkv"].rearrange("(k p) n -> p k n", p=128)), W=["w_mkv"], dma="w0")
    load_gain("mem_norm_g")
    P.add("pool", lambda e: e.memset(Vm.rearrange("p a b c -> p (a b c)"), 1.0), W=["Vm"])
    memt = [P.sb([128, D], F32, "memt%d" % i) for i in range(2)]
    membf = [P.sb([128, D], BF16, "membf%d" % i) for i in range(2)]
    for mt in range(2):
        P.add("sp", lambda e, mt=mt: e.dma_start(out=memt[mt], in_=I["mem_b"][mt * 128:(mt + 1) * 128, :]), W=["memt%d" % mt], dma="memt%d" % mt)
        rms_norm(memt[mt], D, gb, membf[mt], ["memt%d" % mt], ["membf%d" % mt], stk="stm")
        for k in range(8):
            P.add("pe", lambda e, k=k, mt=mt: e.transpose(out=psb(mt)[:, k * 128:(k + 1) * 128], in_=membf[mt][:, k * 128:(k + 1) * 128], identity=identb),
                  R=["membf%d" % mt, "identb"], W=[psk[mt]])
        P.add("act", lambda e, mt=mt: e.copy(out=hmT[:, :, mt * 128:(mt + 1) * 128], in_=psb(mt).rearrange("p (k t) -> p k t", k=8)),
              R=[psk[mt]], W=["hmT"])
    for dt in range(8):
        pk = 2 + dt % 2
        for k in range(8):
            P.add("pe", lambda e, k=k, dt=dt, pk=pk: e.matmul(ps[pk][:, 0:256], lhsT=w_mkv[:, k, dt * 128:(dt + 1) * 128], rhs=hmT[:, k, :],
                                                               start=(k == 0), stop=(k == 7)), R=["w_mkv", "hmT"], W=[psk[pk]])
        P.add("act", lambda e, dt=dt, pk=pk: e.copy(out=KmT[:, dt, :], in_=ps[pk][:, 0:256]), R=[psk[pk]], W=["KmT"])
    for mt in range(2):
        for n in range(2):
            pv = 4 + n
            for k in range(8):
                P.add("pe", lambda e, k=k, mt=mt, n=n, pv=pv: e.matmul(ps[pv][:, :], lhsT=hmT[:, k, mt * 128:(mt + 1) * 128],
                                                                        rhs=w_mkv[:, k, D + n * 512:D + (n + 1) * 512],
                                                                        start=(k == 0), stop=(k == 7)), R=["w_mkv", "hmT"], W=[psk[pv]])
            P.add("dve", lambda e, mt=mt, n=n, pv=pv: e.tensor_copy(out=Vm[:, mt, 2 * n:2 * n + 2, 0:256],
                                                                    in_=ps[pv][:, :].rearrange("p (h c) -> p h c", h=2)),
                  R=[psk[pv]], W=["Vm"])
    P.add("dve", lambda e: e.tensor_tensor(out=ksqm, in0=KmT, in1=KmT, op=ALU.mult), R=["KmT"], W=["ksqm"])
    for hh in range(4):
        for dt in range(2):
            P.add("pe", lambda e, hh=hh, dt=dt: e.matmul(ps[6][0:1, 0:256], lhsT=onesb[:, 0:1], rhs=ksqm[:, 2 * hh + dt, :],
                                                          start=(dt == 0), stop=(dt == 1)), R=["ksqm", "onesb"], W=[psk[6]])
        P.add("dve", lambda e, hh=hh: e.tensor_reduce(out=kbx[0:1, hh:hh + 1], in_=ps[6][0:1, 0:256], axis=AX.X, op=ALU.max),
              R=[psk[6]], W=["kbx%d" % hh])
    P.add("dve", lambda e: e.tensor_reduce(out=kbx[0:1, 4:5], in_=kbx[0:1, 0:4], axis=AX.X, op=ALU.max),
          R=["kbx0", "kbx1", "kbx2", "kbx3"], W=["kbx4"])
    P.add("act", lambda e: e.sqrt(out=kbx[0:1, 5:6], in_=kbx[0:1, 4:5]), R=["kbx4"], W=["kbx5"])
    P.add("dve", lambda e: e.tensor_scalar_mul(out=kbx[0:1, 6:7], in0=kbx[0:1, 5:6], scalar1=-1.04), R=["kbx5"], W=["kbx6"])
    P.barrier()
    P.release()
    w_mq = P.sb([128, 8, D], BF16, "w_mq")
    w_mo = P.sb([128, 8, D], BF16, "w_mo")
    P.add("pool", lambda e: e.dma_start(out=w_mq, in_=I["w_mq"].rearrange("(k p) n -> p k n", p=128)), W=["w_mq"], dma="w0")
    P.add("pool", lambda e: e.dma_start(out=w_mo, in_=I["w_mo"].rearrange("(k p) n -> p k n", p=128)), W=["w_mo"], dma="w1")
    load_gain("cross_norm_g")
    hxbf = [P.sb([128, D], BF16, "hxbf%d" % i) for i in range(2)]
    hxT = P.sb([128, 8, 512], BF16, "hxT")
    qT = P.sb([128, 8, 512], BF16, "qT")
    qsqx = P.sb([128, 8, 512], BF16, "qsqx")
    negm = P.sb([1, 4, 512], BF16, "negm")
    qn1 = P.sb([1, 512], F32, "qn1")
    PTm = [P.sb([128, 512], BF16, "PTm%d" % i) for i in range(2)]
    rdn = P.sb([1, 512], F32, "rdn")
    rdb = P.sb([128, 512], F32, "rdb")
    oTx = P.sb([128, 8, 512], BF16, "oTx")
    for qc in range(4):
        for t4 in range(4):
            j = qc * 4 + t4
            b = t4 % 2
            rms_norm(xres[:, j, :], D, gb, hxbf[b], ["xres"], ["hxbf%d" % b], stk="stx")
            for k in range(8):
                P.add("pe", lambda e, k=k, b=b: e.transpose(out=psb(b)[:, k * 128:(k + 1) * 128], in_=hxbf[b][:, k * 128:(k + 1) * 128], identity=identb),
                      R=["hxbf%d" % b, "identb"], W=[psk[b]])
            P.add("act", lambda e, b=b, t4=t4: e.copy(out=hxT[:, :, t4 * 128:(t4 + 1) * 128], in_=psb(b).rearrange("p (k t) -> p k t", k=8)),
                  R=[psk[b]], W=["hxT"])
        for dt in range(8):
            pq = 6 + dt % 2
            for k in range(8):
                P.add("pe", lambda e, k=k, dt=dt, pq=pq: e.matmul(ps[pq][:, :], lhsT=w_mq[:, k, dt * 128:(dt + 1) * 128], rhs=hxT[:, k, :],
                                                                   start=(k == 0), stop=(k == 7)), R=["w_mq", "hxT"], W=[psk[pq]])
            P.add("act", lambda e, dt=dt, pq=pq: e.mul(out=qT[:, dt, :], in_=ps[pq][:, :], mul=1.0 / 16.0), R=[psk[pq]], W=["qT"])
        P.add("dve", lambda e: e.tensor_tensor(out=qsqx, in0=qT, in1=qT, op=ALU.mult), R=["qT"], W=["qsqx"])
        for hh in range(4):
            for dt in range(2):
                P.add("pe", lambda e, hh=hh, dt=dt: e.matmul(ps[4][0:1, :], lhsT=onesb[:, 0:1], rhs=qsqx[:, 2 * hh + dt, :],
                                                              start=(dt == 0), stop=(dt == 1)), R=["qsqx", "onesb"], W=[psk[4]])
            P.add("act", lambda e: e.sqrt(out=qn1, in_=ps[4][0:1, :]), R=[psk[4]], W=["qn1"])
            P.add("dve", lambda e, hh=hh: e.tensor_scalar_mul(out=negm[0:1, hh, :], in0=qn1, scalar1=kbx[0:1, 6:7]), R=["qn1", "kbx6"], W=["negm"])
        for hh in range(4):
            for mt in range(2):
                for dt in range(2):
                    P.add("pe", lambda e, hh=hh, mt=mt, dt=dt: e.matmul(ps[mt][:, :], lhsT=KmT[:, 2 * hh + dt, mt * 128:(mt + 1) * 128],
                                                                         rhs=qT[:, 2 * hh + dt, :], start=(dt == 0), stop=False),
                          R=["KmT", "qT"], W=[psk[mt]])
                P.add("pe", lambda e, hh=hh, mt=mt: e.matmul(ps[mt][:, :], lhsT=onesb[0:1, 0:128], rhs=negm[0:1, hh, :], start=False, stop=True),
                      R=["negm", "onesb"], W=[psk[mt]])
                P.add("act", lambda e, mt=mt: e.activation(out=PTm[mt], in_=ps[mt][:, :], func=AF.Exp), R=[psk[mt]], W=["PTm%d" % mt])
            for dv in range(2):
                for mt in range(2):
                    P.add("pe", lambda e, hh=hh, mt=mt, dv=dv: e.matmul(ps[2 + dv][:, :], lhsT=Vm[:, mt, hh, dv * 128:(dv + 1) * 128], rhs=PTm[mt],
                                                                         start=(mt == 0), stop=(mt == 1)), R=["Vm", "PTm%d" % mt], W=[psk[2 + dv]])
            for mt in range(2):
                P.add("pe", lambda e, hh=hh, mt=mt: e.matmul(ps[4][0:1, :], lhsT=Vm[:, mt, hh, 256:257], rhs=PTm[mt], start=(mt == 0), stop=(mt == 1)),
                      R=["Vm", "PTm%d" % mt], W=[psk[4]])
            P.add("dve", lambda e: e.reciprocal(out=rdn, in_=ps[4][0:1, :]), R=[psk[4]], W=["rdn"])
            P.add("pe", lambda e: e.matmul(ps[5][:, :], lhsT=onesf[0:1, 0:128], rhs=rdn, start=True, stop=True), R=["rdn", "onesf"], W=[psk[5]])
            P.add("act", lambda e: e.copy(out=rdb, in_=ps[5][:, :]), R=[psk[5]], W=["rdb"])
            for dv in range(2):
                P.add("dve", lambda e, hh=hh, dv=dv: e.tensor_tensor(out=oTx[:, 2 * hh + dv, :], in0=ps[2 + dv][:, :], in1=rdb, op=ALU.mult),
                      R=[psk[2 + dv], "rdb"], W=["oTx"])
        for t4 in range(4):
            j = qc * 4 + t4
            for n in range(2):
                py = 6 + n
                for dt in range(8):
                    P.add("pe", lambda e, dt=dt, t4=t4, n=n, py=py: e.matmul(ps[py][:, :], lhsT=oTx[:, dt, t4 * 128:(t4 + 1) * 128],
                                                                              rhs=w_mo[:, dt, n * 512:(n + 1) * 512], start=(dt == 0), stop=(dt == 7)),
                          R=["oTx", "w_mo"], W=[psk[py]])
                P.add("dve", lambda e, j=j, n=n, py=py: e.tensor_tensor(out=xres[:, j, n * 512:(n + 1) * 512], in0=ps[py][:, :],
                                                                         in1=xres[:, j, n * 512:(n + 1) * 512], op=ALU.add),
                      R=[psk[py], "xres"], W=["xres"])
    P.barrier()
    P.release()
    if "x2" in dbg:
        tx2 = dbg_out("x2", [OWN, D])
        P.add("sp", lambda e: e.dma_start(out=tx2.rearrange("(j p) c -> p j c", p=128), in_=xres), R=["xres"], W=["dbgo"], dma="dbg1")
        P.barrier()
    if stop == "E":
        P.add("sp", None, R=[])
        P.emit()
        return nc, dbg_d

    P.mark()
    tT = P.sb([128, 8, OWN], BF16, "tT")
    combT = P.sb([32, OWN], F32, "combT")
    P.mark()
    load_gain("ffn_norm_g")
    w_r = P.sb([128, 8, 36], F32, "w_r")
    b_r = P.sb([128, 36], F32, "b_r")
    P.add("sp", lambda e: e.dma_start(out=w_r, in_=I["w_route"].rearrange("(k p) n -> p k n", p=128)), W=["w_r"], dma="c3")
    P.add("sp", lambda e: e.dma_start(out=b_r, in_=I["b_route"].partition_broadcast(128)), W=["b_r"], dma="c4")
    tnf = [P.sb([128, D], F32, "tnf%d" % i) for i in range(2)]
    tnb = [P.sb([128, D], BF16, "tnb%d" % i) for i in range(2)]
    tTf = P.sb([128, 8, 128], F32, "tTf")
    T_ = NTO
    lg = P.sb([128, T_, 36], F32, "lg")
    gmx = P.sb([128, T_], F32, "gmx")
    oh = P.sb([128, T_, 4], F32, "oh")
    ge = P.sb([128, T_, 4], F32, "ge")
    gsm = P.sb([128, T_], F32, "gsm")
    pg = P.sb([128, T_], F32, "pg")
    tmp48 = P.sb([128, T_, 4, 8], F32, "tmp48")
    ein = P.sb([128, T_, 8], F32, "ein")
    e2 = P.sb([128, T_, 8], F32, "e2")
    mk1 = P.sb([128, T_, 8], F32, "mk1")
    mk2 = P.sb([128, T_, 8], F32, "mk2")
    m1 = P.sb([128, T_], F32, "m1")
    m2 = P.sb([128, T_], F32, "m2")
    dd = P.sb([128, T_], F32, "dd")
    p1 = P.sb([128, T_], F32, "p1")
    p2 = P.sb([128, T_], F32, "p2")
    we = P.sb([128, T_, 8], F32, "we")
    we2 = P.sb([128, T_, 8], F32, "we2")
    comb = P.sb([128, T_, 4, 8], F32, "comb")
    seq = [0]

    def dv(fn, R, W):
        P.add("dve", fn, R=R, W=W)

    for j in range(NTO):
        b = j % 2
        rms_norm(xres[:, j, :], D, gb, tnf[b], ["xres"], ["tnf%d" % b], stk="stf")
        P.add("act", lambda e, b=b: e.copy(out=tnb[b], in_=tnf[b]), R=["tnf%d" % b], W=["tnb%d" % b])
        for k in range(8):
            P.add("pe", lambda e, k=k, b=b: e.transpose(out=psb(b)[:, k * 128:(k + 1) * 128], in_=tnb[b][:, k * 128:(k + 1) * 128], identity=identb),
                  R=["tnb%d" % b, "identb"], W=[psk[b]])
        P.add("act", lambda e, b=b, j=j: e.copy(out=tT[:, :, j * 128:(j + 1) * 128], in_=psb(b).rearrange("p (k t) -> p k t", k=8)),
              R=[psk[b]], W=["tT"])
        for k in range(8):
            pf = 2 + (k // 4)
            P.add("pe", lambda e, k=k, b=b, pf=pf: e.transpose(out=ps[pf][:, (k % 4) * 128:(k % 4 + 1) * 128], in_=tnf[b][:, k * 128:(k + 1) * 128], identity=identf),
                  R=["tnf%d" % b, "identf"], W=[psk[pf]])
        for hf in range(2):
            P.add("dve" if hf == 0 else "act", (lambda e, hf=hf: e.tensor_copy(out=tTf[:, hf * 4:(hf + 1) * 4, :], in_=ps[2 + hf][:, :].rearrange("p (k t) -> p k t", k=4)))
                  if hf == 0 else (lambda e, hf=hf: e.copy(out=tTf[:, hf * 4:(hf + 1) * 4, :], in_=ps[2 + hf][:, :].rearrange("p (k t) -> p k t", k=4))),
                  R=[psk[2 + hf]], W=["tTf%d" % hf])
        for k in range(8):
            P.add("pe", lambda e, k=k: e.matmul(ps[4][:, 0:36], lhsT=tTf[:, k, :], rhs=w_r[:, k, :], start=(k == 0), stop=(k == 7)),
                  R=["tTf0", "tTf1", "w_r"], W=[psk[4]])
        dv(lambda e, j=j: e.tensor_tensor(out=lg[:, j, :], in0=ps[4][:, 0:36], in1=b_r, op=ALU.add), [psk[4], "b_r"], ["lg"])

    def col(t, n):
        return t.rearrange("p (t o) -> p t o", o=1).broadcast_to([128, T_, n])

    gl = lg[:, :, 0:4]
    el = lg[:, :, 4:36].rearrange("p t (g e) -> p t g e", g=4)
    dv(lambda e: e.tensor_reduce(out=gmx, in_=gl, axis=AX.X, op=ALU.max), ["lg"], ["gmx"])
    dv(lambda e: e.tensor_tensor(out=oh, in0=gl, in1=col(gmx, 4), op=ALU.is_equal), ["lg", "gmx"], ["oh"])
    dv(lambda e: e.tensor_tensor(out=ge, in0=gl, in1=col(gmx, 4), op=ALU.subtract), ["lg", "gmx"], ["ge"])
    P.add("act", lambda e: e.activation(out=ge, in_=ge, func=AF.Exp), R=["ge"], W=["ge"])
    dv(lambda e: e.tensor_reduce(out=gsm, in_=ge, axis=AX.X, op=ALU.add), ["ge"], ["gsm"])
    dv(lambda e: e.reciprocal(out=pg, in_=gsm), ["gsm"], ["pg"])
    dv(lambda e: e.tensor_tensor(out=tmp48, in0=el, in1=oh.rearrange("p t (g o) -> p t g o", o=1).broadcast_to([128, T_, 4, 8]), op=ALU.mult),
       ["lg", "oh"], ["tmp48"])
    dv(lambda e: e.tensor_reduce(out=ein, in_=tmp48.rearrange("p t g e -> p t e g"), axis=AX.X, op=ALU.add), ["tmp48"], ["ein"])
    dv(lambda e: e.tensor_reduce(out=m1, in_=ein, axis=AX.X, op=ALU.max), ["ein"], ["m1"])
    dv(lambda e: e.tensor_tensor(out=mk1, in0=ein, in1=col(m1, 8), op=ALU.is_equal), ["ein", "m1"], ["mk1"])
    dv(lambda e: e.scalar_tensor_tensor(out=e2, in0=mk1, scalar=-1e30, in1=ein, op0=ALU.mult, op1=ALU.add), ["mk1", "ein"], ["e2"])
    dv(lambda e: e.tensor_reduce(out=m2, in_=e2, axis=AX.X, op=ALU.max), ["e2"], ["m2"])
    dv(lambda e: e.tensor_tensor(out=mk2, in0=e2, in1=col(m2, 8), op=ALU.is_equal), ["e2", "m2"], ["mk2"])
    dv(lambda e: e.tensor_tensor(out=dd, in0=m2, in1=m1, op=ALU.subtract), ["m1", "m2"], ["dd"])
    P.add("act", lambda e: e.activation(out=dd, in_=dd, func=AF.Exp), R=["dd"], W=["dd"])
    dv(lambda e: e.tensor_scalar_add(out=p1, in0=dd, scalar1=1.0), ["dd"], ["p1"])
    dv(lambda e: e.reciprocal(out=p1, in_=p1), ["p1"], ["p1"])
    dv(lambda e: e.tensor_tensor(out=p2, in0=dd, in1=p1, op=ALU.mult), ["dd", "p1"], ["p2"])
    dv(lambda e: e.tensor_tensor(out=p1, in0=p1, in1=pg, op=ALU.mult), ["p1", "pg"], ["p1"])
    dv(lambda e: e.tensor_tensor(out=p2, in0=p2, in1=pg, op=ALU.mult), ["p2", "pg"], ["p2"])
    dv(lambda e: e.tensor_tensor(out=we, in0=mk1, in1=col(p1, 8), op=ALU.mult), ["mk1", "p1"], ["we"])
    dv(lambda e: e.tensor_tensor(out=we2, in0=mk2, in1=col(p2, 8), op=ALU.mult), ["mk2", "p2"], ["we2"])
    dv(lambda e: e.tensor_tensor(out=we, in0=we, in1=we2, op=ALU.add), ["we", "we2"], ["we"])
    dv(lambda e: e.tensor_tensor(out=comb, in0=we.rearrange("p t (o e) -> p t o e", o=1).broadcast_to([128, T_, 4, 8]),
                                 in1=oh.rearrange("p t (g o) -> p t g o", o=1).broadcast_to([128, T_, 4, 8]), op=ALU.mult), ["we", "oh"], ["comb"])
    for j in range(NTO):
        pc_ = 5 + j % 2
        P.add("pe", lambda e, j=j, pc_=pc_: e.transpose(out=ps[pc_][0:32, 0:128], in_=comb[:, j, :, :].rearrange("p g e -> p (g e)"), identity=identf),
              R=["comb", "identf"], W=[psk[pc_]])
        dv(lambda e, j=j, pc_=pc_: e.tensor_copy(out=combT[:, j * 128:(j + 1) * 128], in_=ps[pc_][0:32, 0:128]), [psk[pc_]], ["combT"])
    P.add("sp", lambda e: e.dma_start(out=combT_d, in_=combT), R=["combT"], W=["combT_d"], dma="combT")
    if "comb" in dbg:
        tcb = dbg_out("comb", [32, OWN])
        P.add("sp", lambda e: e.dma_start(out=tcb, in_=combT), R=["combT"], W=["dbgo"], dma="dbg1")
    P.barrier()
    P.release()
    NSLOT = 4
    wg = [P.sb([128, 8, 256], BF16, "wg%d" % i) for i in range(NSLOT)]
    wu = [P.sb([128, 8, 256], BF16, "wu%d" % i) for i in range(NSLOT)]
    wd = [P.sb([128, 2, D], BF16, "wd%d" % i) for i in range(NSLOT)]
    CB = [P.sb([128, OWN], F32, "CB%d" % i) for i in range(2)]
    sa = [P.sb([128, 2, 512], F32, "sa%d" % i) for i in range(2)]
    sc = [P.sb([128, 2, 512], F32, "sc%d" % i) for i in range(2)]
    mTe = [P.sb([128, 2, 512], BF16, "mTe%d" % i) for i in range(2)]
    def load_expert(e_):
        sl = e_ % NSLOT
        P.add("pool", lambda e, e_=e_, sl=sl: e.dma_start(out=wg[sl], in_=I["w_gate"][e_].rearrange("(k p) n -> p k n", p=128)), W=["wg%d" % sl], dma="wg%d" % sl)
        P.add("pool", lambda e, e_=e_, sl=sl: e.dma_start(out=wu[sl], in_=I["w_up"][e_].rearrange("(k p) n -> p k n", p=128)), W=["wu%d" % sl], dma="wu%d" % sl)
        P.add("pool", lambda e, e_=e_, sl=sl: e.dma_start(out=wd[sl], in_=I["w_down"][e_].rearrange("(k p) n -> p k n", p=128)), W=["wd%d" % sl], dma="wd%d" % sl)

    for e_ in range(2):
        load_expert(e_)
    yb = 0
    for pr in range(16):
        for ee in range(2):
            if 2 * pr + 2 + ee < 32:
                load_expert(2 * pr + 2 + ee)
        for ee in range(2):
            e_ = 2 * pr + ee
            P.add("sp", lambda e, e_=e_, ee=ee: e.dma_start(out=CB[ee], in_=combT_d[e_:e_ + 1, :].partition_broadcast(128)),
                  R=["combT_d"], W=["CB%d" % ee], dma="CB%d" % ee)
        for c in range(4):
            for ee in range(2):
                e_ = 2 * pr + ee
                sl = e_ % NSLOT
                for f in range(2):
                    for k in range(8):
                        P.add("pe", lambda e, f=f, k=k, sl=sl, c=c: e.matmul(ps[f][:, :], lhsT=wg[sl][:, k, f * 128:(f + 1) * 128],
                                                                            rhs=tT[:, k, c * 512:(c + 1) * 512], start=(k == 0), stop=(k == 7)),
                              R=["wg%d" % sl, "tT"], W=[psk[f]])
                for f in range(2):
                    for k in range(8):
                        P.add("pe", lambda e, f=f, k=k, sl=sl, c=c: e.matmul(ps[2 + f][:, :], lhsT=wu[sl][:, k, f * 128:(f + 1) * 128],
                                                                            rhs=tT[:, k, c * 512:(c + 1) * 512], start=(k == 0), stop=(k == 7)),
                              R=["wu%d" % sl, "tT"], W=[psk[2 + f]])
                for f in range(2):
                    P.add("act", lambda e, f=f, ee=ee: e.activation(out=sa[ee][:, f, :], in_=ps[f][:, :], func=AF.Silu),
                          R=[psk[f]], W=["sa%d_%d" % (ee, f)])
                    P.add("pool", lambda e, f=f, ee=ee, c=c: e.tensor_tensor(out=sc[ee][:, f, :], in0=sa[ee][:, f, :],
                                                                              in1=CB[ee][:, c * 512:(c + 1) * 512], op=ALU.mult),
                          R=["sa%d_%d" % (ee, f), "CB%d" % ee], W=["sc%d_%d" % (ee, f)])
                    P.add("dve", lambda e, f=f, ee=ee: e.tensor_tensor(out=mTe[ee][:, f, :], in0=ps[2 + f][:, :], in1=sc[ee][:, f, :], op=ALU.mult),
                          R=[psk[2 + f], "sc%d_%d" % (ee, f)], W=["mTe%d" % ee])
            for t4 in range(4):
                j = c * 4 + t4
                for n in range(2):
                    py = 4 + (yb % 4)
                    yb += 1
                    cnt_mm = 0
                    for ee in range(2):
                        sl = (2 * pr + ee) % NSLOT
                        for f in range(2):
                            P.add("pe", lambda e, ee=ee, f=f, sl=sl, t4=t4, n=n, py=py, cnt_mm=cnt_mm: e.matmul(
                                ps[py][:, :], lhsT=mTe[ee][:, f, t4 * 128:(t4 + 1) * 128], rhs=wd[sl][:, f, n * 512:(n + 1) * 512],
                                start=(cnt_mm == 0), stop=(cnt_mm == 3)), R=["mTe%d" % ee, "wd%d" % sl], W=[psk[py]])
                            cnt_mm += 1
                    P.add("dve", lambda e, j=j, n=n, py=py: e.tensor_tensor(out=xres[:, j, n * 512:(n + 1) * 512], in0=ps[py][:, :],
                                                                             in1=xres[:, j, n * 512:(n + 1) * 512], op=ALU.add),
                          R=[psk[py], "xres"], W=["xres"])
    P.barrier()
    P.release()
    if "x3" in dbg:
        tx3 = dbg_out("x3", [OWN, D])
        P.add("sp", lambda e: e.dma_start(out=tx3.rearrange("(j p) c -> p j c", p=128), in_=xres), R=["xres"], W=["dbgo"], dma="dbg1")
        P.barrier()

    load_gain("final_norm_g")
    xo = [P.sb([128, D], F32, "xo%d" % i) for i in range(2)]
    for j in range(NTO):
        b = j % 2
        rms_norm(xres[:, j, :], D, gb, xo[b], ["xres"], ["xo%d" % b], stk="stg")
        P.add("sp", lambda e, j=j, b=b: e.dma_start(out=out_d[j * 128:(j + 1) * 128, :], in_=xo[b]),
              R=["xo%d" % b], W=["out_d%d" % b], dma="xo%d" % b)
    P.add("sp", None, R=["out_d0", "out_d1", "dbgo"])
    P.emit()
    return nc, dbg_d


def hyena_phase(nc, P, I, ps, psk, psb, U_d, hout_d, identf, identb, onesb, onesf, halfm, dbg, dbg_out, psall):
    NF = 64
    Hd = nc.dram_tensor("hy_H", [S, 2048], BF16, kind="Internal").ap()
    Bd = [nc.dram_tensor("hy_B%d" % i, [128, NF, 512], BF16, kind="Internal").ap() for i in range(2)]
    Kd = [nc.dram_tensor("hy_K%d" % i, [128, NF, 512], BF16, kind="Internal").ap() for i in range(2)]
    Btd = nc.dram_tensor("hy_Bt", [128, NF, 512], BF16, kind="Internal").ap()
    z1_d = nc.dram_tensor("hy_z1", [S, 512], F32, kind="Internal").ap()
    P.mark()
    Dt = P.sb([64, 64, 128], BF16, "Dt")
    Dinv = P.sb([128, 64, 64], BF16, "Dinv")
    F2a = P.sb([128, 128], BF16, "F2a")
    F2b = P.sb([128, 128], BF16, "F2b")
    Gm = P.sb([128, 128], BF16, "Gm")
    sgn = P.sb([128, 1], F32, "sgn")
    skipb = P.sb([128, 2, 512], F32, "skipb")
    P.add("pool", lambda e: e.dma_start(out=Dt, in_=I["hy_D0"].rearrange("a (b c) -> a b c", b=64)), W=["Dt"], dma="ht0")
    P.add("pool", lambda e: e.dma_start(out=F2a, in_=I["hy_F2a"]), W=["F2a"], dma="ht1")
    P.add("pool", lambda e: e.dma_start(out=F2b, in_=I["hy_F2b"]), W=["F2b"], dma="ht2")
    P.add("pool", lambda e: e.dma_start(out=Gm, in_=I["hy_G"]), W=["Gm"], dma="ht3")
    P.add("pool", lambda e: e.dma_start(out=Dinv, in_=I["hy_Dinv"].rearrange("a (b c) -> a b c", b=64)), W=["Dinv"], dma="ht4")
    P.add("sp", lambda e: e.dma_start(out=sgn, in_=I["hy_sgn"]), W=["sgn"], dma="c3")
    P.add("sp", lambda e: e.dma_start(out=skipb.rearrange("p a b -> p (a b)"), in_=I["hy_skip"].partition_broadcast(128)), W=["skipb"], dma="c4")

    P.mark()
    zT = P.sb([33, S], F32, "zT")
    g1T = P.sb([64, S], F32, "g1T")
    g2T = P.sb([64, S], F32, "g2T")
    hcols = P.sb([64, 4], F32, "hcols")
    w1 = P.sb([33, 64], F32, "w1")
    w2 = P.sb([64, 64], F32, "w2")
    w3 = P.sb([64, 2048], F32, "w3")
    b3r = P.sb([1, 2048], F32, "b3r")
    adec = P.sb([128, 2048], F32, "adec")
    nt01 = P.sb([128, NT], F32, "nt01")
    mpi = P.sb([128, 1], F32, "mpi")
    argt = P.sb([64, 512], F32, "argt")
    argm = P.sb([64, 512], F32, "argm")
    hfo = [P.sb([128, 2048], BF16, "hfo%d" % i) for i in range(2)]
    P.add("sp", lambda e: e.dma_start(out=zT, in_=I["hy_zT"]), W=["zT"], dma="hf0")
    P.add("sp", lambda e: e.dma_start(out=hcols, in_=I["hy_cols"]), W=["hcols"], dma="hf1")
    P.add("sp", lambda e: e.dma_start(out=w1, in_=I["hy_w1"]), W=["w1"], dma="hf2")
    P.add("sp", lambda e: e.dma_start(out=w2, in_=I["hy_w2"]), W=["w2"], dma="hf3")
    P.add("sp", lambda e: e.dma_start(out=w3, in_=I["hy_w3"]), W=["w3"], dma="hf4")
    P.add("sp", lambda e: e.dma_start(out=b3r, in_=I["hy_b3"]), W=["b3r"], dma="hf5")
    P.add("sp", lambda e: e.dma_start(out=adec, in_=I["hy_decay"].partition_broadcast(128)), W=["adec"], dma="hf6")
    P.add("sp", lambda e: e.dma_start(out=nt01, in_=I["hy_nt01"]), W=["nt01"], dma="hf7")
    P.add("pool", lambda e: e.memset(mpi, -math.pi), W=["mpi"])
    P.add("act", lambda e: e.activation(out=adec, in_=adec, func=AF.Abs), R=["adec"], W=["adec"])
    OFFS = math.pi + 16.0 * math.pi
    for (src, wt, kk, bcol, fcol, dst, nm) in ((zT, w1, 33, 0, 2, g1T, "g1T"), (g1T, w2, 64, 1, 3, g2T, "g2T")):
        for ch in range(8):
            pb_ = ch % 2
            P.add("pe", lambda e, src=src, wt=wt, kk=kk, ch=ch, pb_=pb_: e.matmul(ps[pb_][0:64, :], lhsT=wt[0:kk, :], rhs=src[0:kk, ch * 512:(ch + 1) * 512],
                                                                               start=True, stop=True), R=["zT", "g1T", "w1", "w2"], W=[psk[pb_]])
            P.add("dve", lambda e, pb_=pb_, bcol=bcol, fcol=fcol: e.tensor_scalar(out=argt, in0=ps[pb_][0:64, :], scalar1=hcols[:, bcol:bcol + 1],
                                                                                  scalar2=hcols[:, fcol:fcol + 1], op0=ALU.add, op1=ALU.mult),
                  R=[psk[pb_], "hcols"], W=["argt"])
            for _rep in range(2):
                P.add("dve", lambda e: e.tensor_scalar(out=argm, in0=argt, scalar1=math.pi, scalar2=None, op0=ALU.is_gt), R=["argt"], W=["argm"])
                P.add("dve", lambda e: e.scalar_tensor_tensor(out=argt, in0=argm, scalar=-2.0 * math.pi, in1=argt, op0=ALU.mult, op1=ALU.add),
                      R=["argm", "argt"], W=["argt"])
                P.add("dve", lambda e: e.tensor_scalar(out=argm, in0=argt, scalar1=-math.pi, scalar2=None, op0=ALU.is_lt), R=["argt"], W=["argm"])
                P.add("dve", lambda e: e.scalar_tensor_tensor(out=argt, in0=argm, scalar=2.0 * math.pi, in1=argt, op0=ALU.mult, op1=ALU.add),
                      R=["argm", "argt"], W=["argt"])
            P.add("act", lambda e, dst=dst, ch=ch: e.activation(out=dst[:, ch * 512:(ch + 1) * 512], in_=argt, func=AF.Sin),
                  R=["argt"], W=[nm])
    g2b = P.sb([64, S], BF16, "g2b")
    w3b = P.sb([64, 2048], BF16, "w3b")
    b3b = P.sb([1, 2048], BF16, "b3b")
    Etf = [P.sb([128, 2048], F32, "Etf%d" % i) for i in range(2)]
    P.add("act", lambda e: e.copy(out=g2b, in_=g2T), R=["g2T"], W=["g2b"])
    P.add("dve", lambda e: e.tensor_copy(out=w3b, in_=w3), R=["w3"], W=["w3b"])
    P.add("dve", lambda e: e.tensor_copy(out=b3b, in_=b3r), R=["b3r"], W=["b3b"])
    for i in range(NT):
        b = i % 2
        b0 = 4 * b
        for cg in range(4):
            pb_ = b0 + cg
            P.add("pe", lambda e, i=i, cg=cg, pb_=pb_: e.matmul(ps[pb_][:, :], lhsT=g2b[:, i * 128:(i + 1) * 128], rhs=w3b[:, cg * 512:(cg + 1) * 512],
                                                                 start=True, stop=False), R=["g2b", "w3b"], W=[psk[pb_]])
            P.add("pe", lambda e, cg=cg, pb_=pb_: e.matmul(ps[pb_][:, :], lhsT=onesb[0:1, 0:128], rhs=b3b[0:1, cg * 512:(cg + 1) * 512],
                                                            start=False, stop=True), R=["onesb", "b3b"], W=[psk[pb_]])
        P.add("act", lambda e, i=i, b=b: e.activation(out=Etf[b], in_=adec, func=AF.Exp, scale=nt01[:, i:i + 1]),
              R=["adec", "nt01"], W=["Etf%d" % b])
        for hf_ in range(2):
            P.add("dve", lambda e, hf_=hf_, b=b, b0=b0: e.tensor_tensor(out=hfo[b][:, hf_ * 1024:(hf_ + 1) * 1024],
                                                                      in0=psall[:, (b0 + 2 * hf_) * 512:(b0 + 2 * hf_ + 2) * 512],
                                                                      in1=Etf[b][:, hf_ * 1024:(hf_ + 1) * 1024], op=ALU.mult),
                  R=[psk[b0 + 2 * hf_], psk[b0 + 2 * hf_ + 1], "Etf%d" % b], W=["hfo%d" % b])
        if i == 0:
            for o in range(2):
                P.add("pool", lambda e, o=o: e.memset(hfo[0][0:1, o * 1024 + 512:o * 1024 + 1024], 0.0), R=[], W=["hfo0"])
        P.add("sp", lambda e, i=i, b=b: e.dma_start(out=Hd[i * 128:(i + 1) * 128, :], in_=hfo[b]), R=["hfo%d" % b], W=["Hd_%d" % i], dma="hfo%d" % b)
    P.barrier()
    P.release()
    if "hf" in dbg:
        thf = dbg_out("hf", [S, 2048])
        P.mark()
        tbf = P.sb([128, 2048], BF16, "tbf")
        tbf32 = P.sb([128, 2048], F32, "tbf32")
        for i in range(NT):
            P.add("sp", lambda e, i=i: e.dma_start(out=tbf, in_=Hd[i * 128:(i + 1) * 128, :]), R=["Hd_%d" % i], W=["tbf"], dma="dbg0")
            P.add("dve", lambda e: e.tensor_copy(out=tbf32, in_=tbf), R=["tbf"], W=["tbf32"])
            P.add("sp", lambda e, i=i: e.dma_start(out=thf[i * 128:(i + 1) * 128, :], in_=tbf32), R=["tbf32"], W=["dbgo"], dma="dbg1")
        P.barrier()
        P.release()

    ev = [0]
    HDK = ["Hd_%d" % i for i in range(NT)]
    Z1K = ["z1_d_%d" % i for i in range(8)]
    BD0K = ["Bd0_%d" % i for i in range(4)]
    BD1K = ["Bd1_%d" % i for i in range(4)]
    BTDK = ["Btd_%d" % i for i in range(8)]

    def evac(out, in_, Rk, Wk):
        ev[0] += 1
        if ev[0] % 2:
            P.add("act", lambda e: e.copy(out=out, in_=in_), R=Rk, W=Wk)
        else:
            P.add("dve", lambda e: e.tensor_copy(out=out, in_=in_), R=Rk, W=Wk)

    def stage1(src_view, cast, Bdst, bkey, srckeys):
        P.barrier()
        P.mark()
        xsb = [P.sb([64, 16, 512], BF16, "xsb%d" % i) for i in range(2)]
        Bsb = [P.sb([128, 16, 512], BF16, "Bsb%d" % i) for i in range(2)]
        def s1_load(ch):
            xb_ = ch % 2
            q = "pool" if cast else "sp"
            P.add(q, lambda e, ch=ch, xb_=xb_: e.dma_start(out=xsb[xb_], in_=src_view[:, ch * 16:(ch + 1) * 16, :]),
                  R=srckeys, W=["xsb%d" % xb_], dma="xsb%d" % xb_)

        s1_load(0)
        for ch in range(4):
            xb_ = ch % 2
            if ch + 1 < 4:
                s1_load(ch + 1)
            for g4 in range(4):
                b0 = 4 * (g4 % 2)
                for q4 in range(4):
                    s2l = g4 * 4 + q4
                    s2 = ch * 16 + s2l
                    P.add("pe", lambda e, s2=s2, s2l=s2l, xb_=xb_, pb_=b0 + q4: e.matmul(ps[pb_][:, :], lhsT=Dt[0:64, s2, :], rhs=xsb[xb_][0:64, s2l, :],
                                                                                        start=True, stop=True), R=["Dt", "xsb%d" % xb_], W=[psk[b0 + q4]])
                evac(Bsb[xb_][:, g4 * 4:(g4 + 1) * 4, :].rearrange("p a b -> p (a b)"), psall[:, b0 * 512:(b0 + 4) * 512],
                     [psk[b0 + k] for k in range(4)], ["Bsb%d_%d" % (xb_, g4)])
            P.add("sp", lambda e, ch=ch, xb_=xb_: e.dma_start(out=Bdst[:, ch * 16:(ch + 1) * 16, :], in_=Bsb[xb_]),
                  R=["Bsb%d_%d" % (xb_, k) for k in range(4)], W=["%s_%d" % (bkey, ch)], dma="Bsb%d" % xb_)
        P.barrier()
        P.release()

    def blocked(ap2d):
        return ap2d.rearrange("(s1 s2) c -> s1 s2 c", s2=64)

    for o in range(2):
        stage1(blocked(Hd[:, o * 1024:o * 1024 + 512]), False, Bd[0], "Bd0", HDK)
        stage1(blocked(Hd[:, o * 1024 + 512:o * 1024 + 1024]), False, Bd[1], "Bd1", HDK)
        P.mark()
        BTf = [P.sb([128, 8, 512], BF16, "BTf%d" % i) for i in range(2)]
        BTb = [P.sb([128, 8, 512], BF16, "BTb%d" % i) for i in range(2)]
        xfs = [P.sb([128, 1024], F32, "xfs%d" % i) for i in range(2)]
        kst = [P.sb([128, 1024], F32, "kst%d" % i) for i in range(2)]
        Kc = [P.sb([128, 8, 512], BF16, "Kc%d" % i) for i in range(2)]
        def f2_load(fc):
            cb_ = fc % 2
            for r in range(2):
                P.add("sp", lambda e, r=r, fc=fc, cb_=cb_: e.dma_start(
                    out=BTf[cb_][r * 64:(r + 1) * 64, :, :], in_=Bd[0][r * 64 + fc * 8:r * 64 + fc * 8 + 8, :, :].rearrange("f s c -> s f c")),
                    R=BD0K, W=["BTf%d" % cb_], dma="BTf%d_%d" % (cb_, r))
                P.add("sp", lambda e, r=r, fc=fc, cb_=cb_: e.dma_start(
                    out=BTb[cb_][r * 64:(r + 1) * 64, :, :], in_=Bd[1][r * 64 + fc * 8:r * 64 + fc * 8 + 8, :, :].rearrange("f s c -> s f c")),
                    R=BD1K, W=["BTb%d" % cb_], dma="BTb%d_%d" % (cb_, r))

        f2_load(0)
        for fc in range(8):
            cb_ = fc % 2
            if fc + 1 < 8:
                f2_load(fc + 1)
            for gq in range(4):
                t_ = gq % 2
                fa, fb = 2 * t_, 4 + 2 * t_
                for q2 in range(2):
                    f1l = gq * 2 + q2
                    P.add("pe", lambda e, f1l=f1l, cb_=cb_, pb_=fa + q2: e.matmul(ps[pb_][:, :], lhsT=F2a, rhs=BTf[cb_][:, f1l, :], start=True, stop=True),
                          R=["F2a", "BTf%d" % cb_], W=[psk[fa + q2]])
                    P.add("pe", lambda e, f1l=f1l, cb_=cb_, pb_=fb + q2: e.matmul(ps[pb_][:, :], lhsT=F2a, rhs=BTb[cb_][:, f1l, :], start=True, stop=True),
                          R=["F2a", "BTb%d" % cb_], W=[psk[fb + q2]])
                P.add("act", lambda e, fa=fa, t_=t_: e.copy(out=xfs[t_], in_=psall[:, fa * 512:(fa + 2) * 512]), R=[psk[fa], psk[fa + 1]], W=["xfs%d" % t_])
                P.add("dve", lambda e, fb=fb, t_=t_: e.scalar_tensor_tensor(out=kst[t_], in0=psall[:, fb * 512:(fb + 2) * 512], scalar=sgn[:, 0:1], in1=xfs[t_],
                                                                           op0=ALU.mult, op1=ALU.add),
                      R=[psk[fb], psk[fb + 1], "xfs%d" % t_, "sgn"], W=["kst%d" % t_])
                for q2 in range(2):
                    f1l = gq * 2 + q2
                    P.add("pool", lambda e, t_=t_, cb_=cb_, f1l=f1l, q2=q2, o=o: e.tensor_tensor(out=Kc[cb_][0:64, f1l, :], in0=kst[t_][0:64, q2 * 512:(q2 + 1) * 512],
                                                                                          in1=skipb[0:64, o, :], op=ALU.add),
                          R=["kst%d" % t_, "skipb"], W=["Kc%d_a%d" % (cb_, f1l)])
                P.add("act", lambda e, t_=t_, cb_=cb_, gq=gq: e.copy(out=Kc[cb_][64:128, gq * 2:gq * 2 + 2, :].rearrange("p a b -> p (a b)"), in_=kst[t_][64:128, :]),
                      R=["kst%d" % t_], W=["Kc%d_b%d" % (cb_, gq)])
            P.add("sp", lambda e, fc=fc, cb_=cb_, o=o: e.dma_start(out=Kd[o][:, fc * 8:(fc + 1) * 8, :], in_=Kc[cb_]),
                  R=["Kc%d_a%d" % (cb_, k) for k in range(8)] + ["Kc%d_b%d" % (cb_, k) for k in range(4)], W=["Kd%d_%d" % (o, fc)], dma="Kc%d" % cb_)
        P.barrier()
        P.release()
    if "kf" in dbg:
        tkf = dbg_out("kf", [2, 128, NF * 512])
        P.mark()
        tk16 = P.sb([128, 8, 512], BF16, "tk16")
        tk32 = P.sb([128, 8, 512], F32, "tk32")
        for o in range(2):
            for fc in range(8):
                P.add("sp", lambda e, o=o, fc=fc: e.dma_start(out=tk16, in_=Kd[o][:, fc * 8:(fc + 1) * 8, :]), R=["Kd%d_%d" % (o, fc)], W=["tk16"], dma="dbg0")
                P.add("dve", lambda e: e.tensor_copy(out=tk32, in_=tk16), R=["tk16"], W=["tk32"])
                P.add("sp", lambda e, o=o, fc=fc: e.dma_start(out=tkf[o][:, fc * 4096:(fc + 1) * 4096], in_=tk32.rearrange("p a b -> p (a b)")),
                      R=["tk32"], W=["dbgo"], dma="dbg1")
        P.barrier()
        P.release()

    P.add("pool", lambda e: e.dma_start(out=Dt, in_=I["hy_Dc"].rearrange("a (b c) -> a b c", b=64)), W=["Dt"], dma="ht0")
    for o in range(2):
        if o == 0:
            stage1(blocked(U_d[:, 1024:1536]), True, Bd[0], "Bd0", ["U_d"])
        else:
            stage1(blocked(z1_d), True, Bd[0], "Bd0", Z1K)
        P.mark()
        BT = [P.sb([128, 8, 512], BF16, "BT%d" % i) for i in range(2)]
        KA = [P.sb([128, 8, 512], BF16, "KA%d" % i) for i in range(2)]
        KB = [P.sb([128, 8, 512], BF16, "KB%d" % i) for i in range(2)]
        ta = [P.sb([128, 1024], F32, "ta%d" % i) for i in range(2)]
        tb2 = [P.sb([128, 1024], F32, "tb2%d" % i) for i in range(2)]
        Yc = [P.sb([128, 1024], BF16, "Yc%d" % i) for i in range(2)]
        Btsb = [P.sb([128, 8, 512], BF16, "Btsb%d" % i) for i in range(2)]
        def c2_load(fc):
            cb_ = fc % 2
            for r in range(2):
                P.add("sp", lambda e, r=r, fc=fc, cb_=cb_: e.dma_start(
                    out=BT[cb_][r * 64:(r + 1) * 64, :, :], in_=Bd[0][r * 64 + fc * 8:r * 64 + fc * 8 + 8, :, :].rearrange("f s c -> s f c")),
                    R=BD0K, W=["BT%d" % cb_], dma="BT%d_%d" % (cb_, r))
                P.add("sp", lambda e, r=r, fc=fc, cb_=cb_, o=o: e.dma_start(out=KA[cb_][r * 64:(r + 1) * 64, :, :], in_=Kd[o][0:64, fc * 8:(fc + 1) * 8, :]),
                      R=["Kd%d_%d" % (o, fc)], W=["KA%d" % cb_], dma="KA%d_%d" % (cb_, r))
                P.add("sp", lambda e, r=r, fc=fc, cb_=cb_, o=o: e.dma_start(out=KB[cb_][r * 64:(r + 1) * 64, :, :], in_=Kd[o][64:128, fc * 8:(fc + 1) * 8, :]),
                      R=["Kd%d_%d" % (o, fc)], W=["KB%d" % cb_], dma="KB%d_%d" % (cb_, r))

        c2_load(0)
        for fc in range(8):
            cb_ = fc % 2
            if fc + 1 < 8:
                c2_load(fc + 1)
            for gq in range(4):
                t_ = gq % 2
                fa, fb = 2 * t_, 4 + 2 * t_
                f0 = gq * 2
                for q2 in range(2):
                    f1l = f0 + q2
                    P.add("pe", lambda e, f1l=f1l, cb_=cb_, pb_=fa + q2: e.matmul(ps[pb_][:, :], lhsT=F2a, rhs=BT[cb_][:, f1l, :], start=True, stop=True),
                          R=["F2a", "BT%d" % cb_], W=[psk[fa + q2]])
                    P.add("pe", lambda e, f1l=f1l, cb_=cb_, pb_=fb + q2: e.matmul(ps[pb_][:, :], lhsT=F2b, rhs=BT[cb_][:, f1l, :], start=True, stop=True),
                          R=["F2b", "BT%d" % cb_], W=[psk[fb + q2]])
                P.add("dve", lambda e, fa=fa, t_=t_, cb_=cb_, f0=f0: e.tensor_tensor(out=ta[t_], in0=psall[:, fa * 512:(fa + 2) * 512],
                                                                                   in1=KA[cb_][:, f0:f0 + 2, :].rearrange("p a b -> p (a b)"), op=ALU.mult),
                      R=[psk[fa], psk[fa + 1], "KA%d" % cb_], W=["ta%d" % t_])
                P.add("dve", lambda e, fb=fb, t_=t_, cb_=cb_, f0=f0: e.tensor_tensor(out=tb2[t_], in0=psall[:, fb * 512:(fb + 2) * 512],
                                                                                   in1=KB[cb_][:, f0:f0 + 2, :].rearrange("p a b -> p (a b)"), op=ALU.mult),
                      R=[psk[fb], psk[fb + 1], "KB%d" % cb_], W=["tb2%d" % t_])
                P.add("pool", lambda e, t_=t_: e.tensor_tensor(out=Yc[t_], in0=ta[t_], in1=tb2[t_], op=ALU.add),
                      R=["ta%d" % t_, "tb2%d" % t_], W=["Yc%d" % t_])
                for q2 in range(2):
                    P.add("pe", lambda e, t_=t_, q2=q2, pb_=fa + q2: e.matmul(ps[pb_][:, :], lhsT=Gm, rhs=Yc[t_][:, q2 * 512:(q2 + 1) * 512], start=True, stop=True),
                          R=["Gm", "Yc%d" % t_], W=[psk[fa + q2]])
                P.add("act", lambda e, fa=fa, cb_=cb_, f0=f0: e.copy(out=Btsb[cb_][:, f0:f0 + 2, :].rearrange("p a b -> p (a b)"), in_=psall[:, fa * 512:(fa + 2) * 512]),
                      R=[psk[fa], psk[fa + 1]], W=["Btsb%d_%d" % (cb_, gq)])
            P.add("sp", lambda e, fc=fc, cb_=cb_: e.dma_start(out=Btd[:, fc * 8:(fc + 1) * 8, :], in_=Btsb[cb_]),
                  R=["Btsb%d_%d" % (cb_, k) for k in range(4)], W=["Btd_%d" % fc], dma="Btsb%d" % cb_)
        P.barrier()
        P.release()
        P.mark()
        BtT = [P.sb([128, 8, 512], BF16, "BtT%d" % i) for i in range(2)]
        gch = [P.sb([64, 8, 512], F32, "gch%d" % i) for i in range(2)]
        zo = [P.sb([64, 8, 512], F32, "zo%d" % i) for i in range(2)]
        M = 64 if o == 0 else 32
        gcol = 0 if o == 0 else 512
        dst = blocked(z1_d) if o == 0 else blocked(hout_d)
        dkey = "z1_d" if o == 0 else "hout_d"
        def i1_load(tc):
            cb_ = tc % 2
            for r in range(2):
                P.add("sp", lambda e, r=r, tc=tc, cb_=cb_: e.dma_start(
                    out=BtT[cb_][r * 64:(r + 1) * 64, :, :], in_=Btd[r * 64 + tc * 8:r * 64 + tc * 8 + 8, :, :].rearrange("t f c -> f t c")),
                    R=BTDK, W=["BtT%d" % cb_], dma="BtT%d_%d" % (cb_, r))
            P.add("sp", lambda e, tc=tc, cb_=cb_, M=M, gcol=gcol: e.dma_start(
                out=gch[cb_][0:M, :, :], in_=blocked(U_d[0:M * 64, gcol:gcol + 512])[:, tc * 8:(tc + 1) * 8, :]),
                R=["U_d"], W=["gch%d" % cb_], dma="gch%d" % cb_)

        i1_load(0)
        for tc in range(8):
            cb_ = tc % 2
            if tc + 1 < 8:
                i1_load(tc + 1)
            for g4 in range(2):
                b0 = 4 * ((2 * tc + g4) % 2)
                for q4 in range(4):
                    t2l = g4 * 4 + q4
                    t2 = tc * 8 + t2l
                    P.add("pe", lambda e, t2=t2, t2l=t2l, cb_=cb_, pb_=b0 + q4, M=M: e.matmul(ps[pb_][0:M, :], lhsT=Dinv[:, t2, 0:M], rhs=BtT[cb_][:, t2l, :],
                                                                                             start=True, stop=True), R=["Dinv", "BtT%d" % cb_], W=[psk[b0 + q4]])
                P.add("dve", lambda e, g4=g4, cb_=cb_, b0=b0, M=M: e.tensor_tensor(out=zo[cb_][0:M, g4 * 4:(g4 + 1) * 4, :].rearrange("p a b -> p (a b)"),
                                                                                 in0=psall[0:M, b0 * 512:(b0 + 4) * 512],
                                                                                 in1=gch[cb_][0:M, g4 * 4:(g4 + 1) * 4, :].rearrange("p a b -> p (a b)"), op=ALU.mult),
                      R=[psk[b0 + k] for k in range(4)] + ["gch%d" % cb_], W=["zo%d_%d" % (cb_, g4)])
            P.add("sp", lambda e, tc=tc, cb_=cb_, M=M, dst=dst: e.dma_start(out=dst[0:M, tc * 8:(tc + 1) * 8, :], in_=zo[cb_][0:M, :, :]),
                  R=["zo%d_%d" % (cb_, k) for k in range(2)], W=["%s_%d" % (dkey, tc)], dma="zo%d" % cb_)
        P.barrier()
        P.release()
    P.barrier()
    P.release()
    if "z1" in dbg:
        tz1 = dbg_out("z1", [S, 512])
        P.mark()
        tz = P.sb([128, 512], F32, "tz")
        for i in range(NT):
            P.add("sp", lambda e, i=i: e.dma_start(out=tz, in_=z1_d[i * 128:(i + 1) * 128, :]), R=Z1K, W=["tz"], dma="dbg0")
            P.add("sp", lambda e, i=i: e.dma_start(out=tz1[i * 128:(i + 1) * 128, :], in_=tz), R=["tz"], W=["dbgo"], dma="dbg1")
        P.barrier()
        P.release()
    if "h_out" in dbg:
        tho = dbg_out("h_out", [OWN, 512])
        P.mark()
        tz_ = P.sb([128, 512], F32, "tz_")
        for i in range(NTO):
            P.add("sp", lambda e, i=i: e.dma_start(out=tz_, in_=hout_d[i * 128:(i + 1) * 128, :]), R=["hout_d_%d" % k for k in range(8)], W=["tz_"], dma="dbg0")
            P.add("sp", lambda e, i=i: e.dma_start(out=tho[i * 128:(i + 1) * 128, :], in_=tz_), R=["tz_"], W=["dbgo"], dma="dbg1")
        P.barrier()
        P.release()


def hyena_tables(half):
    N = 8192
    f1 = np.arange(64, dtype=np.float64)
    s1 = np.arange(64, dtype=np.float64)
    s2 = np.arange(64, dtype=np.float64)
    th = 2 * np.pi * (f1[None, None, :] + 0.5) * (64 * s1[:, None, None] + s2[None, :, None]) / N
    D0 = np.concatenate([np.cos(th), -np.sin(th)], axis=2)
    perm = (np.arange(64) + 32 * half) % 64
    Dc = D0[perm]
    f2 = np.arange(64, dtype=np.float64)
    ph = 2 * np.pi * np.outer(s2, f2) / 64
    c, s_ = np.cos(ph), np.sin(ph)
    F2a = np.block([[c, -s_], [s_, c]])
    F2b = np.block([[s_, c], [-c, s_]])
    G = np.block([[c, s_], [-s_, c]])
    thi = 2 * np.pi * (f1[:, None, None] + 0.5) * (64 * s1[None, None, :] + s2[None, :, None]) / N
    Dinv0 = np.concatenate([np.cos(thi), -np.sin(thi)], axis=0) * (2.0 / N)
    Dinv = Dinv0[:, :, perm]
    sgn = np.ones((128, 1)); sgn[64:] = -1
    L = S
    t = np.arange(L, dtype=np.float32)
    t01 = t / np.float32(L)
    bands = np.linspace(1e-4, 15, 16, dtype=np.float32)
    ang = (np.float32(2.0 * math.pi) * t[:, None] * bands[None, :] / np.float32(L)).astype(np.float32)
    z = np.concatenate([t01[:, None], np.cos(ang), -np.sin(ang)], axis=-1).astype(np.float32)
    nt01 = (-t01).reshape(NT, 128).T
    f = np.float32
    return {
        "hy_D0": np.ascontiguousarray(D0.reshape(64, 64 * 128).astype(f)), "hy_Dc": np.ascontiguousarray(Dc.reshape(64, 64 * 128).astype(f)),
        "hy_F2a": np.ascontiguousarray(F2a.astype(f)), "hy_F2b": np.ascontiguousarray(F2b.astype(f)), "hy_G": np.ascontiguousarray(G.astype(f)),
        "hy_Dinv": np.ascontiguousarray(Dinv.reshape(128, 64 * 64).astype(f)), "hy_sgn": sgn.astype(f),
        "hy_zT": np.ascontiguousarray(z.T), "hy_nt01": np.ascontiguousarray(nt01.astype(f)),
    }


def host_inputs(inputs, core):
    b, half = divmod(core, 2)
    f32 = np.float32
    x = np.asarray(inputs["x"], dtype=f32)[b]
    own = slice(half * OWN, (half + 1) * OWN)
    oth = slice((1 - half) * OWN, (2 - half) * OWN)
    pos = np.concatenate([np.arange(S)[own], np.arange(S)[oth]])
    m = {}
    m["x_rot"] = np.ascontiguousarray(np.concatenate([x[own], x[oth]], axis=0))
    m["mem_b"] = np.ascontiguousarray(np.asarray(inputs["mem"], dtype=f32)[b])
    for k in ("mix_norm_g", "q_norm_g", "kv_norm_g", "hy_conv_b", "attn_out_g", "hy_out_g", "cross_norm_g",
              "mem_norm_g", "ffn_norm_g"):
        m[k] = np.ascontiguousarray(np.asarray(inputs[k], dtype=f32).reshape(1, -1))
    m["final_norm_g"] = np.ascontiguousarray(np.asarray(inputs["final_norm_g"], dtype=f32).reshape(1, -1))
    for k in ("w_in", "w_uq", "w_ukv", "hy_conv_w", "w_out", "w_mq", "w_mkv", "w_mo"):
        m[k] = np.ascontiguousarray(np.asarray(inputs[k], dtype=f32)[0])
    m["w_route"] = np.ascontiguousarray(np.concatenate([np.asarray(inputs["w_route_group"], f32)[0],
                                                        np.asarray(inputs["w_route_expert"], f32)[0]], axis=1))
    m["b_route"] = np.ascontiguousarray(np.concatenate([np.asarray(inputs["b_route_group"], f32)[0],
                                                        np.asarray(inputs["b_route_expert"], f32)[0]], axis=0).reshape(1, 36))
    m["w_gate"] = np.ascontiguousarray(np.asarray(inputs["w_gate"], f32)[0].reshape(32, D, 256))
    m["w_up"] = np.ascontiguousarray(np.asarray(inputs["w_up"], f32)[0].reshape(32, D, 256))
    m["w_down"] = np.ascontiguousarray(np.asarray(inputs["w_down"], f32)[0].reshape(32, 256, D))
    m["ident"] = np.eye(128, dtype=f32)
    inv = (10000.0 ** (-np.arange(16, dtype=np.float64) / 16)).astype(f32)
    ang = pos.astype(f32)[:, None] * inv[None, :]
    cs = np.concatenate([np.cos(ang), np.sin(ang)], axis=1).astype(f32)
    m["rope_cs"] = np.ascontiguousarray(cs.reshape(NT, 128, 32).transpose(1, 0, 2).reshape(128, NT * 32))
    m.update(hyena_tables(half))
    m["hy_cols"] = np.ascontiguousarray(np.stack([np.asarray(inputs["hy_b1"], f32)[0], np.asarray(inputs["hy_b2"], f32)[0],
                                                  np.asarray(inputs["hy_freq"], f32)[0, 0], np.asarray(inputs["hy_freq"], f32)[0, 1]], axis=1))
    for k in ("hy_w1", "hy_w2", "hy_w3"):
        m[k] = np.ascontiguousarray(np.asarray(inputs[k], f32)[0])
    m["hy_b3"] = np.ascontiguousarray(np.asarray(inputs["hy_b3"], f32).reshape(1, 2048))
    m["hy_decay"] = np.ascontiguousarray(np.asarray(inputs["hy_decay"], f32).reshape(1, 2048))
    m["hy_skip"] = np.ascontiguousarray(np.asarray(inputs["hy_skip"], f32).reshape(1, 1024))
    hm = np.zeros((128, 2), f32)
    hm[:, 0] = half
    hm[:, 1] = 1 - half
    m["halfmask"] = hm
    return m


def kernel(**inputs):
    n = 8
    nc, _ = build()
    in_maps = [host_inputs(inputs, c) for c in range(n)]
    res = run_bass_kernel_spmd(nc, in_maps, core_ids=list(range(n)))
    out = np.zeros((4, S, D), np.float32)
    for c in range(n):
        b, half = divmod(c, 2)
        out[b, half * OWN:(half + 1) * OWN] = res.results[c]["out"]
    return out
```

```python
import math
import os
import contextlib
import numpy as np
import concourse.bass as bass
import concourse.mybir as mybir
from concourse.bass_utils import run_bass_kernel_spmd

F32 = mybir.dt.float32
BF16 = mybir.dt.bfloat16
AF = mybir.ActivationFunctionType
ALU = mybir.AluOpType
AX = mybir.AxisListType
ENGS = ("pe", "act", "dve", "pool", "sp")

D = 1024
S = 4096
OWN = 2048
NT = 32
NTO = 16
EPS = 1e-6
HD = 96
NH = 8


class Op:
    __slots__ = ("eng", "fn", "deps", "dma", "flag", "seq", "idx", "dmaval")

    def __init__(self, eng, fn, dma):
        self.eng = eng
        self.fn = fn
        self.deps = set()
        self.dma = dma
        self.flag = False
        self.seq = 0
        self.dmaval = 0


class Prog:
    ARENA_WORDS = 52000

    def __init__(self, nc):
        self.nc = nc
        self.ops = []
        self.lastw = {}
        self.readers = {}
        self.dma_count = {}
        self.sb_off = 0
        self.sb_marks = []
        self.arena = None
        self.dma_slots = {}

    def sb(self, shape, dtype, name=None):
        if self.arena is None:
            self.arena = self.nc.alloc_sbuf_tensor("arena", [128, self.ARENA_WORDS], F32)
        esz = 4 if dtype == F32 else 2
        nel = int(np.prod(shape[1:]))
        nwords = (nel * esz + 3) // 4
        nwords = (nwords + 15) // 16 * 16
        o = self.sb_off
        self.sb_off += nwords
        assert self.sb_off <= self.ARENA_WORDS, ("SBUF overflow", self.sb_off * 4, name)
        v = self.arena[0:shape[0], o:o + nwords]
        if esz == 2:
            v = v.bitcast(dtype)[:, 0:nel]
        else:
            v = v[:, 0:nel]
        if len(shape) > 2:
            names = " ".join("a%d" % i for i in range(len(shape) - 1))
            kw = {"a%d" % i: int(shape[i + 1]) for i in range(len(shape) - 1)}
            v = v.rearrange("p (%s) -> p %s" % (names, names), **kw)
        return v

    def mark(self):
        self.sb_marks.append(self.sb_off)

    def release(self):
        self.sb_off = self.sb_marks.pop()

    def add(self, eng, fn, R=(), W=(), dma=None):
        if dma is not None:
            slots = self.dma_slots.setdefault(eng, {"free": [], "n": 0, "map": {}})
            if dma not in slots["map"]:
                if slots["free"]:
                    slots["map"][dma] = slots["free"].pop()
                else:
                    slots["map"][dma] = slots["n"]
                    slots["n"] += 1
            dma = (eng, slots["map"][dma])
        op = Op(eng, fn, dma)
        op.idx = len(self.ops)
        if eng != "pe":
            psr = [r for r in R if isinstance(r, str) and r.startswith("ps") and r[2:].isdigit()]
            if psr:
                R = [r for r in R if r not in psr]
                W = list(W) + psr
        deps = set()
        for r in R:
            lw = self.lastw.get(r)
            if lw is not None:
                deps.add(lw)
        for w in W:
            lw = self.lastw.get(w)
            if lw is not None:
                deps.add(lw)
            for rd in self.readers.get(w, ()):
                deps.add(rd)
        if dma is not None:
            k = ("__dmasem", dma)
            lw = self.lastw.get(k)
            if lw is not None:
                deps.add(lw)
            self.lastw[k] = op
            self.dma_count[dma] = self.dma_count.get(dma, 0) + 1
            op.dmaval = 16 * self.dma_count[dma]
        deps.discard(op)
        for d in deps:
            if d.dma is None and d.eng == "pe" and eng == "pe" and dma is None:
                continue
            op.deps.add(d)
            d.flag = True
        for r in R:
            self.readers.setdefault(r, []).append(op)
        for w in W:
            self.lastw[w] = op
            self.readers[w] = []
        self.ops.append(op)
        return op

    def barrier(self):
        fr = {}
        dmas = set()
        allops = set(self.lastw.values())
        for v in self.readers.values():
            allops.update(v)
        for o in allops:
            if o.dma is not None:
                dmas.add(o)
            elif o.eng not in fr or fr[o.eng].idx < o.idx:
                fr[o.eng] = o
        for e in ENGS:
            op = Op(e, None, None)
            op.idx = len(self.ops)
            for d in list(fr.values()) + list(dmas):
                op.deps.add(d)
                d.flag = True
            self.ops.append(op)
        self.lastw = {}
        self.readers = {}
        for sl in self.dma_slots.values():
            sl["free"].extend(sl["map"].values())
            sl["map"].clear()

    def emit(self):
        nc = self.nc
        with contextlib.ExitStack() as st:
            esem = {e: st.enter_context(nc.semaphore("s_" + e)) for e in ENGS}
            dsem = {}
            for k in self.dma_count:
                dsem[k] = st.enter_context(nc.semaphore("d_%d" % len(dsem)))
            cnt = {e: 0 for e in ENGS}
            for op in self.ops:
                if op.dma is None and op.flag:
                    cnt[op.eng] += 1
                    op.seq = cnt[op.eng]
            byeng = {e: [o for o in self.ops if o.eng == e] for e in ENGS}
            if os.environ.get("KDEBUG"):
                print("sem counts", cnt, "ndma sems", len(dsem), "nops", {e: len(v) for e, v in byeng.items()})
            block = st.enter_context(nc.Block())

            def run(e, eng):
                waited = {}
                for op in byeng[e]:
                    need = {}
                    for d in op.deps:
                        if d.dma is not None:
                            s, v = dsem[d.dma], d.dmaval
                        else:
                            s, v = esem[d.eng], d.seq
                        key = id(s)
                        if waited.get(key, 0) >= v:
                            continue
                        if key not in need or need[key][1] < v:
                            need[key] = (s, v)
                    for key, (s, v) in need.items():
                        eng.wait_ge(s, v)
                        waited[key] = v
                    if op.fn is None:
                        continue
                    ins = op.fn(eng)
                    if op.dma is not None:
                        ins.then_inc(dsem[op.dma], 16)
                    elif op.flag:
                        ins.then_inc(esem[e], 1)

            @block.tensor
            def _(eng):
                run("pe", eng)

            @block.scalar
            def _(eng):
                run("act", eng)

            @block.vector
            def _(eng):
                run("dve", eng)

            @block.gpsimd
            def _(eng):
                run("pool", eng)

            @block.sync
            def _(eng):
                run("sp", eng)


INPUT_SHAPES = {
    "x_rot": [S, D], "mem_b": [256, D],
    "mix_norm_g": [1, D], "w_in": [D, 1952], "q_norm_g": [1, 256], "kv_norm_g": [1, 128],
    "w_uq": [256, 768], "w_ukv": [128, 1024], "hy_conv_w": [3, 1536], "hy_conv_b": [1, 1536],
    "attn_out_g": [1, 512], "hy_out_g": [1, 512], "w_out": [D, D],
    "cross_norm_g": [1, D], "mem_norm_g": [1, D], "w_mq": [D, D], "w_mkv": [D, 2 * D], "w_mo": [D, D],
    "ffn_norm_g": [1, D], "w_route": [D, 36], "b_route": [1, 36],
    "w_gate": [32, D, 256], "w_up": [32, D, 256], "w_down": [32, 256, D], "final_norm_g": [1, D],
    "ident": [128, 128], "rope_cs": [128, NT * 32], "halfmask": [128, 2],
    "hy_D0": [64, 64 * 128], "hy_Dc": [64, 64 * 128], "hy_F2a": [128, 128], "hy_F2b": [128, 128], "hy_G": [128, 128],
    "hy_Dinv": [128, 64 * 64], "hy_sgn": [128, 1], "hy_zT": [33, S], "hy_nt01": [128, NT],
    "hy_cols": [64, 4], "hy_w1": [33, 64], "hy_w2": [64, 64], "hy_w3": [64, 2048], "hy_b3": [1, 2048], "hy_decay": [1, 2048],
    "hy_skip": [1, 1024], "hy_conv_wT": [128, 36], "hy_conv_bT": [128, 12],
}


def build(stop=None, dbg=()):
    nc = bass.Bass("TRN2", target_bir_lowering=False)
    I = {k: nc.dram_tensor(k, v, F32, kind="ExternalInput").ap() for k, v in INPUT_SHAPES.items()}
    out_d = nc.dram_tensor("out", [OWN, D], F32, kind="ExternalOutput").ap()
    dbg_d = {}
    U_d = nc.dram_tensor("U_scr", [S, 1536], F32, kind="Internal").ap()
    hout_d = nc.dram_tensor("hout_scr", [OWN, 512], F32, kind="Internal").ap()
    combT_d = nc.dram_tensor("combT_scr", [32, OWN], F32, kind="Internal").ap()

    P = Prog(nc)
    psall = nc.alloc_psum_tensor("psall", [128, 4096], F32)
    ps = [psall[:, i * 512:(i + 1) * 512] for i in range(8)]
    psk = ["ps%d" % i for i in range(8)]

    def psb(i):
        return ps[i].bitcast(BF16)

    def dbg_out(name, shape):
        t = nc.dram_tensor("dbg_" + name, shape, F32, kind="ExternalOutput").ap()
        dbg_d[name] = t
        return t

    cnt = [0]

    def uid(s):
        cnt[0] += 1
        return "%s_%d" % (s, cnt[0])

    identf = P.sb([128, 128], F32, "identf")
    identb = P.sb([128, 128], BF16, "identb")
    halfm = P.sb([128, 2], F32, "halfm")
    st = P.sb([128, 8], F32, "st")
    junk = P.sb([128, 1024], F32, "junk")
    gb = P.sb([128, 1024], F32, "gb")
    onesb = P.sb([128, 128], BF16, "onesb")
    onesf = P.sb([128, 128], F32, "onesf")
    P.add("sp", lambda e: e.dma_start(out=identf, in_=I["ident"]), W=["identf"], dma="c0")
    P.add("sp", lambda e: e.dma_start(out=halfm, in_=I["halfmask"]), W=["halfm"], dma="c1")
    P.add("dve", lambda e: e.tensor_copy(out=identb, in_=identf), R=["identf"], W=["identb"])
    epst = P.sb([128, 1], F32, "epst")
    P.add("pool", lambda e: e.memset(epst, EPS), W=["epst"])
    P.add("pool", lambda e: e.memset(onesb, 1.0), W=["onesb"])
    P.add("pool", lambda e: e.memset(onesf, 1.0), W=["onesf"])

    def load_gain(name, n=D, key="gb"):
        P.add("sp", lambda e: e.dma_start(out=gb[:, 0:n], in_=I[name].partition_broadcast(128)), W=[key], dma="gain")

    st_tiles = {}
    for _n in ("st", "stq", "stk", "sta", "sth", "stm", "stx", "stf", "stg"):
        st_tiles[_n] = P.sb([128, 4], F32, "st_" + _n)

    def rms_norm(src, n, gview, out_bf, Rk, Wk, stk="st"):
        if stk not in st_tiles:
            st_tiles[stk] = P.sb([128, 4], F32, "st_" + stk)
        st = st_tiles[stk]
        P.add("act", lambda e: e.activation(out=junk[:, 0:n], in_=src, func=AF.Square, accum_out=st[:, 0:1]),
              R=Rk, W=["junk", stk + "0"])
        P.add("act", lambda e: e.activation(out=st[:, 2:3], in_=st[:, 0:1], func=AF.Sqrt, scale=1.0 / n, bias=epst[:, 0:1]),
              R=[stk + "0", "epst"], W=[stk + "2"])
        P.add("dve", lambda e: e.reciprocal(out=st[:, 3:4], in_=st[:, 2:3]), R=[stk + "2"], W=[stk + "3"])
        P.add("dve", lambda e: e.scalar_tensor_tensor(out=out_bf, in0=src, scalar=st[:, 3:4], in1=gview,
                                                      op0=ALU.mult, op1=ALU.mult),
              R=list(Rk) + [stk + "3", "gb"], W=Wk)

    P.mark()
    hqT = P.sb([128, 2, OWN], BF16, "hqT")
    hkvT = P.sb([128, S], BF16, "hkvT")
    krot = P.sb([128, NT, 32], F32, "krot")
    ropecs = P.sb([128, NT, 32], F32, "ropecs")
    P.add("sp", lambda e: e.dma_start(out=ropecs.rearrange("p a b -> p (a b)"), in_=I["rope_cs"]), W=["ropecs"], dma="c2")

    P.mark()
    hT = P.sb([128, 8, 2, OWN + 2], BF16, "hT")
    P.mark()
    xt = [P.sb([128, D], F32, "xt%d" % i) for i in range(4)]
    xn = [P.sb([128, D], BF16, "xn%d" % i) for i in range(4)]
    load_gain("mix_norm_g")
    for i in range(NT):
        b = i % 4
        seg, j = divmod(i, NTO)
        P.add("sp", lambda e, i=i, b=b: e.dma_start(out=xt[b], in_=I["x_rot"][i * 128:(i + 1) * 128, :]),
              W=["xt%d" % b], dma="xt%d" % b)
        rms_norm(xt[b], D, gb, xn[b], ["xt%d" % b], ["xn%d" % b])
        pb = 0 + b
        for k in range(8):
            P.add("pe", lambda e, k=k, b=b, pb=pb: e.transpose(out=psb(pb)[:, k * 128:(k + 1) * 128],
                                                                 in_=xn[b][:, k * 128:(k + 1) * 128], identity=identb),
                  R=["xn%d" % b, "identb"], W=[psk[pb]])
        eng = "act" if b % 2 == 0 else "dve"
        dst = hT[:, :, seg, 1 + j * 128:1 + (j + 1) * 128]
        src = psb(pb).rearrange("p (k t) -> p k t", k=8)
        if eng == "act":
            P.add("act", lambda e, dst=dst, src=src: e.copy(out=dst, in_=src), R=[psk[pb]], W=["hT"])
        else:
            P.add("dve", lambda e, dst=dst, src=src: e.tensor_copy(out=dst, in_=src), R=[psk[pb]], W=["hT"])
    for (ds, dc, ss_, sc, m) in ((0, 0, 1, OWN, 0), (0, OWN + 1, 1, 1, 1), (1, 0, 0, OWN, 1), (1, OWN + 1, 0, 1, 0)):
        P.add("dve", lambda e, ds=ds, dc=dc, ss_=ss_, sc=sc, m=m: e.tensor_scalar_mul(
            out=hT[:, :, ds, dc:dc + 1], in0=hT[:, :, ss_, sc:sc + 1], scalar1=halfm[:, m:m + 1]),
            R=["hT", "halfm"], W=["hT"])

    P.barrier()
    P.release()
    P.mark()
    w_mla = P.sb([128, 8, 416], BF16, "w_mla")
    P.add("pool", lambda e: e.dma_start(out=w_mla, in_=I["w_in"][:, 0:416].rearrange("(k p) n -> p k n", p=128)),
          W=["w_mla"], dma="w0")
    gq = P.sb([128, 256], F32, "gq")
    gkv = P.sb([128, 128], F32, "gkv")
    P.add("sp", lambda e: e.dma_start(out=gq, in_=I["q_norm_g"].partition_broadcast(128)), W=["gq"], dma="c3")
    P.add("sp", lambda e: e.dma_start(out=gkv, in_=I["kv_norm_g"].partition_broadcast(128)), W=["gkv"], dma="c4")
    hqn = [P.sb([128, 256], BF16, "hqn%d" % i) for i in range(2)]
    hkvn = [P.sb([128, 128], BF16, "hkvn%d" % i) for i in range(2)]
    tmp16 = P.sb([128, 4, 16], F32, "tmp16")
    for i in range(NT):
        b = i % 2
        seg, j = divmod(i, NTO)
        pb = 2 + b
        for k in range(8):
            P.add("pe", lambda e, k=k, pb=pb, seg=seg, j=j: e.matmul(
                ps[pb][:, 0:416], lhsT=hT[:, k, seg, 1 + j * 128:1 + (j + 1) * 128], rhs=w_mla[:, k, :],
                start=(k == 0), stop=(k == 7)), R=["hT", "w_mla"], W=[psk[pb]])
        if seg == 0:
            rms_norm(ps[pb][:, 0:256], 256, gq, hqn[b], [psk[pb], "gq"], ["hqn%d" % b], stk="stq")
            pt = 4 + b
            for k in range(2):
                P.add("pe", lambda e, k=k, b=b, pt=pt: e.transpose(out=psb(pt)[:, k * 128:(k + 1) * 128],
                                                                     in_=hqn[b][:, k * 128:(k + 1) * 128], identity=identb),
                      R=["hqn%d" % b, "identb"], W=[psk[pt]])
            P.add("act", lambda e, pt=pt, j=j: e.copy(out=hqT[:, :, j * 128:(j + 1) * 128],
                                                       in_=psb(pt)[:, 0:256].rearrange("p (k t) -> p k t", k=2)),
                  R=[psk[pt]], W=["hqT"])
        rms_norm(ps[pb][:, 256:384], 128, gkv, hkvn[b], [psk[pb], "gkv"], ["hkvn%d" % b], stk="stk")
        pt = 6 + b
        P.add("pe", lambda e, b=b, pt=pt: e.transpose(out=psb(pt)[:, 0:128], in_=hkvn[b], identity=identb),
              R=["hkvn%d" % b, "identb"], W=[psk[pt]])
        P.add("act", lambda e, pt=pt, i=i: e.copy(out=hkvT[:, i * 128:(i + 1) * 128], in_=psb(pt)[:, 0:128]),
              R=[psk[pt]], W=["hkvT"])
        x1 = ps[pb][:, 384:400]
        x2 = ps[pb][:, 400:416]
        c = ropecs[:, i, 0:16]
        s_ = ropecs[:, i, 16:32]
        P.add("dve", lambda e, x1=x1, c=c: e.tensor_tensor(out=tmp16[:, 0, :], in0=x1, in1=c, op=ALU.mult), R=[psk[pb], "ropecs"], W=["t16a"])
        P.add("dve", lambda e, x2=x2, s_=s_: e.tensor_tensor(out=tmp16[:, 1, :], in0=x2, in1=s_, op=ALU.mult), R=[psk[pb], "ropecs"], W=["t16b"])
        P.add("dve", lambda e, x1=x1, s_=s_: e.tensor_tensor(out=tmp16[:, 2, :], in0=x1, in1=s_, op=ALU.mult), R=[psk[pb], "ropecs"], W=["t16c"])
        P.add("dve", lambda e, x2=x2, c=c: e.tensor_tensor(out=tmp16[:, 3, :], in0=x2, in1=c, op=ALU.mult), R=[psk[pb], "ropecs"], W=["t16d"])
        P.add("dve", lambda e, i=i: e.tensor_tensor(out=krot[:, i, 0:16], in0=tmp16[:, 0, :], in1=tmp16[:, 1, :], op=ALU.subtract),
              R=["t16a", "t16b"], W=["krot"])
        P.add("dve", lambda e, i=i: e.tensor_tensor(out=krot[:, i, 16:32], in0=tmp16[:, 2, :], in1=tmp16[:, 3, :], op=ALU.add),
              R=["t16c", "t16d"], W=["krot"])

    P.barrier()
    P.release()
    w_hy = P.sb([128, 8, 512], BF16, "w_hy")
    cwT = P.sb([128, 12, 3], F32, "cwT")
    cbT = P.sb([128, 12], F32, "cbT")
    uT = [P.sb([128, 2, OWN + 2], F32, "uT0")] * 2
    tTc = P.sb([128, 4, 2, OWN], F32, "tTc")
    tmpP = P.sb([128, OWN], F32, "tmpP")
    uo = [P.sb([128, 512], F32, "uo%d" % i) for i in range(2)]
    P.add("sp", lambda e: e.dma_start(out=cwT.rearrange("p a b -> p (a b)"), in_=I["hy_conv_wT"]), W=["cwT"], dma="cw0")
    P.add("sp", lambda e: e.dma_start(out=cbT, in_=I["hy_conv_bT"]), W=["cbT"], dma="cw1")
    nev = 0
    for c3 in range(3):
        c0 = 416 + c3 * 512
        P.add("pool", lambda e, c0=c0: e.dma_start(out=w_hy, in_=I["w_in"][:, c0:c0 + 512].rearrange("(k p) n -> p k n", p=128)),
              W=["w_hy"], dma="w1")
        for c4 in range(4):
            ct = c3 * 4 + c4
            ub = 0
            u_ = uT[ub]
            for seg in range(2):
                for tc in range(4):
                    pb = 2 + (nev % 4)
                    for k in range(8):
                        P.add("pe", lambda e, k=k, pb=pb, seg=seg, tc=tc, c4=c4: e.matmul(
                            ps[pb][:, :], lhsT=w_hy[:, k, c4 * 128:(c4 + 1) * 128], rhs=hT[:, k, seg, 1 + tc * 512:1 + (tc + 1) * 512],
                            start=(k == 0), stop=(k == 7)), R=["hT", "w_hy"], W=[psk[pb]])
                    dst = u_[:, seg, 1 + tc * 512:1 + (tc + 1) * 512]
                    if nev % 2 == 0:
                        P.add("act", lambda e, pb=pb, dst=dst: e.copy(out=dst, in_=ps[pb][:, :]), R=[psk[pb]], W=["uT%d" % ub])
                    else:
                        P.add("dve", lambda e, pb=pb, dst=dst: e.tensor_copy(out=dst, in_=ps[pb][:, :]), R=[psk[pb]], W=["uT%d" % ub])
                    nev += 1
            for (ds, dc, ss_, sc, m) in ((0, 0, 1, OWN, 0), (0, OWN + 1, 1, 1, 1), (1, 0, 0, OWN, 1), (1, OWN + 1, 0, 1, 0)):
                P.add("dve", lambda e, u_=u_, ds=ds, dc=dc, ss_=ss_, sc=sc, m=m: e.tensor_scalar_mul(
                    out=u_[:, ds, dc:dc + 1], in0=u_[:, ss_, sc:sc + 1], scalar1=halfm[:, m:m + 1]),
                    R=["uT%d" % ub, "halfm"], W=["uT%d" % ub])
            for seg in range(2):
                t_ = tTc[:, c4, seg, :]
                P.add("act", lambda e, u_=u_, seg=seg, t_=t_, ct=ct: e.activation(out=t_, in_=u_[:, seg, 1:OWN + 1], func=AF.Identity,
                                                                               scale=cwT[:, ct, 1:2], bias=cbT[:, ct:ct + 1]),
                      R=["uT%d" % ub, "cwT", "cbT"], W=["tTc_%d_%d" % (c4, seg)])
                P.add("dve", lambda e, u_=u_, seg=seg, t_=t_, ct=ct: e.scalar_tensor_tensor(out=t_, in0=u_[:, seg, 0:OWN], scalar=cwT[:, ct, 0:1], in1=t_,
                                                                                         op0=ALU.mult, op1=ALU.add),
                      R=["uT%d" % ub, "cwT", "tTc_%d_%d" % (c4, seg)], W=["tTc_%d_%d" % (c4, seg)])
                P.add("act", lambda e, u_=u_, seg=seg, ct=ct: e.activation(out=tmpP, in_=u_[:, seg, 2:OWN + 2], func=AF.Copy, scale=cwT[:, ct, 2:3]),
                      R=["uT%d" % ub, "cwT"], W=["tmpP"])
                P.add("pool", lambda e, t_=t_: e.tensor_tensor(out=t_, in0=t_, in1=tmpP, op=ALU.add),
                      R=["tmpP", "tTc_%d_%d" % (c4, seg)], W=["tTc_%d_%d" % (c4, seg)])
        for i in range(NT):
            b = i % 2
            seg, j = divmod(i, NTO)
            pb = 6 + b
            for c4 in range(4):
                P.add("pe", lambda e, c4=c4, seg=seg, j=j, pb=pb: e.transpose(out=ps[pb][:, c4 * 128:(c4 + 1) * 128],
                                                                           in_=tTc[:, c4, seg, j * 128:(j + 1) * 128], identity=identf),
                      R=["tTc_%d_%d" % (c4, seg), "identf"], W=[psk[pb]])
            if b == 0:
                P.add("act", lambda e, pb=pb, b=b: e.copy(out=uo[b], in_=ps[pb][:, :]), R=[psk[pb]], W=["uo%d" % b])
            else:
                P.add("dve", lambda e, pb=pb, b=b: e.tensor_copy(out=uo[b], in_=ps[pb][:, :]), R=[psk[pb]], W=["uo%d" % b])
            P.add("sp", lambda e, i=i, b=b, c3=c3: e.dma_start(out=U_d[i * 128:(i + 1) * 128, c3 * 512:(c3 + 1) * 512], in_=uo[b]),
                  R=["uo%d" % b], W=["U_d"], dma="uo%d" % b)
    P.barrier()
    P.release()

    if stop == "A0":
        P.add("sp", None, R=[])
        P.emit()
        return nc, dbg_d
    if "uc" in dbg:
        tu = dbg_out("uc", [S, 1536])
        P.mark()
        tb = P.sb([128, 1536], F32, "dbgt")
        for i in range(NT):
            P.add("sp", lambda e, i=i: e.dma_start(out=tb, in_=U_d[i * 128:(i + 1) * 128, :]), R=["U_d"], W=["dbgt"], dma="dbg0")
            P.add("sp", lambda e, i=i: e.dma_start(out=tu[i * 128:(i + 1) * 128, :], in_=tb), R=["dbgt"], W=["dbgo"], dma="dbg1")
        P.barrier()
        P.release()

    if stop == "A":
        P.add("sp", None, R=["dbgo"])
        P.emit()
        return nc, dbg_d
    aout_d = nc.dram_tensor("aout_scr", [OWN, 512], F32, kind="Internal").ap()
    P.mark()
    G4 = 8
    KT = P.sb([128, G4, S], BF16, "KT")
    QT = P.sb([128, G4, OWN], BF16, "QT")
    Vaug = P.sb([128, NT, G4, 68], BF16, "Vaug")
    w_ukv = P.sb([128, 1024], BF16, "w_ukv")
    w_uq = P.sb([128, 2, 768], BF16, "w_uq")
    P.add("pool", lambda e: e.dma_start(out=w_ukv, in_=I["w_ukv"]), W=["w_ukv"], dma="w0")
    P.add("pool", lambda e: e.dma_start(out=w_uq, in_=I["w_uq"].rearrange("(k p) n -> p k n", p=128)), W=["w_uq"], dma="w1")
    Kaug = [P.sb([128, G4, 100], BF16, "Kaug%d" % i) for i in range(2)]
    Qaug = [P.sb([128, G4, 100], BF16, "Qaug%d" % i) for i in range(2)]
    ksq = P.sb([128, G4, 96], F32, "ksq")
    kn2 = P.sb([128, G4], F32, "kn2")
    kmax = P.sb([128, G4], F32, "kmax")
    kb = P.sb([128, 4], F32, "kb")
    qs = P.sb([128, G4, 96], F32, "qs")
    qsq = ksq
    qn = P.sb([128, G4], F32, "qn")
    qt4 = P.sb([128, 4, G4, 16], F32, "qt4")
    PT = [P.sb([128, 512], BF16, "PT%d" % i) for i in range(3)]
    oTs = P.sb([65, 512], F32, "oTs")
    rden = P.sb([128, 4], F32, "rden")
    astage = [P.sb([128, 4, 512], F32, "astage0")] * 2
    scale = HD ** -0.5
    it = 0
    for g in range(1):
        P.add("pool", lambda e: e.memset(Vaug.rearrange("p a b c -> p (a b c)"), 1.0), W=["Vaug"])
        for b in range(2):
            P.add("pool", lambda e, b=b: e.memset(Kaug[b].rearrange("p a b -> p (a b)"), 1.0), W=["Kaug%d" % b])
        P.add("pool", lambda e: e.memset(kmax, 0.0), W=["kmax"])
        for i in range(NT):
            b = i % 2
            pbk = 2 * b
            for hh in range(2):
                P.add("pe", lambda e, hh=hh, pbk=pbk, i=i: e.matmul(ps[pbk + hh][:, :], lhsT=hkvT[:, i * 128:(i + 1) * 128],
                                                                     rhs=w_ukv[:, hh * 512:(hh + 1) * 512], start=True, stop=True),
                      R=["hkvT", "w_ukv"], W=[psk[pbk + hh]])
            v = psall[:, pbk * 512:(pbk + 2) * 512].rearrange("p (h c) -> p h c", h=8)
            P.add("act", lambda e, v=v, i=i: e.copy(out=Vaug[:, i, :, 0:64], in_=v[:, :, 64:128]), R=[psk[pbk], psk[pbk + 1]], W=["Vaug"])
            P.add("dve", lambda e, v=v, b=b: e.tensor_copy(out=Kaug[b][:, :, 0:64], in_=v[:, :, 0:64]), R=[psk[pbk], psk[pbk + 1]], W=["Kaug%d" % b])
            for h in range(G4):
                P.add("pool", lambda e, h=h, b=b, i=i: e.tensor_copy(out=Kaug[b][:, h, 64:96], in_=krot[:, i, :]),
                      R=["krot"], W=["Kaug%d" % b])
            P.add("dve", lambda e, b=b: e.tensor_tensor(out=ksq, in0=Kaug[b][:, :, 0:96], in1=Kaug[b][:, :, 0:96], op=ALU.mult),
                  R=["Kaug%d" % b], W=["ksq"])
            P.add("dve", lambda e: e.tensor_reduce(out=kn2, in_=ksq, axis=AX.X, op=ALU.add), R=["ksq"], W=["kn2"])
            P.add("dve", lambda e: e.tensor_tensor(out=kmax, in0=kmax, in1=kn2, op=ALU.max), R=["kn2", "kmax"], W=["kmax"])
            pt = 4 + b
            for h in range(G4):
                P.add("pe", lambda e, h=h, b=b, pt=pt: e.transpose(out=psb(pt)[0:97, h * 128:(h + 1) * 128], in_=Kaug[b][:, h, 0:97], identity=identb),
                      R=["Kaug%d" % b, "identb"], W=[psk[pt]])
            P.add("act", lambda e, pt=pt, i=i: e.copy(out=KT[0:97, :, i * 128:(i + 1) * 128],
                                                       in_=psb(pt)[0:97, 0:G4 * 128].rearrange("p (h t) -> p h t", h=G4)),
                  R=[psk[pt]], W=["KT"])
        P.add("dve", lambda e: e.tensor_reduce(out=kb[:, 1:2], in_=kmax, axis=AX.X, op=ALU.max), R=["kmax"], W=["kb1"])
        P.add("pe", lambda e: e.transpose(out=ps[6][0:1, 0:128], in_=kb[:, 1:2], identity=identf), R=["kb1", "identf"], W=[psk[6]])
        P.add("dve", lambda e: e.tensor_reduce(out=kb[0:1, 2:3], in_=ps[6][0:1, 0:128], axis=AX.X, op=ALU.max), R=[psk[6]], W=["kb2"])
        P.add("pe", lambda e: e.matmul(ps[7][:, 0:1], lhsT=onesf[0:1, 0:128], rhs=kb[0:1, 2:3], start=True, stop=True),
              R=["kb2", "onesf"], W=[psk[7]])
        P.add("act", lambda e: e.sqrt(out=kb[:, 0:1], in_=ps[7][:, 0:1]), R=[psk[7]], W=["kb0"])
        for j in range(NTO):
            b = j % 2
            pa = 2 * b
            for (pq, c0, ncol) in ((pa, 0, 480), (pa + 1, 480, 288)):
                for k in range(2):
                    P.add("pe", lambda e, pq=pq, c0=c0, ncol=ncol, k=k, j=j: e.matmul(
                        ps[pq][:, 0:ncol], lhsT=hqT[:, k, j * 128:(j + 1) * 128], rhs=w_uq[:, k, c0:c0 + ncol],
                        start=(k == 0), stop=(k == 1)), R=["hqT", "w_uq"], W=[psk[pq]])
            P.add("act", lambda e, pa=pa: e.mul(out=qs[:, 0:5, :], in_=ps[pa][:, 0:480].rearrange("p (h c) -> p h c", h=5), mul=scale),
                  R=[psk[pa]], W=["qs"])
            P.add("act", lambda e, pa=pa: e.mul(out=qs[:, 5:8, :], in_=ps[pa + 1][:, 0:288].rearrange("p (h c) -> p h c", h=3), mul=scale),
                  R=[psk[pa + 1]], W=["qs"])
            c = ropecs[:, j:j + 1, 0:16].broadcast_to([128, G4, 16])
            s_ = ropecs[:, j:j + 1, 16:32].broadcast_to([128, G4, 16])
            x1 = qs[:, :, 64:80]
            x2 = qs[:, :, 80:96]
            P.add("dve", lambda e, x1=x1, c=c: e.tensor_tensor(out=qt4[:, 0], in0=x1, in1=c, op=ALU.mult), R=["qs", "ropecs"], W=["qt4a"])
            P.add("dve", lambda e, x2=x2, s_=s_: e.tensor_tensor(out=qt4[:, 1], in0=x2, in1=s_, op=ALU.mult), R=["qs", "ropecs"], W=["qt4b"])
            P.add("pool", lambda e, x1=x1, s_=s_: e.tensor_tensor(out=qt4[:, 2], in0=x1, in1=s_, op=ALU.mult), R=["qs", "ropecs"], W=["qt4c"])
            P.add("pool", lambda e, x2=x2, c=c: e.tensor_tensor(out=qt4[:, 3], in0=x2, in1=c, op=ALU.mult), R=["qs", "ropecs"], W=["qt4d"])
            P.add("dve", lambda e: e.tensor_tensor(out=qs[:, :, 64:80], in0=qt4[:, 0], in1=qt4[:, 1], op=ALU.subtract),
                  R=["qt4a", "qt4b"], W=["qs"])
            P.add("dve", lambda e: e.tensor_tensor(out=qs[:, :, 80:96], in0=qt4[:, 2], in1=qt4[:, 3], op=ALU.add),
                  R=["qt4c", "qt4d"], W=["qs"])
            P.add("dve", lambda e: e.tensor_tensor(out=qsq, in0=qs, in1=qs, op=ALU.mult), R=["qs"], W=["ksq"])
            P.add("dve", lambda e: e.tensor_reduce(out=qn, in_=qsq, axis=AX.X, op=ALU.add), R=["ksq"], W=["qn"])
            P.add("act", lambda e: e.sqrt(out=qn, in_=qn), R=["qn"], W=["qn"])
            P.add("dve", lambda e, b=b: e.tensor_scalar(out=Qaug[b][:, :, 96:97], in0=qn.rearrange("p (h o) -> p h o", o=1),
                                                        scalar1=kb[:, 0:1], scalar2=-1.0, op0=ALU.mult, op1=ALU.mult),
                  R=["qn", "kb0"], W=["Qaug%d" % b])
            P.add("act", lambda e, b=b: e.copy(out=Qaug[b][:, :, 0:96], in_=qs), R=["qs"], W=["Qaug%d" % b])
            pt = 4 + b
            for h in range(G4):
                P.add("pe", lambda e, h=h, b=b, pt=pt: e.transpose(out=psb(pt)[0:97, h * 128:(h + 1) * 128], in_=Qaug[b][:, h, 0:97], identity=identb),
                      R=["Qaug%d" % b, "identb"], W=[psk[pt]])
            P.add("dve", lambda e, pt=pt, j=j: e.tensor_copy(out=QT[0:97, :, j * 128:(j + 1) * 128],
                                                             in_=psb(pt)[0:97, 0:G4 * 128].rearrange("p (h t) -> p h t", h=G4)),
                  R=[psk[pt]], W=["QT"])
        items = [(qc, h, kt) for qc in range(4) for h in range(G4) for kt in range(NT)]
        LA = 2

        def emit_scores(idx):
            qc, h, kt = items[idx]
            pb_ = idx % 3
            P.add("pe", lambda e, h=h, qc=qc, kt=kt, pb_=pb_: e.matmul(
                ps[pb_][:, :], lhsT=KT[0:97, h, kt * 128:(kt + 1) * 128], rhs=QT[0:97, h, qc * 512:(qc + 1) * 512],
                start=True, stop=True), R=["KT", "QT"], W=[psk[pb_]])

        def emit_epilogue(qc, h):
            po = 6 + h % 2
            sb_ = qc % 2
            P.add("dve", lambda e, po=po: e.tensor_copy(out=oTs, in_=ps[po][0:65, :]), R=[psk[po]], W=["oTs"])
            for t4 in range(4):
                pt = 3 + (t4 % 2)
                P.add("pe", lambda e, t4=t4, pt=pt: e.transpose(out=ps[pt][:, 0:65], in_=oTs[:, t4 * 128:(t4 + 1) * 128], identity=identf[0:65, 0:65]),
                      R=["oTs", "identf"], W=[psk[pt]])
                P.add("dve", lambda e, pt=pt, t4=t4: e.reciprocal(out=rden[:, t4:t4 + 1], in_=ps[pt][:, 64:65]), R=[psk[pt]], W=["rden%d" % t4])
                P.add("dve", lambda e, pt=pt, t4=t4, h=h, sb_=sb_: e.tensor_scalar_mul(
                    out=astage[sb_][:, t4, h * 64:(h + 1) * 64], in0=ps[pt][:, 0:64], scalar1=rden[:, t4:t4 + 1]),
                    R=[psk[pt], "rden%d" % t4], W=["astage0"])
            if h == G4 - 1:
                P.add("sp", lambda e, qc=qc, g=g, sb_=sb_: e.dma_start(
                    out=aout_d[qc * 512:(qc + 1) * 512, :].rearrange("(t p) c -> p t c", p=128), in_=astage[sb_]),
                    R=["astage0"], W=["aout_d"], dma="ast0")

        for idx in range(min(LA, len(items))):
            emit_scores(idx)
        pending = None
        for idx, (qc, h, kt) in enumerate(items):
            pb_ = idx % 3
            po = 6 + h % 2
            P.add("act", lambda e, pb_=pb_: e.activation(out=PT[pb_], in_=ps[pb_][:, :], func=AF.Exp),
                  R=[psk[pb_]], W=["PT%d" % pb_])
            if idx + LA < len(items):
                emit_scores(idx + LA)
            P.add("pe", lambda e, h=h, kt=kt, pb_=pb_, po=po: e.matmul(
                ps[po][0:65, :], lhsT=Vaug[:, kt, h, 0:65], rhs=PT[pb_], start=(kt == 0), stop=(kt == NT - 1)),
                R=["Vaug", "PT%d" % pb_], W=[psk[po]])
            if pending is not None and kt == 3:
                emit_epilogue(*pending)
                pending = None
            if kt == NT - 1:
                pending = (qc, h)
        if pending is not None:
            emit_epilogue(*pending)
    P.barrier()
    P.release()
    P.release()

    if "a_out" in dbg:
        ta = dbg_out("a_out", [OWN, 512])
        P.mark()
        tba = P.sb([128, NTO, 512], F32, "dbgt2")
        P.add("sp", lambda e: e.dma_start(out=tba, in_=aout_d.rearrange("(j p) c -> p j c", p=128)), R=["aout_d"], W=["dbgt2"], dma="dbg0")
        P.add("sp", lambda e: e.dma_start(out=ta.rearrange("(j p) c -> p j c", p=128), in_=tba), R=["dbgt2"], W=["dbgo"], dma="dbg1")
        P.barrier()
        P.release()

    if stop == "attn":
        P.add("sp", None, R=[])
        P.emit()
        return nc, dbg_d

    if "hout_in" in dbg:
        hin = nc.dram_tensor("dbg_hout_in", [OWN, 512], F32, kind="ExternalInput").ap()
        P.mark()
        tbh = P.sb([128, NTO, 512], F32, "tbh")
        P.add("sp", lambda e: e.dma_start(out=tbh, in_=hin.rearrange("(j p) c -> p j c", p=128)), W=["tbh"], dma="dbg0")
        P.add("sp", lambda e: e.dma_start(out=hout_d.rearrange("(j p) c -> p j c", p=128), in_=tbh), R=["tbh"], W=["hout_d"], dma="dbg1")
        P.barrier()
        P.release()
    else:
        hyena_phase(nc, P, I, ps, psk, psb, U_d, hout_d, identf, identb, onesb, onesf, halfm, dbg, dbg_out, psall)

    if stop == "C":
        P.add("sp", None, R=[])
        P.emit()
        return nc, dbg_d
    xres = P.sb([128, NTO, D], F32, "xres")
    P.add("sp", lambda e: e.dma_start(out=xres, in_=I["x_rot"][0:OWN, :].rearrange("(j p) c -> p j c", p=128)), W=["xres"], dma="xres")
    P.mark()
    w_out = P.sb([128, 8, D], BF16, "w_out")
    P.add("pool", lambda e: e.dma_start(out=w_out, in_=I["w_out"].rearrange("(k p) n -> p k n", p=128)), W=["w_out"], dma="w0")
    P.add("sp", lambda e: e.dma_start(out=gb[:, 0:512], in_=I["attn_out_g"].partition_broadcast(128)), W=["gb"], dma="gain")
    P.add("sp", lambda e: e.dma_start(out=gb[:, 512:1024], in_=I["hy_out_g"].partition_broadcast(128)), W=["gb"], dma="gain")
    mixin = [P.sb([128, D], F32, "mixin%d" % i) for i in range(2)]
    mixbf = [P.sb([128, D], BF16, "mixbf%d" % i) for i in range(2)]
    mT = [P.sb([128, 8, 128], BF16, "mT%d" % i) for i in range(2)]
    for j in range(NTO):
        b = j % 2
        P.add("sp", lambda e, j=j, b=b: e.dma_start(out=mixin[b][:, 0:512], in_=aout_d[j * 128:(j + 1) * 128, :]),
              R=["aout_d"], W=["mixin%d" % b], dma="mixa%d" % b)
        P.add("sp", lambda e, j=j, b=b: e.dma_start(out=mixin[b][:, 512:1024], in_=hout_d[j * 128:(j + 1) * 128, :]),
              R=["hout_d"] + ["hout_d_%d" % k for k in range(8)], W=["mixin%d" % b], dma="mixh%d" % b)
        rms_norm(mixin[b][:, 0:512], 512, gb[:, 0:512], mixbf[b][:, 0:512], ["mixin%d" % b], ["mixbfa%d" % b], stk="sta")
        rms_norm(mixin[b][:, 512:1024], 512, gb[:, 512:1024], mixbf[b][:, 512:1024], ["mixin%d" % b], ["mixbfh%d" % b], stk="sth")
        pt = 0 + b
        for k in range(8):
            P.add("pe", lambda e, k=k, b=b, pt=pt: e.transpose(out=psb(pt)[:, k * 128:(k + 1) * 128], in_=mixbf[b][:, k * 128:(k + 1) * 128], identity=identb),
                  R=["mixbfa%d" % b, "mixbfh%d" % b, "identb"], W=[psk[pt]])
        P.add("act", lambda e, b=b, pt=pt: e.copy(out=mT[b].rearrange("p k t -> p (k t)"), in_=psb(pt)), R=[psk[pt]], W=["mT%d" % b])
        for n in range(2):
            py = 2 + 2 * b + n
            for k in range(8):
                P.add("pe", lambda e, k=k, b=b, n=n, py=py: e.matmul(ps[py][:, :], lhsT=mT[b][:, k, :], rhs=w_out[:, k, n * 512:(n + 1) * 512],
                                                                      start=(k == 0), stop=(k == 7)), R=["mT%d" % b, "w_out"], W=[psk[py]])
            P.add("dve", lambda e, j=j, n=n, py=py: e.tensor_tensor(out=xres[:, j, n * 512:(n + 1) * 512], in0=ps[py][:, :],
                                                                     in1=xres[:, j, n * 512:(n + 1) * 512], op=ALU.add),
                  R=[psk[py], "xres"], W=["xres"])
    P.barrier()
    P.release()
    if stop == "D":
        P.add("sp", None, R=[])
        P.emit()
        return nc, dbg_d
    if "x1" in dbg:
        tx1 = dbg_out("x1", [OWN, D])
        P.add("sp", lambda e: e.dma_start(out=tx1.rearrange("(j p) c -> p j c", p=128), in_=xres), R=["xres"], W=["dbgo"], dma="dbg1")
        P.barrier()

    P.mark()
    hmT = P.sb([128, 8, 256], BF16, "hmT")
    KmT = P.sb([128, 8, 256], BF16, "KmT")
    Vm = P.sb([128, 2, 4, 260], BF16, "Vm")
    ksqm = P.sb([128, 8, 256], BF16, "ksqm")
    kbx = P.sb([1, 8], F32, "kbx")
    P.mark()
    w_mkv = P.sb([128, 8, 2 * D], BF16, "w_mkv")
    P.add("pool", lambda e: e.dma_start(out=w_mkv, in_=I["w_mkv"].rearrange("(k p) n -> p k n", p=128)), W=["w_mkv"], dma="w0")
    load_gain("mem_norm_g")
    P.add("pool", lambda e: e.memset(Vm.rearrange("p a b c -> p (a b c)"), 1.0), W=["Vm"])
    memt = [P.sb([128, D], F32, "memt%d" % i) for i in range(2)]
    membf = [P.sb([128, D], BF16, "membf%d" % i) for i in range(2)]
    for mt in range(2):
        P.add("sp", lambda e, mt=mt: e.dma_start(out=memt[mt], in_=I["mem_b"][mt * 128:(mt + 1) * 128, :]), W=["memt%d" % mt], dma="memt%d" % mt)
        rms_norm(memt[mt], D, gb, membf[mt], ["memt%d" % mt], ["membf%d" % mt], stk="stm")
        for k in range(8):
            P.add("pe", lambda e, k=k, mt=mt: e.transpose(out=psb(mt)[:, k * 128:(k + 1) * 128], in_=membf[mt][:, k * 128:(k + 1) * 128], identity=identb),
                  R=["membf%d" % mt, "identb"], W=[psk[mt]])
        P.add("act", lambda e, mt=mt: e.copy(out=hmT[:, :, mt * 128:(mt + 1) * 128], in_=psb(mt).rearrange("p (k t) -> p k t", k=8)),
              R=[psk[mt]], W=["hmT"])
    for dt in range(8):
        pk = 2 + dt % 2
        for k in range(8):
            P.add("pe", lambda e, k=k, dt=dt, pk=pk: e.matmul(ps[pk][:, 0:256], lhsT=w_mkv[:, k, dt * 128:(dt + 1) * 128], rhs=hmT[:, k, :],
                                                               start=(k == 0), stop=(k == 7)), R=["w_mkv", "hmT"], W=[psk[pk]])
        P.add("act", lambda e, dt=dt, pk=pk: e.copy(out=KmT[:, dt, :], in_=ps[pk][:, 0:256]), R=[psk[pk]], W=["KmT"])
    for mt in range(2):
        for n in range(2):
            pv = 4 + n
            for k in range(8):
                P.add("pe", lambda e, k=k, mt=mt, n=n, pv=pv: e.matmul(ps[pv][:, :], lhsT=hmT[:, k, mt * 128:(mt + 1) * 128],
                                                                        rhs=w_mkv[:, k, D + n * 512:D + (n + 1) * 512],
                                                                        start=(k == 0), stop=(k == 7)), R=["w_mkv", "hmT"], W=[psk[pv]])
            P.add("dve", lambda e, mt=mt, n=n, pv=pv: e.tensor_copy(out=Vm[:, mt, 2 * n:2 * n + 2, 0:256],
                                                                    in_=ps[pv][:, :].rearrange("p (h c) -> p h c", h=2)),
                  R=[psk[pv]], W=["Vm"])
    P.add("dve", lambda e: e.tensor_tensor(out=ksqm, in0=KmT, in1=KmT, op=ALU.mult), R=["KmT"], W=["ksqm"])
    for hh in range(4):
        for dt in range(2):
            P.add("pe", lambda e, hh=hh, dt=dt: e.matmul(ps[6][0:1, 0:256], lhsT=onesb[:, 0:1], rhs=ksqm[:, 2 * hh + dt, :],
                                                          start=(dt == 0), stop=(dt == 1)), R=["ksqm", "onesb"], W=[psk[6]])
        P.add("dve", lambda e, hh=hh: e.tensor_reduce(out=kbx[0:1, hh:hh + 1], in_=ps[6][0:1, 0:256], axis=AX.X, op=ALU.max),
              R=[psk[6]], W=["kbx%d" % hh])
    P.add("dve", lambda e: e.tensor_reduce(out=kbx[0:1, 4:5], in_=kbx[0:1, 0:4], axis=AX.X, op=ALU.max),
          R=["kbx0", "kbx1", "kbx2", "kbx3"], W=["kbx4"])
    P.add("act", lambda e: e.sqrt(out=kbx[0:1, 5:6], in_=kbx[0:1, 4:5]), R=["kbx4"], W=["kbx5"])
    P.add("dve", lambda e: e.tensor_scalar_mul(out=kbx[0:1, 6:7], in0=kbx[0:1, 5:6], scalar1=-1.04), R=["kbx5"], W=["kbx6"])
    P.barrier()
    P.release()
    w_mq = P.sb([128, 8, D], BF16, "w_mq")
    w_mo = P.sb([128, 8, D], BF16, "w_mo")
    P.add("pool", lambda e: e.dma_start(out=w_mq, in_=I["w_mq"].rearrange("(k p) n -> p k n", p=128)), W=["w_mq"], dma="w0")
    P.add("pool", lambda e: e.dma_start(out=w_mo, in_=I["w_mo"].rearrange("(k p) n -> p k n", p=128)), W=["w_mo"], dma="w1")
    load_gain("cross_norm_g")
    hxbf = [P.sb([128, D], BF16, "hxbf%d" % i) for i in range(2)]
    hxT = P.sb([128, 8, 512], BF16, "hxT")
    qT = P.sb([128, 8, 512], BF16, "qT")
    qsqx = P.sb([128, 8, 512], BF16, "qsqx")
    negm = P.sb([1, 4, 512], BF16, "negm")
    qn1 = P.sb([1, 512], F32, "qn1")
    PTm = [P.sb([128, 512], BF16, "PTm%d" % i) for i in range(2)]
    rdn = P.sb([1, 512], F32, "rdn")
    rdb = P.sb([128, 512], F32, "rdb")
    oTx = P.sb([128, 8, 512], BF16, "oTx")
    for qc in range(4):
        for t4 in range(4):
            j = qc * 4 + t4
            b = t4 % 2
            rms_norm(xres[:, j, :], D, gb, hxbf[b], ["xres"], ["hxbf%d" % b], stk="stx")
            for k in range(8):
                P.add("pe", lambda e, k=k, b=b: e.transpose(out=psb(b)[:, k * 128:(k + 1) * 128], in_=hxbf[b][:, k * 128:(k + 1) * 128], identity=identb),
                      R=["hxbf%d" % b, "identb"], W=[psk[b]])
            P.add("act", lambda e, b=b, t4=t4: e.copy(out=hxT[:, :, t4 * 128:(t4 + 1) * 128], in_=psb(b).rearrange("p (k t) -> p k t", k=8)),
                  R=[psk[b]], W=["hxT"])
        for dt in range(8):
            pq = 6 + dt % 2
            for k in range(8):
                P.add("pe", lambda e, k=k, dt=dt, pq=pq: e.matmul(ps[pq][:, :], lhsT=w_mq[:, k, dt * 128:(dt + 1) * 128], rhs=hxT[:, k, :],
                                                                   start=(k == 0), stop=(k == 7)), R=["w_mq", "hxT"], W=[psk[pq]])
            P.add("act", lambda e, dt=dt, pq=pq: e.mul(out=qT[:, dt, :], in_=ps[pq][:, :], mul=1.0 / 16.0), R=[psk[pq]], W=["qT"])
        P.add("dve", lambda e: e.tensor_tensor(out=qsqx, in0=qT, in1=qT, op=ALU.mult), R=["qT"], W=["qsqx"])
        for hh in range(4):
            for dt in range(2):
                P.add("pe", lambda e, hh=hh, dt=dt: e.matmul(ps[4][0:1, :], lhsT=onesb[:, 0:1], rhs=qsqx[:, 2 * hh + dt, :],
                                                              start=(dt == 0), stop=(dt == 1)), R=["qsqx", "onesb"], W=[psk[4]])
            P.add("act", lambda e: e.sqrt(out=qn1, in_=ps[4][0:1, :]), R=[psk[4]], W=["qn1"])
            P.add("dve", lambda e, hh=hh: e.tensor_scalar_mul(out=negm[0:1, hh, :], in0=qn1, scalar1=kbx[0:1, 6:7]), R=["qn1", "kbx6"], W=["negm"])
        for hh in range(4):
            for mt in range(2):
                for dt in range(2):
                    P.add("pe", lambda e, hh=hh, mt=mt, dt=dt: e.matmul(ps[mt][:, :], lhsT=KmT[:, 2 * hh + dt, mt * 128:(mt + 1) * 128],
                                                                         rhs=qT[:, 2 * hh + dt, :], start=(dt == 0), stop=False),
                          R=["KmT", "qT"], W=[psk[mt]])
                P.add("pe", lambda e, hh=hh, mt=mt: e.matmul(ps[mt][:, :], lhsT=onesb[0:1, 0:128], rhs=negm[0:1, hh, :], start=False, stop=True),
                      R=["negm", "onesb"], W=[psk[mt]])
                P.add("act", lambda e, mt=mt: e.activation(out=PTm[mt], in_=ps[mt][:, :], func=AF.Exp), R=[psk[mt]], W=["PTm%d" % mt])
            for dv in range(2):
                for mt in range(2):
                    P.add("pe", lambda e, hh=hh, mt=mt, dv=dv: e.matmul(ps[2 + dv][:, :], lhsT=Vm[:, mt, hh, dv * 128:(dv + 1) * 128], rhs=PTm[mt],
                                                                         start=(mt == 0), stop=(mt == 1)), R=["Vm", "PTm%d" % mt], W=[psk[2 + dv]])
            for mt in range(2):
                P.add("pe", lambda e, hh=hh, mt=mt: e.matmul(ps[4][0:1, :], lhsT=Vm[:, mt, hh, 256:257], rhs=PTm[mt], start=(mt == 0), stop=(mt == 1)),
                      R=["Vm", "PTm%d" % mt], W=[psk[4]])
            P.add("dve", lambda e: e.reciprocal(out=rdn, in_=ps[4][0:1, :]), R=[psk[4]], W=["rdn"])
            P.add("pe", lambda e: e.matmul(ps[5][:, :], lhsT=onesf[0:1, 0:128], rhs=rdn, start=True, stop=True), R=["rdn", "onesf"], W=[psk[5]])
            P.add("act", lambda e: e.copy(out=rdb, in_=ps[5][:, :]), R=[psk[5]], W=["rdb"])
            for dv in range(2):
                P.add("dve", lambda e, hh=hh, dv=dv: e.tensor_tensor(out=oTx[:, 2 * hh + dv, :], in0=ps[2 + dv][:, :], in1=rdb, op=ALU.mult),
                      R=[psk[2 + dv], "rdb"], W=["oTx"])
        for t4 in range(4):
            j = qc * 4 + t4
            for n in range(2):
                py = 6 + n
                for dt in range(8):
                    P.add("pe", lambda e, dt=dt, t4=t4, n=n, py=py: e.matmul(ps[py][:, :], lhsT=oTx[:, dt, t4 * 128:(t4 + 1) * 128],
                                                                              rhs=w_mo[:, dt, n * 512:(n + 1) * 512], start=(dt == 0), stop=(dt == 7)),
                          R=["oTx", "w_mo"], W=[psk[py]])
                P.add("dve", lambda e, j=j, n=n, py=py: e.tensor_tensor(out=xres[:, j, n * 512:(n + 1) * 512], in0=ps[py][:, :],
                                                                         in1=xres[:, j, n * 512:(n + 1) * 512], op=ALU.add),
                      R=[psk[py], "xres"], W=["xres"])
    P.barrier()
    P.release()
    if "x2" in dbg:
        tx2 = dbg_out("x2", [OWN, D])
        P.add("sp", lambda e: e.dma_start(out=tx2.rearrange("(j p) c -> p j c", p=128), in_=xres), R=["xres"], W=["dbgo"], dma="dbg1")
        P.barrier()
    if stop == "E":
        P.add("sp", None, R=[])
        P.emit()
        return nc, dbg_d

    P.mark()
    tT = P.sb([128, 8, OWN], BF16, "tT")
    combT = P.sb([32, OWN], F32, "combT")
    P.mark()
    load_gain("ffn_norm_g")
    w_r = P.sb([128, 8, 36], F32, "w_r")
    b_r = P.sb([128, 36], F32, "b_r")
    P.add("sp", lambda e: e.dma_start(out=w_r, in_=I["w_route"].rearrange("(k p) n -> p k n", p=128)), W=["w_r"], dma="c3")
    P.add("sp", lambda e: e.dma_start(out=b_r, in_=I["b_route"].partition_broadcast(128)), W=["b_r"], dma="c4")
    tnf = [P.sb([128, D], F32, "tnf%d" % i) for i in range(2)]
    tnb = [P.sb([128, D], BF16, "tnb%d" % i) for i in range(2)]
    tTf = P.sb([128, 8, 128], F32, "tTf")
    T_ = NTO
    lg = P.sb([128, T_, 36], F32, "lg")
    gmx = P.sb([128, T_], F32, "gmx")
    oh = P.sb([128, T_, 4], F32, "oh")
    ge = P.sb([128, T_, 4], F32, "ge")
    gsm = P.sb([128, T_], F32, "gsm")
    pg = P.sb([128, T_], F32, "pg")
    tmp48 = P.sb([128, T_, 4, 8], F32, "tmp48")
    ein = P.sb([128, T_, 8], F32, "ein")
    e2 = P.sb([128, T_, 8], F32, "e2")
    mk1 = P.sb([128, T_, 8], F32, "mk1")
    mk2 = P.sb([128, T_, 8], F32, "mk2")
    m1 = P.sb([128, T_], F32, "m1")
    m2 = P.sb([128, T_], F32, "m2")
    dd = P.sb([128, T_], F32, "dd")
    p1 = P.sb([128, T_], F32, "p1")
    p2 = P.sb([128, T_], F32, "p2")
    we = P.sb([128, T_, 8], F32, "we")
    we2 = P.sb([128, T_, 8], F32, "we2")
    comb = P.sb([128, T_, 4, 8], F32, "comb")
    seq = [0]

    def dv(fn, R, W):
        P.add("dve", fn, R=R, W=W)

    for j in range(NTO):
        b = j % 2
        rms_norm(xres[:, j, :], D, gb, tnf[b], ["xres"], ["tnf%d" % b], stk="stf")
        P.add("act", lambda e, b=b: e.copy(out=tnb[b], in_=tnf[b]), R=["tnf%d" % b], W=["tnb%d" % b])
        for k in range(8):
            P.add("pe", lambda e, k=k, b=b: e.transpose(out=psb(b)[:, k * 128:(k + 1) * 128], in_=tnb[b][:, k * 128:(k + 1) * 128], identity=identb),
                  R=["tnb%d" % b, "identb"], W=[psk[b]])
        P.add("act", lambda e, b=b, j=j: e.copy(out=tT[:, :, j * 128:(j + 1) * 128], in_=psb(b).rearrange("p (k t) -> p k t", k=8)),
              R=[psk[b]], W=["tT"])
        for k in range(8):
            pf = 2 + (k // 4)
            P.add("pe", lambda e, k=k, b=b, pf=pf: e.transpose(out=ps[pf][:, (k % 4) * 128:(k % 4 + 1) * 128], in_=tnf[b][:, k * 128:(k + 1) * 128], identity=identf),
                  R=["tnf%d" % b, "identf"], W=[psk[pf]])
        for hf in range(2):
            P.add("dve" if hf == 0 else "act", (lambda e, hf=hf: e.tensor_copy(out=tTf[:, hf * 4:(hf + 1) * 4, :], in_=ps[2 + hf][:, :].rearrange("p (k t) -> p k t", k=4)))
                  if hf == 0 else (lambda e, hf=hf: e.copy(out=tTf[:, hf * 4:(hf + 1) * 4, :], in_=ps[2 + hf][:, :].rearrange("p (k t) -> p k t", k=4))),
                  R=[psk[2 + hf]], W=["tTf%d" % hf])
        for k in range(8):
            P.add("pe", lambda e, k=k: e.matmul(ps[4][:, 0:36], lhsT=tTf[:, k, :], rhs=w_r[:, k, :], start=(k == 0), stop=(k == 7)),
                  R=["tTf0", "tTf1", "w_r"], W=[psk[4]])
        dv(lambda e, j=j: e.tensor_tensor(out=lg[:, j, :], in0=ps[4][:, 0:36], in1=b_r, op=ALU.add), [psk[4], "b_r"], ["lg"])

    def col(t, n):
        return t.rearrange("p (t o) -> p t o", o=1).broadcast_to([128, T_, n])

    gl = lg[:, :, 0:4]
    el = lg[:, :, 4:36].rearrange("p t (g e) -> p t g e", g=4)
    dv(lambda e: e.tensor_reduce(out=gmx, in_=gl, axis=AX.X, op=ALU.max), ["lg"], ["gmx"])
    dv(lambda e: e.tensor_tensor(out=oh, in0=gl, in1=col(gmx, 4), op=ALU.is_equal), ["lg", "gmx"], ["oh"])
    dv(lambda e: e.tensor_tensor(out=ge, in0=gl, in1=col(gmx, 4), op=ALU.subtract), ["lg", "gmx"], ["ge"])
    P.add("act", lambda e: e.activation(out=ge, in_=ge, func=AF.Exp), R=["ge"], W=["ge"])
    dv(lambda e: e.tensor_reduce(out=gsm, in_=ge, axis=AX.X, op=ALU.add), ["ge"], ["gsm"])
    dv(lambda e: e.reciprocal(out=pg, in_=gsm), ["gsm"], ["pg"])
    dv(lambda e: e.tensor_tensor(out=tmp48, in0=el, in1=oh.rearrange("p t (g o) -> p t g o", o=1).broadcast_to([128, T_, 4, 8]), op=ALU.mult),
       ["lg", "oh"], ["tmp48"])
    dv(lambda e: e.tensor_reduce(out=ein, in_=tmp48.rearrange("p t g e -> p t e g"), axis=AX.X, op=ALU.add), ["tmp48"], ["ein"])
    dv(lambda e: e.tensor_reduce(out=m1, in_=ein, axis=AX.X, op=ALU.max), ["ein"], ["m1"])
    dv(lambda e: e.tensor_tensor(out=mk1, in0=ein, in1=col(m1, 8), op=ALU.is_equal), ["ein", "m1"], ["mk1"])
    dv(lambda e: e.scalar_tensor_tensor(out=e2, in0=mk1, scalar=-1e30, in1=ein, op0=ALU.mult, op1=ALU.add), ["mk1", "ein"], ["e2"])
    dv(lambda e: e.tensor_reduce(out=m2, in_=e2, axis=AX.X, op=ALU.max), ["e2"], ["m2"])
    dv(lambda e: e.tensor_tensor(out=mk2, in0=e2, in1=col(m2, 8), op=ALU.is_equal), ["e2", "m2"], ["mk2"])
    dv(lambda e: e.tensor_tensor(out=dd, in0=m2, in1=m1, op=ALU.subtract), ["m1", "m2"], ["dd"])
    P.add("act", lambda e: e.activation(out=dd, in_=dd, func=AF.Exp), R=["dd"], W=["dd"])
    dv(lambda e: e.tensor_scalar_add(out=p1, in0=dd, scalar1=1.0), ["dd"], ["p1"])
    dv(lambda e: e.reciprocal(out=p1, in_=p1), ["p1"], ["p1"])
    dv(lambda e: e.tensor_tensor(out=p2, in0=dd, in1=p1, op=ALU.mult), ["dd", "p1"], ["p2"])
    dv(lambda e: e.tensor_tensor(out=p1, in0=p1, in1=pg, op=ALU.mult), ["p1", "pg"], ["p1"])
    dv(lambda e: e.tensor_tensor(out=p2, in0=p2, in1=pg, op=ALU.mult), ["p2", "pg"], ["p2"])
    dv(lambda e: e.tensor_tensor(out=we, in0=mk1, in1=col(p1, 8), op=ALU.mult), ["mk1", "p1"], ["we"])
    dv(lambda e: e.tensor_tensor(out=we2, in0=mk2, in1=col(p2, 8), op=ALU.mult), ["mk2", "p2"], ["we2"])
    dv(lambda e: e.tensor_tensor(out=we, in0=we, in1=we2, op=ALU.add), ["we", "we2"], ["we"])
    dv(lambda e: e.tensor_tensor(out=comb, in0=we.rearrange("p t (o e) -> p t o e", o=1).broadcast_to([128, T_, 4, 8]),
                                 in1=oh.rearrange("p t (g o) -> p t g o", o=1).broadcast_to([128, T_, 4, 8]), op=ALU.mult), ["we", "oh"], ["comb"])
    for j in range(NTO):
        pc_ = 5 + j % 2
        P.add("pe", lambda e, j=j, pc_=pc_: e.transpose(out=ps[pc_][0:32, 0:128], in_=comb[:, j, :, :].rearrange("p g e -> p (g e)"), identity=identf),
              R=["comb", "identf"], W=[psk[pc_]])
        dv(lambda e, j=j, pc_=pc_: e.tensor_copy(out=combT[:, j * 128:(j + 1) * 128], in_=ps[pc_][0:32, 0:128]), [psk[pc_]], ["combT"])
    P.add("sp", lambda e: e.dma_start(out=combT_d, in_=combT), R=["combT"], W=["combT_d"], dma="combT")
    if "comb" in dbg:
        tcb = dbg_out("comb", [32, OWN])
        P.add("sp", lambda e: e.dma_start(out=tcb, in_=combT), R=["combT"], W=["dbgo"], dma="dbg1")
    P.barrier()
    P.release()
    NSLOT = 4
    wg = [P.sb([128, 8, 256], BF16, "wg%d" % i) for i in range(NSLOT)]
    wu = [P.sb([128, 8, 256], BF16, "wu%d" % i) for i in range(NSLOT)]
    wd = [P.sb([128, 2, D], BF16, "wd%d" % i) for i in range(NSLOT)]
    CB = [P.sb([128, OWN], F32, "CB%d" % i) for i in range(2)]
    sa = [P.sb([128, 2, 512], F32, "sa%d" % i) for i in range(2)]
    sc = [P.sb([128, 2, 512], F32, "sc%d" % i) for i in range(2)]
    mTe = [P.sb([128, 2, 512], BF16, "mTe%d" % i) for i in range(2)]
    def load_expert(e_):
        sl = e_ % NSLOT
        P.add("pool", lambda e, e_=e_, sl=sl: e.dma_start(out=wg[sl], in_=I["w_gate"][e_].rearrange("(k p) n -> p k n", p=128)), W=["wg%d" % sl], dma="wg%d" % sl)
        P.add("pool", lambda e, e_=e_, sl=sl: e.dma_start(out=wu[sl], in_=I["w_up"][e_].rearrange("(k p) n -> p k n", p=128)), W=["wu%d" % sl], dma="wu%d" % sl)
        P.add("pool", lambda e, e_=e_, sl=sl: e.dma_start(out=wd[sl], in_=I["w_down"][e_].rearrange("(k p) n -> p k n", p=128)), W=["wd%d" % sl], dma="wd%d" % sl)

    for e_ in range(2):
        load_expert(e_)
    yb = 0
    for pr in range(16):
        for ee in range(2):
            if 2 * pr + 2 + ee < 32:
                load_expert(2 * pr + 2 + ee)
        for ee in range(2):
            e_ = 2 * pr + ee
            P.add("sp", lambda e, e_=e_, ee=ee: e.dma_start(out=CB[ee], in_=combT_d[e_:e_ + 1, :].partition_broadcast(128)),
                  R=["combT_d"], W=["CB%d" % ee], dma="CB%d" % ee)
        for c in range(4):
            for ee in range(2):
                e_ = 2 * pr + ee
                sl = e_ % NSLOT
                for f in range(2):
                    for k in range(8):
                        P.add("pe", lambda e, f=f, k=k, sl=sl, c=c: e.matmul(ps[f][:, :], lhsT=wg[sl][:, k, f * 128:(f + 1) * 128],
                                                                            rhs=tT[:, k, c * 512:(c + 1) * 512], start=(k == 0), stop=(k == 7)),
                              R=["wg%d" % sl, "tT"], W=[psk[f]])
                for f in range(2):
                    for k in range(8):
                        P.add("pe", lambda e, f=f, k=k, sl=sl, c=c: e.matmul(ps[2 + f][:, :], lhsT=wu[sl][:, k, f * 128:(f + 1) * 128],
                                                                            rhs=tT[:, k, c * 512:(c + 1) * 512], start=(k == 0), stop=(k == 7)),
                              R=["wu%d" % sl, "tT"], W=[psk[2 + f]])
                for f in range(2):
                    P.add("act", lambda e, f=f, ee=ee: e.activation(out=sa[ee][:, f, :], in_=ps[f][:, :], func=AF.Silu),
                          R=[psk[f]], W=["sa%d_%d" % (ee, f)])
                    P.add("pool", lambda e, f=f, ee=ee, c=c: e.tensor_tensor(out=sc[ee][:, f, :], in0=sa[ee][:, f, :],
                                                                              in1=CB[ee][:, c * 512:(c + 1) * 512], op=ALU.mult),
                          R=["sa%d_%d" % (ee, f), "CB%d" % ee], W=["sc%d_%d" % (ee, f)])
                    P.add("dve", lambda e, f=f, ee=ee: e.tensor_tensor(out=mTe[ee][:, f, :], in0=ps[2 + f][:, :], in1=sc[ee][:, f, :], op=ALU.mult),
                          R=[psk[2 + f], "sc%d_%d" % (ee, f)], W=["mTe%d" % ee])
            for t4 in range(4):
                j = c * 4 + t4
                for n in range(2):
                    py = 4 + (yb % 4)
                    yb += 1
                    cnt_mm = 0
                    for ee in range(2):
                        sl = (2 * pr + ee) % NSLOT
                        for f in range(2):
                            P.add("pe", lambda e, ee=ee, f=f, sl=sl, t4=t4, n=n, py=py, cnt_mm=cnt_mm: e.matmul(
                                ps[py][:, :], lhsT=mTe[ee][:, f, t4 * 128:(t4 + 1) * 128], rhs=wd[sl][:, f, n * 512:(n + 1) * 512],
                                start=(cnt_mm == 0), stop=(cnt_mm == 3)), R=["mTe%d" % ee, "wd%d" % sl], W=[psk[py]])
                            cnt_mm += 1
                    P.add("dve", lambda e, j=j, n=n, py=py: e.tensor_tensor(out=xres[:, j, n * 512:(n + 1) * 512], in0=ps[py][:, :],
                                                                             in1=xres[:, j, n * 512:(n + 1) * 512], op=ALU.add),
                          R=[psk[py], "xres"], W=["xres"])
    P.barrier()
    P.release()
    if "x3" in dbg:
        tx3 = dbg_out("x3", [OWN, D])
        P.add("sp", lambda e: e.dma_start(out=tx3.rearrange("(j p) c -> p j c", p=128), in_=xres), R=["xres"], W=["dbgo"], dma="dbg1")
        P.barrier()

    load_gain("final_norm_g")
    xo = [P.sb([128, D], F32, "xo%d" % i) for i in range(2)]
    for j in range(NTO):
        b = j % 2
        rms_norm(xres[:, j, :], D, gb, xo[b], ["xres"], ["xo%d" % b], stk="stg")
        P.add("sp", lambda e, j=j, b=b: e.dma_start(out=out_d[j * 128:(j + 1) * 128, :], in_=xo[b]),
              R=["xo%d" % b], W=["out_d%d" % b], dma="xo%d" % b)
    P.add("sp", None, R=["out_d0", "out_d1", "dbgo"])
    P.emit()
    return nc, dbg_d


def hyena_phase(nc, P, I, ps, psk, psb, U_d, hout_d, identf, identb, onesb, onesf, halfm, dbg, dbg_out, psall):
    NF = 64
    Hd = nc.dram_tensor("hy_H", [S, 2048], BF16, kind="Internal").ap()
    Bd = [nc.dram_tensor("hy_B%d" % i, [128, NF, 512], BF16, kind="Internal").ap() for i in range(2)]
    Kd = [nc.dram_tensor("hy_K%d" % i, [128, NF, 512], BF16, kind="Internal").ap() for i in range(2)]
    Btd = nc.dram_tensor("hy_Bt", [128, NF, 512], BF16, kind="Internal").ap()
    z1_d = nc.dram_tensor("hy_z1", [S, 512], F32, kind="Internal").ap()
    P.mark()
    Dt = P.sb([64, 64, 128], BF16, "Dt")
    Dinv = P.sb([128, 64, 64], BF16, "Dinv")
    F2a = P.sb([128, 128], BF16, "F2a")
    F2b = P.sb([128, 128], BF16, "F2b")
    Gm = P.sb([128, 128], BF16, "Gm")
    sgn = P.sb([128, 1], F32, "sgn")
    skipb = P.sb([128, 2, 512], F32, "skipb")
    P.add("pool", lambda e: e.dma_start(out=Dt, in_=I["hy_D0"].rearrange("a (b c) -> a b c", b=64)), W=["Dt"], dma="ht0")
    P.add("pool", lambda e: e.dma_start(out=F2a, in_=I["hy_F2a"]), W=["F2a"], dma="ht1")
    P.add("pool", lambda e: e.dma_start(out=F2b, in_=I["hy_F2b"]), W=["F2b"], dma="ht2")
    P.add("pool", lambda e: e.dma_start(out=Gm, in_=I["hy_G"]), W=["Gm"], dma="ht3")
    P.add("pool", lambda e: e.dma_start(out=Dinv, in_=I["hy_Dinv"].rearrange("a (b c) -> a b c", b=64)), W=["Dinv"], dma="ht4")
    P.add("sp", lambda e: e.dma_start(out=sgn, in_=I["hy_sgn"]), W=["sgn"], dma="c3")
    P.add("sp", lambda e: e.dma_start(out=skipb.rearrange("p a b -> p (a b)"), in_=I["hy_skip"].partition_broadcast(128)), W=["skipb"], dma="c4")

    P.mark()
    zT = P.sb([33, S], F32, "zT")
    g1T = P.sb([64, S], F32, "g1T")
    g2T = P.sb([64, S], F32, "g2T")
    hcols = P.sb([64, 4], F32, "hcols")
    w1 = P.sb([33, 64], F32, "w1")
    w2 = P.sb([64, 64], F32, "w2")
    w3 = P.sb([64, 2048], F32, "w3")
    b3r = P.sb([1, 2048], F32, "b3r")
    adec = P.sb([128, 2048], F32, "adec")
    nt01 = P.sb([128, NT], F32, "nt01")
    mpi = P.sb([128, 1], F32, "mpi")
    argt = P.sb([64, 512], F32, "argt")
    argm = P.sb([64, 512], F32, "argm")
    hfo = [P.sb([128, 2048], BF16, "hfo%d" % i) for i in range(2)]
    P.add("sp", lambda e: e.dma_start(out=zT, in_=I["hy_zT"]), W=["zT"], dma="hf0")
    P.add("sp", lambda e: e.dma_start(out=hcols, in_=I["hy_cols"]), W=["hcols"], dma="hf1")
    P.add("sp", lambda e: e.dma_start(out=w1, in_=I["hy_w1"]), W=["w1"], dma="hf2")
    P.add("sp", lambda e: e.dma_start(out=w2, in_=I["hy_w2"]), W=["w2"], dma="hf3")
    P.add("sp", lambda e: e.dma_start(out=w3, in_=I["hy_w3"]), W=["w3"], dma="hf4")
    P.add("sp", lambda e: e.dma_start(out=b3r, in_=I["hy_b3"]), W=["b3r"], dma="hf5")
    P.add("sp", lambda e: e.dma_start(out=adec, in_=I["hy_decay"].partition_broadcast(128)), W=["adec"], dma="hf6")
    P.add("sp", lambda e: e.dma_start(out=nt01, in_=I["hy_nt01"]), W=["nt01"], dma="hf7")
    P.add("pool", lambda e: e.memset(mpi, -math.pi), W=["mpi"])
    P.add("act", lambda e: e.activation(out=adec, in_=adec, func=AF.Abs), R=["adec"], W=["adec"])
    OFFS = math.pi + 16.0 * math.pi
    for (src, wt, kk, bcol, fcol, dst, nm) in ((zT, w1, 33, 0, 2, g1T, "g1T"), (g1T, w2, 64, 1, 3, g2T, "g2T")):
        for ch in range(8):
            pb_ = ch % 2
            P.add("pe", lambda e, src=src, wt=wt, kk=kk, ch=ch, pb_=pb_: e.matmul(ps[pb_][0:64, :], lhsT=wt[0:kk, :], rhs=src[0:kk, ch * 512:(ch + 1) * 512],
                                                                               start=True, stop=True), R=["zT", "g1T", "w1", "w2"], W=[psk[pb_]])
            P.add("dve", lambda e, pb_=pb_, bcol=bcol, fcol=fcol: e.tensor_scalar(out=argt, in0=ps[pb_][0:64, :], scalar1=hcols[:, bcol:bcol + 1],
                                                                                  scalar2=hcols[:, fcol:fcol + 1], op0=ALU.add, op1=ALU.mult),
                  R=[psk[pb_], "hcols"], W=["argt"])
            for _rep in range(2):
                P.add("dve", lambda e: e.tensor_scalar(out=argm, in0=argt, scalar1=math.pi, scalar2=None, op0=ALU.is_gt), R=["argt"], W=["argm"])
                P.add("dve", lambda e: e.scalar_tensor_tensor(out=argt, in0=argm, scalar=-2.0 * math.pi, in1=argt, op0=ALU.mult, op1=ALU.add),
                      R=["argm", "argt"], W=["argt"])
                P.add("dve", lambda e: e.tensor_scalar(out=argm, in0=argt, scalar1=-math.pi, scalar2=None, op0=ALU.is_lt), R=["argt"], W=["argm"])
                P.add("dve", lambda e: e.scalar_tensor_tensor(out=argt, in0=argm, scalar=2.0 * math.pi, in1=argt, op0=ALU.mult, op1=ALU.add),
                      R=["argm", "argt"], W=["argt"])
            P.add("act", lambda e, dst=dst, ch=ch: e.activation(out=dst[:, ch * 512:(ch + 1) * 512], in_=argt, func=AF.Sin),
                  R=["argt"], W=[nm])
    g2b = P.sb([64, S], BF16, "g2b")
    w3b = P.sb([64, 2048], BF16, "w3b")
    b3b = P.sb([1, 2048], BF16, "b3b")
    Etf = [P.sb([128, 2048], F32, "Etf%d" % i) for i in range(2)]
    P.add("act", lambda e: e.copy(out=g2b, in_=g2T), R=["g2T"], W=["g2b"])
    P.add("dve", lambda e: e.tensor_copy(out=w3b, in_=w3), R=["w3"], W=["w3b"])
    P.add("dve", lambda e: e.tensor_copy(out=b3b, in_=b3r), R=["b3r"], W=["b3b"])
    for i in range(NT):
        b = i % 2
        b0 = 4 * b
        for cg in range(4):
            pb_ = b0 + cg
            P.add("pe", lambda e, i=i, cg=cg, pb_=pb_: e.matmul(ps[pb_][:, :], lhsT=g2b[:, i * 128:(i + 1) * 128], rhs=w3b[:, cg * 512:(cg + 1) * 512],
                                                                 start=True, stop=False), R=["g2b", "w3b"], W=[psk[pb_]])
            P.add("pe", lambda e, cg=cg, pb_=pb_: e.matmul(ps[pb_][:, :], lhsT=onesb[0:1, 0:128], rhs=b3b[0:1, cg * 512:(cg + 1) * 512],
                                                            start=False, stop=True), R=["onesb", "b3b"], W=[psk[pb_]])
        P.add("act", lambda e, i=i, b=b: e.activation(out=Etf[b], in_=adec, func=AF.Exp, scale=nt01[:, i:i + 1]),
              R=["adec", "nt01"], W=["Etf%d" % b])
        for hf_ in range(2):
            P.add("dve", lambda e, hf_=hf_, b=b, b0=b0: e.tensor_tensor(out=hfo[b][:, hf_ * 1024:(hf_ + 1) * 1024],
                                                                      in0=psall[:, (b0 + 2 * hf_) * 512:(b0 + 2 * hf_ + 2) * 512],
                                                                      in1=Etf[b][:, hf_ * 1024:(hf_ + 1) * 1024], op=ALU.mult),
                  R=[psk[b0 + 2 * hf_], psk[b0 + 2 * hf_ + 1], "Etf%d" % b], W=["hfo%d" % b])
        if i == 0:
            for o in range(2):
                P.add("pool", lambda e, o=o: e.memset(hfo[0][0:1, o * 1024 + 512:o * 1024 + 1024], 0.0), R=[], W=["hfo0"])
        P.add("sp", lambda e, i=i, b=b: e.dma_start(out=Hd[i * 128:(i + 1) * 128, :], in_=hfo[b]), R=["hfo%d" % b], W=["Hd_%d" % i], dma="hfo%d" % b)
    P.barrier()
    P.release()
    if "hf" in dbg:
        thf = dbg_out("hf", [S, 2048])
        P.mark()
        tbf = P.sb([128, 2048], BF16, "tbf")
        tbf32 = P.sb([128, 2048], F32, "tbf32")
        for i in range(NT):
            P.add("sp", lambda e, i=i: e.dma_start(out=tbf, in_=Hd[i * 128:(i + 1) * 128, :]), R=["Hd_%d" % i], W=["tbf"], dma="dbg0")
            P.add("dve", lambda e: e.tensor_copy(out=tbf32, in_=tbf), R=["tbf"], W=["tbf32"])
            P.add("sp", lambda e, i=i: e.dma_start(out=thf[i * 128:(i + 1) * 128, :], in_=tbf32), R=["tbf32"], W=["dbgo"], dma="dbg1")
        P.barrier()
        P.release()

    ev = [0]
    HDK = ["Hd_%d" % i for i in range(NT)]
    Z1K = ["z1_d_%d" % i for i in range(8)]
    BD0K = ["Bd0_%d" % i for i in range(4)]
    BD1K = ["Bd1_%d" % i for i in range(4)]
    BTDK = ["Btd_%d" % i for i in range(8)]

    def evac(out, in_, Rk, Wk):
        ev[0] += 1
        if ev[0] % 2:
            P.add("act", lambda e: e.copy(out=out, in_=in_), R=Rk, W=Wk)
        else:
            P.add("dve", lambda e: e.tensor_copy(out=out, in_=in_), R=Rk, W=Wk)

    def stage1(src_view, cast, Bdst, bkey, srckeys):
        P.barrier()
        P.mark()
        xsb = [P.sb([64, 16, 512], BF16, "xsb%d" % i) for i in range(2)]
        Bsb = [P.sb([128, 16, 512], BF16, "Bsb%d" % i) for i in range(2)]
        def s1_load(ch):
            xb_ = ch % 2
            q = "pool" if cast else "sp"
            P.add(q, lambda e, ch=ch, xb_=xb_: e.dma_start(out=xsb[xb_], in_=src_view[:, ch * 16:(ch + 1) * 16, :]),
                  R=srckeys, W=["xsb%d" % xb_], dma="xsb%d" % xb_)

        s1_load(0)
        for ch in range(4):
            xb_ = ch % 2
            if ch + 1 < 4:
                s1_load(ch + 1)
            for g4 in range(4):
                b0 = 4 * (g4 % 2)
                for q4 in range(4):
                    s2l = g4 * 4 + q4
                    s2 = ch * 16 + s2l
                    P.add("pe", lambda e, s2=s2, s2l=s2l, xb_=xb_, pb_=b0 + q4: e.matmul(ps[pb_][:, :], lhsT=Dt[0:64, s2, :], rhs=xsb[xb_][0:64, s2l, :],
                                                                                        start=True, stop=True), R=["Dt", "xsb%d" % xb_], W=[psk[b0 + q4]])
                evac(Bsb[xb_][:, g4 * 4:(g4 + 1) * 4, :].rearrange("p a b -> p (a b)"), psall[:, b0 * 512:(b0 + 4) * 512],
                     [psk[b0 + k] for k in range(4)], ["Bsb%d_%d" % (xb_, g4)])
            P.add("sp", lambda e, ch=ch, xb_=xb_: e.dma_start(out=Bdst[:, ch * 16:(ch + 1) * 16, :], in_=Bsb[xb_]),
                  R=["Bsb%d_%d" % (xb_, k) for k in range(4)], W=["%s_%d" % (bkey, ch)], dma="Bsb%d" % xb_)
        P.barrier()
        P.release()

    def blocked(ap2d):
        return ap2d.rearrange("(s1 s2) c -> s1 s2 c", s2=64)

    for o in range(2):
        stage1(blocked(Hd[:, o * 1024:o * 1024 + 512]), False, Bd[0], "Bd0", HDK)
        stage1(blocked(Hd[:, o * 1024 + 512:o * 1024 + 1024]), False, Bd[1], "Bd1", HDK)
        P.mark()
        BTf = [P.sb([128, 8, 512], BF16, "BTf%d" % i) for i in range(2)]
        BTb = [P.sb([128, 8, 512], BF16, "BTb%d" % i) for i in range(2)]
        xfs = [P.sb([128, 1024], F32, "xfs%d" % i) for i in range(2)]
        kst = [P.sb([128, 1024], F32, "kst%d" % i) for i in range(2)]
        Kc = [P.sb([128, 8, 512], BF16, "Kc%d" % i) for i in range(2)]
        def f2_load(fc):
            cb_ = fc % 2
            for r in range(2):
                P.add("sp", lambda e, r=r, fc=fc, cb_=cb_: e.dma_start(
                    out=BTf[cb_][r * 64:(r + 1) * 64, :, :], in_=Bd[0][r * 64 + fc * 8:r * 64 + fc * 8 + 8, :, :].rearrange("f s c -> s f c")),
                    R=BD0K, W=["BTf%d" % cb_], dma="BTf%d_%d" % (cb_, r))
                P.add("sp", lambda e, r=r, fc=fc, cb_=cb_: e.dma_start(
                    out=BTb[cb_][r * 64:(r + 1) * 64, :, :], in_=Bd[1][r * 64 + fc * 8:r * 64 + fc * 8 + 8, :, :].rearrange("f s c -> s f c")),
                    R=BD1K, W=["BTb%d" % cb_], dma="BTb%d_%d" % (cb_, r))

        f2_load(0)
        for fc in range(8):
            cb_ = fc % 2
            if fc + 1 < 8:
                f2_load(fc + 1)
            for gq in range(4):
                t_ = gq % 2
                fa, fb = 2 * t_, 4 + 2 * t_
                for q2 in range(2):
                    f1l = gq * 2 + q2
                    P.add("pe", lambda e, f1l=f1l, cb_=cb_, pb_=fa + q2: e.matmul(ps[pb_][:, :], lhsT=F2a, rhs=BTf[cb_][:, f1l, :], start=True, stop=True),
                          R=["F2a", "BTf%d" % cb_], W=[psk[fa + q2]])
                    P.add("pe", lambda e, f1l=f1l, cb_=cb_, pb_=fb + q2: e.matmul(ps[pb_][:, :], lhsT=F2a, rhs=BTb[cb_][:, f1l, :], start=True, stop=True),
                          R=["F2a", "BTb%d" % cb_], W=[psk[fb + q2]])
                P.add("act", lambda e, fa=fa, t_=t_: e.copy(out=xfs[t_], in_=psall[:, fa * 512:(fa + 2) * 512]), R=[psk[fa], psk[fa + 1]], W=["xfs%d" % t_])
                P.add("dve", lambda e, fb=fb, t_=t_: e.scalar_tensor_tensor(out=kst[t_], in0=psall[:, fb * 512:(fb + 2) * 512], scalar=sgn[:, 0:1], in1=xfs[t_],
                                                                           op0=ALU.mult, op1=ALU.add),
                      R=[psk[fb], psk[fb + 1], "xfs%d" % t_, "sgn"], W=["kst%d" % t_])
                for q2 in range(2):
                    f1l = gq * 2 + q2
                    P.add("pool", lambda e, t_=t_, cb_=cb_, f1l=f1l, q2=q2, o=o: e.tensor_tensor(out=Kc[cb_][0:64, f1l, :], in0=kst[t_][0:64, q2 * 512:(q2 + 1) * 512],
                                                                                          in1=skipb[0:64, o, :], op=ALU.add),
                          R=["kst%d" % t_, "skipb"], W=["Kc%d_a%d" % (cb_, f1l)])
                P.add("act", lambda e, t_=t_, cb_=cb_, gq=gq: e.copy(out=Kc[cb_][64:128, gq * 2:gq * 2 + 2, :].rearrange("p a b -> p (a b)"), in_=kst[t_][64:128, :]),
                      R=["kst%d" % t_], W=["Kc%d_b%d" % (cb_, gq)])
            P.add("sp", lambda e, fc=fc, cb_=cb_, o=o: e.dma_start(out=Kd[o][:, fc * 8:(fc + 1) * 8, :], in_=Kc[cb_]),
                  R=["Kc%d_a%d" % (cb_, k) for k in range(8)] + ["Kc%d_b%d" % (cb_, k) for k in range(4)], W=["Kd%d_%d" % (o, fc)], dma="Kc%d" % cb_)
        P.barrier()
        P.release()
    if "kf" in dbg:
        tkf = dbg_out("kf", [2, 128, NF * 512])
        P.mark()
        tk16 = P.sb([128, 8, 512], BF16, "tk16")
        tk32 = P.sb([128, 8, 512], F32, "tk32")
        for o in range(2):
            for fc in range(8):
                P.add("sp", lambda e, o=o, fc=fc: e.dma_start(out=tk16, in_=Kd[o][:, fc * 8:(fc + 1) * 8, :]), R=["Kd%d_%d" % (o, fc)], W=["tk16"], dma="dbg0")
                P.add("dve", lambda e: e.tensor_copy(out=tk32, in_=tk16), R=["tk16"], W=["tk32"])
                P.add("sp", lambda e, o=o, fc=fc: e.dma_start(out=tkf[o][:, fc * 4096:(fc + 1) * 4096], in_=tk32.rearrange("p a b -> p (a b)")),
                      R=["tk32"], W=["dbgo"], dma="dbg1")
        P.barrier()
        P.release()

    P.add("pool", lambda e: e.dma_start(out=Dt, in_=I["hy_Dc"].rearrange("a (b c) -> a b c", b=64)), W=["Dt"], dma="ht0")
    for o in range(2):
        if o == 0:
            stage1(blocked(U_d[:, 1024:1536]), True, Bd[0], "Bd0", ["U_d"])
        else:
            stage1(blocked(z1_d), True, Bd[0], "Bd0", Z1K)
        P.mark()
        BT = [P.sb([128, 8, 512], BF16, "BT%d" % i) for i in range(2)]
        KA = [P.sb([128, 8, 512], BF16, "KA%d" % i) for i in range(2)]
        KB = [P.sb([128, 8, 512], BF16, "KB%d" % i) for i in range(2)]
        ta = [P.sb([128, 1024], F32, "ta%d" % i) for i in range(2)]
        tb2 = [P.sb([128, 1024], F32, "tb2%d" % i) for i in range(2)]
        Yc = [P.sb([128, 1024], BF16, "Yc%d" % i) for i in range(2)]
        Btsb = [P.sb([128, 8, 512], BF16, "Btsb%d" % i) for i in range(2)]
        def c2_load(fc):
            cb_ = fc % 2
            for r in range(2):
                P.add("sp", lambda e, r=r, fc=fc, cb_=cb_: e.dma_start(
                    out=BT[cb_][r * 64:(r + 1) * 64, :, :], in_=Bd[0][r * 64 + fc * 8:r * 64 + fc * 8 + 8, :, :].rearrange("f s c -> s f c")),
                    R=BD0K, W=["BT%d" % cb_], dma="BT%d_%d" % (cb_, r))
                P.add("sp", lambda e, r=r, fc=fc, cb_=cb_, o=o: e.dma_start(out=KA[cb_][r * 64:(r + 1) * 64, :, :], in_=Kd[o][0:64, fc * 8:(fc + 1) * 8, :]),
                      R=["Kd%d_%d" % (o, fc)], W=["KA%d" % cb_], dma="KA%d_%d" % (cb_, r))
                P.add("sp", lambda e, r=r, fc=fc, cb_=cb_, o=o: e.dma_start(out=KB[cb_][r * 64:(r + 1) * 64, :, :], in_=Kd[o][64:128, fc * 8:(fc + 1) * 8, :]),
                      R=["Kd%d_%d" % (o, fc)], W=["KB%d" % cb_], dma="KB%d_%d" % (cb_, r))

        c2_load(0)
        for fc in range(8):
            cb_ = fc % 2
            if fc + 1 < 8:
                c2_load(fc + 1)
            for gq in range(4):
                t_ = gq % 2
                fa, fb = 2 * t_, 4 + 2 * t_
                f0 = gq * 2
                for q2 in range(2):
                    f1l = f0 + q2
                    P.add("pe", lambda e, f1l=f1l, cb_=cb_, pb_=fa + q2: e.matmul(ps[pb_][:, :], lhsT=F2a, rhs=BT[cb_][:, f1l, :], start=True, stop=True),
                          R=["F2a", "BT%d" % cb_], W=[psk[fa + q2]])
                    P.add("pe", lambda e, f1l=f1l, cb_=cb_, pb_=fb + q2: e.matmul(ps[pb_][:, :], lhsT=F2b, rhs=BT[cb_][:, f1l, :], start=True, stop=True),
                          R=["F2b", "BT%d" % cb_], W=[psk[fb + q2]])
                P.add("dve", lambda e, fa=fa, t_=t_, cb_=cb_, f0=f0: e.tensor_tensor(out=ta[t_], in0=psall[:, fa * 512:(fa + 2) * 512],
                                                                                   in1=KA[cb_][:, f0:f0 + 2, :].rearrange("p a b -> p (a b)"), op=ALU.mult),
                      R=[psk[fa], psk[fa + 1], "KA%d" % cb_], W=["ta%d" % t_])
                P.add("dve", lambda e, fb=fb, t_=t_, cb_=cb_, f0=f0: e.tensor_tensor(out=tb2[t_], in0=psall[:, fb * 512:(fb + 2) * 512],
                                                                                   in1=KB[cb_][:, f0:f0 + 2, :].rearrange("p a b -> p (a b)"), op=ALU.mult),
                      R=[psk[fb], psk[fb + 1], "KB%d" % cb_], W=["tb2%d" % t_])
                P.add("pool", lambda e, t_=t_: e.tensor_tensor(out=Yc[t_], in0=ta[t_], in1=tb2[t_], op=ALU.add),
                      R=["ta%d" % t_, "tb2%d" % t_], W=["Yc%d" % t_])
                for q2 in range(2):
                    P.add("pe", lambda e, t_=t_, q2=q2, pb_=fa + q2: e.matmul(ps[pb_][:, :], lhsT=Gm, rhs=Yc[t_][:, q2 * 512:(q2 + 1) * 512], start=True, stop=True),
                          R=["Gm", "Yc%d" % t_], W=[psk[fa + q2]])
                P.add("act", lambda e, fa=fa, cb_=cb_, f0=f0: e.copy(out=Btsb[cb_][:, f0:f0 + 2, :].rearrange("p a b -> p (a b)"), in_=psall[:, fa * 512:(fa + 2) * 512]),
                      R=[psk[fa], psk[fa + 1]], W=["Btsb%d_%d" % (cb_, gq)])
            P.add("sp", lambda e, fc=fc, cb_=cb_: e.dma_start(out=Btd[:, fc * 8:(fc + 1) * 8, :], in_=Btsb[cb_]),
                  R=["Btsb%d_%d" % (cb_, k) for k in range(4)], W=["Btd_%d" % fc], dma="Btsb%d" % cb_)
        P.barrier()
        P.release()
        P.mark()
        BtT = [P.sb([128, 8, 512], BF16, "BtT%d" % i) for i in range(2)]
        gch = [P.sb([64, 8, 512], F32, "gch%d" % i) for i in range(2)]
        zo = [P.sb([64, 8, 512], F32, "zo%d" % i) for i in range(2)]
        M = 64 if o == 0 else 32
        gcol = 0 if o == 0 else 512
        dst = blocked(z1_d) if o == 0 else blocked(hout_d)
        dkey = "z1_d" if o == 0 else "hout_d"
        def i1_load(tc):
            cb_ = tc % 2
            for r in range(2):
                P.add("sp", lambda e, r=r, tc=tc, cb_=cb_: e.dma_start(
                    out=BtT[cb_][r * 64:(r + 1) * 64, :, :], in_=Btd[r * 64 + tc * 8:r * 64 + tc * 8 + 8, :, :].rearrange("t f c -> f t c")),
                    R=BTDK, W=["BtT%d" % cb_], dma="BtT%d_%d" % (cb_, r))
            P.add("sp", lambda e, tc=tc, cb_=cb_, M=M, gcol=gcol: e.dma_start(
                out=gch[cb_][0:M, :, :], in_=blocked(U_d[0:M * 64, gcol:gcol + 512])[:, tc * 8:(tc + 1) * 8, :]),
                R=["U_d"], W=["gch%d" % cb_], dma="gch%d" % cb_)

        i1_load(0)
        for tc in range(8):
            cb_ = tc % 2
            if tc + 1 < 8:
                i1_load(tc + 1)
            for g4 in range(2):
                b0 = 4 * ((2 * tc + g4) % 2)
                for q4 in range(4):
                    t2l = g4 * 4 + q4
                    t2 = tc * 8 + t2l
                    P.add("pe", lambda e, t2=t2, t2l=t2l, cb_=cb_, pb_=b0 + q4, M=M: e.matmul(ps[pb_][0:M, :], lhsT=Dinv[:, t2, 0:M], rhs=BtT[cb_][:, t2l, :],
                                                                                             start=True, stop=True), R=["Dinv", "BtT%d" % cb_], W=[psk[b0 + q4]])
                P.add("dve", lambda e, g4=g4, cb_=cb_, b0=b0, M=M: e.tensor_tensor(out=zo[cb_][0:M, g4 * 4:(g4 + 1) * 4, :].rearrange("p a b -> p (a b)"),
                                                                                 in0=psall[0:M, b0 * 512:(b0 + 4) * 512],
                                                                                 in1=gch[cb_][0:M, g4 * 4:(g4 + 1) * 4, :].rearrange("p a b -> p (a b)"), op=ALU.mult),
                      R=[psk[b0 + k] for k in range(4)] + ["gch%d" % cb_], W=["zo%d_%d" % (cb_, g4)])
            P.add("sp", lambda e, tc=tc, cb_=cb_, M=M, dst=dst: e.dma_start(out=dst[0:M, tc * 8:(tc + 1) * 8, :], in_=zo[cb_][0:M, :, :]),
                  R=["zo%d_%d" % (cb_, k) for k in range(2)], W=["%s_%d" % (dkey, tc)], dma="zo%d" % cb_)
        P.barrier()
        P.release()
    P.barrier()
    P.release()
    if "z1" in dbg:
        tz1 = dbg_out("z1", [S, 512])
        P.mark()
        tz = P.sb([128, 512], F32, "tz")
        for i in range(NT):
            P.add("sp", lambda e, i=i: e.dma_start(out=tz, in_=z1_d[i * 128:(i + 1) * 128, :]), R=Z1K, W=["tz"], dma="dbg0")
            P.add("sp", lambda e, i=i: e.dma_start(out=tz1[i * 128:(i + 1) * 128, :], in_=tz), R=["tz"], W=["dbgo"], dma="dbg1")
        P.barrier()
        P.release()
    if "h_out" in dbg:
        tho = dbg_out("h_out", [OWN, 512])
        P.mark()
        tz_ = P.sb([128, 512], F32, "tz_")
        for i in range(NTO):
            P.add("sp", lambda e, i=i: e.dma_start(out=tz_, in_=hout_d[i * 128:(i + 1) * 128, :]), R=["hout_d_%d" % k for k in range(8)], W=["tz_"], dma="dbg0")
            P.add("sp", lambda e, i=i: e.dma_start(out=tho[i * 128:(i + 1) * 128, :], in_=tz_), R=["tz_"], W=["dbgo"], dma="dbg1")
        P.barrier()
        P.release()


def hyena_tables(half):
    N = 8192
    f1 = np.arange(64, dtype=np.float64)
    s1 = np.arange(64, dtype=np.float64)
    s2 = np.arange(64, dtype=np.float64)
    th = 2 * np.pi * (f1[None, None, :] + 0.5) * (64 * s1[:, None, None] + s2[None, :, None]) / N
    D0 = np.concatenate([np.cos(th), -np.sin(th)], axis=2)
    perm = (np.arange(64) + 32 * half) % 64
    Dc = D0[perm]
    f2 = np.arange(64, dtype=np.float64)
    ph = 2 * np.pi * np.outer(s2, f2) / 64
    c, s_ = np.cos(ph), np.sin(ph)
    F2a = np.block([[c, -s_], [s_, c]])
    F2b = np.block([[s_, c], [-c, s_]])
    G = np.block([[c, s_], [-s_, c]])
    thi = 2 * np.pi * (f1[:, None, None] + 0.5) * (64 * s1[None, None, :] + s2[None, :, None]) / N
    Dinv0 = np.concatenate([np.cos(thi), -np.sin(thi)], axis=0) * (2.0 / N)
    Dinv = Dinv0[:, :, perm]
    sgn = np.ones((128, 1)); sgn[64:] = -1
    L = S
    t = np.arange(L, dtype=np.float32)
    t01 = t / np.float32(L)
    bands = np.linspace(1e-4, 15, 16, dtype=np.float32)
    ang = (np.float32(2.0 * math.pi) * t[:, None] * bands[None, :] / np.float32(L)).astype(np.float32)
    z = np.concatenate([t01[:, None], np.cos(ang), -np.sin(ang)], axis=-1).astype(np.float32)
    nt01 = (-t01).reshape(NT, 128).T
    f = np.float32
    return {
        "hy_D0": np.ascontiguousarray(D0.reshape(64, 64 * 128).astype(f)), "hy_Dc": np.ascontiguousarray(Dc.reshape(64, 64 * 128).astype(f)),
        "hy_F2a": np.ascontiguousarray(F2a.astype(f)), "hy_F2b": np.ascontiguousarray(F2b.astype(f)), "hy_G": np.ascontiguousarray(G.astype(f)),
        "hy_Dinv": np.ascontiguousarray(Dinv.reshape(128, 64 * 64).astype(f)), "hy_sgn": sgn.astype(f),
        "hy_zT": np.ascontiguousarray(z.T), "hy_nt01": np.ascontiguousarray(nt01.astype(f)),
    }


def host_inputs(inputs, core):
    b, half = divmod(core, 2)
    f32 = np.float32
    x = np.asarray(inputs["x"], dtype=f32)[b]
    own = slice(half * OWN, (half + 1) * OWN)
    oth = slice((1 - half) * OWN, (2 - half) * OWN)
    pos = np.concatenate([np.arange(S)[own], np.arange(S)[oth]])
    m = {}
    m["x_rot"] = np.ascontiguousarray(np.concatenate([x[own], x[oth]], axis=0))
    m["mem_b"] = np.ascontiguousarray(np.asarray(inputs["mem"], dtype=f32)[b])
    for k in ("mix_norm_g", "q_norm_g", "kv_norm_g", "hy_conv_b", "attn_out_g", "hy_out_g", "cross_norm_g",
              "mem_norm_g", "ffn_norm_g"):
        m[k] = np.ascontiguousarray(np.asarray(inputs[k], dtype=f32).reshape(1, -1))
    m["final_norm_g"] = np.ascontiguousarray(np.asarray(inputs["final_norm_g"], dtype=f32).reshape(1, -1))
    for k in ("w_in", "w_uq", "w_ukv", "hy_conv_w", "w_out", "w_mq", "w_mkv", "w_mo"):
        m[k] = np.ascontiguousarray(np.asarray(inputs[k], dtype=f32)[0])
    m["w_route"] = np.ascontiguousarray(np.concatenate([np.asarray(inputs["w_route_group"], f32)[0],
                                                        np.asarray(inputs["w_route_expert"], f32)[0]], axis=1))
    m["b_route"] = np.ascontiguousarray(np.concatenate([np.asarray(inputs["b_route_group"], f32)[0],
                                                        np.asarray(inputs["b_route_expert"], f32)[0]], axis=0).reshape(1, 36))
    m["w_gate"] = np.ascontiguousarray(np.asarray(inputs["w_gate"], f32)[0].reshape(32, D, 256))
    m["w_up"] = np.ascontiguousarray(np.asarray(inputs["w_up"], f32)[0].reshape(32, D, 256))
    m["w_down"] = np.ascontiguousarray(np.asarray(inputs["w_down"], f32)[0].reshape(32, 256, D))
    m["ident"] = np.eye(128, dtype=f32)
    inv = (10000.0 ** (-np.arange(16, dtype=np.float64) / 16)).astype(f32)
    ang = pos.astype(f32)[:, None] * inv[None, :]
    cs = np.concatenate([np.cos(ang), np.sin(ang)], axis=1).astype(f32)
    m["rope_cs"] = np.ascontiguousarray(cs.reshape(NT, 128, 32).transpose(1, 0, 2).reshape(128, NT * 32))
    m.update(hyena_tables(half))
    m["hy_cols"] = np.ascontiguousarray(np.stack([np.asarray(inputs["hy_b1"], f32)[0], np.asarray(inputs["hy_b2"], f32)[0],
                                                  np.asarray(inputs["hy_freq"], f32)[0, 0], np.asarray(inputs["hy_freq"], f32)[0, 1]], axis=1))
    for k in ("hy_w1", "hy_w2", "hy_w3"):
        m[k] = np.ascontiguousarray(np.asarray(inputs[k], f32)[0])
    m["hy_b3"] = np.ascontiguousarray(np.asarray(inputs["hy_b3"], f32).reshape(1, 2048))
    m["hy_decay"] = np.ascontiguousarray(np.asarray(inputs["hy_decay"], f32).reshape(1, 2048))
    m["hy_skip"] = np.ascontiguousarray(np.asarray(inputs["hy_skip"], f32).reshape(1, 1024))
    cw_ = np.asarray(inputs["hy_conv_w"], f32)[0]
    m["hy_conv_wT"] = np.ascontiguousarray(cw_.reshape(3, 12, 128).transpose(2, 1, 0).reshape(128, 36))
    m["hy_conv_bT"] = np.ascontiguousarray(np.asarray(inputs["hy_conv_b"], f32)[0].reshape(12, 128).T)
    hm = np.zeros((128, 2), f32)
    hm[:, 0] = half
    hm[:, 1] = 1 - half
    m["halfmask"] = hm
    return m


def kernel(**inputs):
    n = 8
    nc, _ = build()
    in_maps = [host_inputs(inputs, c) for c in range(n)]
    res = run_bass_kernel_spmd(nc, in_maps, core_ids=list(range(n)))
    out = np.zeros((4, S, D), np.float32)
    for c in range(n):
        b, half = divmod(c, 2)
        out[b, half * OWN:(half + 1) * OWN] = res.results[c]["out"]
    return out
```

```python
import math
import os
import contextlib
import numpy as np
import concourse.bass as bass
import concourse.mybir as mybir
from concourse.bass_utils import run_bass_kernel_spmd

F32 = mybir.dt.float32
BF16 = mybir.dt.bfloat16
AF = mybir.ActivationFunctionType
ALU = mybir.AluOpType
AX = mybir.AxisListType
ENGS = ("pe", "act", "dve", "pool", "sp")

D = 1024
S = 4096
OWN = 2048
NT = 32
NTO = 16
EPS = 1e-6
HD = 96
NH = 8


class Op:
    __slots__ = ("eng", "fn", "deps", "dma", "flag", "seq", "idx", "dmaval")

    def __init__(self, eng, fn, dma):
        self.eng = eng
        self.fn = fn
        self.deps = set()
        self.dma = dma
        self.flag = False
        self.seq = 0
        self.dmaval = 0


class Prog:
    ARENA_WORDS = 52000

    def __init__(self, nc):
        self.nc = nc
        self.ops = []
        self.lastw = {}
        self.readers = {}
        self.dma_count = {}
        self.sb_off = 0
        self.sb_marks = []
        self.arena = None
        self.dma_slots = {}

    def sb(self, shape, dtype, name=None):
        if self.arena is None:
            self.arena = self.nc.alloc_sbuf_tensor("arena", [128, self.ARENA_WORDS], F32)
        esz = 4 if dtype == F32 else 2
        nel = int(np.prod(shape[1:]))
        nwords = (nel * esz + 3) // 4
        nwords = (nwords + 15) // 16 * 16
        o = self.sb_off
        self.sb_off += nwords
        assert self.sb_off <= self.ARENA_WORDS, ("SBUF overflow", self.sb_off * 4, name)
        v = self.arena[0:shape[0], o:o + nwords]
        if esz == 2:
            v = v.bitcast(dtype)[:, 0:nel]
        else:
            v = v[:, 0:nel]
        if len(shape) > 2:
            names = " ".join("a%d" % i for i in range(len(shape) - 1))
            kw = {"a%d" % i: int(shape[i + 1]) for i in range(len(shape) - 1)}
            v = v.rearrange("p (%s) -> p %s" % (names, names), **kw)
        return v

    def mark(self):
        self.sb_marks.append(self.sb_off)

    def release(self):
        self.sb_off = self.sb_marks.pop()

    def add(self, eng, fn, R=(), W=(), dma=None):
        if dma is not None:
            slots = self.dma_slots.setdefault(eng, {"free": [], "n": 0, "map": {}})
            if dma not in slots["map"]:
                if slots["free"]:
                    slots["map"][dma] = slots["free"].pop()
                else:
                    slots["map"][dma] = slots["n"]
                    slots["n"] += 1
            dma = (eng, slots["map"][dma])
        op = Op(eng, fn, dma)
        op.idx = len(self.ops)
        if eng != "pe":
            psr = [r for r in R if isinstance(r, str) and r.startswith("ps") and r[2:].isdigit()]
            if psr:
                R = [r for r in R if r not in psr]
                W = list(W) + psr
        deps = set()
        for r in R:
            lw = self.lastw.get(r)
            if lw is not None:
                deps.add(lw)
        for w in W:
            lw = self.lastw.get(w)
            if lw is not None:
                deps.add(lw)
            for rd in self.readers.get(w, ()):
                deps.add(rd)
        if dma is not None:
            k = ("__dmasem", dma)
            lw = self.lastw.get(k)
            if lw is not None:
                deps.add(lw)
            self.lastw[k] = op
            self.dma_count[dma] = self.dma_count.get(dma, 0) + 1
            op.dmaval = 16 * self.dma_count[dma]
        deps.discard(op)
        for d in deps:
            if d.dma is None and d.eng == "pe" and eng == "pe" and dma is None:
                continue
            op.deps.add(d)
            d.flag = True
        for r in R:
            self.readers.setdefault(r, []).append(op)
        for w in W:
            self.lastw[w] = op
            self.readers[w] = []
        self.ops.append(op)
        return op

    def barrier(self):
        fr = {}
        dmas = set()
        allops = set(self.lastw.values())
        for v in self.readers.values():
            allops.update(v)
        for o in allops:
            if o.dma is not None:
                dmas.add(o)
            elif o.eng not in fr or fr[o.eng].idx < o.idx:
                fr[o.eng] = o
        for e in ENGS:
            op = Op(e, None, None)
            op.idx = len(self.ops)
            for d in list(fr.values()) + list(dmas):
                op.deps.add(d)
                d.flag = True
            self.ops.append(op)
        self.lastw = {}
        self.readers = {}
        for sl in self.dma_slots.values():
            sl["free"].extend(sl["map"].values())
            sl["map"].clear()

    def emit(self):
        nc = self.nc
        with contextlib.ExitStack() as st:
            esem = {e: st.enter_context(nc.semaphore("s_" + e)) for e in ENGS}
            dsem = {}
            for k in self.dma_count:
                dsem[k] = st.enter_context(nc.semaphore("d_%d" % len(dsem)))
            cnt = {e: 0 for e in ENGS}
            for op in self.ops:
                if op.dma is None and op.flag:
                    cnt[op.eng] += 1
                    op.seq = cnt[op.eng]
            byeng = {e: [o for o in self.ops if o.eng == e] for e in ENGS}
            if os.environ.get("KDEBUG"):
                print("sem counts", cnt, "ndma sems", len(dsem), "nops", {e: len(v) for e, v in byeng.items()})
            block = st.enter_context(nc.Block())

            def run(e, eng):
                waited = {}
                for op in byeng[e]:
                    need = {}
                    for d in op.deps:
                        if d.dma is not None:
                            s, v = dsem[d.dma], d.dmaval
                        else:
                            s, v = esem[d.eng], d.seq
                        key = id(s)
                        if waited.get(key, 0) >= v:
                            continue
                        if key not in need or need[key][1] < v:
                            need[key] = (s, v)
                    for key, (s, v) in need.items():
                        eng.wait_ge(s, v)
                        waited[key] = v
                    if op.fn is None:
                        continue
                    ins = op.fn(eng)
                    if op.dma is not None:
                        ins.then_inc(dsem[op.dma], 16)
                    elif op.flag:
                        ins.then_inc(esem[e], 1)

            @block.tensor
            def _(eng):
                run("pe", eng)

            @block.scalar
            def _(eng):
                run("act", eng)

            @block.vector
            def _(eng):
                run("dve", eng)

            @block.gpsimd
            def _(eng):
                run("pool", eng)

            @block.sync
            def _(eng):
                run("sp", eng)


INPUT_SHAPES = {
    "x_rot": [S, D], "mem_b": [256, D],
    "mix_norm_g": [1, D], "w_in": [D, 1952], "q_norm_g": [1, 256], "kv_norm_g": [1, 128],
    "w_uq": [256, 768], "w_ukv": [128, 1024], "hy_conv_w": [3, 1536], "hy_conv_b": [1, 1536],
    "attn_out_g": [1, 512], "hy_out_g": [1, 512], "w_out": [D, D],
    "cross_norm_g": [1, D], "mem_norm_g": [1, D], "w_mq": [D, D], "w_mkv": [D, 2 * D], "w_mo": [D, D],
    "ffn_norm_g": [1, D], "w_route": [D, 36], "b_route": [1, 36],
    "w_gate": [32, D, 256], "w_up": [32, D, 256], "w_down": [32, 256, D], "final_norm_g": [1, D],
    "ident": [128, 128], "rope_cs": [128, NT * 32], "halfmask": [128, 2],
    "hy_D0": [64, 64 * 128], "hy_Dc": [64, 64 * 128], "hy_F2a": [128, 128], "hy_F2b": [128, 128], "hy_G": [128, 128],
    "hy_Dinv": [128, 64 * 64], "hy_sgn": [128, 1], "hy_zT": [33, S], "hy_nt01": [128, NT],
    "hy_cols": [64, 4], "hy_w1": [33, 64], "hy_w2": [64, 64], "hy_w3": [64, 2048], "hy_b3": [1, 2048], "hy_decay": [1, 2048],
    "hy_skip": [1, 1024], "hy_conv_wT": [128, 36], "hy_conv_bT": [128, 12],
}


def build(stop=None, dbg=()):
    nc = bass.Bass("TRN2", target_bir_lowering=False)
    I = {k: nc.dram_tensor(k, v, F32, kind="ExternalInput").ap() for k, v in INPUT_SHAPES.items()}
    out_d = nc.dram_tensor("out", [OWN, D], F32, kind="ExternalOutput").ap()
    dbg_d = {}
    U_d = nc.dram_tensor("U_scr", [S, 1536], F32, kind="Internal").ap()
    hout_d = nc.dram_tensor("hout_scr", [OWN, 512], F32, kind="Internal").ap()
    combT_d = nc.dram_tensor("combT_scr", [32, OWN], F32, kind="Internal").ap()

    P = Prog(nc)
    psall = nc.alloc_psum_tensor("psall", [128, 4096], F32)
    ps = [psall[:, i * 512:(i + 1) * 512] for i in range(8)]
    psk = ["ps%d" % i for i in range(8)]

    def psb(i):
        return ps[i].bitcast(BF16)

    def dbg_out(name, shape):
        t = nc.dram_tensor("dbg_" + name, shape, F32, kind="ExternalOutput").ap()
        dbg_d[name] = t
        return t

    cnt = [0]

    def uid(s):
        cnt[0] += 1
        return "%s_%d" % (s, cnt[0])

    identf = P.sb([128, 128], F32, "identf")
    identb = P.sb([128, 128], BF16, "identb")
    halfm = P.sb([128, 2], F32, "halfm")
    st = P.sb([128, 8], F32, "st")
    junk = P.sb([128, 1024], F32, "junk")
    gb = P.sb([128, 1024], F32, "gb")
    onesb = P.sb([128, 128], BF16, "onesb")
    onesf = P.sb([128, 128], F32, "onesf")
    P.add("sp", lambda e: e.dma_start(out=identf, in_=I["ident"]), W=["identf"], dma="c0")
    P.add("sp", lambda e: e.dma_start(out=halfm, in_=I["halfmask"]), W=["halfm"], dma="c1")
    P.add("dve", lambda e: e.tensor_copy(out=identb, in_=identf), R=["identf"], W=["identb"])
    epst = P.sb([128, 1], F32, "epst")
    P.add("pool", lambda e: e.memset(epst, EPS), W=["epst"])
    P.add("pool", lambda e: e.memset(onesb, 1.0), W=["onesb"])
    P.add("pool", lambda e: e.memset(onesf, 1.0), W=["onesf"])

    def pipeline(n, stages):
        K_ = len(stages)
        for s_ in range(n + K_ - 1):
            for k_ in range(K_):
                i_ = s_ - k_
                if 0 <= i_ < n:
                    stages[k_](i_)

    def load_gain(name, n=D, key="gb"):
        P.add("sp", lambda e: e.dma_start(out=gb[:, 0:n], in_=I[name].partition_broadcast(128)), W=[key], dma="gain")

    st_tiles = {}
    for _n in ("st", "stq", "stk", "sta", "sth", "stm", "stx", "stf", "stg"):
        st_tiles[_n] = P.sb([128, 4], F32, "st_" + _n)

    def rms_norm(src, n, gview, out_bf, Rk, Wk, stk="st"):
        if stk not in st_tiles:
            st_tiles[stk] = P.sb([128, 4], F32, "st_" + stk)
        st = st_tiles[stk]
        P.add("act", lambda e: e.activation(out=junk[:, 0:n], in_=src, func=AF.Square, accum_out=st[:, 0:1]),
              R=Rk, W=["junk", stk + "0"])
        P.add("act", lambda e: e.activation(out=st[:, 2:3], in_=st[:, 0:1], func=AF.Sqrt, scale=1.0 / n, bias=epst[:, 0:1]),
              R=[stk + "0", "epst"], W=[stk + "2"])
        P.add("dve", lambda e: e.reciprocal(out=st[:, 3:4], in_=st[:, 2:3]), R=[stk + "2"], W=[stk + "3"])
        P.add("dve", lambda e: e.scalar_tensor_tensor(out=out_bf, in0=src, scalar=st[:, 3:4], in1=gview,
                                                      op0=ALU.mult, op1=ALU.mult),
              R=list(Rk) + [stk + "3", "gb"], W=Wk)

    P.mark()
    hqT = P.sb([128, 2, OWN], BF16, "hqT")
    hkvT = P.sb([128, S], BF16, "hkvT")
    krot = P.sb([128, NT, 32], F32, "krot")
    ropecs = P.sb([128, NT, 32], F32, "ropecs")
    P.add("sp", lambda e: e.dma_start(out=ropecs.rearrange("p a b -> p (a b)"), in_=I["rope_cs"]), W=["ropecs"], dma="c2")

    P.mark()
    hT = P.sb([128, 8, 2, OWN + 2], BF16, "hT")
    P.mark()
    xt = [P.sb([128, D], F32, "xt%d" % i) for i in range(4)]
    xn = [P.sb([128, D], BF16, "xn%d" % i) for i in range(4)]
    load_gain("mix_norm_g")
    def a1S0(i):
        b = i % 4
        P.add("sp", lambda e, i=i, b=b: e.dma_start(out=xt[b], in_=I["x_rot"][i * 128:(i + 1) * 128, :]),
              W=["xt%d" % b], dma="xt%d" % b)
        rms_norm(xt[b], D, gb, xn[b], ["xt%d" % b], ["xn%d" % b])

    def a1S1(i):
        b = i % 4
        pb = i % 4
        for k in range(8):
            P.add("pe", lambda e, k=k, b=b, pb=pb: e.transpose(out=psb(pb)[:, k * 128:(k + 1) * 128],
                                                                 in_=xn[b][:, k * 128:(k + 1) * 128], identity=identb),
                  R=["xn%d" % b, "identb"], W=[psk[pb]])

    def a1S2(i):
        pb = i % 4
        seg, j = divmod(i, NTO)
        dst = hT[:, :, seg, 1 + j * 128:1 + (j + 1) * 128]
        src = psb(pb).rearrange("p (k t) -> p k t", k=8)
        if i % 2 == 0:
            P.add("act", lambda e, dst=dst, src=src: e.copy(out=dst, in_=src), R=[psk[pb]], W=["hT"])
        else:
            P.add("dve", lambda e, dst=dst, src=src: e.tensor_copy(out=dst, in_=src), R=[psk[pb]], W=["hT"])

    pipeline(NT, [a1S0, a1S1, a1S2])
    for (ds, dc, ss_, sc, m) in ((0, 0, 1, OWN, 0), (0, OWN + 1, 1, 1, 1), (1, 0, 0, OWN, 1), (1, OWN + 1, 0, 1, 0)):
        P.add("dve", lambda e, ds=ds, dc=dc, ss_=ss_, sc=sc, m=m: e.tensor_scalar_mul(
            out=hT[:, :, ds, dc:dc + 1], in0=hT[:, :, ss_, sc:sc + 1], scalar1=halfm[:, m:m + 1]),
            R=["hT", "halfm"], W=["hT"])

    P.barrier()
    P.release()
    P.mark()
    w_mla = P.sb([128, 8, 416], BF16, "w_mla")
    P.add("pool", lambda e: e.dma_start(out=w_mla, in_=I["w_in"][:, 0:416].rearrange("(k p) n -> p k n", p=128)),
          W=["w_mla"], dma="w0")
    gq = P.sb([128, 256], F32, "gq")
    gkv = P.sb([128, 128], F32, "gkv")
    P.add("sp", lambda e: e.dma_start(out=gq, in_=I["q_norm_g"].partition_broadcast(128)), W=["gq"], dma="c3")
    P.add("sp", lambda e: e.dma_start(out=gkv, in_=I["kv_norm_g"].partition_broadcast(128)), W=["gkv"], dma="c4")
    hqn = [P.sb([128, 256], BF16, "hqn%d" % i) for i in range(3)]
    hkvn = [P.sb([128, 128], BF16, "hkvn%d" % i) for i in range(3)]
    tmp16 = P.sb([128, 4, 16], F32, "tmp16")
    def a2S0(i):
        seg, j = divmod(i, NTO)
        pb = i % 3
        for k in range(8):
            P.add("pe", lambda e, k=k, pb=pb, seg=seg, j=j: e.matmul(
                ps[pb][:, 0:416], lhsT=hT[:, k, seg, 1 + j * 128:1 + (j + 1) * 128], rhs=w_mla[:, k, :],
                start=(k == 0), stop=(k == 7)), R=["hT", "w_mla"], W=[psk[pb]])

    def a2S1(i):
        seg, j = divmod(i, NTO)
        pb = i % 3
        b = i % 3
        if seg == 0:
            rms_norm(ps[pb][:, 0:256], 256, gq, hqn[b], [psk[pb], "gq"], ["hqn%d" % b], stk="stq")
        rms_norm(ps[pb][:, 256:384], 128, gkv, hkvn[b], [psk[pb], "gkv"], ["hkvn%d" % b], stk="stk")
        x1 = ps[pb][:, 384:400]
        x2 = ps[pb][:, 400:416]
        c = ropecs[:, i, 0:16]
        s_ = ropecs[:, i, 16:32]
        P.add("dve", lambda e, x1=x1, c=c: e.tensor_tensor(out=tmp16[:, 0, :], in0=x1, in1=c, op=ALU.mult), R=[psk[pb], "ropecs"], W=["t16a"])
        P.add("dve", lambda e, x2=x2, s_=s_: e.tensor_tensor(out=tmp16[:, 1, :], in0=x2, in1=s_, op=ALU.mult), R=[psk[pb], "ropecs"], W=["t16b"])
        P.add("dve", lambda e, x1=x1, s_=s_: e.tensor_tensor(out=tmp16[:, 2, :], in0=x1, in1=s_, op=ALU.mult), R=[psk[pb], "ropecs"], W=["t16c"])
        P.add("dve", lambda e, x2=x2, c=c: e.tensor_tensor(out=tmp16[:, 3, :], in0=x2, in1=c, op=ALU.mult), R=[psk[pb], "ropecs"], W=["t16d"])
        P.add("dve", lambda e, i=i: e.tensor_tensor(out=krot[:, i, 0:16], in0=tmp16[:, 0, :], in1=tmp16[:, 1, :], op=ALU.subtract),
              R=["t16a", "t16b"], W=["krot"])
        P.add("dve", lambda e, i=i: e.tensor_tensor(out=krot[:, i, 16:32], in0=tmp16[:, 2, :], in1=tmp16[:, 3, :], op=ALU.add),
              R=["t16c", "t16d"], W=["krot"])

    def a2S2(i):
        seg, j = divmod(i, NTO)
        b = i % 3
        pt = 4 + i % 2
        if seg == 0:
            for k in range(2):
                P.add("pe", lambda e, k=k, b=b, pt=pt: e.transpose(out=psb(pt)[:, k * 128:(k + 1) * 128],
                                                                     in_=hqn[b][:, k * 128:(k + 1) * 128], identity=identb),
                      R=["hqn%d" % b, "identb"], W=[psk[pt]])
        P.add("pe", lambda e, b=b, pt=pt: e.transpose(out=psb(pt)[:, 256:384], in_=hkvn[b], identity=identb),
              R=["hkvn%d" % b, "identb"], W=[psk[pt]])

    def a2S3(i):
        seg, j = divmod(i, NTO)
        pt = 4 + i % 2
        if seg == 0:
            P.add("act", lambda e, pt=pt, j=j: e.copy(out=hqT[:, :, j * 128:(j + 1) * 128],
                                                       in_=psb(pt)[:, 0:256].rearrange("p (k t) -> p k t", k=2)),
                  R=[psk[pt]], W=["hqT"])
        P.add("act", lambda e, pt=pt, i=i: e.copy(out=hkvT[:, i * 128:(i + 1) * 128], in_=psb(pt)[:, 256:384]),
              R=[psk[pt]], W=["hkvT"])

    pipeline(NT, [a2S0, a2S1, a2S2, a2S3])

    P.barrier()
    P.release()
    w_hy = P.sb([128, 8, 512], BF16, "w_hy")
    cwT = P.sb([128, 12, 3], F32, "cwT")
    cbT = P.sb([128, 12], F32, "cbT")
    uT = [P.sb([128, 2, OWN + 2], F32, "uT0")] * 2
    tTc = P.sb([128, 4, 2, OWN], F32, "tTc")
    tmpP = P.sb([128, OWN], F32, "tmpP")
    uo = [P.sb([128, 512], F32, "uo%d" % i) for i in range(2)]
    P.add("sp", lambda e: e.dma_start(out=cwT.rearrange("p a b -> p (a b)"), in_=I["hy_conv_wT"]), W=["cwT"], dma="cw0")
    P.add("sp", lambda e: e.dma_start(out=cbT, in_=I["hy_conv_bT"]), W=["cbT"], dma="cw1")
    nev = 0
    for c3 in range(3):
        c0 = 416 + c3 * 512
        P.add("pool", lambda e, c0=c0: e.dma_start(out=w_hy, in_=I["w_in"][:, c0:c0 + 512].rearrange("(k p) n -> p k n", p=128)),
              W=["w_hy"], dma="w1")
        for c4 in range(4):
            ct = c3 * 4 + c4
            ub = 0
            u_ = uT[ub]
            for seg in range(2):
                for tc in range(4):
                    pb = 2 + (nev % 4)
                    for k in range(8):
                        P.add("pe", lambda e, k=k, pb=pb, seg=seg, tc=tc, c4=c4: e.matmul(
                            ps[pb][:, :], lhsT=w_hy[:, k, c4 * 128:(c4 + 1) * 128], rhs=hT[:, k, seg, 1 + tc * 512:1 + (tc + 1) * 512],
                            start=(k == 0), stop=(k == 7)), R=["hT", "w_hy"], W=[psk[pb]])
                    dst = u_[:, seg, 1 + tc * 512:1 + (tc + 1) * 512]
                    if nev % 2 == 0:
                        P.add("act", lambda e, pb=pb, dst=dst: e.copy(out=dst, in_=ps[pb][:, :]), R=[psk[pb]], W=["uT%d" % ub])
                    else:
                        P.add("dve", lambda e, pb=pb, dst=dst: e.tensor_copy(out=dst, in_=ps[pb][:, :]), R=[psk[pb]], W=["uT%d" % ub])
                    nev += 1
            for (ds, dc, ss_, sc, m) in ((0, 0, 1, OWN, 0), (0, OWN + 1, 1, 1, 1), (1, 0, 0, OWN, 1), (1, OWN + 1, 0, 1, 0)):
                P.add("dve", lambda e, u_=u_, ds=ds, dc=dc, ss_=ss_, sc=sc, m=m: e.tensor_scalar_mul(
                    out=u_[:, ds, dc:dc + 1], in0=u_[:, ss_, sc:sc + 1], scalar1=halfm[:, m:m + 1]),
                    R=["uT%d" % ub, "halfm"], W=["uT%d" % ub])
            for seg in range(2):
                t_ = tTc[:, c4, seg, :]
                P.add("act", lambda e, u_=u_, seg=seg, t_=t_, ct=ct: e.activation(out=t_, in_=u_[:, seg, 1:OWN + 1], func=AF.Identity,
                                                                               scale=cwT[:, ct, 1:2], bias=cbT[:, ct:ct + 1]),
                      R=["uT%d" % ub, "cwT", "cbT"], W=["tTc_%d_%d" % (c4, seg)])
                P.add("dve", lambda e, u_=u_, seg=seg, t_=t_, ct=ct: e.scalar_tensor_tensor(out=t_, in0=u_[:, seg, 0:OWN], scalar=cwT[:, ct, 0:1], in1=t_,
                                                                                         op0=ALU.mult, op1=ALU.add),
                      R=["uT%d" % ub, "cwT", "tTc_%d_%d" % (c4, seg)], W=["tTc_%d_%d" % (c4, seg)])
                P.add("act", lambda e, u_=u_, seg=seg, ct=ct: e.activation(out=tmpP, in_=u_[:, seg, 2:OWN + 2], func=AF.Copy, scale=cwT[:, ct, 2:3]),
                      R=["uT%d" % ub, "cwT"], W=["tmpP"])
                P.add("pool", lambda e, t_=t_: e.tensor_tensor(out=t_, in0=t_, in1=tmpP, op=ALU.add),
                      R=["tmpP", "tTc_%d_%d" % (c4, seg)], W=["tTc_%d_%d" % (c4, seg)])
        for i in range(NT):
            b = i % 2
            seg, j = divmod(i, NTO)
            pb = 6 + b
            for c4 in range(4):
                P.add("pe", lambda e, c4=c4, seg=seg, j=j, pb=pb: e.transpose(out=ps[pb][:, c4 * 128:(c4 + 1) * 128],
                                                                           in_=tTc[:, c4, seg, j * 128:(j + 1) * 128], identity=identf),
                      R=["tTc_%d_%d" % (c4, seg), "identf"], W=[psk[pb]])
            if b == 0:
                P.add("act", lambda e, pb=pb, b=b: e.copy(out=uo[b], in_=ps[pb][:, :]), R=[psk[pb]], W=["uo%d" % b])
            else:
                P.add("dve", lambda e, pb=pb, b=b: e.tensor_copy(out=uo[b], in_=ps[pb][:, :]), R=[psk[pb]], W=["uo%d" % b])
            P.add("sp", lambda e, i=i, b=b, c3=c3: e.dma_start(out=U_d[i * 128:(i + 1) * 128, c3 * 512:(c3 + 1) * 512], in_=uo[b]),
                  R=["uo%d" % b], W=["U_d"], dma="uo%d" % b)
    P.barrier()
    P.release()

    if stop == "A0":
        P.add("sp", None, R=[])
        P.emit()
        return nc, dbg_d
    if "uc" in dbg:
        tu = dbg_out("uc", [S, 1536])
        P.mark()
        tb = P.sb([128, 1536], F32, "dbgt")
        for i in range(NT):
            P.add("sp", lambda e, i=i: e.dma_start(out=tb, in_=U_d[i * 128:(i + 1) * 128, :]), R=["U_d"], W=["dbgt"], dma="dbg0")
            P.add("sp", lambda e, i=i: e.dma_start(out=tu[i * 128:(i + 1) * 128, :], in_=tb), R=["dbgt"], W=["dbgo"], dma="dbg1")
        P.barrier()
        P.release()

    if stop == "A":
        P.add("sp", None, R=["dbgo"])
        P.emit()
        return nc, dbg_d
    aout_d = nc.dram_tensor("aout_scr", [OWN, 512], F32, kind="Internal").ap()
    P.mark()
    G4 = 8
    KT = P.sb([128, G4, S], BF16, "KT")
    QT = P.sb([128, G4, OWN], BF16, "QT")
    Vaug = P.sb([128, NT, G4, 68], BF16, "Vaug")
    w_ukv = P.sb([128, 1024], BF16, "w_ukv")
    w_uq = P.sb([128, 2, 768], BF16, "w_uq")
    P.add("pool", lambda e: e.dma_start(out=w_ukv, in_=I["w_ukv"]), W=["w_ukv"], dma="w0")
    P.add("pool", lambda e: e.dma_start(out=w_uq, in_=I["w_uq"].rearrange("(k p) n -> p k n", p=128)), W=["w_uq"], dma="w1")
    Kaug = [P.sb([128, G4, 100], BF16, "Kaug%d" % i) for i in range(2)]
    Kaug2 = P.sb([128, G4, 100], BF16, "Kaug2")
    ksq2 = [P.sb([128, G4, 96], F32, "ksq2_%d" % i) for i in range(2)]
    ksq = ksq2[0]
    kn2 = P.sb([128, G4], F32, "kn2")
    kmax = P.sb([128, G4], F32, "kmax")
    kb = P.sb([128, 4], F32, "kb")
    qs2 = [P.sb([128, G4, 96], F32, "qs%d" % i) for i in range(2)]
    qn2 = [P.sb([128, G4], F32, "qn%d" % i) for i in range(2)]
    qt42 = [P.sb([128, 4, G4, 16], F32, "qt4_0")] * 2
    PT = [P.sb([128, 512], BF16, "PT%d" % i) for i in range(3)]
    oTs = P.sb([65, 512], F32, "oTs")
    rden = P.sb([128, 4], F32, "rden")
    astage = [P.sb([128, 4, 512], F32, "astage0")] * 2
    scale = HD ** -0.5
    it = 0
    for g in range(1):
        P.add("pool", lambda e: e.memset(Vaug.rearrange("p a b c -> p (a b c)"), 1.0), W=["Vaug"])
        for b in range(2):
            P.add("pool", lambda e, b=b: e.memset(Kaug[b].rearrange("p a b -> p (a b)"), 1.0), W=["Kaug%d" % b])
        P.add("pool", lambda e: e.memset(kmax, 0.0), W=["kmax"])
        ND = 3
        KaugN = [Kaug[0], Kaug[1], Kaug2]
        P.add("pool", lambda e: e.memset(Kaug2.rearrange("p a b -> p (a b)"), 1.0), W=["Kaug2"])

        def kS0(i):
            pbk = 2 * (i % 2)
            for hh in range(2):
                P.add("pe", lambda e, hh=hh, pbk=pbk, i=i: e.matmul(ps[pbk + hh][:, :], lhsT=hkvT[:, i * 128:(i + 1) * 128],
                                                                     rhs=w_ukv[:, hh * 512:(hh + 1) * 512], start=True, stop=True),
                      R=["hkvT", "w_ukv"], W=[psk[pbk + hh]])

        def kS1(i):
            pbk = 2 * (i % 2)
            kb_ = i % ND
            Ka = KaugN[kb_]
            v = psall[:, pbk * 512:(pbk + 2) * 512].rearrange("p (h c) -> p h c", h=8)
            P.add("act", lambda e, v=v, i=i: e.copy(out=Vaug[:, i, :, 0:64], in_=v[:, :, 64:128]), R=[psk[pbk], psk[pbk + 1]], W=["Vaug"])
            P.add("dve", lambda e, v=v, Ka=Ka: e.tensor_copy(out=Ka[:, :, 0:64], in_=v[:, :, 0:64]), R=[psk[pbk], psk[pbk + 1]], W=["Kaug%d" % kb_])
            P.add("pool", lambda e, Ka=Ka, i=i: e.tensor_copy(out=Ka[:, :, 64:96], in_=krot[:, i:i + 1, :].broadcast_to([128, G4, 32])),
                  R=["krot"], W=["Kaug%d" % kb_])
            P.add("dve", lambda e, Ka=Ka, kb_=kb_: e.tensor_tensor(out=ksq2[kb_ % 2], in0=Ka[:, :, 0:96], in1=Ka[:, :, 0:96], op=ALU.mult),
                  R=["Kaug%d" % kb_], W=["ksq2_%d" % (kb_ % 2)])
            P.add("dve", lambda e, kb_=kb_: e.tensor_reduce(out=kn2, in_=ksq2[kb_ % 2], axis=AX.X, op=ALU.add), R=["ksq2_%d" % (kb_ % 2)], W=["kn2"])
            P.add("dve", lambda e: e.tensor_tensor(out=kmax, in0=kmax, in1=kn2, op=ALU.max), R=["kn2", "kmax"], W=["kmax"])

        def kS2(i):
            kb_ = i % ND
            Ka = KaugN[kb_]
            pt = 4 + i % 2
            for h in range(G4):
                P.add("pe", lambda e, h=h, Ka=Ka, pt=pt: e.transpose(out=psb(pt)[0:97, h * 128:(h + 1) * 128], in_=Ka[:, h, 0:97], identity=identb),
                      R=["Kaug%d" % kb_, "identb"], W=[psk[pt]])
            P.add("act", lambda e, pt=pt, i=i: e.copy(out=KT[0:97, :, i * 128:(i + 1) * 128],
                                                       in_=psb(pt)[0:97, 0:G4 * 128].rearrange("p (h t) -> p h t", h=G4)),
                  R=[psk[pt]], W=["KT"])

        pipeline(NT, [kS0, kS1, kS2])
        P.add("dve", lambda e: e.tensor_reduce(out=kb[:, 1:2], in_=kmax, axis=AX.X, op=ALU.max), R=["kmax"], W=["kb1"])
        P.add("pe", lambda e: e.transpose(out=ps[6][0:1, 0:128], in_=kb[:, 1:2], identity=identf), R=["kb1", "identf"], W=[psk[6]])
        P.add("dve", lambda e: e.tensor_reduce(out=kb[0:1, 2:3], in_=ps[6][0:1, 0:128], axis=AX.X, op=ALU.max), R=[psk[6]], W=["kb2"])
        P.add("pe", lambda e: e.matmul(ps[7][:, 0:1], lhsT=onesf[0:1, 0:128], rhs=kb[0:1, 2:3], start=True, stop=True),
              R=["kb2", "onesf"], W=[psk[7]])
        P.add("act", lambda e: e.sqrt(out=kb[:, 0:1], in_=ps[7][:, 0:1]), R=[psk[7]], W=["kb0"])
        QaugN = KaugN

        def qS0(j):
            pa = 2 * (j % 2)
            for (pq, c0, ncol) in ((pa, 0, 480), (pa + 1, 480, 288)):
                for k in range(2):
                    P.add("pe", lambda e, pq=pq, c0=c0, ncol=ncol, k=k, j=j: e.matmul(
                        ps[pq][:, 0:ncol], lhsT=hqT[:, k, j * 128:(j + 1) * 128], rhs=w_uq[:, k, c0:c0 + ncol],
                        start=(k == 0), stop=(k == 1)), R=["hqT", "w_uq"], W=[psk[pq]])

        def qS1(j):
            pa = 2 * (j % 2)
            d_ = j % 2
            qb_ = j % ND
            Qa = QaugN[qb_]
            qs_ = qs2[d_]
            q4 = qt42[d_]
            P.add("act", lambda e, pa=pa, qs_=qs_: e.mul(out=qs_[:, 0:5, :], in_=ps[pa][:, 0:480].rearrange("p (h c) -> p h c", h=5), mul=scale),
                  R=[psk[pa]], W=["qs%d" % d_])
            P.add("act", lambda e, pa=pa, qs_=qs_: e.mul(out=qs_[:, 5:8, :], in_=ps[pa + 1][:, 0:288].rearrange("p (h c) -> p h c", h=3), mul=scale),
                  R=[psk[pa + 1]], W=["qs%d" % d_])
            c = ropecs[:, j:j + 1, 0:16].broadcast_to([128, G4, 16])
            s_ = ropecs[:, j:j + 1, 16:32].broadcast_to([128, G4, 16])
            x1 = qs_[:, :, 64:80]
            x2 = qs_[:, :, 80:96]
            P.add("dve", lambda e, x1=x1, c=c, q4=q4: e.tensor_tensor(out=q4[:, 0], in0=x1, in1=c, op=ALU.mult), R=["qs%d" % d_, "ropecs"], W=["qt4a"])
            P.add("dve", lambda e, x2=x2, s_=s_, q4=q4: e.tensor_tensor(out=q4[:, 1], in0=x2, in1=s_, op=ALU.mult), R=["qs%d" % d_, "ropecs"], W=["qt4b"])
            P.add("pool", lambda e, x1=x1, s_=s_, q4=q4: e.tensor_tensor(out=q4[:, 2], in0=x1, in1=s_, op=ALU.mult), R=["qs%d" % d_, "ropecs"], W=["qt4c"])
            P.add("pool", lambda e, x2=x2, c=c, q4=q4: e.tensor_tensor(out=q4[:, 3], in0=x2, in1=c, op=ALU.mult), R=["qs%d" % d_, "ropecs"], W=["qt4d"])
            P.add("dve", lambda e, qs_=qs_, q4=q4: e.tensor_tensor(out=qs_[:, :, 64:80], in0=q4[:, 0], in1=q4[:, 1], op=ALU.subtract),
                  R=["qt4a", "qt4b"], W=["qs%d" % d_])
            P.add("dve", lambda e, qs_=qs_, q4=q4: e.tensor_tensor(out=qs_[:, :, 80:96], in0=q4[:, 2], in1=q4[:, 3], op=ALU.add),
                  R=["qt4c", "qt4d"], W=["qs%d" % d_])
            P.add("dve", lambda e, qs_=qs_, d_=d_: e.tensor_tensor(out=ksq2[d_], in0=qs_, in1=qs_, op=ALU.mult), R=["qs%d" % d_], W=["ksq2_%d" % d_])
            P.add("dve", lambda e, d_=d_: e.tensor_reduce(out=qn2[d_], in_=ksq2[d_], axis=AX.X, op=ALU.add), R=["ksq2_%d" % d_], W=["qn%d" % d_])
            P.add("act", lambda e, d_=d_: e.sqrt(out=qn2[d_], in_=qn2[d_]), R=["qn%d" % d_], W=["qn%d" % d_])
            P.add("dve", lambda e, Qa=Qa, d_=d_: e.tensor_scalar(out=Qa[:, :, 96:97], in0=qn2[d_].rearrange("p (h o) -> p h o", o=1),
                                                                 scalar1=kb[:, 0:1], scalar2=-1.0, op0=ALU.mult, op1=ALU.mult),
                  R=["qn%d" % d_, "kb0"], W=["Kaug%d" % qb_])
            P.add("act", lambda e, Qa=Qa, qs_=qs_: e.copy(out=Qa[:, :, 0:96], in_=qs_), R=["qs%d" % d_], W=["Kaug%d" % qb_])

        def qS2(j):
            qb_ = j % ND
            Qa = QaugN[qb_]
            pt = 4 + j % 2
            for h in range(G4):
                P.add("pe", lambda e, h=h, Qa=Qa, pt=pt: e.transpose(out=psb(pt)[0:97, h * 128:(h + 1) * 128], in_=Qa[:, h, 0:97], identity=identb),
                      R=["Kaug%d" % qb_, "identb"], W=[psk[pt]])
            P.add("dve", lambda e, pt=pt, j=j: e.tensor_copy(out=QT[0:97, :, j * 128:(j + 1) * 128],
                                                             in_=psb(pt)[0:97, 0:G4 * 128].rearrange("p (h t) -> p h t", h=G4)),
                  R=[psk[pt]], W=["QT"])

        pipeline(NTO, [qS0, qS1, qS2])
        items = [(qc, h, kt) for qc in range(4) for h in range(G4) for kt in range(NT)]
        LA = 2

        def emit_scores(idx):
            qc, h, kt = items[idx]
            pb_ = idx % 3
            P.add("pe", lambda e, h=h, qc=qc, kt=kt, pb_=pb_: e.matmul(
                ps[pb_][:, :], lhsT=KT[0:97, h, kt * 128:(kt + 1) * 128], rhs=QT[0:97, h, qc * 512:(qc + 1) * 512],
                start=True, stop=True), R=["KT", "QT"], W=[psk[pb_]])

        def emit_epilogue(qc, h):
            po = 6 + h % 2
            sb_ = qc % 2
            P.add("dve", lambda e, po=po: e.tensor_copy(out=oTs, in_=ps[po][0:65, :]), R=[psk[po]], W=["oTs"])
            for t4 in range(4):
                pt = 3 + (t4 % 2)
                P.add("pe", lambda e, t4=t4, pt=pt: e.transpose(out=ps[pt][:, 0:65], in_=oTs[:, t4 * 128:(t4 + 1) * 128], identity=identf[0:65, 0:65]),
                      R=["oTs", "identf"], W=[psk[pt]])
                P.add("dve", lambda e, pt=pt, t4=t4: e.reciprocal(out=rden[:, t4:t4 + 1], in_=ps[pt][:, 64:65]), R=[psk[pt]], W=["rden%d" % t4])
                P.add("dve", lambda e, pt=pt, t4=t4, h=h, sb_=sb_: e.tensor_scalar_mul(
                    out=astage[sb_][:, t4, h * 64:(h + 1) * 64], in0=ps[pt][:, 0:64], scalar1=rden[:, t4:t4 + 1]),
                    R=[psk[pt], "rden%d" % t4], W=["astage0"])
            if h == G4 - 1:
                P.add("sp", lambda e, qc=qc, g=g, sb_=sb_: e.dma_start(
                    out=aout_d[qc * 512:(qc + 1) * 512, :].rearrange("(t p) c -> p t c", p=128), in_=astage[sb_]),
                    R=["astage0"], W=["aout_d"], dma="ast0")

        for idx in range(min(LA, len(items))):
            emit_scores(idx)
        pending = None
        for idx, (qc, h, kt) in enumerate(items):
            pb_ = idx % 3
            po = 6 + h % 2
            P.add("act", lambda e, pb_=pb_: e.activation(out=PT[pb_], in_=ps[pb_][:, :], func=AF.Exp),
                  R=[psk[pb_]], W=["PT%d" % pb_])
            if idx + LA < len(items):
                emit_scores(idx + LA)
            P.add("pe", lambda e, h=h, kt=kt, pb_=pb_, po=po: e.matmul(
                ps[po][0:65, :], lhsT=Vaug[:, kt, h, 0:65], rhs=PT[pb_], start=(kt == 0), stop=(kt == NT - 1)),
                R=["Vaug", "PT%d" % pb_], W=[psk[po]])
            if pending is not None and kt == 3:
                emit_epilogue(*pending)
                pending = None
            if kt == NT - 1:
                pending = (qc, h)
        if pending is not None:
            emit_epilogue(*pending)
    P.barrier()
    P.release()
    P.release()

    if "a_out" in dbg:
        ta = dbg_out("a_out", [OWN, 512])
        P.mark()
        tba = P.sb([128, NTO, 512], F32, "dbgt2")
        P.add("sp", lambda e: e.dma_start(out=tba, in_=aout_d.rearrange("(j p) c -> p j c", p=128)), R=["aout_d"], W=["dbgt2"], dma="dbg0")
        P.add("sp", lambda e: e.dma_start(out=ta.rearrange("(j p) c -> p j c", p=128), in_=tba), R=["dbgt2"], W=["dbgo"], dma="dbg1")
        P.barrier()
        P.release()

    if stop == "attn":
        P.add("sp", None, R=[])
        P.emit()
        return nc, dbg_d

    if "hout_in" in dbg:
        hin = nc.dram_tensor("dbg_hout_in", [OWN, 512], F32, kind="ExternalInput").ap()
        P.mark()
        tbh = P.sb([128, NTO, 512], F32, "tbh")
        P.add("sp", lambda e: e.dma_start(out=tbh, in_=hin.rearrange("(j p) c -> p j c", p=128)), W=["tbh"], dma="dbg0")
        P.add("sp", lambda e: e.dma_start(out=hout_d.rearrange("(j p) c -> p j c", p=128), in_=tbh), R=["tbh"], W=["hout_d"], dma="dbg1")
        P.barrier()
        P.release()
    else:
        hyena_phase(nc, P, I, ps, psk, psb, U_d, hout_d, identf, identb, onesb, onesf, halfm, dbg, dbg_out, psall)

    if stop == "C":
        P.add("sp", None, R=[])
        P.emit()
        return nc, dbg_d
    xres = P.sb([128, NTO, D], F32, "xres")
    P.add("sp", lambda e: e.dma_start(out=xres, in_=I["x_rot"][0:OWN, :].rearrange("(j p) c -> p j c", p=128)), W=["xres"], dma="xres")
    P.mark()
    w_out = P.sb([128, 8, D], BF16, "w_out")
    P.add("pool", lambda e: e.dma_start(out=w_out, in_=I["w_out"].rearrange("(k p) n -> p k n", p=128)), W=["w_out"], dma="w0")
    P.add("sp", lambda e: e.dma_start(out=gb[:, 0:512], in_=I["attn_out_g"].partition_broadcast(128)), W=["gb"], dma="gain")
    P.add("sp", lambda e: e.dma_start(out=gb[:, 512:1024], in_=I["hy_out_g"].partition_broadcast(128)), W=["gb"], dma="gain")
    ND_ = 3
    mixin = [P.sb([128, D], F32, "mixin%d" % i) for i in range(ND_)]
    mixbf = [P.sb([128, D], BF16, "mixbf%d" % i) for i in range(ND_)]
    mT = [P.sb([128, 8, 128], BF16, "mT%d" % i) for i in range(ND_)]

    def dS0(j):
        b = j % ND_
        P.add("sp", lambda e, j=j, b=b: e.dma_start(out=mixin[b][:, 0:512], in_=aout_d[j * 128:(j + 1) * 128, :]),
              R=["aout_d"], W=["mixin%d" % b], dma="mixa%d" % b)
        P.add("sp", lambda e, j=j, b=b: e.dma_start(out=mixin[b][:, 512:1024], in_=hout_d[j * 128:(j + 1) * 128, :]),
              R=["hout_d"] + ["hout_d_%d" % k for k in range(8)], W=["mixin%d" % b], dma="mixh%d" % b)

    def dS1(j):
        b = j % ND_
        rms_norm(mixin[b][:, 0:512], 512, gb[:, 0:512], mixbf[b][:, 0:512], ["mixin%d" % b], ["mixbfa%d" % b], stk="sta")
        rms_norm(mixin[b][:, 512:1024], 512, gb[:, 512:1024], mixbf[b][:, 512:1024], ["mixin%d" % b], ["mixbfh%d" % b], stk="sth")

    def dS2(j):
        b = j % ND_
        pt = j % 2
        for k in range(8):
            P.add("pe", lambda e, k=k, b=b, pt=pt: e.transpose(out=psb(pt)[:, k * 128:(k + 1) * 128], in_=mixbf[b][:, k * 128:(k + 1) * 128], identity=identb),
                  R=["mixbfa%d" % b, "mixbfh%d" % b, "identb"], W=[psk[pt]])

    def dS3(j):
        b = j % ND_
        pt = j % 2
        P.add("act", lambda e, b=b, pt=pt: e.copy(out=mT[b].rearrange("p k t -> p (k t)"), in_=psb(pt)), R=[psk[pt]], W=["mT%d" % b])

    def dS4(j):
        b = j % ND_
        for n in range(2):
            py = 2 + 2 * (j % 2) + n
            for k in range(8):
                P.add("pe", lambda e, k=k, b=b, n=n, py=py: e.matmul(ps[py][:, :], lhsT=mT[b][:, k, :], rhs=w_out[:, k, n * 512:(n + 1) * 512],
                                                                      start=(k == 0), stop=(k == 7)), R=["mT%d" % b, "w_out"], W=[psk[py]])
            P.add("dve", lambda e, j=j, n=n, py=py: e.tensor_tensor(out=xres[:, j, n * 512:(n + 1) * 512], in0=ps[py][:, :],
                                                                     in1=xres[:, j, n * 512:(n + 1) * 512], op=ALU.add),
                  R=[psk[py], "xres"], W=["xres"])

    pipeline(NTO, [dS0, dS1, dS2, dS3, dS4])
    P.barrier()
    P.release()
    if stop == "D":
        P.add("sp", None, R=[])
        P.emit()
        return nc, dbg_d
    if "x1" in dbg:
        tx1 = dbg_out("x1", [OWN, D])
        P.add("sp", lambda e: e.dma_start(out=tx1.rearrange("(j p) c -> p j c", p=128), in_=xres), R=["xres"], W=["dbgo"], dma="dbg1")
        P.barrier()

    P.mark()
    hmT = P.sb([128, 8, 256], BF16, "hmT")
    KmT = P.sb([128, 8, 256], BF16, "KmT")
    Vm = P.sb([128, 2, 4, 260], BF16, "Vm")
    ksqm = P.sb([128, 8, 256], BF16, "ksqm")
    kbx = P.sb([1, 8], F32, "kbx")
    P.mark()
    w_mkv = P.sb([128, 8, 2 * D], BF16, "w_mkv")
    P.add("pool", lambda e: e.dma_start(out=w_mkv, in_=I["w_mkv"].rearrange("(k p) n -> p k n", p=128)), W=["w_mkv"], dma="w0")
    load_gain("mem_norm_g")
    P.add("pool", lambda e: e.memset(Vm.rearrange("p a b c -> p (a b c)"), 1.0), W=["Vm"])
    memt = [P.sb([128, D], F32, "memt%d" % i) for i in range(2)]
    membf = [P.sb([128, D], BF16, "membf%d" % i) for i in range(2)]
    for mt in range(2):
        P.add("sp", lambda e, mt=mt: e.dma_start(out=memt[mt], in_=I["mem_b"][mt * 128:(mt + 1) * 128, :]), W=["memt%d" % mt], dma="memt%d" % mt)
        rms_norm(memt[mt], D, gb, membf[mt], ["memt%d" % mt], ["membf%d" % mt], stk="stm")
        for k in range(8):
            P.add("pe", lambda e, k=k, mt=mt: e.transpose(out=psb(mt)[:, k * 128:(k + 1) * 128], in_=membf[mt][:, k * 128:(k + 1) * 128], identity=identb),
                  R=["membf%d" % mt, "identb"], W=[psk[mt]])
        P.add("act", lambda e, mt=mt: e.copy(out=hmT[:, :, mt * 128:(mt + 1) * 128], in_=psb(mt).rearrange("p (k t) -> p k t", k=8)),
              R=[psk[mt]], W=["hmT"])
    for dt in range(8):
        pk = 2 + dt % 2
        for k in range(8):
            P.add("pe", lambda e, k=k, dt=dt, pk=pk: e.matmul(ps[pk][:, 0:256], lhsT=w_mkv[:, k, dt * 128:(dt + 1) * 128], rhs=hmT[:, k, :],
                                                               start=(k == 0), stop=(k == 7)), R=["w_mkv", "hmT"], W=[psk[pk]])
        P.add("act", lambda e, dt=dt, pk=pk: e.copy(out=KmT[:, dt, :], in_=ps[pk][:, 0:256]), R=[psk[pk]], W=["KmT"])
    for mt in range(2):
        for n in range(2):
            pv = 4 + n
            for k in range(8):
                P.add("pe", lambda e, k=k, mt=mt, n=n, pv=pv: e.matmul(ps[pv][:, :], lhsT=hmT[:, k, mt * 128:(mt + 1) * 128],
                                                                        rhs=w_mkv[:, k, D + n * 512:D + (n + 1) * 512],
                                                                        start=(k == 0), stop=(k == 7)), R=["w_mkv", "hmT"], W=[psk[pv]])
            P.add("dve", lambda e, mt=mt, n=n, pv=pv: e.tensor_copy(out=Vm[:, mt, 2 * n:2 * n + 2, 0:256],
                                                                    in_=ps[pv][:, :].rearrange("p (h c) -> p h c", h=2)),
                  R=[psk[pv]], W=["Vm"])
    P.add("dve", lambda e: e.tensor_tensor(out=ksqm, in0=KmT, in1=KmT, op=ALU.mult), R=["KmT"], W=["ksqm"])
    for hh in range(4):
        for dt in range(2):
            P.add("pe", lambda e, hh=hh, dt=dt: e.matmul(ps[6][0:1, 0:256], lhsT=onesb[:, 0:1], rhs=ksqm[:, 2 * hh + dt, :],
                                                          start=(dt == 0), stop=(dt == 1)), R=["ksqm", "onesb"], W=[psk[6]])
        P.add("dve", lambda e, hh=hh: e.tensor_reduce(out=kbx[0:1, hh:hh + 1], in_=ps[6][0:1, 0:256], axis=AX.X, op=ALU.max),
              R=[psk[6]], W=["kbx%d" % hh])
    P.add("dve", lambda e: e.tensor_reduce(out=kbx[0:1, 4:5], in_=kbx[0:1, 0:4], axis=AX.X, op=ALU.max),
          R=["kbx0", "kbx1", "kbx2", "kbx3"], W=["kbx4"])
    P.add("act", lambda e: e.sqrt(out=kbx[0:1, 5:6], in_=kbx[0:1, 4:5]), R=["kbx4"], W=["kbx5"])
    P.add("dve", lambda e: e.tensor_scalar_mul(out=kbx[0:1, 6:7], in0=kbx[0:1, 5:6], scalar1=-1.04), R=["kbx5"], W=["kbx6"])
    P.barrier()
    P.release()
    w_mq = P.sb([128, 8, D], BF16, "w_mq")
    w_mo = P.sb([128, 8, D], BF16, "w_mo")
    P.add("pool", lambda e: e.dma_start(out=w_mq, in_=I["w_mq"].rearrange("(k p) n -> p k n", p=128)), W=["w_mq"], dma="w0")
    P.add("pool", lambda e: e.dma_start(out=w_mo, in_=I["w_mo"].rearrange("(k p) n -> p k n", p=128)), W=["w_mo"], dma="w1")
    load_gain("cross_norm_g")
    hxbf = [P.sb([128, D], BF16, "hxbf%d" % i) for i in range(3)]
    hxT = P.sb([128, 8, 512], BF16, "hxT")
    qT = P.sb([128, 8, 512], BF16, "qT")
    qsqx = P.sb([128, 8, 512], BF16, "qsqx")
    negm = P.sb([1, 4, 512], BF16, "negm")
    qn1 = P.sb([1, 512], F32, "qn1")
    PTm = [P.sb([128, 2, 512], BF16, "PTm%d" % i) for i in range(2)]
    rdn = [P.sb([1, 512], F32, "rdn%d" % i) for i in range(2)]
    rdb = [P.sb([128, 512], F32, "rdb%d" % i) for i in range(2)]
    oTx = P.sb([128, 8, 512], BF16, "oTx")
    for qc in range(4):
        def eS0(t4):
            j = qc * 4 + t4
            b = t4 % 3
            rms_norm(xres[:, j, :], D, gb, hxbf[b], ["xres"], ["hxbf%d" % b], stk="stx")

        def eS1(t4):
            b = t4 % 3
            pb = t4 % 2
            for k in range(8):
                P.add("pe", lambda e, k=k, b=b, pb=pb: e.transpose(out=psb(pb)[:, k * 128:(k + 1) * 128], in_=hxbf[b][:, k * 128:(k + 1) * 128], identity=identb),
                      R=["hxbf%d" % b, "identb"], W=[psk[pb]])

        def eS2(t4):
            pb = t4 % 2
            P.add("act", lambda e, pb=pb, t4=t4: e.copy(out=hxT[:, :, t4 * 128:(t4 + 1) * 128], in_=psb(pb).rearrange("p (k t) -> p k t", k=8)),
                  R=[psk[pb]], W=["hxT"])

        pipeline(4, [eS0, eS1, eS2])
        for dt in range(8):
            pq = 4 + dt % 2
            for k in range(8):
                P.add("pe", lambda e, k=k, dt=dt, pq=pq: e.matmul(ps[pq][:, :], lhsT=w_mq[:, k, dt * 128:(dt + 1) * 128], rhs=hxT[:, k, :],
                                                                   start=(k == 0), stop=(k == 7)), R=["w_mq", "hxT"], W=[psk[pq]])
            if dt % 2 == 0:
                P.add("act", lambda e, dt=dt, pq=pq: e.mul(out=qT[:, dt, :], in_=ps[pq][:, :], mul=1.0 / 16.0), R=[psk[pq]], W=["qT%d" % dt])
            else:
                P.add("dve", lambda e, dt=dt, pq=pq: e.tensor_scalar_mul(out=qT[:, dt, :], in0=ps[pq][:, :], scalar1=1.0 / 16.0), R=[psk[pq]], W=["qT%d" % dt])
        QTK = ["qT%d" % k for k in range(8)]
        P.add("pool", lambda e: e.tensor_tensor(out=qsqx, in0=qT, in1=qT, op=ALU.mult), R=QTK, W=["qsqx"])
        for hh in range(4):
            for dt in range(2):
                P.add("pe", lambda e, hh=hh, dt=dt: e.matmul(ps[6][0:1, :], lhsT=onesb[:, 0:1], rhs=qsqx[:, 2 * hh + dt, :],
                                                              start=(dt == 0), stop=(dt == 1)), R=["qsqx", "onesb"], W=[psk[6]])
            P.add("act", lambda e: e.sqrt(out=qn1, in_=ps[6][0:1, :]), R=[psk[6]], W=["qn1"])
            P.add("dve", lambda e, hh=hh: e.tensor_scalar_mul(out=negm[0:1, hh, :], in0=qn1, scalar1=kbx[0:1, 6:7]), R=["qn1", "kbx6"], W=["negm"])

        def hA(hh):
            s0 = 2 * (hh % 2)
            for mt in range(2):
                for dt in range(2):
                    P.add("pe", lambda e, hh=hh, mt=mt, dt=dt, s0=s0: e.matmul(ps[s0 + mt][:, :], lhsT=KmT[:, 2 * hh + dt, mt * 128:(mt + 1) * 128],
                                                                                rhs=qT[:, 2 * hh + dt, :], start=(dt == 0), stop=False),
                          R=["KmT"] + QTK, W=[psk[s0 + mt]])
                P.add("pe", lambda e, hh=hh, mt=mt, s0=s0: e.matmul(ps[s0 + mt][:, :], lhsT=onesb[0:1, 0:128], rhs=negm[0:1, hh, :], start=False, stop=True),
                      R=["negm", "onesb"], W=[psk[s0 + mt]])

        def hB(hh):
            s0 = 2 * (hh % 2)
            pb_ = hh % 2
            P.add("act", lambda e, s0=s0, pb_=pb_: e.activation(out=PTm[pb_].rearrange("p a b -> p (a b)"), in_=psall[:, s0 * 512:(s0 + 2) * 512], func=AF.Exp),
                  R=[psk[s0], psk[s0 + 1]], W=["PTm%d" % pb_])

        def hC(hh):
            if hh > 0:
                hD(hh - 1)
            pb_ = hh % 2
            for dv_ in range(2):
                for mt in range(2):
                    P.add("pe", lambda e, hh=hh, mt=mt, dv_=dv_, pb_=pb_: e.matmul(ps[4 + dv_][:, :], lhsT=Vm[:, mt, hh, dv_ * 128:(dv_ + 1) * 128], rhs=PTm[pb_][:, mt, :],
                                                                                   start=(mt == 0), stop=(mt == 1)), R=["Vm", "PTm%d" % pb_], W=[psk[4 + dv_]])
            for mt in range(2):
                P.add("pe", lambda e, hh=hh, mt=mt, pb_=pb_: e.matmul(ps[6][0:1, :], lhsT=Vm[:, mt, hh, 256:257], rhs=PTm[pb_][:, mt, :], start=(mt == 0), stop=(mt == 1)),
                      R=["Vm", "PTm%d" % pb_], W=[psk[6]])
            P.add("dve", lambda e, pb_=pb_: e.reciprocal(out=rdn[pb_], in_=ps[6][0:1, :]), R=[psk[6]], W=["rdn%d" % pb_])

        def hD(hh):
            pb_ = hh % 2
            P.add("pe", lambda e, pb_=pb_: e.matmul(ps[7][:, :], lhsT=onesf[0:1, 0:128], rhs=rdn[pb_], start=True, stop=True), R=["rdn%d" % pb_, "onesf"], W=[psk[7]])
            P.add("act", lambda e, pb_=pb_: e.copy(out=rdb[pb_], in_=ps[7][:, :]), R=[psk[7]], W=["rdb%d" % pb_])
            for dv_ in range(2):
                P.add("dve", lambda e, hh=hh, dv_=dv_, pb_=pb_: e.tensor_tensor(out=oTx[:, 2 * hh + dv_, :], in0=ps[4 + dv_][:, :], in1=rdb[pb_], op=ALU.mult),
                      R=[psk[4 + dv_], "rdb%d" % pb_], W=["oTx%d" % (2 * hh + dv_)])

        pipeline(4, [hA, hB, hC])
        hD(3)
        OTK = ["oTx%d" % k for k in range(8)]
        for t4 in range(4):
            j = qc * 4 + t4
            for n in range(2):
                py = (0, 1, 2, 3)[(t4 * 2 + n) % 4]
                for dt in range(8):
                    P.add("pe", lambda e, dt=dt, t4=t4, n=n, py=py: e.matmul(ps[py][:, :], lhsT=oTx[:, dt, t4 * 128:(t4 + 1) * 128],
                                                                              rhs=w_mo[:, dt, n * 512:(n + 1) * 512], start=(dt == 0), stop=(dt == 7)),
                          R=OTK + ["w_mo"], W=[psk[py]])
                P.add("dve", lambda e, j=j, n=n, py=py: e.tensor_tensor(out=xres[:, j, n * 512:(n + 1) * 512], in0=ps[py][:, :],
                                                                         in1=xres[:, j, n * 512:(n + 1) * 512], op=ALU.add),
                      R=[psk[py], "xres"], W=["xres"])
    P.barrier()
    P.release()
    if "x2" in dbg:
        tx2 = dbg_out("x2", [OWN, D])
        P.add("sp", lambda e: e.dma_start(out=tx2.rearrange("(j p) c -> p j c", p=128), in_=xres), R=["xres"], W=["dbgo"], dma="dbg1")
        P.barrier()
    if stop == "E":
        P.add("sp", None, R=[])
        P.emit()
        return nc, dbg_d

    P.mark()
    tT = P.sb([128, 8, OWN], BF16, "tT")
    combT = P.sb([32, OWN], F32, "combT")
    P.mark()
    load_gain("ffn_norm_g")
    w_r = P.sb([128, 8, 36], F32, "w_r")
    b_r = P.sb([128, 36], F32, "b_r")
    P.add("sp", lambda e: e.dma_start(out=w_r, in_=I["w_route"].rearrange("(k p) n -> p k n", p=128)), W=["w_r"], dma="c3")
    P.add("sp", lambda e: e.dma_start(out=b_r, in_=I["b_route"].partition_broadcast(128)), W=["b_r"], dma="c4")
    tnf = [P.sb([128, D], F32, "tnf%d" % i) for i in range(2)]
    tnb = [P.sb([128, D], BF16, "tnb%d" % i) for i in range(2)]
    tTf = P.sb([128, 8, 128], F32, "tTf")
    T_ = NTO
    lg = P.sb([128, T_, 36], F32, "lg")
    gmx = P.sb([128, T_], F32, "gmx")
    oh = P.sb([128, T_, 4], F32, "oh")
    ge = P.sb([128, T_, 4], F32, "ge")
    gsm = P.sb([128, T_], F32, "gsm")
    pg = P.sb([128, T_], F32, "pg")
    tmp48 = P.sb([128, T_, 4, 8], F32, "tmp48")
    ein = P.sb([128, T_, 8], F32, "ein")
    e2 = P.sb([128, T_, 8], F32, "e2")
    mk1 = P.sb([128, T_, 8], F32, "mk1")
    mk2 = P.sb([128, T_, 8], F32, "mk2")
    m1 = P.sb([128, T_], F32, "m1")
    m2 = P.sb([128, T_], F32, "m2")
    dd = P.sb([128, T_], F32, "dd")
    p1 = P.sb([128, T_], F32, "p1")
    p2 = P.sb([128, T_], F32, "p2")
    we = P.sb([128, T_, 8], F32, "we")
    we2 = P.sb([128, T_, 8], F32, "we2")
    comb = P.sb([128, T_, 4, 8], F32, "comb")
    seq = [0]

    def dv(fn, R, W):
        P.add("dve", fn, R=R, W=W)

    for j in range(NTO):
        b = j % 2
        rms_norm(xres[:, j, :], D, gb, tnf[b], ["xres"], ["tnf%d" % b], stk="stf")
        P.add("act", lambda e, b=b: e.copy(out=tnb[b], in_=tnf[b]), R=["tnf%d" % b], W=["tnb%d" % b])
        for k in range(8):
            P.add("pe", lambda e, k=k, b=b: e.transpose(out=psb(b)[:, k * 128:(k + 1) * 128], in_=tnb[b][:, k * 128:(k + 1) * 128], identity=identb),
                  R=["tnb%d" % b, "identb"], W=[psk[b]])
        P.add("act", lambda e, b=b, j=j: e.copy(out=tT[:, :, j * 128:(j + 1) * 128], in_=psb(b).rearrange("p (k t) -> p k t", k=8)),
              R=[psk[b]], W=["tT"])
        for k in range(8):
            pf = 2 + (k // 4)
            P.add("pe", lambda e, k=k, b=b, pf=pf: e.transpose(out=ps[pf][:, (k % 4) * 128:(k % 4 + 1) * 128], in_=tnf[b][:, k * 128:(k + 1) * 128], identity=identf),
                  R=["tnf%d" % b, "identf"], W=[psk[pf]])
        for hf in range(2):
            P.add("dve" if hf == 0 else "act", (lambda e, hf=hf: e.tensor_copy(out=tTf[:, hf * 4:(hf + 1) * 4, :], in_=ps[2 + hf][:, :].rearrange("p (k t) -> p k t", k=4)))
                  if hf == 0 else (lambda e, hf=hf: e.copy(out=tTf[:, hf * 4:(hf + 1) * 4, :], in_=ps[2 + hf][:, :].rearrange("p (k t) -> p k t", k=4))),
                  R=[psk[2 + hf]], W=["tTf%d" % hf])
        for k in range(8):
            P.add("pe", lambda e, k=k: e.matmul(ps[4][:, 0:36], lhsT=tTf[:, k, :], rhs=w_r[:, k, :], start=(k == 0), stop=(k == 7)),
                  R=["tTf0", "tTf1", "w_r"], W=[psk[4]])
        dv(lambda e, j=j: e.tensor_tensor(out=lg[:, j, :], in0=ps[4][:, 0:36], in1=b_r, op=ALU.add), [psk[4], "b_r"], ["lg"])

    def col(t, n):
        return t.rearrange("p (t o) -> p t o", o=1).broadcast_to([128, T_, n])

    gl = lg[:, :, 0:4]
    el = lg[:, :, 4:36].rearrange("p t (g e) -> p t g e", g=4)
    dv(lambda e: e.tensor_reduce(out=gmx, in_=gl, axis=AX.X, op=ALU.max), ["lg"], ["gmx"])
    dv(lambda e: e.tensor_tensor(out=oh, in0=gl, in1=col(gmx, 4), op=ALU.is_equal), ["lg", "gmx"], ["oh"])
    dv(lambda e: e.tensor_tensor(out=ge, in0=gl, in1=col(gmx, 4), op=ALU.subtract), ["lg", "gmx"], ["ge"])
    P.add("act", lambda e: e.activation(out=ge, in_=ge, func=AF.Exp), R=["ge"], W=["ge"])
    dv(lambda e: e.tensor_reduce(out=gsm, in_=ge, axis=AX.X, op=ALU.add), ["ge"], ["gsm"])
    dv(lambda e: e.reciprocal(out=pg, in_=gsm), ["gsm"], ["pg"])
    dv(lambda e: e.tensor_tensor(out=tmp48, in0=el, in1=oh.rearrange("p t (g o) -> p t g o", o=1).broadcast_to([128, T_, 4, 8]), op=ALU.mult),
       ["lg", "oh"], ["tmp48"])
    dv(lambda e: e.tensor_reduce(out=ein, in_=tmp48.rearrange("p t g e -> p t e g"), axis=AX.X, op=ALU.add), ["tmp48"], ["ein"])
    dv(lambda e: e.tensor_reduce(out=m1, in_=ein, axis=AX.X, op=ALU.max), ["ein"], ["m1"])
    dv(lambda e: e.tensor_tensor(out=mk1, in0=ein, in1=col(m1, 8), op=ALU.is_equal), ["ein", "m1"], ["mk1"])
    dv(lambda e: e.scalar_tensor_tensor(out=e2, in0=mk1, scalar=-1e30, in1=ein, op0=ALU.mult, op1=ALU.add), ["mk1", "ein"], ["e2"])
    dv(lambda e: e.tensor_reduce(out=m2, in_=e2, axis=AX.X, op=ALU.max), ["e2"], ["m2"])
    dv(lambda e: e.tensor_tensor(out=mk2, in0=e2, in1=col(m2, 8), op=ALU.is_equal), ["e2", "m2"], ["mk2"])
    dv(lambda e: e.tensor_tensor(out=dd, in0=m2, in1=m1, op=ALU.subtract), ["m1", "m2"], ["dd"])
    P.add("act", lambda e: e.activation(out=dd, in_=dd, func=AF.Exp), R=["dd"], W=["dd"])
    dv(lambda e: e.tensor_scalar_add(out=p1, in0=dd, scalar1=1.0), ["dd"], ["p1"])
    dv(lambda e: e.reciprocal(out=p1, in_=p1), ["p1"], ["p1"])
    dv(lambda e: e.tensor_tensor(out=p2, in0=dd, in1=p1, op=ALU.mult), ["dd", "p1"], ["p2"])
    dv(lambda e: e.tensor_tensor(out=p1, in0=p1, in1=pg, op=ALU.mult), ["p1", "pg"], ["p1"])
    dv(lambda e: e.tensor_tensor(out=p2, in0=p2, in1=pg, op=ALU.mult), ["p2", "pg"], ["p2"])
    dv(lambda e: e.tensor_tensor(out=we, in0=mk1, in1=col(p1, 8), op=ALU.mult), ["mk1", "p1"], ["we"])
    dv(lambda e: e.tensor_tensor(out=we2, in0=mk2, in1=col(p2, 8), op=ALU.mult), ["mk2", "p2"], ["we2"])
    dv(lambda e: e.tensor_tensor(out=we, in0=we, in1=we2, op=ALU.add), ["we", "we2"], ["we"])
    dv(lambda e: e.tensor_tensor(out=comb, in0=we.rearrange("p t (o e) -> p t o e", o=1).broadcast_to([128, T_, 4, 8]),
                                 in1=oh.rearrange("p t (g o) -> p t g o", o=1).broadcast_to([128, T_, 4, 8]), op=ALU.mult), ["we", "oh"], ["comb"])
    for j in range(NTO):
        pc_ = 5 + j % 2
        P.add("pe", lambda e, j=j, pc_=pc_: e.transpose(out=ps[pc_][0:32, 0:128], in_=comb[:, j, :, :].rearrange("p g e -> p (g e)"), identity=identf),
              R=["comb", "identf"], W=[psk[pc_]])
        dv(lambda e, j=j, pc_=pc_: e.tensor_copy(out=combT[:, j * 128:(j + 1) * 128], in_=ps[pc_][0:32, 0:128]), [psk[pc_]], ["combT"])
    P.add("sp", lambda e: e.dma_start(out=combT_d, in_=combT), R=["combT"], W=["combT_d"], dma="combT")
    if "comb" in dbg:
        tcb = dbg_out("comb", [32, OWN])
        P.add("sp", lambda e: e.dma_start(out=tcb, in_=combT), R=["combT"], W=["dbgo"], dma="dbg1")
    P.barrier()
    P.release()
    NSLOT = 4
    wg = [P.sb([128, 8, 256], BF16, "wg%d" % i) for i in range(NSLOT)]
    wu = [P.sb([128, 8, 256], BF16, "wu%d" % i) for i in range(NSLOT)]
    wd = [P.sb([128, 2, D], BF16, "wd%d" % i) for i in range(NSLOT)]
    CB = [P.sb([128, OWN], F32, "CB%d" % i) for i in range(2)]
    sa = [P.sb([128, 2, 512], F32, "sa%d" % i) for i in range(2)]
    sc = [P.sb([128, 2, 512], F32, "sc%d" % i) for i in range(2)]
    mTe = [P.sb([128, 2, 512], BF16, "mTe%d" % i) for i in range(2)]
    def load_expert(e_):
        sl = e_ % NSLOT
        P.add("pool", lambda e, e_=e_, sl=sl: e.dma_start(out=wg[sl], in_=I["w_gate"][e_].rearrange("(k p) n -> p k n", p=128)), W=["wg%d" % sl], dma="wg%d" % sl)
        P.add("pool", lambda e, e_=e_, sl=sl: e.dma_start(out=wu[sl], in_=I["w_up"][e_].rearrange("(k p) n -> p k n", p=128)), W=["wu%d" % sl], dma="wu%d" % sl)
        P.add("pool", lambda e, e_=e_, sl=sl: e.dma_start(out=wd[sl], in_=I["w_down"][e_].rearrange("(k p) n -> p k n", p=128)), W=["wd%d" % sl], dma="wd%d" % sl)

    for e_ in range(2):
        load_expert(e_)
    yb = 0
    for pr in range(16):
        for ee in range(2):
            if 2 * pr + 2 + ee < 32:
                load_expert(2 * pr + 2 + ee)
        for ee in range(2):
            e_ = 2 * pr + ee
            P.add("sp", lambda e, e_=e_, ee=ee: e.dma_start(out=CB[ee], in_=combT_d[e_:e_ + 1, :].partition_broadcast(128)),
                  R=["combT_d"], W=["CB%d" % ee], dma="CB%d" % ee)
        for c in range(4):
            for ee in range(2):
                e_ = 2 * pr + ee
                sl = e_ % NSLOT
                for f in range(2):
                    for k in range(8):
                        P.add("pe", lambda e, f=f, k=k, sl=sl, c=c: e.matmul(ps[f][:, :], lhsT=wg[sl][:, k, f * 128:(f + 1) * 128],
                                                                            rhs=tT[:, k, c * 512:(c + 1) * 512], start=(k == 0), stop=(k == 7)),
                              R=["wg%d" % sl, "tT"], W=[psk[f]])
                for f in range(2):
                    for k in range(8):
                        P.add("pe", lambda e, f=f, k=k, sl=sl, c=c: e.matmul(ps[2 + f][:, :], lhsT=wu[sl][:, k, f * 128:(f + 1) * 128],
                                                                            rhs=tT[:, k, c * 512:(c + 1) * 512], start=(k == 0), stop=(k == 7)),
                              R=["wu%d" % sl, "tT"], W=[psk[2 + f]])
                for f in range(2):
                    P.add("act", lambda e, f=f, ee=ee: e.activation(out=sa[ee][:, f, :], in_=ps[f][:, :], func=AF.Silu),
                          R=[psk[f]], W=["sa%d_%d" % (ee, f)])
                    P.add("pool", lambda e, f=f, ee=ee, c=c: e.tensor_tensor(out=sc[ee][:, f, :], in0=sa[ee][:, f, :],
                                                                              in1=CB[ee][:, c * 512:(c + 1) * 512], op=ALU.mult),
                          R=["sa%d_%d" % (ee, f), "CB%d" % ee], W=["sc%d_%d" % (ee, f)])
                    P.add("dve", lambda e, f=f, ee=ee: e.tensor_tensor(out=mTe[ee][:, f, :], in0=ps[2 + f][:, :], in1=sc[ee][:, f, :], op=ALU.mult),
                          R=[psk[2 + f], "sc%d_%d" % (ee, f)], W=["mTe%d" % ee])
            for t4 in range(4):
                j = c * 4 + t4
                for n in range(2):
                    py = 4 + (yb % 4)
                    yb += 1
                    cnt_mm = 0
                    for ee in range(2):
                        sl = (2 * pr + ee) % NSLOT
                        for f in range(2):
                            P.add("pe", lambda e, ee=ee, f=f, sl=sl, t4=t4, n=n, py=py, cnt_mm=cnt_mm: e.matmul(
                                ps[py][:, :], lhsT=mTe[ee][:, f, t4 * 128:(t4 + 1) * 128], rhs=wd[sl][:, f, n * 512:(n + 1) * 512],
                                start=(cnt_mm == 0), stop=(cnt_mm == 3)), R=["mTe%d" % ee, "wd%d" % sl], W=[psk[py]])
                            cnt_mm += 1
                    P.add("dve", lambda e, j=j, n=n, py=py: e.tensor_tensor(out=xres[:, j, n * 512:(n + 1) * 512], in0=ps[py][:, :],
                                                                             in1=xres[:, j, n * 512:(n + 1) * 512], op=ALU.add),
                          R=[psk[py], "xres"], W=["xres"])
    P.barrier()
    P.release()
    if "x3" in dbg:
        tx3 = dbg_out("x3", [OWN, D])
        P.add("sp", lambda e: e.dma_start(out=tx3.rearrange("(j p) c -> p j c", p=128), in_=xres), R=["xres"], W=["dbgo"], dma="dbg1")
        P.barrier()

    load_gain("final_norm_g")
    xo = [P.sb([128, D], F32, "xo%d" % i) for i in range(2)]
    for j in range(NTO):
        b = j % 2
        rms_norm(xres[:, j, :], D, gb, xo[b], ["xres"], ["xo%d" % b], stk="stg")
        P.add("sp", lambda e, j=j, b=b: e.dma_start(out=out_d[j * 128:(j + 1) * 128, :], in_=xo[b]),
              R=["xo%d" % b], W=["out_d%d" % b], dma="xo%d" % b)
    P.add("sp", None, R=["out_d0", "out_d1", "dbgo"])
    P.emit()
    return nc, dbg_d


def hyena_phase(nc, P, I, ps, psk, psb, U_d, hout_d, identf, identb, onesb, onesf, halfm, dbg, dbg_out, psall):
    NF = 64
    Hd = nc.dram_tensor("hy_H", [S, 2048], BF16, kind="Internal").ap()
    Bd = [nc.dram_tensor("hy_B%d" % i, [128, NF, 512], BF16, kind="Internal").ap() for i in range(2)]
    Kd = [nc.dram_tensor("hy_K%d" % i, [128, NF, 512], BF16, kind="Internal").ap() for i in range(2)]
    Btd = nc.dram_tensor("hy_Bt", [128, NF, 512], BF16, kind="Internal").ap()
    z1_d = nc.dram_tensor("hy_z1", [S, 512], F32, kind="Internal").ap()
    P.mark()
    Dt = P.sb([64, 64, 128], BF16, "Dt")
    Dinv = P.sb([128, 64, 64], BF16, "Dinv")
    F2a = P.sb([128, 128], BF16, "F2a")
    F2b = P.sb([128, 128], BF16, "F2b")
    Gm = P.sb([128, 128], BF16, "Gm")
    sgn = P.sb([128, 1], F32, "sgn")
    skipb = P.sb([128, 2, 512], F32, "skipb")
    P.add("pool", lambda e: e.dma_start(out=Dt, in_=I["hy_D0"].rearrange("a (b c) -> a b c", b=64)), W=["Dt"], dma="ht0")
    P.add("pool", lambda e: e.dma_start(out=F2a, in_=I["hy_F2a"]), W=["F2a"], dma="ht1")
    P.add("pool", lambda e: e.dma_start(out=F2b, in_=I["hy_F2b"]), W=["F2b"], dma="ht2")
    P.add("pool", lambda e: e.dma_start(out=Gm, in_=I["hy_G"]), W=["Gm"], dma="ht3")
    P.add("pool", lambda e: e.dma_start(out=Dinv, in_=I["hy_Dinv"].rearrange("a (b c) -> a b c", b=64)), W=["Dinv"], dma="ht4")
    P.add("sp", lambda e: e.dma_start(out=sgn, in_=I["hy_sgn"]), W=["sgn"], dma="c3")
    P.add("sp", lambda e: e.dma_start(out=skipb.rearrange("p a b -> p (a b)"), in_=I["hy_skip"].partition_broadcast(128)), W=["skipb"], dma="c4")

    P.mark()
    zT = P.sb([33, S], F32, "zT")
    g1T = P.sb([64, S], F32, "g1T")
    g2T = P.sb([64, S], F32, "g2T")
    hcols = P.sb([64, 4], F32, "hcols")
    w1 = P.sb([33, 64], F32, "w1")
    w2 = P.sb([64, 64], F32, "w2")
    w3 = P.sb([64, 2048], F32, "w3")
    b3r = P.sb([1, 2048], F32, "b3r")
    adec = P.sb([128, 2048], F32, "adec")
    nt01 = P.sb([128, NT], F32, "nt01")
    mpi = P.sb([128, 1], F32, "mpi")
    argt = P.sb([64, 512], F32, "argt")
    argm = P.sb([64, 512], F32, "argm")
    hfo = [P.sb([128, 2048], BF16, "hfo%d" % i) for i in range(2)]
    P.add("sp", lambda e: e.dma_start(out=zT, in_=I["hy_zT"]), W=["zT"], dma="hf0")
    P.add("sp", lambda e: e.dma_start(out=hcols, in_=I["hy_cols"]), W=["hcols"], dma="hf1")
    P.add("sp", lambda e: e.dma_start(out=w1, in_=I["hy_w1"]), W=["w1"], dma="hf2")
    P.add("sp", lambda e: e.dma_start(out=w2, in_=I["hy_w2"]), W=["w2"], dma="hf3")
    P.add("sp", lambda e: e.dma_start(out=w3, in_=I["hy_w3"]), W=["w3"], dma="hf4")
    P.add("sp", lambda e: e.dma_start(out=b3r, in_=I["hy_b3"]), W=["b3r"], dma="hf5")
    P.add("sp", lambda e: e.dma_start(out=adec, in_=I["hy_decay"].partition_broadcast(128)), W=["adec"], dma="hf6")
    P.add("sp", lambda e: e.dma_start(out=nt01, in_=I["hy_nt01"]), W=["nt01"], dma="hf7")
    P.add("pool", lambda e: e.memset(mpi, -math.pi), W=["mpi"])
    P.add("act", lambda e: e.activation(out=adec, in_=adec, func=AF.Abs), R=["adec"], W=["adec"])
    OFFS = math.pi + 16.0 * math.pi
    for (src, wt, kk, bcol, fcol, dst, nm) in ((zT, w1, 33, 0, 2, g1T, "g1T"), (g1T, w2, 64, 1, 3, g2T, "g2T")):
        for ch in range(8):
            pb_ = ch % 2
            P.add("pe", lambda e, src=src, wt=wt, kk=kk, ch=ch, pb_=pb_: e.matmul(ps[pb_][0:64, :], lhsT=wt[0:kk, :], rhs=src[0:kk, ch * 512:(ch + 1) * 512],
                                                                               start=True, stop=True), R=["zT", "g1T", "w1", "w2"], W=[psk[pb_]])
            P.add("dve", lambda e, pb_=pb_, bcol=bcol, fcol=fcol: e.tensor_scalar(out=argt, in0=ps[pb_][0:64, :], scalar1=hcols[:, bcol:bcol + 1],
                                                                                  scalar2=hcols[:, fcol:fcol + 1], op0=ALU.add, op1=ALU.mult),
                  R=[psk[pb_], "hcols"], W=["argt"])
            for _rep in range(2):
                P.add("dve", lambda e: e.tensor_scalar(out=argm, in0=argt, scalar1=math.pi, scalar2=None, op0=ALU.is_gt), R=["argt"], W=["argm"])
                P.add("dve", lambda e: e.scalar_tensor_tensor(out=argt, in0=argm, scalar=-2.0 * math.pi, in1=argt, op0=ALU.mult, op1=ALU.add),
                      R=["argm", "argt"], W=["argt"])
                P.add("dve", lambda e: e.tensor_scalar(out=argm, in0=argt, scalar1=-math.pi, scalar2=None, op0=ALU.is_lt), R=["argt"], W=["argm"])
                P.add("dve", lambda e: e.scalar_tensor_tensor(out=argt, in0=argm, scalar=2.0 * math.pi, in1=argt, op0=ALU.mult, op1=ALU.add),
                      R=["argm", "argt"], W=["argt"])
            P.add("act", lambda e, dst=dst, ch=ch: e.activation(out=dst[:, ch * 512:(ch + 1) * 512], in_=argt, func=AF.Sin),
                  R=["argt"], W=[nm])
    g2b = P.sb([64, S], BF16, "g2b")
    w3b = P.sb([64, 2048], BF16, "w3b")
    b3b = P.sb([1, 2048], BF16, "b3b")
    Etf = [P.sb([128, 2048], F32, "Etf%d" % i) for i in range(2)]
    P.add("act", lambda e: e.copy(out=g2b, in_=g2T), R=["g2T"], W=["g2b"])
    P.add("dve", lambda e: e.tensor_copy(out=w3b, in_=w3), R=["w3"], W=["w3b"])
    P.add("dve", lambda e: e.tensor_copy(out=b3b, in_=b3r), R=["b3r"], W=["b3b"])
    for i in range(NT):
        b = i % 2
        b0 = 4 * b
        for cg in range(4):
            pb_ = b0 + cg
            P.add("pe", lambda e, i=i, cg=cg, pb_=pb_: e.matmul(ps[pb_][:, :], lhsT=g2b[:, i * 128:(i + 1) * 128], rhs=w3b[:, cg * 512:(cg + 1) * 512],
                                                                 start=True, stop=False), R=["g2b", "w3b"], W=[psk[pb_]])
            P.add("pe", lambda e, cg=cg, pb_=pb_: e.matmul(ps[pb_][:, :], lhsT=onesb[0:1, 0:128], rhs=b3b[0:1, cg * 512:(cg + 1) * 512],
                                                            start=False, stop=True), R=["onesb", "b3b"], W=[psk[pb_]])
        P.add("act", lambda e, i=i, b=b: e.activation(out=Etf[b], in_=adec, func=AF.Exp, scale=nt01[:, i:i + 1]),
              R=["adec", "nt01"], W=["Etf%d" % b])
        for hf_ in range(2):
            P.add("dve", lambda e, hf_=hf_, b=b, b0=b0: e.tensor_tensor(out=hfo[b][:, hf_ * 1024:(hf_ + 1) * 1024],
                                                                      in0=psall[:, (b0 + 2 * hf_) * 512:(b0 + 2 * hf_ + 2) * 512],
                                                                      in1=Etf[b][:, hf_ * 1024:(hf_ + 1) * 1024], op=ALU.mult),
                  R=[psk[b0 + 2 * hf_], psk[b0 + 2 * hf_ + 1], "Etf%d" % b], W=["hfo%d" % b])
        if i == 0:
            for o in range(2):
                P.add("pool", lambda e, o=o: e.memset(hfo[0][0:1, o * 1024 + 512:o * 1024 + 1024], 0.0), R=[], W=["hfo0"])
        P.add("sp", lambda e, i=i, b=b: e.dma_start(out=Hd[i * 128:(i + 1) * 128, :], in_=hfo[b]), R=["hfo%d" % b], W=["Hd_%d" % i], dma="hfo%d" % b)
    P.barrier()
    P.release()
    if "hf" in dbg:
        thf = dbg_out("hf", [S, 2048])
        P.mark()
        tbf = P.sb([128, 2048], BF16, "tbf")
        tbf32 = P.sb([128, 2048], F32, "tbf32")
        for i in range(NT):
            P.add("sp", lambda e, i=i: e.dma_start(out=tbf, in_=Hd[i * 128:(i + 1) * 128, :]), R=["Hd_%d" % i], W=["tbf"], dma="dbg0")
            P.add("dve", lambda e: e.tensor_copy(out=tbf32, in_=tbf), R=["tbf"], W=["tbf32"])
            P.add("sp", lambda e, i=i: e.dma_start(out=thf[i * 128:(i + 1) * 128, :], in_=tbf32), R=["tbf32"], W=["dbgo"], dma="dbg1")
        P.barrier()
        P.release()

    ev = [0]
    HDK = ["Hd_%d" % i for i in range(NT)]
    Z1K = ["z1_d_%d" % i for i in range(8)]
    BD0K = ["Bd0_%d" % i for i in range(4)]
    BD1K = ["Bd1_%d" % i for i in range(4)]
    BTDK = ["Btd_%d" % i for i in range(8)]

    def evac(out, in_, Rk, Wk):
        ev[0] += 1
        if ev[0] % 2:
            P.add("act", lambda e: e.copy(out=out, in_=in_), R=Rk, W=Wk)
        else:
            P.add("dve", lambda e: e.tensor_copy(out=out, in_=in_), R=Rk, W=Wk)

    def stage1(src_view, cast, Bdst, bkey, srckeys):
        P.barrier()
        P.mark()
        xsb = [P.sb([64, 16, 512], BF16, "xsb%d" % i) for i in range(2)]
        Bsb = [P.sb([128, 16, 512], BF16, "Bsb%d" % i) for i in range(2)]
        def s1_load(ch):
            xb_ = ch % 2
            q = "pool" if cast else "sp"
            P.add(q, lambda e, ch=ch, xb_=xb_: e.dma_start(out=xsb[xb_], in_=src_view[:, ch * 16:(ch + 1) * 16, :]),
                  R=srckeys, W=["xsb%d" % xb_], dma="xsb%d" % xb_)

        s1_load(0)
        for ch in range(4):
            xb_ = ch % 2
            if ch + 1 < 4:
                s1_load(ch + 1)
            for g4 in range(4):
                b0 = 4 * (g4 % 2)
                for q4 in range(4):
                    s2l = g4 * 4 + q4
                    s2 = ch * 16 + s2l
                    P.add("pe", lambda e, s2=s2, s2l=s2l, xb_=xb_, pb_=b0 + q4: e.matmul(ps[pb_][:, :], lhsT=Dt[0:64, s2, :], rhs=xsb[xb_][0:64, s2l, :],
                                                                                        start=True, stop=True), R=["Dt", "xsb%d" % xb_], W=[psk[b0 + q4]])
                evac(Bsb[xb_][:, g4 * 4:(g4 + 1) * 4, :].rearrange("p a b -> p (a b)"), psall[:, b0 * 512:(b0 + 4) * 512],
                     [psk[b0 + k] for k in range(4)], ["Bsb%d_%d" % (xb_, g4)])
            P.add("sp", lambda e, ch=ch, xb_=xb_: e.dma_start(out=Bdst[:, ch * 16:(ch + 1) * 16, :], in_=Bsb[xb_]),
                  R=["Bsb%d_%d" % (xb_, k) for k in range(4)], W=["%s_%d" % (bkey, ch)], dma="Bsb%d" % xb_)
        P.barrier()
        P.release()

    def blocked(ap2d):
        return ap2d.rearrange("(s1 s2) c -> s1 s2 c", s2=64)

    for o in range(2):
        stage1(blocked(Hd[:, o * 1024:o * 1024 + 512]), False, Bd[0], "Bd0", HDK)
        stage1(blocked(Hd[:, o * 1024 + 512:o * 1024 + 1024]), False, Bd[1], "Bd1", HDK)
        P.mark()
        BTf = [P.sb([128, 8, 512], BF16, "BTf%d" % i) for i in range(2)]
        BTb = [P.sb([128, 8, 512], BF16, "BTb%d" % i) for i in range(2)]
        xfs = [P.sb([128, 1024], F32, "xfs%d" % i) for i in range(2)]
        kst = [P.sb([128, 1024], F32, "kst%d" % i) for i in range(2)]
        Kc = [P.sb([128, 8, 512], BF16, "Kc%d" % i) for i in range(2)]
        def f2_load(fc):
            cb_ = fc % 2
            for r in range(2):
                P.add("sp", lambda e, r=r, fc=fc, cb_=cb_: e.dma_start(
                    out=BTf[cb_][r * 64:(r + 1) * 64, :, :], in_=Bd[0][r * 64 + fc * 8:r * 64 + fc * 8 + 8, :, :].rearrange("f s c -> s f c")),
                    R=BD0K, W=["BTf%d" % cb_], dma="BTf%d_%d" % (cb_, r))
                P.add("sp", lambda e, r=r, fc=fc, cb_=cb_: e.dma_start(
                    out=BTb[cb_][r * 64:(r + 1) * 64, :, :], in_=Bd[1][r * 64 + fc * 8:r * 64 + fc * 8 + 8, :, :].rearrange("f s c -> s f c")),
                    R=BD1K, W=["BTb%d" % cb_], dma="BTb%d_%d" % (cb_, r))

        f2_load(0)
        for fc in range(8):
            cb_ = fc % 2
            if fc + 1 < 8:
                f2_load(fc + 1)
            for gq in range(4):
                t_ = gq % 2
                fa, fb = 2 * t_, 4 + 2 * t_
                for q2 in range(2):
                    f1l = gq * 2 + q2
                    P.add("pe", lambda e, f1l=f1l, cb_=cb_, pb_=fa + q2: e.matmul(ps[pb_][:, :], lhsT=F2a, rhs=BTf[cb_][:, f1l, :], start=True, stop=True),
                          R=["F2a", "BTf%d" % cb_], W=[psk[fa + q2]])
                    P.add("pe", lambda e, f1l=f1l, cb_=cb_, pb_=fb + q2: e.matmul(ps[pb_][:, :], lhsT=F2a, rhs=BTb[cb_][:, f1l, :], start=True, stop=True),
                          R=["F2a", "BTb%d" % cb_], W=[psk[fb + q2]])
                P.add("act", lambda e, fa=fa, t_=t_: e.copy(out=xfs[t_], in_=psall[:, fa * 512:(fa + 2) * 512]), R=[psk[fa], psk[fa + 1]], W=["xfs%d" % t_])
                P.add("dve", lambda e, fb=fb, t_=t_: e.scalar_tensor_tensor(out=kst[t_], in0=psall[:, fb * 512:(fb + 2) * 512], scalar=sgn[:, 0:1], in1=xfs[t_],
                                                                           op0=ALU.mult, op1=ALU.add),
                      R=[psk[fb], psk[fb + 1], "xfs%d" % t_, "sgn"], W=["kst%d" % t_])
                for q2 in range(2):
                    f1l = gq * 2 + q2
                    P.add("pool", lambda e, t_=t_, cb_=cb_, f1l=f1l, q2=q2, o=o: e.tensor_tensor(out=Kc[cb_][0:64, f1l, :], in0=kst[t_][0:64, q2 * 512:(q2 + 1) * 512],
                                                                                          in1=skipb[0:64, o, :], op=ALU.add),
                          R=["kst%d" % t_, "skipb"], W=["Kc%d_a%d" % (cb_, f1l)])
                P.add("act", lambda e, t_=t_, cb_=cb_, gq=gq: e.copy(out=Kc[cb_][64:128, gq * 2:gq * 2 + 2, :].rearrange("p a b -> p (a b)"), in_=kst[t_][64:128, :]),
                      R=["kst%d" % t_], W=["Kc%d_b%d" % (cb_, gq)])
            P.add("sp", lambda e, fc=fc, cb_=cb_, o=o: e.dma_start(out=Kd[o][:, fc * 8:(fc + 1) * 8, :], in_=Kc[cb_]),
                  R=["Kc%d_a%d" % (cb_, k) for k in range(8)] + ["Kc%d_b%d" % (cb_, k) for k in range(4)], W=["Kd%d_%d" % (o, fc)], dma="Kc%d" % cb_)
        P.barrier()
        P.release()
    if "kf" in dbg:
        tkf = dbg_out("kf", [2, 128, NF * 512])
        P.mark()
        tk16 = P.sb([128, 8, 512], BF16, "tk16")
        tk32 = P.sb([128, 8, 512], F32, "tk32")
        for o in range(2):
            for fc in range(8):
                P.add("sp", lambda e, o=o, fc=fc: e.dma_start(out=tk16, in_=Kd[o][:, fc * 8:(fc + 1) * 8, :]), R=["Kd%d_%d" % (o, fc)], W=["tk16"], dma="dbg0")
                P.add("dve", lambda e: e.tensor_copy(out=tk32, in_=tk16), R=["tk16"], W=["tk32"])
                P.add("sp", lambda e, o=o, fc=fc: e.dma_start(out=tkf[o][:, fc * 4096:(fc + 1) * 4096], in_=tk32.rearrange("p a b -> p (a b)")),
                      R=["tk32"], W=["dbgo"], dma="dbg1")
        P.barrier()
        P.release()

    P.add("pool", lambda e: e.dma_start(out=Dt, in_=I["hy_Dc"].rearrange("a (b c) -> a b c", b=64)), W=["Dt"], dma="ht0")
    for o in range(2):
        if o == 0:
            stage1(blocked(U_d[:, 1024:1536]), True, Bd[0], "Bd0", ["U_d"])
        else:
            stage1(blocked(z1_d), True, Bd[0], "Bd0", Z1K)
        P.mark()
        BT = [P.sb([128, 8, 512], BF16, "BT%d" % i) for i in range(2)]
        KA = [P.sb([128, 8, 512], BF16, "KA%d" % i) for i in range(2)]
        KB = [P.sb([128, 8, 512], BF16, "KB%d" % i) for i in range(2)]
        ta = [P.sb([128, 1024], F32, "ta%d" % i) for i in range(2)]
        tb2 = [P.sb([128, 1024], F32, "tb2%d" % i) for i in range(2)]
        Yc = [P.sb([128, 1024], BF16, "Yc%d" % i) for i in range(2)]
        Btsb = [P.sb([128, 8, 512], BF16, "Btsb%d" % i) for i in range(2)]
        def c2_load(fc):
            cb_ = fc % 2
            for r in range(2):
                P.add("sp", lambda e, r=r, fc=fc, cb_=cb_: e.dma_start(
                    out=BT[cb_][r * 64:(r + 1) * 64, :, :], in_=Bd[0][r * 64 + fc * 8:r * 64 + fc * 8 + 8, :, :].rearrange("f s c -> s f c")),
                    R=BD0K, W=["BT%d" % cb_], dma="BT%d_%d" % (cb_, r))
                P.add("sp", lambda e, r=r, fc=fc, cb_=cb_, o=o: e.dma_start(out=KA[cb_][r * 64:(r + 1) * 64, :, :], in_=Kd[o][0:64, fc * 8:(fc + 1) * 8, :]),
                      R=["Kd%d_%d" % (o, fc)], W=["KA%d" % cb_], dma="KA%d_%d" % (cb_, r))
                P.add("sp", lambda e, r=r, fc=fc, cb_=cb_, o=o: e.dma_start(out=KB[cb_][r * 64:(r + 1) * 64, :, :], in_=Kd[o][64:128, fc * 8:(fc + 1) * 8, :]),
                      R=["Kd%d_%d" % (o, fc)], W=["KB%d" % cb_], dma="KB%d_%d" % (cb_, r))

        c2_load(0)
        for fc in range(8):
            cb_ = fc % 2
            if fc + 1 < 8:
                c2_load(fc + 1)
            for gq in range(4):
                t_ = gq % 2
                fa, fb = 2 * t_, 4 + 2 * t_
                f0 = gq * 2
                for q2 in range(2):
                    f1l = f0 + q2
                    P.add("pe", lambda e, f1l=f1l, cb_=cb_, pb_=fa + q2: e.matmul(ps[pb_][:, :], lhsT=F2a, rhs=BT[cb_][:, f1l, :], start=True, stop=True),
                          R=["F2a", "BT%d" % cb_], W=[psk[fa + q2]])
                    P.add("pe", lambda e, f1l=f1l, cb_=cb_, pb_=fb + q2: e.matmul(ps[pb_][:, :], lhsT=F2b, rhs=BT[cb_][:, f1l, :], start=True, stop=True),
                          R=["F2b", "BT%d" % cb_], W=[psk[fb + q2]])
                P.add("dve", lambda e, fa=fa, t_=t_, cb_=cb_, f0=f0: e.tensor_tensor(out=ta[t_], in0=psall[:, fa * 512:(fa + 2) * 512],
                                                                                   in1=KA[cb_][:, f0:f0 + 2, :].rearrange("p a b -> p (a b)"), op=ALU.mult),
                      R=[psk[fa], psk[fa + 1], "KA%d" % cb_], W=["ta%d" % t_])
                P.add("dve", lambda e, fb=fb, t_=t_, cb_=cb_, f0=f0: e.tensor_tensor(out=tb2[t_], in0=psall[:, fb * 512:(fb + 2) * 512],
                                                                                   in1=KB[cb_][:, f0:f0 + 2, :].rearrange("p a b -> p (a b)"), op=ALU.mult),
                      R=[psk[fb], psk[fb + 1], "KB%d" % cb_], W=["tb2%d" % t_])
                P.add("pool", lambda e, t_=t_: e.tensor_tensor(out=Yc[t_], in0=ta[t_], in1=tb2[t_], op=ALU.add),
                      R=["ta%d" % t_, "tb2%d" % t_], W=["Yc%d" % t_])
                for q2 in range(2):
                    P.add("pe", lambda e, t_=t_, q2=q2, pb_=fa + q2: e.matmul(ps[pb_][:, :], lhsT=Gm, rhs=Yc[t_][:, q2 * 512:(q2 + 1) * 512], start=True, stop=True),
                          R=["Gm", "Yc%d" % t_], W=[psk[fa + q2]])
                P.add("act", lambda e, fa=fa, cb_=cb_, f0=f0: e.copy(out=Btsb[cb_][:, f0:f0 + 2, :].rearrange("p a b -> p (a b)"), in_=psall[:, fa * 512:(fa + 2) * 512]),
                      R=[psk[fa], psk[fa + 1]], W=["Btsb%d_%d" % (cb_, gq)])
            P.add("sp", lambda e, fc=fc, cb_=cb_: e.dma_start(out=Btd[:, fc * 8:(fc + 1) * 8, :], in_=Btsb[cb_]),
                  R=["Btsb%d_%d" % (cb_, k) for k in range(4)], W=["Btd_%d" % fc], dma="Btsb%d" % cb_)
        P.barrier()
        P.release()
        P.mark()
        BtT = [P.sb([128, 8, 512], BF16, "BtT%d" % i) for i in range(2)]
        gch = [P.sb([64, 8, 512], F32, "gch%d" % i) for i in range(2)]
        zo = [P.sb([64, 8, 512], F32, "zo%d" % i) for i in range(2)]
        M = 64 if o == 0 else 32
        gcol = 0 if o == 0 else 512
        dst = blocked(z1_d) if o == 0 else blocked(hout_d)
        dkey = "z1_d" if o == 0 else "hout_d"
        def i1_load(tc):
            cb_ = tc % 2
            for r in range(2):
                P.add("sp", lambda e, r=r, tc=tc, cb_=cb_: e.dma_start(
                    out=BtT[cb_][r * 64:(r + 1) * 64, :, :], in_=Btd[r * 64 + tc * 8:r * 64 + tc * 8 + 8, :, :].rearrange("t f c -> f t c")),
                    R=BTDK, W=["BtT%d" % cb_], dma="BtT%d_%d" % (cb_, r))
            P.add("sp", lambda e, tc=tc, cb_=cb_, M=M, gcol=gcol: e.dma_start(
                out=gch[cb_][0:M, :, :], in_=blocked(U_d[0:M * 64, gcol:gcol + 512])[:, tc * 8:(tc + 1) * 8, :]),
                R=["U_d"], W=["gch%d" % cb_], dma="gch%d" % cb_)

        i1_load(0)
        for tc in range(8):
            cb_ = tc % 2
            if tc + 1 < 8:
                i1_load(tc + 1)
            for g4 in range(2):
                b0 = 4 * ((2 * tc + g4) % 2)
                for q4 in range(4):
                    t2l = g4 * 4 + q4
                    t2 = tc * 8 + t2l
                    P.add("pe", lambda e, t2=t2, t2l=t2l, cb_=cb_, pb_=b0 + q4, M=M: e.matmul(ps[pb_][0:M, :], lhsT=Dinv[:, t2, 0:M], rhs=BtT[cb_][:, t2l, :],
                                                                                             start=True, stop=True), R=["Dinv", "BtT%d" % cb_], W=[psk[b0 + q4]])
                P.add("dve", lambda e, g4=g4, cb_=cb_, b0=b0, M=M: e.tensor_tensor(out=zo[cb_][0:M, g4 * 4:(g4 + 1) * 4, :].rearrange("p a b -> p (a b)"),
                                                                                 in0=psall[0:M, b0 * 512:(b0 + 4) * 512],
                                                                                 in1=gch[cb_][0:M, g4 * 4:(g4 + 1) * 4, :].rearrange("p a b -> p (a b)"), op=ALU.mult),
                      R=[psk[b0 + k] for k in range(4)] + ["gch%d" % cb_], W=["zo%d_%d" % (cb_, g4)])
            P.add("sp", lambda e, tc=tc, cb_=cb_, M=M, dst=dst: e.dma_start(out=dst[0:M, tc * 8:(tc + 1) * 8, :], in_=zo[cb_][0:M, :, :]),
                  R=["zo%d_%d" % (cb_, k) for k in range(2)], W=["%s_%d" % (dkey, tc)], dma="zo%d" % cb_)
        P.barrier()
        P.release()
    P.barrier()
    P.release()
    if "z1" in dbg:
        tz1 = dbg_out("z1", [S, 512])
        P.mark()
        tz = P.sb([128, 512], F32, "tz")
        for i in range(NT):
            P.add("sp", lambda e, i=i: e.dma_start(out=tz, in_=z1_d[i * 128:(i + 1) * 128, :]), R=Z1K, W=["tz"], dma="dbg0")
            P.add("sp", lambda e, i=i: e.dma_start(out=tz1[i * 128:(i + 1) * 128, :], in_=tz), R=["tz"], W=["dbgo"], dma="dbg1")
        P.barrier()
        P.release()
    if "h_out" in dbg:
        tho = dbg_out("h_out", [OWN, 512])
        P.mark()
        tz_ = P.sb([128, 512], F32, "tz_")
        for i in range(NTO):
            P.add("sp", lambda e, i=i: e.dma_start(out=tz_, in_=hout_d[i * 128:(i + 1) * 128, :]), R=["hout_d_%d" % k for k in range(8)], W=["tz_"], dma="dbg0")
            P.add("sp", lambda e, i=i: e.dma_start(out=tho[i * 128:(i + 1) * 128, :], in_=tz_), R=["tz_"], W=["dbgo"], dma="dbg1")
        P.barrier()
        P.release()


def hyena_tables(half):
    N = 8192
    f1 = np.arange(64, dtype=np.float64)
    s1 = np.arange(64, dtype=np.float64)
    s2 = np.arange(64, dtype=np.float64)
    th = 2 * np.pi * (f1[None, None, :] + 0.5) * (64 * s1[:, None, None] + s2[None, :, None]) / N
    D0 = np.concatenate([np.cos(th), -np.sin(th)], axis=2)
    perm = (np.arange(64) + 32 * half) % 64
    Dc = D0[perm]
    f2 = np.arange(64, dtype=np.float64)
    ph = 2 * np.pi * np.outer(s2, f2) / 64
    c, s_ = np.cos(ph), np.sin(ph)
    F2a = np.block([[c, -s_], [s_, c]])
    F2b = np.block([[s_, c], [-c, s_]])
    G = np.block([[c, s_], [-s_, c]])
    thi = 2 * np.pi * (f1[:, None, None] + 0.5) * (64 * s1[None, None, :] + s2[None, :, None]) / N
    Dinv0 = np.concatenate([np.cos(thi), -np.sin(thi)], axis=0) * (2.0 / N)
    Dinv = Dinv0[:, :, perm]
    sgn = np.ones((128, 1)); sgn[64:] = -1
    L = S
    t = np.arange(L, dtype=np.float32)
    t01 = t / np.float32(L)
    bands = np.linspace(1e-4, 15, 16, dtype=np.float32)
    ang = (np.float32(2.0 * math.pi) * t[:, None] * bands[None, :] / np.float32(L)).astype(np.float32)
    z = np.concatenate([t01[:, None], np.cos(ang), -np.sin(ang)], axis=-1).astype(np.float32)
    nt01 = (-t01).reshape(NT, 128).T
    f = np.float32
    return {
        "hy_D0": np.ascontiguousarray(D0.reshape(64, 64 * 128).astype(f)), "hy_Dc": np.ascontiguousarray(Dc.reshape(64, 64 * 128).astype(f)),
        "hy_F2a": np.ascontiguousarray(F2a.astype(f)), "hy_F2b": np.ascontiguousarray(F2b.astype(f)), "hy_G": np.ascontiguousarray(G.astype(f)),
        "hy_Dinv": np.ascontiguousarray(Dinv.reshape(128, 64 * 64).astype(f)), "hy_sgn": sgn.astype(f),
        "hy_zT": np.ascontiguousarray(z.T), "hy_nt01": np.ascontiguousarray(nt01.astype(f)),
    }


def host_inputs(inputs, core):
    b, half = divmod(core, 2)
    f32 = np.float32
    x = np.asarray(inputs["x"], dtype=f32)[b]
    own = slice(half * OWN, (half + 1) * OWN)
    oth = slice((1 - half) * OWN, (2 - half) * OWN)
    pos = np.concatenate([np.arange(S)[own], np.arange(S)[oth]])
    m = {}
    m["x_rot"] = np.ascontiguousarray(np.concatenate([x[own], x[oth]], axis=0))
    m["mem_b"] = np.ascontiguousarray(np.asarray(inputs["mem"], dtype=f32)[b])
    for k in ("mix_norm_g", "q_norm_g", "kv_norm_g", "hy_conv_b", "attn_out_g", "hy_out_g", "cross_norm_g",
              "mem_norm_g", "ffn_norm_g"):
        m[k] = np.ascontiguousarray(np.asarray(inputs[k], dtype=f32).reshape(1, -1))
    m["final_norm_g"] = np.ascontiguousarray(np.asarray(inputs["final_norm_g"], dtype=f32).reshape(1, -1))
    for k in ("w_in", "w_uq", "w_ukv", "hy_conv_w", "w_out", "w_mq", "w_mkv", "w_mo"):
        m[k] = np.ascontiguousarray(np.asarray(inputs[k], dtype=f32)[0])
    m["w_route"] = np.ascontiguousarray(np.concatenate([np.asarray(inputs["w_route_group"], f32)[0],
                                                        np.asarray(inputs["w_route_expert"], f32)[0]], axis=1))
    m["b_route"] = np.ascontiguousarray(np.concatenate([np.asarray(inputs["b_route_group"], f32)[0],
                                                        np.asarray(inputs["b_route_expert"], f32)[0]], axis=0).reshape(1, 36))
    m["w_gate"] = np.ascontiguousarray(np.asarray(inputs["w_gate"], f32)[0].reshape(32, D, 256))
    m["w_up"] = np.ascontiguousarray(np.asarray(inputs["w_up"], f32)[0].reshape(32, D, 256))
    m["w_down"] = np.ascontiguousarray(np.asarray(inputs["w_down"], f32)[0].reshape(32, 256, D))
    m["ident"] = np.eye(128, dtype=f32)
    inv = (10000.0 ** (-np.arange(16, dtype=np.float64) / 16)).astype(f32)
    ang = pos.astype(f32)[:, None] * inv[None, :]
    cs = np.concatenate([np.cos(ang), np.sin(ang)], axis=1).astype(f32)
    m["rope_cs"] = np.ascontiguousarray(cs.reshape(NT, 128, 32).transpose(1, 0, 2).reshape(128, NT * 32))
    m.update(hyena_tables(half))
    m["hy_cols"] = np.ascontiguousarray(np.stack([np.asarray(inputs["hy_b1"], f32)[0], np.asarray(inputs["hy_b2"], f32)[0],
                                                  np.asarray(inputs["hy_freq"], f32)[0, 0], np.asarray(inputs["hy_freq"], f32)[0, 1]], axis=1))
    for k in ("hy_w1", "hy_w2", "hy_w3"):
        m[k] = np.ascontiguousarray(np.asarray(inputs[k], f32)[0])
    m["hy_b3"] = np.ascontiguousarray(np.asarray(inputs["hy_b3"], f32).reshape(1, 2048))
    m["hy_decay"] = np.ascontiguousarray(np.asarray(inputs["hy_decay"], f32).reshape(1, 2048))
    m["hy_skip"] = np.ascontiguousarray(np.asarray(inputs["hy_skip"], f32).reshape(1, 1024))
    cw_ = np.asarray(inputs["hy_conv_w"], f32)[0]
    m["hy_conv_wT"] = np.ascontiguousarray(cw_.reshape(3, 12, 128).transpose(2, 1, 0).reshape(128, 36))
    m["hy_conv_bT"] = np.ascontiguousarray(np.asarray(inputs["hy_conv_b"], f32)[0].reshape(12, 128).T)
    hm = np.zeros((128, 2), f32)
    hm[:, 0] = half
    hm[:, 1] = 1 - half
    m["halfmask"] = hm
    return m


def kernel(**inputs):
    n = 8
    nc, _ = build()
    in_maps = [host_inputs(inputs, c) for c in range(n)]
    res = run_bass_kernel_spmd(nc, in_maps, core_ids=list(range(n)))
    out = np.zeros((4, S, D), np.float32)
    for c in range(n):
        b, half = divmod(c, 2)
        out[b, half * OWN:(half + 1) * OWN] = res.results[c]["out"]
    return out
```

```python
import math
import os
import contextlib
import numpy as np
import concourse.bass as bass
import concourse.mybir as mybir
from concourse.bass_utils import run_bass_kernel_spmd

F32 = mybir.dt.float32
BF16 = mybir.dt.bfloat16
AF = mybir.ActivationFunctionType
ALU = mybir.AluOpType
AX = mybir.AxisListType
ENGS = ("pe", "act", "dve", "pool", "sp")

D = 1024
S = 4096
OWN = 2048
NT = 32
NTO = 16
EPS = 1e-6
HD = 96
NH = 8


class Op:
    __slots__ = ("eng", "fn", "deps", "dma", "flag", "seq", "idx", "dmaval")

    def __init__(self, eng, fn, dma):
        self.eng = eng
        self.fn = fn
        self.deps = set()
        self.dma = dma
        self.flag = False
        self.seq = 0
        self.dmaval = 0


class Prog:
    ARENA_WORDS = 52000

    def __init__(self, nc):
        self.nc = nc
        self.ops = []
        self.lastw = {}
        self.readers = {}
        self.dma_count = {}
        self.sb_off = 0
        self.sb_marks = []
        self.arena = None
        self.dma_slots = {}

    def sb(self, shape, dtype, name=None):
        if self.arena is None:
            self.arena = self.nc.alloc_sbuf_tensor("arena", [128, self.ARENA_WORDS], F32)
        esz = 4 if dtype == F32 else 2
        nel = int(np.prod(shape[1:]))
        nwords = (nel * esz + 3) // 4
        nwords = (nwords + 15) // 16 * 16
        o = self.sb_off
        self.sb_off += nwords
        assert self.sb_off <= self.ARENA_WORDS, ("SBUF overflow", self.sb_off * 4, name)
        v = self.arena[0:shape[0], o:o + nwords]
        if esz == 2:
            v = v.bitcast(dtype)[:, 0:nel]
        else:
            v = v[:, 0:nel]
        if len(shape) > 2:
            names = " ".join("a%d" % i for i in range(len(shape) - 1))
            kw = {"a%d" % i: int(shape[i + 1]) for i in range(len(shape) - 1)}
            v = v.rearrange("p (%s) -> p %s" % (names, names), **kw)
        return v

    def mark(self):
        self.sb_marks.append(self.sb_off)

    def release(self):
        self.sb_off = self.sb_marks.pop()

    def add(self, eng, fn, R=(), W=(), dma=None):
        if dma is not None:
            slots = self.dma_slots.setdefault(eng, {"free": [], "n": 0, "map": {}})
            if dma not in slots["map"]:
                if slots["free"]:
                    slots["map"][dma] = slots["free"].pop()
                else:
                    slots["map"][dma] = slots["n"]
                    slots["n"] += 1
            dma = (eng, slots["map"][dma])
        op = Op(eng, fn, dma)
        op.idx = len(self.ops)
        if eng != "pe":
            psr = [r for r in R if isinstance(r, str) and r.startswith("ps") and r[2:].isdigit()]
            if psr:
                R = [r for r in R if r not in psr]
                W = list(W) + psr
        deps = set()
        for r in R:
            lw = self.lastw.get(r)
            if lw is not None:
                deps.add(lw)
        for w in W:
            lw = self.lastw.get(w)
            if lw is not None:
                deps.add(lw)
            for rd in self.readers.get(w, ()):
                deps.add(rd)
        if dma is not None:
            k = ("__dmasem", dma)
            lw = self.lastw.get(k)
            if lw is not None:
                deps.add(lw)
            self.lastw[k] = op
            self.dma_count[dma] = self.dma_count.get(dma, 0) + 1
            op.dmaval = 16 * self.dma_count[dma]
        deps.discard(op)
        for d in deps:
            if d.dma is None and d.eng == "pe" and eng == "pe" and dma is None:
                continue
            op.deps.add(d)
            d.flag = True
        for r in R:
            self.readers.setdefault(r, []).append(op)
        for w in W:
            self.lastw[w] = op
            self.readers[w] = []
        self.ops.append(op)
        return op

    def barrier(self):
        fr = {}
        dmas = set()
        allops = set(self.lastw.values())
        for v in self.readers.values():
            allops.update(v)
        for o in allops:
            if o.dma is not None:
                dmas.add(o)
            elif o.eng not in fr or fr[o.eng].idx < o.idx:
                fr[o.eng] = o
        for e in ENGS:
            op = Op(e, None, None)
            op.idx = len(self.ops)
            for d in list(fr.values()) + list(dmas):
                op.deps.add(d)
                d.flag = True
            self.ops.append(op)
        self.lastw = {}
        self.readers = {}
        for sl in self.dma_slots.values():
            sl["free"].extend(sl["map"].values())
            sl["map"].clear()

    def emit(self):
        nc = self.nc
        with contextlib.ExitStack() as st:
            esem = {e: st.enter_context(nc.semaphore("s_" + e)) for e in ENGS}
            dsem = {}
            for k in self.dma_count:
                dsem[k] = st.enter_context(nc.semaphore("d_%d" % len(dsem)))
            cnt = {e: 0 for e in ENGS}
            for op in self.ops:
                if op.dma is None and op.flag:
                    cnt[op.eng] += 1
                    op.seq = cnt[op.eng]
            byeng = {e: [o for o in self.ops if o.eng == e] for e in ENGS}
            if os.environ.get("KDEBUG"):
                print("sem counts", cnt, "ndma sems", len(dsem), "nops", {e: len(v) for e, v in byeng.items()})
            block = st.enter_context(nc.Block())

            def run(e, eng):
                waited = {}
                for op in byeng[e]:
                    need = {}
                    for d in op.deps:
                        if d.dma is not None:
                            s, v = dsem[d.dma], d.dmaval
                        else:
                            s, v = esem[d.eng], d.seq
                        key = id(s)
                        if waited.get(key, 0) >= v:
                            continue
                        if key not in need or need[key][1] < v:
                            need[key] = (s, v)
                    for key, (s, v) in need.items():
                        eng.wait_ge(s, v)
                        waited[key] = v
                    if op.fn is None:
                        continue
                    ins = op.fn(eng)
                    if op.dma is not None:
                        ins.then_inc(dsem[op.dma], 16)
                    elif op.flag:
                        ins.then_inc(esem[e], 1)

            @block.tensor
            def _(eng):
                run("pe", eng)

            @block.scalar
            def _(eng):
                run("act", eng)

            @block.vector
            def _(eng):
                run("dve", eng)

            @block.gpsimd
            def _(eng):
                run("pool", eng)

            @block.sync
            def _(eng):
                run("sp", eng)


INPUT_SHAPES = {
    "x_rot": [S, D], "mem_b": [256, D],
    "mix_norm_g": [1, D], "w_in": [D, 1952], "q_norm_g": [1, 256], "kv_norm_g": [1, 128],
    "w_uq": [256, 768], "w_ukv": [128, 1024], "hy_conv_w": [3, 1536], "hy_conv_b": [1, 1536],
    "attn_out_g": [1, 512], "hy_out_g": [1, 512], "w_out": [D, D],
    "cross_norm_g": [1, D], "mem_norm_g": [1, D], "w_mq": [D, D], "w_mkv": [D, 2 * D], "w_mo": [D, D],
    "ffn_norm_g": [1, D], "w_route": [D, 36], "b_route": [1, 36],
    "w_gate": [32, D, 256], "w_up": [32, D, 256], "w_down": [32, 256, D], "final_norm_g": [1, D],
    "ident": [128, 128], "rope_cs": [128, NT * 32], "halfmask": [128, 2],
    "hy_D0": [64, 64 * 128], "hy_Dc": [64, 64 * 128], "hy_F2a": [128, 128], "hy_F2b": [128, 128], "hy_G": [128, 128],
    "hy_Dinv": [128, 64 * 64], "hy_sgn": [128, 1], "hy_zT": [33, S], "hy_nt01": [128, NT],
    "hy_cols": [64, 4], "hy_w1": [33, 64], "hy_w2": [64, 64], "hy_w3": [64, 2048], "hy_b3": [1, 2048], "hy_decay": [1, 2048],
    "hy_skip": [1, 1024], "hy_conv_wT": [128, 36], "hy_conv_bT": [128, 12],
}


def build(stop=None, dbg=()):
    nc = bass.Bass("TRN2", target_bir_lowering=False)
    I = {k: nc.dram_tensor(k, v, F32, kind="ExternalInput").ap() for k, v in INPUT_SHAPES.items()}
    out_d = nc.dram_tensor("out", [OWN, D], F32, kind="ExternalOutput").ap()
    dbg_d = {}
    U_d = nc.dram_tensor("U_scr", [S, 1536], F32, kind="Internal").ap()
    hout_d = nc.dram_tensor("hout_scr", [OWN, 512], F32, kind="Internal").ap()
    combT_d = nc.dram_tensor("combT_scr", [32, OWN], F32, kind="Internal").ap()

    P = Prog(nc)
    psall = nc.alloc_psum_tensor("psall", [128, 4096], F32)
    ps = [psall[:, i * 512:(i + 1) * 512] for i in range(8)]
    psk = ["ps%d" % i for i in range(8)]

    def psb(i):
        return ps[i].bitcast(BF16)

    def dbg_out(name, shape):
        t = nc.dram_tensor("dbg_" + name, shape, F32, kind="ExternalOutput").ap()
        dbg_d[name] = t
        return t

    cnt = [0]

    def uid(s):
        cnt[0] += 1
        return "%s_%d" % (s, cnt[0])

    identf = P.sb([128, 128], F32, "identf")
    identb = P.sb([128, 128], BF16, "identb")
    halfm = P.sb([128, 2], F32, "halfm")
    st = P.sb([128, 8], F32, "st")
    junk = P.sb([128, 1024], F32, "junk")
    gb = P.sb([128, 1024], F32, "gb")
    onesb = P.sb([128, 128], BF16, "onesb")
    onesf = P.sb([128, 128], F32, "onesf")
    P.add("sp", lambda e: e.dma_start(out=identf, in_=I["ident"]), W=["identf"], dma="c0")
    P.add("sp", lambda e: e.dma_start(out=halfm, in_=I["halfmask"]), W=["halfm"], dma="c1")
    P.add("dve", lambda e: e.tensor_copy(out=identb, in_=identf), R=["identf"], W=["identb"])
    epst = P.sb([128, 1], F32, "epst")
    P.add("pool", lambda e: e.memset(epst, EPS), W=["epst"])
    P.add("pool", lambda e: e.memset(onesb, 1.0), W=["onesb"])
    P.add("pool", lambda e: e.memset(onesf, 1.0), W=["onesf"])

    def pipeline(n, stages):
        K_ = len(stages)
        for s_ in range(n + K_ - 1):
            for k_ in range(K_):
                i_ = s_ - k_
                if 0 <= i_ < n:
                    stages[k_](i_)

    def load_gain(name, n=D, key="gb"):
        P.add("sp", lambda e: e.dma_start(out=gb[:, 0:n], in_=I[name].partition_broadcast(128)), W=[key], dma="gain")

    st_tiles = {}
    for _n in ("st", "stq", "stk", "sta", "sth", "stm", "stx", "stf", "stg"):
        st_tiles[_n] = P.sb([128, 4], F32, "st_" + _n)

    def rms_norm(src, n, gview, out_bf, Rk, Wk, stk="st"):
        if stk not in st_tiles:
            st_tiles[stk] = P.sb([128, 4], F32, "st_" + stk)
        st = st_tiles[stk]
        P.add("act", lambda e: e.activation(out=junk[:, 0:n], in_=src, func=AF.Square, accum_out=st[:, 0:1]),
              R=Rk, W=["junk", stk + "0"])
        P.add("act", lambda e: e.activation(out=st[:, 2:3], in_=st[:, 0:1], func=AF.Sqrt, scale=1.0 / n, bias=epst[:, 0:1]),
              R=[stk + "0", "epst"], W=[stk + "2"])
        P.add("dve", lambda e: e.reciprocal(out=st[:, 3:4], in_=st[:, 2:3]), R=[stk + "2"], W=[stk + "3"])
        P.add("dve", lambda e: e.scalar_tensor_tensor(out=out_bf, in0=src, scalar=st[:, 3:4], in1=gview,
                                                      op0=ALU.mult, op1=ALU.mult),
              R=list(Rk) + [stk + "3", "gb"], W=Wk)

    P.mark()
    hqT = P.sb([128, 2, OWN], BF16, "hqT")
    hkvT = P.sb([128, S], BF16, "hkvT")
    krot = P.sb([128, NT, 32], F32, "krot")
    ropecs = P.sb([128, NT, 32], F32, "ropecs")
    P.add("sp", lambda e: e.dma_start(out=ropecs.rearrange("p a b -> p (a b)"), in_=I["rope_cs"]), W=["ropecs"], dma="c2")

    P.mark()
    hT = P.sb([128, 8, 2, OWN + 2], BF16, "hT")
    P.mark()
    xt = [P.sb([128, D], F32, "xt%d" % i) for i in range(4)]
    xn = [P.sb([128, D], BF16, "xn%d" % i) for i in range(4)]
    load_gain("mix_norm_g")
    def a1S0(i):
        b = i % 4
        P.add("sp", lambda e, i=i, b=b: e.dma_start(out=xt[b], in_=I["x_rot"][i * 128:(i + 1) * 128, :]),
              W=["xt%d" % b], dma="xt%d" % b)
        rms_norm(xt[b], D, gb, xn[b], ["xt%d" % b], ["xn%d" % b])

    def a1S1(i):
        b = i % 4
        pb = i % 4
        for k in range(8):
            P.add("pe", lambda e, k=k, b=b, pb=pb: e.transpose(out=psb(pb)[:, k * 128:(k + 1) * 128],
                                                                 in_=xn[b][:, k * 128:(k + 1) * 128], identity=identb),
                  R=["xn%d" % b, "identb"], W=[psk[pb]])

    def a1S2(i):
        pb = i % 4
        seg, j = divmod(i, NTO)
        dst = hT[:, :, seg, 1 + j * 128:1 + (j + 1) * 128]
        src = psb(pb).rearrange("p (k t) -> p k t", k=8)
        if i % 2 == 0:
            P.add("act", lambda e, dst=dst, src=src: e.copy(out=dst, in_=src), R=[psk[pb]], W=["hT"])
        else:
            P.add("dve", lambda e, dst=dst, src=src: e.tensor_copy(out=dst, in_=src), R=[psk[pb]], W=["hT"])

    pipeline(NT, [a1S0, a1S1, a1S2])
    for (ds, dc, ss_, sc, m) in ((0, 0, 1, OWN, 0), (0, OWN + 1, 1, 1, 1), (1, 0, 0, OWN, 1), (1, OWN + 1, 0, 1, 0)):
        P.add("dve", lambda e, ds=ds, dc=dc, ss_=ss_, sc=sc, m=m: e.tensor_scalar_mul(
            out=hT[:, :, ds, dc:dc + 1], in0=hT[:, :, ss_, sc:sc + 1], scalar1=halfm[:, m:m + 1]),
            R=["hT", "halfm"], W=["hT"])

    P.barrier()
    P.release()
    P.mark()
    w_mla = P.sb([128, 8, 416], BF16, "w_mla")
    P.add("pool", lambda e: e.dma_start(out=w_mla, in_=I["w_in"][:, 0:416].rearrange("(k p) n -> p k n", p=128)),
          W=["w_mla"], dma="w0")
    gq = P.sb([128, 256], F32, "gq")
    gkv = P.sb([128, 128], F32, "gkv")
    P.add("sp", lambda e: e.dma_start(out=gq, in_=I["q_norm_g"].partition_broadcast(128)), W=["gq"], dma="c3")
    P.add("sp", lambda e: e.dma_start(out=gkv, in_=I["kv_norm_g"].partition_broadcast(128)), W=["gkv"], dma="c4")
    hqn = [P.sb([128, 256], BF16, "hqn%d" % i) for i in range(3)]
    hkvn = [P.sb([128, 128], BF16, "hkvn%d" % i) for i in range(3)]
    tmp16 = P.sb([128, 4, 16], F32, "tmp16")
    def a2S0(i):
        seg, j = divmod(i, NTO)
        pb = i % 3
        for k in range(8):
            P.add("pe", lambda e, k=k, pb=pb, seg=seg, j=j: e.matmul(
                ps[pb][:, 0:416], lhsT=hT[:, k, seg, 1 + j * 128:1 + (j + 1) * 128], rhs=w_mla[:, k, :],
                start=(k == 0), stop=(k == 7)), R=["hT", "w_mla"], W=[psk[pb]])

    def a2S1(i):
        seg, j = divmod(i, NTO)
        pb = i % 3
        b = i % 3
        if seg == 0:
            rms_norm(ps[pb][:, 0:256], 256, gq, hqn[b], [psk[pb], "gq"], ["hqn%d" % b], stk="stq")
        rms_norm(ps[pb][:, 256:384], 128, gkv, hkvn[b], [psk[pb], "gkv"], ["hkvn%d" % b], stk="stk")
        x1 = ps[pb][:, 384:400]
        x2 = ps[pb][:, 400:416]
        c = ropecs[:, i, 0:16]
        s_ = ropecs[:, i, 16:32]
        P.add("dve", lambda e, x1=x1, c=c: e.tensor_tensor(out=tmp16[:, 0, :], in0=x1, in1=c, op=ALU.mult), R=[psk[pb], "ropecs"], W=["t16a"])
        P.add("dve", lambda e, x2=x2, s_=s_: e.tensor_tensor(out=tmp16[:, 1, :], in0=x2, in1=s_, op=ALU.mult), R=[psk[pb], "ropecs"], W=["t16b"])
        P.add("dve", lambda e, x1=x1, s_=s_: e.tensor_tensor(out=tmp16[:, 2, :], in0=x1, in1=s_, op=ALU.mult), R=[psk[pb], "ropecs"], W=["t16c"])
        P.add("dve", lambda e, x2=x2, c=c: e.tensor_tensor(out=tmp16[:, 3, :], in0=x2, in1=c, op=ALU.mult), R=[psk[pb], "ropecs"], W=["t16d"])
        P.add("dve", lambda e, i=i: e.tensor_tensor(out=krot[:, i, 0:16], in0=tmp16[:, 0, :], in1=tmp16[:, 1, :], op=ALU.subtract),
              R=["t16a", "t16b"], W=["krot"])
        P.add("dve", lambda e, i=i: e.tensor_tensor(out=krot[:, i, 16:32], in0=tmp16[:, 2, :], in1=tmp16[:, 3, :], op=ALU.add),
              R=["t16c", "t16d"], W=["krot"])

    def a2S2(i):
        seg, j = divmod(i, NTO)
        b = i % 3
        pt = 4 + i % 2
        if seg == 0:
            for k in range(2):
                P.add("pe", lambda e, k=k, b=b, pt=pt: e.transpose(out=psb(pt)[:, k * 128:(k + 1) * 128],
                                                                     in_=hqn[b][:, k * 128:(k + 1) * 128], identity=identb),
                      R=["hqn%d" % b, "identb"], W=[psk[pt]])
        P.add("pe", lambda e, b=b, pt=pt: e.transpose(out=psb(pt)[:, 256:384], in_=hkvn[b], identity=identb),
              R=["hkvn%d" % b, "identb"], W=[psk[pt]])

    def a2S3(i):
        seg, j = divmod(i, NTO)
        pt = 4 + i % 2
        if seg == 0:
            P.add("act", lambda e, pt=pt, j=j: e.copy(out=hqT[:, :, j * 128:(j + 1) * 128],
                                                       in_=psb(pt)[:, 0:256].rearrange("p (k t) -> p k t", k=2)),
                  R=[psk[pt]], W=["hqT"])
        P.add("act", lambda e, pt=pt, i=i: e.copy(out=hkvT[:, i * 128:(i + 1) * 128], in_=psb(pt)[:, 256:384]),
              R=[psk[pt]], W=["hkvT"])

    pipeline(NT, [a2S0, a2S1, a2S2, a2S3])

    P.barrier()
    P.release()
    w_hy = P.sb([128, 8, 512], BF16, "w_hy")
    cwT = P.sb([128, 12, 3], F32, "cwT")
    cbT = P.sb([128, 12], F32, "cbT")
    uT = [P.sb([128, 2, OWN + 2], F32, "uT0")] * 2
    tTc = P.sb([128, 4, 2, OWN], F32, "tTc")
    tmpP = P.sb([128, OWN], F32, "tmpP")
    uo = [P.sb([128, 512], F32, "uo%d" % i) for i in range(2)]
    P.add("sp", lambda e: e.dma_start(out=cwT.rearrange("p a b -> p (a b)"), in_=I["hy_conv_wT"]), W=["cwT"], dma="cw0")
    P.add("sp", lambda e: e.dma_start(out=cbT, in_=I["hy_conv_bT"]), W=["cbT"], dma="cw1")
    nev = 0
    for c3 in range(3):
        c0 = 416 + c3 * 512
        P.add("pool", lambda e, c0=c0: e.dma_start(out=w_hy, in_=I["w_in"][:, c0:c0 + 512].rearrange("(k p) n -> p k n", p=128)),
              W=["w_hy"], dma="w1")
        for c4 in range(4):
            ct = c3 * 4 + c4
            ub = 0
            u_ = uT[ub]
            for seg in range(2):
                for tc in range(4):
                    pb = 2 + (nev % 4)
                    for k in range(8):
                        P.add("pe", lambda e, k=k, pb=pb, seg=seg, tc=tc, c4=c4: e.matmul(
                            ps[pb][:, :], lhsT=w_hy[:, k, c4 * 128:(c4 + 1) * 128], rhs=hT[:, k, seg, 1 + tc * 512:1 + (tc + 1) * 512],
                            start=(k == 0), stop=(k == 7)), R=["hT", "w_hy"], W=[psk[pb]])
                    dst = u_[:, seg, 1 + tc * 512:1 + (tc + 1) * 512]
                    if nev % 2 == 0:
                        P.add("act", lambda e, pb=pb, dst=dst: e.copy(out=dst, in_=ps[pb][:, :]), R=[psk[pb]], W=["uT%d" % ub])
                    else:
                        P.add("dve", lambda e, pb=pb, dst=dst: e.tensor_copy(out=dst, in_=ps[pb][:, :]), R=[psk[pb]], W=["uT%d" % ub])
                    nev += 1
            for (ds, dc, ss_, sc, m) in ((0, 0, 1, OWN, 0), (0, OWN + 1, 1, 1, 1), (1, 0, 0, OWN, 1), (1, OWN + 1, 0, 1, 0)):
                P.add("dve", lambda e, u_=u_, ds=ds, dc=dc, ss_=ss_, sc=sc, m=m: e.tensor_scalar_mul(
                    out=u_[:, ds, dc:dc + 1], in0=u_[:, ss_, sc:sc + 1], scalar1=halfm[:, m:m + 1]),
                    R=["uT%d" % ub, "halfm"], W=["uT%d" % ub])
            for seg in range(2):
                t_ = tTc[:, c4, seg, :]
                P.add("act", lambda e, u_=u_, seg=seg, t_=t_, ct=ct: e.activation(out=t_, in_=u_[:, seg, 1:OWN + 1], func=AF.Identity,
                                                                               scale=cwT[:, ct, 1:2], bias=cbT[:, ct:ct + 1]),
                      R=["uT%d" % ub, "cwT", "cbT"], W=["tTc_%d_%d" % (c4, seg)])
                P.add("dve", lambda e, u_=u_, seg=seg, t_=t_, ct=ct: e.scalar_tensor_tensor(out=t_, in0=u_[:, seg, 0:OWN], scalar=cwT[:, ct, 0:1], in1=t_,
                                                                                         op0=ALU.mult, op1=ALU.add),
                      R=["uT%d" % ub, "cwT", "tTc_%d_%d" % (c4, seg)], W=["tTc_%d_%d" % (c4, seg)])
                P.add("act", lambda e, u_=u_, seg=seg, ct=ct: e.activation(out=tmpP, in_=u_[:, seg, 2:OWN + 2], func=AF.Copy, scale=cwT[:, ct, 2:3]),
                      R=["uT%d" % ub, "cwT"], W=["tmpP"])
                P.add("pool", lambda e, t_=t_: e.tensor_tensor(out=t_, in0=t_, in1=tmpP, op=ALU.add),
                      R=["tmpP", "tTc_%d_%d" % (c4, seg)], W=["tTc_%d_%d" % (c4, seg)])
        for i in range(NT):
            b = i % 2
            seg, j = divmod(i, NTO)
            pb = 6 + b
            for c4 in range(4):
                P.add("pe", lambda e, c4=c4, seg=seg, j=j, pb=pb: e.transpose(out=ps[pb][:, c4 * 128:(c4 + 1) * 128],
                                                                           in_=tTc[:, c4, seg, j * 128:(j + 1) * 128], identity=identf),
                      R=["tTc_%d_%d" % (c4, seg), "identf"], W=[psk[pb]])
            if b == 0:
                P.add("act", lambda e, pb=pb, b=b: e.copy(out=uo[b], in_=ps[pb][:, :]), R=[psk[pb]], W=["uo%d" % b])
            else:
                P.add("dve", lambda e, pb=pb, b=b: e.tensor_copy(out=uo[b], in_=ps[pb][:, :]), R=[psk[pb]], W=["uo%d" % b])
            P.add("sp", lambda e, i=i, b=b, c3=c3: e.dma_start(out=U_d[i * 128:(i + 1) * 128, c3 * 512:(c3 + 1) * 512], in_=uo[b]),
                  R=["uo%d" % b], W=["U_d"], dma="uo%d" % b)
    P.barrier()
    P.release()

    if stop == "A0":
        P.add("sp", None, R=[])
        P.emit()
        return nc, dbg_d
    if "uc" in dbg:
        tu = dbg_out("uc", [S, 1536])
        P.mark()
        tb = P.sb([128, 1536], F32, "dbgt")
        for i in range(NT):
            P.add("sp", lambda e, i=i: e.dma_start(out=tb, in_=U_d[i * 128:(i + 1) * 128, :]), R=["U_d"], W=["dbgt"], dma="dbg0")
            P.add("sp", lambda e, i=i: e.dma_start(out=tu[i * 128:(i + 1) * 128, :], in_=tb), R=["dbgt"], W=["dbgo"], dma="dbg1")
        P.barrier()
        P.release()

    if stop == "A":
        P.add("sp", None, R=["dbgo"])
        P.emit()
        return nc, dbg_d
    aout_d = nc.dram_tensor("aout_scr", [OWN, 512], F32, kind="Internal").ap()
    P.mark()
    G4 = 8
    KT = P.sb([128, G4, S], BF16, "KT")
    QT = P.sb([128, G4, OWN], BF16, "QT")
    Vaug = P.sb([128, NT, G4, 68], BF16, "Vaug")
    w_ukv = P.sb([128, 1024], BF16, "w_ukv")
    w_uq = P.sb([128, 2, 768], BF16, "w_uq")
    P.add("pool", lambda e: e.dma_start(out=w_ukv, in_=I["w_ukv"]), W=["w_ukv"], dma="w0")
    P.add("pool", lambda e: e.dma_start(out=w_uq, in_=I["w_uq"].rearrange("(k p) n -> p k n", p=128)), W=["w_uq"], dma="w1")
    Kaug = [P.sb([128, G4, 100], BF16, "Kaug%d" % i) for i in range(2)]
    Kaug2 = P.sb([128, G4, 100], BF16, "Kaug2")
    ksq2 = [P.sb([128, G4, 96], F32, "ksq2_%d" % i) for i in range(2)]
    ksq = ksq2[0]
    kn2 = P.sb([128, G4], F32, "kn2")
    kmax = P.sb([128, G4], F32, "kmax")
    kb = P.sb([128, 4], F32, "kb")
    qs2 = [P.sb([128, G4, 96], F32, "qs%d" % i) for i in range(2)]
    qn2 = [P.sb([128, G4], F32, "qn%d" % i) for i in range(2)]
    qt42 = [P.sb([128, 4, G4, 16], F32, "qt4_0")] * 2
    PT = [P.sb([128, 512], BF16, "PT%d" % i) for i in range(3)]
    oTs = P.sb([65, 512], F32, "oTs")
    rden = P.sb([128, 4], F32, "rden")
    astage = [P.sb([128, 4, 512], F32, "astage0")] * 2
    scale = HD ** -0.5
    it = 0
    for g in range(1):
        P.add("pool", lambda e: e.memset(Vaug.rearrange("p a b c -> p (a b c)"), 1.0), W=["Vaug"])
        for b in range(2):
            P.add("pool", lambda e, b=b: e.memset(Kaug[b].rearrange("p a b -> p (a b)"), 1.0), W=["Kaug%d" % b])
        P.add("pool", lambda e: e.memset(kmax, 0.0), W=["kmax"])
        ND = 3
        KaugN = [Kaug[0], Kaug[1], Kaug2]
        P.add("pool", lambda e: e.memset(Kaug2.rearrange("p a b -> p (a b)"), 1.0), W=["Kaug2"])

        def kS0(i):
            pbk = 2 * (i % 2)
            for hh in range(2):
                P.add("pe", lambda e, hh=hh, pbk=pbk, i=i: e.matmul(ps[pbk + hh][:, :], lhsT=hkvT[:, i * 128:(i + 1) * 128],
                                                                     rhs=w_ukv[:, hh * 512:(hh + 1) * 512], start=True, stop=True),
                      R=["hkvT", "w_ukv"], W=[psk[pbk + hh]])

        def kS1(i):
            pbk = 2 * (i % 2)
            kb_ = i % ND
            Ka = KaugN[kb_]
            v = psall[:, pbk * 512:(pbk + 2) * 512].rearrange("p (h c) -> p h c", h=8)
            P.add("act", lambda e, v=v, i=i: e.copy(out=Vaug[:, i, :, 0:64], in_=v[:, :, 64:128]), R=[psk[pbk], psk[pbk + 1]], W=["Vaug"])
            P.add("dve", lambda e, v=v, Ka=Ka: e.tensor_copy(out=Ka[:, :, 0:64], in_=v[:, :, 0:64]), R=[psk[pbk], psk[pbk + 1]], W=["Kaug%d" % kb_])
            P.add("pool", lambda e, Ka=Ka, i=i: e.tensor_copy(out=Ka[:, :, 64:96], in_=krot[:, i:i + 1, :].broadcast_to([128, G4, 32])),
                  R=["krot"], W=["Kaug%d" % kb_])
            P.add("dve", lambda e, Ka=Ka, kb_=kb_: e.tensor_tensor(out=ksq2[kb_ % 2], in0=Ka[:, :, 0:96], in1=Ka[:, :, 0:96], op=ALU.mult),
                  R=["Kaug%d" % kb_], W=["ksq2_%d" % (kb_ % 2)])
            P.add("dve", lambda e, kb_=kb_: e.tensor_reduce(out=kn2, in_=ksq2[kb_ % 2], axis=AX.X, op=ALU.add), R=["ksq2_%d" % (kb_ % 2)], W=["kn2"])
            P.add("dve", lambda e: e.tensor_tensor(out=kmax, in0=kmax, in1=kn2, op=ALU.max), R=["kn2", "kmax"], W=["kmax"])

        def kS2(i):
            kb_ = i % ND
            Ka = KaugN[kb_]
            pt = 4 + i % 2
            for h in range(G4):
                P.add("pe", lambda e, h=h, Ka=Ka, pt=pt: e.transpose(out=psb(pt)[0:97, h * 128:(h + 1) * 128], in_=Ka[:, h, 0:97], identity=identb),
                      R=["Kaug%d" % kb_, "identb"], W=[psk[pt]])
            P.add("act", lambda e, pt=pt, i=i: e.copy(out=KT[0:97, :, i * 128:(i + 1) * 128],
                                                       in_=psb(pt)[0:97, 0:G4 * 128].rearrange("p (h t) -> p h t", h=G4)),
                  R=[psk[pt]], W=["KT"])

        pipeline(NT, [kS0, kS1, kS2])
        P.add("dve", lambda e: e.tensor_reduce(out=kb[:, 1:2], in_=kmax, axis=AX.X, op=ALU.max), R=["kmax"], W=["kb1"])
        P.add("pe", lambda e: e.transpose(out=ps[6][0:1, 0:128], in_=kb[:, 1:2], identity=identf), R=["kb1", "identf"], W=[psk[6]])
        P.add("dve", lambda e: e.tensor_reduce(out=kb[0:1, 2:3], in_=ps[6][0:1, 0:128], axis=AX.X, op=ALU.max), R=[psk[6]], W=["kb2"])
        P.add("pe", lambda e: e.matmul(ps[7][:, 0:1], lhsT=onesf[0:1, 0:128], rhs=kb[0:1, 2:3], start=True, stop=True),
              R=["kb2", "onesf"], W=[psk[7]])
        P.add("act", lambda e: e.sqrt(out=kb[:, 0:1], in_=ps[7][:, 0:1]), R=[psk[7]], W=["kb0"])
        QaugN = KaugN

        def qS0(j):
            pa = 2 * (j % 2)
            for (pq, c0, ncol) in ((pa, 0, 480), (pa + 1, 480, 288)):
                for k in range(2):
                    P.add("pe", lambda e, pq=pq, c0=c0, ncol=ncol, k=k, j=j: e.matmul(
                        ps[pq][:, 0:ncol], lhsT=hqT[:, k, j * 128:(j + 1) * 128], rhs=w_uq[:, k, c0:c0 + ncol],
                        start=(k == 0), stop=(k == 1)), R=["hqT", "w_uq"], W=[psk[pq]])

        def qS1(j):
            pa = 2 * (j % 2)
            d_ = j % 2
            qb_ = j % ND
            Qa = QaugN[qb_]
            qs_ = qs2[d_]
            q4 = qt42[d_]
            P.add("act", lambda e, pa=pa, qs_=qs_: e.mul(out=qs_[:, 0:5, :], in_=ps[pa][:, 0:480].rearrange("p (h c) -> p h c", h=5), mul=scale),
                  R=[psk[pa]], W=["qs%d" % d_])
            P.add("act", lambda e, pa=pa, qs_=qs_: e.mul(out=qs_[:, 5:8, :], in_=ps[pa + 1][:, 0:288].rearrange("p (h c) -> p h c", h=3), mul=scale),
                  R=[psk[pa + 1]], W=["qs%d" % d_])
            c = ropecs[:, j:j + 1, 0:16].broadcast_to([128, G4, 16])
            s_ = ropecs[:, j:j + 1, 16:32].broadcast_to([128, G4, 16])
            x1 = qs_[:, :, 64:80]
            x2 = qs_[:, :, 80:96]
            P.add("dve", lambda e, x1=x1, c=c, q4=q4: e.tensor_tensor(out=q4[:, 0], in0=x1, in1=c, op=ALU.mult), R=["qs%d" % d_, "ropecs"], W=["qt4a"])
            P.add("dve", lambda e, x2=x2, s_=s_, q4=q4: e.tensor_tensor(out=q4[:, 1], in0=x2, in1=s_, op=ALU.mult), R=["qs%d" % d_, "ropecs"], W=["qt4b"])
            P.add("pool", lambda e, x1=x1, s_=s_, q4=q4: e.tensor_tensor(out=q4[:, 2], in0=x1, in1=s_, op=ALU.mult), R=["qs%d" % d_, "ropecs"], W=["qt4c"])
            P.add("pool", lambda e, x2=x2, c=c, q4=q4: e.tensor_tensor(out=q4[:, 3], in0=x2, in1=c, op=ALU.mult), R=["qs%d" % d_, "ropecs"], W=["qt4d"])
            P.add("dve", lambda e, qs_=qs_, q4=q4: e.tensor_tensor(out=qs_[:, :, 64:80], in0=q4[:, 0], in1=q4[:, 1], op=ALU.subtract),
                  R=["qt4a", "qt4b"], W=["qs%d" % d_])
            P.add("dve", lambda e, qs_=qs_, q4=q4: e.tensor_tensor(out=qs_[:, :, 80:96], in0=q4[:, 2], in1=q4[:, 3], op=ALU.add),
                  R=["qt4c", "qt4d"], W=["qs%d" % d_])
            P.add("dve", lambda e, qs_=qs_, d_=d_: e.tensor_tensor(out=ksq2[d_], in0=qs_, in1=qs_, op=ALU.mult), R=["qs%d" % d_], W=["ksq2_%d" % d_])
            P.add("dve", lambda e, d_=d_: e.tensor_reduce(out=qn2[d_], in_=ksq2[d_], axis=AX.X, op=ALU.add), R=["ksq2_%d" % d_], W=["qn%d" % d_])
            P.add("act", lambda e, d_=d_: e.sqrt(out=qn2[d_], in_=qn2[d_]), R=["qn%d" % d_], W=["qn%d" % d_])
            P.add("dve", lambda e, Qa=Qa, d_=d_: e.tensor_scalar(out=Qa[:, :, 96:97], in0=qn2[d_].rearrange("p (h o) -> p h o", o=1),
                                                                 scalar1=kb[:, 0:1], scalar2=-1.0, op0=ALU.mult, op1=ALU.mult),
                  R=["qn%d" % d_, "kb0"], W=["Kaug%d" % qb_])
            P.add("act", lambda e, Qa=Qa, qs_=qs_: e.copy(out=Qa[:, :, 0:96], in_=qs_), R=["qs%d" % d_], W=["Kaug%d" % qb_])

        def qS2(j):
            qb_ = j % ND
            Qa = QaugN[qb_]
            pt = 4 + j % 2
            for h in range(G4):
                P.add("pe", lambda e, h=h, Qa=Qa, pt=pt: e.transpose(out=psb(pt)[0:97, h * 128:(h + 1) * 128], in_=Qa[:, h, 0:97], identity=identb),
                      R=["Kaug%d" % qb_, "identb"], W=[psk[pt]])
            P.add("dve", lambda e, pt=pt, j=j: e.tensor_copy(out=QT[0:97, :, j * 128:(j + 1) * 128],
                                                             in_=psb(pt)[0:97, 0:G4 * 128].rearrange("p (h t) -> p h t", h=G4)),
                  R=[psk[pt]], W=["QT"])

        pipeline(NTO, [qS0, qS1, qS2])
        items = [(qc, h, kt) for qc in range(4) for h in range(G4) for kt in range(NT)]
        LA = 2

        def emit_scores(idx):
            qc, h, kt = items[idx]
            pb_ = idx % 3
            P.add("pe", lambda e, h=h, qc=qc, kt=kt, pb_=pb_: e.matmul(
                ps[pb_][:, :], lhsT=KT[0:97, h, kt * 128:(kt + 1) * 128], rhs=QT[0:97, h, qc * 512:(qc + 1) * 512],
                start=True, stop=True), R=["KT", "QT"], W=[psk[pb_]])

        def emit_epilogue(qc, h):
            po = 6 + h % 2
            sb_ = qc % 2
            P.add("dve", lambda e, po=po: e.tensor_copy(out=oTs, in_=ps[po][0:65, :]), R=[psk[po]], W=["oTs"])
            for t4 in range(4):
                pt = 3 + (t4 % 2)
                P.add("pe", lambda e, t4=t4, pt=pt: e.transpose(out=ps[pt][:, 0:65], in_=oTs[:, t4 * 128:(t4 + 1) * 128], identity=identf[0:65, 0:65]),
                      R=["oTs", "identf"], W=[psk[pt]])
                P.add("dve", lambda e, pt=pt, t4=t4: e.reciprocal(out=rden[:, t4:t4 + 1], in_=ps[pt][:, 64:65]), R=[psk[pt]], W=["rden%d" % t4])
                P.add("dve", lambda e, pt=pt, t4=t4, h=h, sb_=sb_: e.tensor_scalar_mul(
                    out=astage[sb_][:, t4, h * 64:(h + 1) * 64], in0=ps[pt][:, 0:64], scalar1=rden[:, t4:t4 + 1]),
                    R=[psk[pt], "rden%d" % t4], W=["astage0"])
            if h == G4 - 1:
                P.add("sp", lambda e, qc=qc, g=g, sb_=sb_: e.dma_start(
                    out=aout_d[qc * 512:(qc + 1) * 512, :].rearrange("(t p) c -> p t c", p=128), in_=astage[sb_]),
                    R=["astage0"], W=["aout_d"], dma="ast0")

        for idx in range(min(LA, len(items))):
            emit_scores(idx)
        pending = None
        for idx, (qc, h, kt) in enumerate(items):
            pb_ = idx % 3
            po = 6 + h % 2
            P.add("act", lambda e, pb_=pb_: e.activation(out=PT[pb_], in_=ps[pb_][:, :], func=AF.Exp),
                  R=[psk[pb_]], W=["PT%d" % pb_])
            if idx + LA < len(items):
                emit_scores(idx + LA)
            P.add("pe", lambda e, h=h, kt=kt, pb_=pb_, po=po: e.matmul(
                ps[po][0:65, :], lhsT=Vaug[:, kt, h, 0:65], rhs=PT[pb_], start=(kt == 0), stop=(kt == NT - 1)),
                R=["Vaug", "PT%d" % pb_], W=[psk[po]])
            if pending is not None and kt == 3:
                emit_epilogue(*pending)
                pending = None
            if kt == NT - 1:
                pending = (qc, h)
        if pending is not None:
            emit_epilogue(*pending)
    P.barrier()
    P.release()
    P.release()

    if "a_out" in dbg:
        ta = dbg_out("a_out", [OWN, 512])
        P.mark()
        tba = P.sb([128, NTO, 512], F32, "dbgt2")
        P.add("sp", lambda e: e.dma_start(out=tba, in_=aout_d.rearrange("(j p) c -> p j c", p=128)), R=["aout_d"], W=["dbgt2"], dma="dbg0")
        P.add("sp", lambda e: e.dma_start(out=ta.rearrange("(j p) c -> p j c", p=128), in_=tba), R=["dbgt2"], W=["dbgo"], dma="dbg1")
        P.barrier()
        P.release()

    if stop == "attn":
        P.add("sp", None, R=[])
        P.emit()
        return nc, dbg_d

    if "hout_in" in dbg:
        hin = nc.dram_tensor("dbg_hout_in", [OWN, 512], F32, kind="ExternalInput").ap()
        P.mark()
        tbh = P.sb([128, NTO, 512], F32, "tbh")
        P.add("sp", lambda e: e.dma_start(out=tbh, in_=hin.rearrange("(j p) c -> p j c", p=128)), W=["tbh"], dma="dbg0")
        P.add("sp", lambda e: e.dma_start(out=hout_d.rearrange("(j p) c -> p j c", p=128), in_=tbh), R=["tbh"], W=["hout_d"], dma="dbg1")
        P.barrier()
        P.release()
    else:
        hyena_phase(nc, P, I, ps, psk, psb, U_d, hout_d, identf, identb, onesb, onesf, halfm, dbg, dbg_out, psall)

    if stop == "C":
        P.add("sp", None, R=[])
        P.emit()
        return nc, dbg_d
    xres = P.sb([128, NTO, D], F32, "xres")
    P.add("sp", lambda e: e.dma_start(out=xres, in_=I["x_rot"][0:OWN, :].rearrange("(j p) c -> p j c", p=128)), W=["xres"], dma="xres")
    P.mark()
    w_out = P.sb([128, 8, D], BF16, "w_out")
    P.add("pool", lambda e: e.dma_start(out=w_out, in_=I["w_out"].rearrange("(k p) n -> p k n", p=128)), W=["w_out"], dma="w0")
    P.add("sp", lambda e: e.dma_start(out=gb[:, 0:512], in_=I["attn_out_g"].partition_broadcast(128)), W=["gb"], dma="gain")
    P.add("sp", lambda e: e.dma_start(out=gb[:, 512:1024], in_=I["hy_out_g"].partition_broadcast(128)), W=["gb"], dma="gain")
    ND_ = 3
    mixin = [P.sb([128, D], F32, "mixin%d" % i) for i in range(ND_)]
    mixbf = [P.sb([128, D], BF16, "mixbf%d" % i) for i in range(ND_)]
    mT = [P.sb([128, 8, 128], BF16, "mT%d" % i) for i in range(ND_)]

    def dS0(j):
        b = j % ND_
        P.add("sp", lambda e, j=j, b=b: e.dma_start(out=mixin[b][:, 0:512], in_=aout_d[j * 128:(j + 1) * 128, :]),
              R=["aout_d"], W=["mixin%d" % b], dma="mixa%d" % b)
        P.add("sp", lambda e, j=j, b=b: e.dma_start(out=mixin[b][:, 512:1024], in_=hout_d[j * 128:(j + 1) * 128, :]),
              R=["hout_d"] + ["hout_d_%d" % k for k in range(8)], W=["mixin%d" % b], dma="mixh%d" % b)

    def dS1(j):
        b = j % ND_
        rms_norm(mixin[b][:, 0:512], 512, gb[:, 0:512], mixbf[b][:, 0:512], ["mixin%d" % b], ["mixbfa%d" % b], stk="sta")
        rms_norm(mixin[b][:, 512:1024], 512, gb[:, 512:1024], mixbf[b][:, 512:1024], ["mixin%d" % b], ["mixbfh%d" % b], stk="sth")

    def dS2(j):
        b = j % ND_
        pt = j % 2
        for k in range(8):
            P.add("pe", lambda e, k=k, b=b, pt=pt: e.transpose(out=psb(pt)[:, k * 128:(k + 1) * 128], in_=mixbf[b][:, k * 128:(k + 1) * 128], identity=identb),
                  R=["mixbfa%d" % b, "mixbfh%d" % b, "identb"], W=[psk[pt]])

    def dS3(j):
        b = j % ND_
        pt = j % 2
        P.add("act", lambda e, b=b, pt=pt: e.copy(out=mT[b].rearrange("p k t -> p (k t)"), in_=psb(pt)), R=[psk[pt]], W=["mT%d" % b])

    def dS4(j):
        b = j % ND_
        for n in range(2):
            py = 2 + 2 * (j % 2) + n
            for k in range(8):
                P.add("pe", lambda e, k=k, b=b, n=n, py=py: e.matmul(ps[py][:, :], lhsT=mT[b][:, k, :], rhs=w_out[:, k, n * 512:(n + 1) * 512],
                                                                      start=(k == 0), stop=(k == 7)), R=["mT%d" % b, "w_out"], W=[psk[py]])
            P.add("dve", lambda e, j=j, n=n, py=py: e.tensor_tensor(out=xres[:, j, n * 512:(n + 1) * 512], in0=ps[py][:, :],
                                                                     in1=xres[:, j, n * 512:(n + 1) * 512], op=ALU.add),
                  R=[psk[py], "xres"], W=["xres"])

    pipeline(NTO, [dS0, dS1, dS2, dS3, dS4])
    P.barrier()
    P.release()
    if stop == "D":
        P.add("sp", None, R=[])
        P.emit()
        return nc, dbg_d
    if "x1" in dbg:
        tx1 = dbg_out("x1", [OWN, D])
        P.add("sp", lambda e: e.dma_start(out=tx1.rearrange("(j p) c -> p j c", p=128), in_=xres), R=["xres"], W=["dbgo"], dma="dbg1")
        P.barrier()

    P.mark()
    hmT = P.sb([128, 8, 256], BF16, "hmT")
    KmT = P.sb([128, 8, 256], BF16, "KmT")
    Vm = P.sb([128, 2, 4, 260], BF16, "Vm")
    ksqm = P.sb([128, 8, 256], BF16, "ksqm")
    kbx = P.sb([1, 8], F32, "kbx")
    P.mark()
    w_mkv = P.sb([128, 8, 2 * D], BF16, "w_mkv")
    P.add("pool", lambda e: e.dma_start(out=w_mkv, in_=I["w_mkv"].rearrange("(k p) n -> p k n", p=128)), W=["w_mkv"], dma="w0")
    load_gain("mem_norm_g")
    P.add("pool", lambda e: e.memset(Vm.rearrange("p a b c -> p (a b c)"), 1.0), W=["Vm"])
    memt = [P.sb([128, D], F32, "memt%d" % i) for i in range(2)]
    membf = [P.sb([128, D], BF16, "membf%d" % i) for i in range(2)]
    for mt in range(2):
        P.add("sp", lambda e, mt=mt: e.dma_start(out=memt[mt], in_=I["mem_b"][mt * 128:(mt + 1) * 128, :]), W=["memt%d" % mt], dma="memt%d" % mt)
        rms_norm(memt[mt], D, gb, membf[mt], ["memt%d" % mt], ["membf%d" % mt], stk="stm")
        for k in range(8):
            P.add("pe", lambda e, k=k, mt=mt: e.transpose(out=psb(mt)[:, k * 128:(k + 1) * 128], in_=membf[mt][:, k * 128:(k + 1) * 128], identity=identb),
                  R=["membf%d" % mt, "identb"], W=[psk[mt]])
        P.add("act", lambda e, mt=mt: e.copy(out=hmT[:, :, mt * 128:(mt + 1) * 128], in_=psb(mt).rearrange("p (k t) -> p k t", k=8)),
              R=[psk[mt]], W=["hmT"])
    for dt in range(8):
        pk = 2 + dt % 2
        for k in range(8):
            P.add("pe", lambda e, k=k, dt=dt, pk=pk: e.matmul(ps[pk][:, 0:256], lhsT=w_mkv[:, k, dt * 128:(dt + 1) * 128], rhs=hmT[:, k, :],
                                                               start=(k == 0), stop=(k == 7)), R=["w_mkv", "hmT"], W=[psk[pk]])
        P.add("act", lambda e, dt=dt, pk=pk: e.copy(out=KmT[:, dt, :], in_=ps[pk][:, 0:256]), R=[psk[pk]], W=["KmT"])
    for mt in range(2):
        for n in range(2):
            pv = 4 + n
            for k in range(8):
                P.add("pe", lambda e, k=k, mt=mt, n=n, pv=pv: e.matmul(ps[pv][:, :], lhsT=hmT[:, k, mt * 128:(mt + 1) * 128],
                                                                        rhs=w_mkv[:, k, D + n * 512:D + (n + 1) * 512],
                                                                        start=(k == 0), stop=(k == 7)), R=["w_mkv", "hmT"], W=[psk[pv]])
            P.add("dve", lambda e, mt=mt, n=n, pv=pv: e.tensor_copy(out=Vm[:, mt, 2 * n:2 * n + 2, 0:256],
                                                                    in_=ps[pv][:, :].rearrange("p (h c) -> p h c", h=2)),
                  R=[psk[pv]], W=["Vm"])
    P.add("dve", lambda e: e.tensor_tensor(out=ksqm, in0=KmT, in1=KmT, op=ALU.mult), R=["KmT"], W=["ksqm"])
    for hh in range(4):
        for dt in range(2):
            P.add("pe", lambda e, hh=hh, dt=dt: e.matmul(ps[6][0:1, 0:256], lhsT=onesb[:, 0:1], rhs=ksqm[:, 2 * hh + dt, :],
                                                          start=(dt == 0), stop=(dt == 1)), R=["ksqm", "onesb"], W=[psk[6]])
        P.add("dve", lambda e, hh=hh: e.tensor_reduce(out=kbx[0:1, hh:hh + 1], in_=ps[6][0:1, 0:256], axis=AX.X, op=ALU.max),
              R=[psk[6]], W=["kbx%d" % hh])
    P.add("dve", lambda e: e.tensor_reduce(out=kbx[0:1, 4:5], in_=kbx[0:1, 0:4], axis=AX.X, op=ALU.max),
          R=["kbx0", "kbx1", "kbx2", "kbx3"], W=["kbx4"])
    P.add("act", lambda e: e.sqrt(out=kbx[0:1, 5:6], in_=kbx[0:1, 4:5]), R=["kbx4"], W=["kbx5"])
    P.add("dve", lambda e: e.tensor_scalar_mul(out=kbx[0:1, 6:7], in0=kbx[0:1, 5:6], scalar1=-1.04), R=["kbx5"], W=["kbx6"])
    P.barrier()
    P.release()
    w_mq = P.sb([128, 8, D], BF16, "w_mq")
    w_mo = P.sb([128, 8, D], BF16, "w_mo")
    P.add("pool", lambda e: e.dma_start(out=w_mq, in_=I["w_mq"].rearrange("(k p) n -> p k n", p=128)), W=["w_mq"], dma="w0")
    P.add("pool", lambda e: e.dma_start(out=w_mo, in_=I["w_mo"].rearrange("(k p) n -> p k n", p=128)), W=["w_mo"], dma="w1")
    load_gain("cross_norm_g")
    hxbf = [P.sb([128, D], BF16, "hxbf%d" % i) for i in range(2)]
    hxT2 = [P.sb([128, 8, 512], BF16, "hxT0")] * 2
    qT2 = [P.sb([128, 8, 512], BF16, "qT%d" % i) for i in range(2)]
    qsqx2 = [P.sb([128, 8, 512], BF16, "qsqx0")] * 2
    negm2 = [P.sb([1, 4, 512], BF16, "negm%d" % i) for i in range(2)]
    qn4 = P.sb([1, 2048], F32, "qn4")
    PTm = [P.sb([128, 2, 512], BF16, "PTm%d" % i) for i in range(2)]
    rdn = [P.sb([1, 512], F32, "rdn%d" % i) for i in range(2)]
    rdb = [P.sb([128, 512], F32, "rdb%d" % i) for i in range(2)]
    oTx2 = [P.sb([128, 8, 512], BF16, "oTx%d" % i) for i in range(2)]
    def e_front(qc):
        pq_ = qc % 2
        def eS0(t4):
            j = qc * 4 + t4
            b = t4 % 2
            rms_norm(xres[:, j, :], D, gb, hxbf[b], ["xres"], ["hxbf%d" % b], stk="stx")

        def eS1(t4):
            b = t4 % 2
            pb = t4 % 2
            for k in range(8):
                P.add("pe", lambda e, k=k, b=b, pb=pb: e.transpose(out=psb(pb)[:, k * 128:(k + 1) * 128], in_=hxbf[b][:, k * 128:(k + 1) * 128], identity=identb),
                      R=["hxbf%d" % b, "identb"], W=[psk[pb]])

        def eS2(t4):
            pb = t4 % 2
            P.add("act", lambda e, pb=pb, t4=t4: e.copy(out=hxT2[pq_][:, :, t4 * 128:(t4 + 1) * 128], in_=psb(pb).rearrange("p (k t) -> p k t", k=8)),
                  R=[psk[pb]], W=["hxT0"])

        pipeline(4, [eS0, eS1, eS2])
        for dt in range(8):
            pq = 4 + dt % 2
            for k in range(8):
                P.add("pe", lambda e, k=k, dt=dt, pq=pq: e.matmul(ps[pq][:, :], lhsT=w_mq[:, k, dt * 128:(dt + 1) * 128], rhs=hxT2[pq_][:, k, :],
                                                                   start=(k == 0), stop=(k == 7)), R=["w_mq", "hxT0"], W=[psk[pq]])
            if dt % 2 == 0:
                P.add("act", lambda e, dt=dt, pq=pq: e.mul(out=qT2[pq_][:, dt, :], in_=ps[pq][:, :], mul=1.0 / 16.0), R=[psk[pq]], W=["qT%d_%d" % (pq_, dt)])
            else:
                P.add("dve", lambda e, dt=dt, pq=pq: e.tensor_scalar_mul(out=qT2[pq_][:, dt, :], in0=ps[pq][:, :], scalar1=1.0 / 16.0), R=[psk[pq]], W=["qT%d_%d" % (pq_, dt)])
        QTK = ["qT%d_%d" % (pq_, k) for k in range(8)]
        P.add("pool", lambda e: e.tensor_tensor(out=qsqx2[pq_], in0=qT2[pq_], in1=qT2[pq_], op=ALU.mult), R=QTK, W=["qsqx0"])
        for hh in range(4):
            for dt in range(2):
                P.add("pe", lambda e, hh=hh, dt=dt: e.matmul(ps[4 + hh][0:1, :], lhsT=onesb[:, 0:1], rhs=qsqx2[pq_][:, 2 * hh + dt, :],
                                                              start=(dt == 0), stop=(dt == 1)), R=["qsqx0", "onesb"], W=[psk[4 + hh]])
        P.add("act", lambda e: e.sqrt(out=qn4, in_=psall[0:1, 4 * 512:8 * 512]), R=[psk[4], psk[5], psk[6], psk[7]], W=["qn4"])
        P.add("dve", lambda e: e.tensor_scalar_mul(out=negm2[pq_].rearrange("p a b -> p (a b)"), in0=qn4, scalar1=kbx[0:1, 6:7]), R=["qn4", "kbx6"], W=["negm%d" % pq_])


    def e_heads(qc):
        pq_ = qc % 2
        QTK = ["qT%d_%d" % (pq_, k) for k in range(8)]
        def hA(hh):
            s0 = 2 * (hh % 2)
            for mt in range(2):
                for dt in range(2):
                    P.add("pe", lambda e, hh=hh, mt=mt, dt=dt, s0=s0: e.matmul(ps[s0 + mt][:, :], lhsT=KmT[:, 2 * hh + dt, mt * 128:(mt + 1) * 128],
                                                                                rhs=qT2[pq_][:, 2 * hh + dt, :], start=(dt == 0), stop=False),
                          R=["KmT"] + QTK, W=[psk[s0 + mt]])
                P.add("pe", lambda e, hh=hh, mt=mt, s0=s0: e.matmul(ps[s0 + mt][:, :], lhsT=onesb[0:1, 0:128], rhs=negm2[pq_][0:1, hh, :], start=False, stop=True),
                      R=["negm%d" % pq_, "onesb"], W=[psk[s0 + mt]])

        def hB(hh):
            s0 = 2 * (hh % 2)
            pb_ = hh % 2
            P.add("act", lambda e, s0=s0, pb_=pb_: e.activation(out=PTm[pb_].rearrange("p a b -> p (a b)"), in_=psall[:, s0 * 512:(s0 + 2) * 512], func=AF.Exp),
                  R=[psk[s0], psk[s0 + 1]], W=["PTm%d" % pb_])

        def hC(hh):
            if hh > 0:
                hD(hh - 1)
            pb_ = hh % 2
            for dv_ in range(2):
                for mt in range(2):
                    P.add("pe", lambda e, hh=hh, mt=mt, dv_=dv_, pb_=pb_: e.matmul(ps[4 + dv_][:, :], lhsT=Vm[:, mt, hh, dv_ * 128:(dv_ + 1) * 128], rhs=PTm[pb_][:, mt, :],
                                                                                   start=(mt == 0), stop=(mt == 1)), R=["Vm", "PTm%d" % pb_], W=[psk[4 + dv_]])
            for mt in range(2):
                P.add("pe", lambda e, hh=hh, mt=mt, pb_=pb_: e.matmul(ps[6][0:1, :], lhsT=Vm[:, mt, hh, 256:257], rhs=PTm[pb_][:, mt, :], start=(mt == 0), stop=(mt == 1)),
                      R=["Vm", "PTm%d" % pb_], W=[psk[6]])
            P.add("dve", lambda e, pb_=pb_: e.reciprocal(out=rdn[pb_], in_=ps[6][0:1, :]), R=[psk[6]], W=["rdn%d" % pb_])

        def hD(hh):
            pb_ = hh % 2
            P.add("pe", lambda e, pb_=pb_: e.matmul(ps[7][:, :], lhsT=onesf[0:1, 0:128], rhs=rdn[pb_], start=True, stop=True), R=["rdn%d" % pb_, "onesf"], W=[psk[7]])
            P.add("act", lambda e, pb_=pb_: e.copy(out=rdb[pb_], in_=ps[7][:, :]), R=[psk[7]], W=["rdb%d" % pb_])
            for dv_ in range(2):
                P.add("dve", lambda e, hh=hh, dv_=dv_, pb_=pb_: e.tensor_tensor(out=oTx2[pq_][:, 2 * hh + dv_, :], in0=ps[4 + dv_][:, :], in1=rdb[pb_], op=ALU.mult),
                      R=[psk[4 + dv_], "rdb%d" % pb_], W=["oTx%d_%d" % (pq_, 2 * hh + dv_)])

        pipeline(4, [hA, hB, hC])
        hD(3)

    def e_yproj(qc):
        pq_ = qc % 2
        OTK = ["oTx%d_%d" % (pq_, k) for k in range(8)]
        for t4 in range(4):
            j = qc * 4 + t4
            for n in range(2):
                py = (0, 1, 2, 3)[(t4 * 2 + n) % 4]
                for dt in range(8):
                    P.add("pe", lambda e, dt=dt, t4=t4, n=n, py=py: e.matmul(ps[py][:, :], lhsT=oTx2[pq_][:, dt, t4 * 128:(t4 + 1) * 128],
                                                                              rhs=w_mo[:, dt, n * 512:(n + 1) * 512], start=(dt == 0), stop=(dt == 7)),
                          R=OTK + ["w_mo"], W=[psk[py]])
                P.add("dve", lambda e, j=j, n=n, py=py: e.tensor_tensor(out=xres[:, j, n * 512:(n + 1) * 512], in0=ps[py][:, :],
                                                                         in1=xres[:, j, n * 512:(n + 1) * 512], op=ALU.add),
                      R=[psk[py], "xres"], W=["xres"])

    pipeline(4, [e_front, e_heads, e_yproj])
    P.barrier()
    P.release()
    if "x2" in dbg:
        tx2 = dbg_out("x2", [OWN, D])
        P.add("sp", lambda e: e.dma_start(out=tx2.rearrange("(j p) c -> p j c", p=128), in_=xres), R=["xres"], W=["dbgo"], dma="dbg1")
        P.barrier()
    if stop == "E":
        P.add("sp", None, R=[])
        P.emit()
        return nc, dbg_d

    P.mark()
    tT = P.sb([128, 8, OWN], BF16, "tT")
    combT = P.sb([32, OWN], F32, "combT")
    P.mark()
    load_gain("ffn_norm_g")
    w_r = P.sb([128, 8, 36], F32, "w_r")
    b_r = P.sb([128, 36], F32, "b_r")
    P.add("sp", lambda e: e.dma_start(out=w_r, in_=I["w_route"].rearrange("(k p) n -> p k n", p=128)), W=["w_r"], dma="c3")
    P.add("sp", lambda e: e.dma_start(out=b_r, in_=I["b_route"].partition_broadcast(128)), W=["b_r"], dma="c4")
    tnf = [P.sb([128, D], F32, "tnf%d" % i) for i in range(2)]
    tnb = [P.sb([128, D], BF16, "tnb%d" % i) for i in range(2)]
    tTf = P.sb([128, 8, 128], F32, "tTf")
    T_ = NTO
    lg = P.sb([128, T_, 36], F32, "lg")
    gmx = P.sb([128, T_], F32, "gmx")
    oh = P.sb([128, T_, 4], F32, "oh")
    ge = P.sb([128, T_, 4], F32, "ge")
    gsm = P.sb([128, T_], F32, "gsm")
    pg = P.sb([128, T_], F32, "pg")
    tmp48 = P.sb([128, T_, 4, 8], F32, "tmp48")
    ein = P.sb([128, T_, 8], F32, "ein")
    e2 = P.sb([128, T_, 8], F32, "e2")
    mk1 = P.sb([128, T_, 8], F32, "mk1")
    mk2 = P.sb([128, T_, 8], F32, "mk2")
    m1 = P.sb([128, T_], F32, "m1")
    m2 = P.sb([128, T_], F32, "m2")
    dd = P.sb([128, T_], F32, "dd")
    p1 = P.sb([128, T_], F32, "p1")
    p2 = P.sb([128, T_], F32, "p2")
    we = P.sb([128, T_, 8], F32, "we")
    we2 = P.sb([128, T_, 8], F32, "we2")
    comb = P.sb([128, T_, 4, 8], F32, "comb")
    seq = [0]

    def dv(fn, R, W):
        P.add("dve", fn, R=R, W=W)

    for j in range(NTO):
        b = j % 2
        rms_norm(xres[:, j, :], D, gb, tnf[b], ["xres"], ["tnf%d" % b], stk="stf")
        P.add("act", lambda e, b=b: e.copy(out=tnb[b], in_=tnf[b]), R=["tnf%d" % b], W=["tnb%d" % b])
        for k in range(8):
            P.add("pe", lambda e, k=k, b=b: e.transpose(out=psb(b)[:, k * 128:(k + 1) * 128], in_=tnb[b][:, k * 128:(k + 1) * 128], identity=identb),
                  R=["tnb%d" % b, "identb"], W=[psk[b]])
        P.add("act", lambda e, b=b, j=j: e.copy(out=tT[:, :, j * 128:(j + 1) * 128], in_=psb(b).rearrange("p (k t) -> p k t", k=8)),
              R=[psk[b]], W=["tT"])
        for k in range(8):
            pf = 2 + (k // 4)
            P.add("pe", lambda e, k=k, b=b, pf=pf: e.transpose(out=ps[pf][:, (k % 4) * 128:(k % 4 + 1) * 128], in_=tnf[b][:, k * 128:(k + 1) * 128], identity=identf),
                  R=["tnf%d" % b, "identf"], W=[psk[pf]])
        for hf in range(2):
            P.add("dve" if hf == 0 else "act", (lambda e, hf=hf: e.tensor_copy(out=tTf[:, hf * 4:(hf + 1) * 4, :], in_=ps[2 + hf][:, :].rearrange("p (k t) -> p k t", k=4)))
                  if hf == 0 else (lambda e, hf=hf: e.copy(out=tTf[:, hf * 4:(hf + 1) * 4, :], in_=ps[2 + hf][:, :].rearrange("p (k t) -> p k t", k=4))),
                  R=[psk[2 + hf]], W=["tTf%d" % hf])
        for k in range(8):
            P.add("pe", lambda e, k=k: e.matmul(ps[4][:, 0:36], lhsT=tTf[:, k, :], rhs=w_r[:, k, :], start=(k == 0), stop=(k == 7)),
                  R=["tTf0", "tTf1", "w_r"], W=[psk[4]])
        dv(lambda e, j=j: e.tensor_tensor(out=lg[:, j, :], in0=ps[4][:, 0:36], in1=b_r, op=ALU.add), [psk[4], "b_r"], ["lg"])

    def col(t, n):
        return t.rearrange("p (t o) -> p t o", o=1).broadcast_to([128, T_, n])

    gl = lg[:, :, 0:4]
    el = lg[:, :, 4:36].rearrange("p t (g e) -> p t g e", g=4)
    dv(lambda e: e.tensor_reduce(out=gmx, in_=gl, axis=AX.X, op=ALU.max), ["lg"], ["gmx"])
    dv(lambda e: e.tensor_tensor(out=oh, in0=gl, in1=col(gmx, 4), op=ALU.is_equal), ["lg", "gmx"], ["oh"])
    dv(lambda e: e.tensor_tensor(out=ge, in0=gl, in1=col(gmx, 4), op=ALU.subtract), ["lg", "gmx"], ["ge"])
    P.add("act", lambda e: e.activation(out=ge, in_=ge, func=AF.Exp), R=["ge"], W=["ge"])
    dv(lambda e: e.tensor_reduce(out=gsm, in_=ge, axis=AX.X, op=ALU.add), ["ge"], ["gsm"])
    dv(lambda e: e.reciprocal(out=pg, in_=gsm), ["gsm"], ["pg"])
    dv(lambda e: e.tensor_tensor(out=tmp48, in0=el, in1=oh.rearrange("p t (g o) -> p t g o", o=1).broadcast_to([128, T_, 4, 8]), op=ALU.mult),
       ["lg", "oh"], ["tmp48"])
    dv(lambda e: e.tensor_reduce(out=ein, in_=tmp48.rearrange("p t g e -> p t e g"), axis=AX.X, op=ALU.add), ["tmp48"], ["ein"])
    dv(lambda e: e.tensor_reduce(out=m1, in_=ein, axis=AX.X, op=ALU.max), ["ein"], ["m1"])
    dv(lambda e: e.tensor_tensor(out=mk1, in0=ein, in1=col(m1, 8), op=ALU.is_equal), ["ein", "m1"], ["mk1"])
    dv(lambda e: e.scalar_tensor_tensor(out=e2, in0=mk1, scalar=-1e30, in1=ein, op0=ALU.mult, op1=ALU.add), ["mk1", "ein"], ["e2"])
    dv(lambda e: e.tensor_reduce(out=m2, in_=e2, axis=AX.X, op=ALU.max), ["e2"], ["m2"])
    dv(lambda e: e.tensor_tensor(out=mk2, in0=e2, in1=col(m2, 8), op=ALU.is_equal), ["e2", "m2"], ["mk2"])
    dv(lambda e: e.tensor_tensor(out=dd, in0=m2, in1=m1, op=ALU.subtract), ["m1", "m2"], ["dd"])
    P.add("act", lambda e: e.activation(out=dd, in_=dd, func=AF.Exp), R=["dd"], W=["dd"])
    dv(lambda e: e.tensor_scalar_add(out=p1, in0=dd, scalar1=1.0), ["dd"], ["p1"])
    dv(lambda e: e.reciprocal(out=p1, in_=p1), ["p1"], ["p1"])
    dv(lambda e: e.tensor_tensor(out=p2, in0=dd, in1=p1, op=ALU.mult), ["dd", "p1"], ["p2"])
    dv(lambda e: e.tensor_tensor(out=p1, in0=p1, in1=pg, op=ALU.mult), ["p1", "pg"], ["p1"])
    dv(lambda e: e.tensor_tensor(out=p2, in0=p2, in1=pg, op=ALU.mult), ["p2", "pg"], ["p2"])
    dv(lambda e: e.tensor_tensor(out=we, in0=mk1, in1=col(p1, 8), op=ALU.mult), ["mk1", "p1"], ["we"])
    dv(lambda e: e.tensor_tensor(out=we2, in0=mk2, in1=col(p2, 8), op=ALU.mult), ["mk2", "p2"], ["we2"])
    dv(lambda e: e.tensor_tensor(out=we, in0=we, in1=we2, op=ALU.add), ["we", "we2"], ["we"])
    dv(lambda e: e.tensor_tensor(out=comb, in0=we.rearrange("p t (o e) -> p t o e", o=1).broadcast_to([128, T_, 4, 8]),
                                 in1=oh.rearrange("p t (g o) -> p t g o", o=1).broadcast_to([128, T_, 4, 8]), op=ALU.mult), ["we", "oh"], ["comb"])
    for j in range(NTO):
        pc_ = 5 + j % 2
        P.add("pe", lambda e, j=j, pc_=pc_: e.transpose(out=ps[pc_][0:32, 0:128], in_=comb[:, j, :, :].rearrange("p g e -> p (g e)"), identity=identf),
              R=["comb", "identf"], W=[psk[pc_]])
        dv(lambda e, j=j, pc_=pc_: e.tensor_copy(out=combT[:, j * 128:(j + 1) * 128], in_=ps[pc_][0:32, 0:128]), [psk[pc_]], ["combT"])
    P.add("sp", lambda e: e.dma_start(out=combT_d, in_=combT), R=["combT"], W=["combT_d"], dma="combT")
    if "comb" in dbg:
        tcb = dbg_out("comb", [32, OWN])
        P.add("sp", lambda e: e.dma_start(out=tcb, in_=combT), R=["combT"], W=["dbgo"], dma="dbg1")
    P.barrier()
    P.release()
    NSLOT = 4
    wg = [P.sb([128, 8, 256], BF16, "wg%d" % i) for i in range(NSLOT)]
    wu = [P.sb([128, 8, 256], BF16, "wu%d" % i) for i in range(NSLOT)]
    wd = [P.sb([128, 2, D], BF16, "wd%d" % i) for i in range(NSLOT)]
    CB = [P.sb([128, OWN], F32, "CB%d" % i) for i in range(2)]
    sa = [P.sb([128, 2, 512], F32, "sa%d" % i) for i in range(2)]
    sc = [P.sb([128, 2, 512], F32, "sc%d" % i) for i in range(2)]
    mTe = [P.sb([128, 2, 512], BF16, "mTe%d" % i) for i in range(2)]
    def load_expert(e_):
        sl = e_ % NSLOT
        P.add("pool", lambda e, e_=e_, sl=sl: e.dma_start(out=wg[sl], in_=I["w_gate"][e_].rearrange("(k p) n -> p k n", p=128)), W=["wg%d" % sl], dma="wg%d" % sl)
        P.add("pool", lambda e, e_=e_, sl=sl: e.dma_start(out=wu[sl], in_=I["w_up"][e_].rearrange("(k p) n -> p k n", p=128)), W=["wu%d" % sl], dma="wu%d" % sl)
        P.add("pool", lambda e, e_=e_, sl=sl: e.dma_start(out=wd[sl], in_=I["w_down"][e_].rearrange("(k p) n -> p k n", p=128)), W=["wd%d" % sl], dma="wd%d" % sl)

    for e_ in range(2):
        load_expert(e_)
    yb = 0
    for pr in range(16):
        for ee in range(2):
            if 2 * pr + 2 + ee < 32:
                load_expert(2 * pr + 2 + ee)
        for ee in range(2):
            e_ = 2 * pr + ee
            P.add("sp", lambda e, e_=e_, ee=ee: e.dma_start(out=CB[ee], in_=combT_d[e_:e_ + 1, :].partition_broadcast(128)),
                  R=["combT_d"], W=["CB%d" % ee], dma="CB%d" % ee)
        for c in range(4):
            for ee in range(2):
                e_ = 2 * pr + ee
                sl = e_ % NSLOT
                for f in range(2):
                    for k in range(8):
                        P.add("pe", lambda e, f=f, k=k, sl=sl, c=c: e.matmul(ps[f][:, :], lhsT=wg[sl][:, k, f * 128:(f + 1) * 128],
                                                                            rhs=tT[:, k, c * 512:(c + 1) * 512], start=(k == 0), stop=(k == 7)),
                              R=["wg%d" % sl, "tT"], W=[psk[f]])
                for f in range(2):
                    for k in range(8):
                        P.add("pe", lambda e, f=f, k=k, sl=sl, c=c: e.matmul(ps[2 + f][:, :], lhsT=wu[sl][:, k, f * 128:(f + 1) * 128],
                                                                            rhs=tT[:, k, c * 512:(c + 1) * 512], start=(k == 0), stop=(k == 7)),
                              R=["wu%d" % sl, "tT"], W=[psk[2 + f]])
                for f in range(2):
                    P.add("act", lambda e, f=f, ee=ee: e.activation(out=sa[ee][:, f, :], in_=ps[f][:, :], func=AF.Silu),
                          R=[psk[f]], W=["sa%d_%d" % (ee, f)])
                    P.add("pool", lambda e, f=f, ee=ee, c=c: e.tensor_tensor(out=sc[ee][:, f, :], in0=sa[ee][:, f, :],
                                                                              in1=CB[ee][:, c * 512:(c + 1) * 512], op=ALU.mult),
                          R=["sa%d_%d" % (ee, f), "CB%d" % ee], W=["sc%d_%d" % (ee, f)])
                    P.add("dve", lambda e, f=f, ee=ee: e.tensor_tensor(out=mTe[ee][:, f, :], in0=ps[2 + f][:, :], in1=sc[ee][:, f, :], op=ALU.mult),
                          R=[psk[2 + f], "sc%d_%d" % (ee, f)], W=["mTe%d" % ee])
            for t4 in range(4):
                j = c * 4 + t4
                for n in range(2):
                    py = 4 + (yb % 4)
                    yb += 1
                    cnt_mm = 0
                    for ee in range(2):
                        sl = (2 * pr + ee) % NSLOT
                        for f in range(2):
                            P.add("pe", lambda e, ee=ee, f=f, sl=sl, t4=t4, n=n, py=py, cnt_mm=cnt_mm: e.matmul(
                                ps[py][:, :], lhsT=mTe[ee][:, f, t4 * 128:(t4 + 1) * 128], rhs=wd[sl][:, f, n * 512:(n + 1) * 512],
                                start=(cnt_mm == 0), stop=(cnt_mm == 3)), R=["mTe%d" % ee, "wd%d" % sl], W=[psk[py]])
                            cnt_mm += 1
                    P.add("dve", lambda e, j=j, n=n, py=py: e.tensor_tensor(out=xres[:, j, n * 512:(n + 1) * 512], in0=ps[py][:, :],
                                                                             in1=xres[:, j, n * 512:(n + 1) * 512], op=ALU.add),
                          R=[psk[py], "xres"], W=["xres"])
    P.barrier()
    P.release()
    if "x3" in dbg:
        tx3 = dbg_out("x3", [OWN, D])
        P.add("sp", lambda e: e.dma_start(out=tx3.rearrange("(j p) c -> p j c", p=128), in_=xres), R=["xres"], W=["dbgo"], dma="dbg1")
        P.barrier()

    load_gain("final_norm_g")
    xo = [P.sb([128, D], F32, "xo%d" % i) for i in range(2)]
    for j in range(NTO):
        b = j % 2
        rms_norm(xres[:, j, :], D, gb, xo[b], ["xres"], ["xo%d" % b], stk="stg")
        P.add("sp", lambda e, j=j, b=b: e.dma_start(out=out_d[j * 128:(j + 1) * 128, :], in_=xo[b]),
              R=["xo%d" % b], W=["out_d%d" % b], dma="xo%d" % b)
    P.add("sp", None, R=["out_d0", "out_d1", "dbgo"])
    P.emit()
    return nc, dbg_d


def hyena_phase(nc, P, I, ps, psk, psb, U_d, hout_d, identf, identb, onesb, onesf, halfm, dbg, dbg_out, psall):
    NF = 64
    Hd = nc.dram_tensor("hy_H", [S, 2048], BF16, kind="Internal").ap()
    Bd = [nc.dram_tensor("hy_B%d" % i, [128, NF, 512], BF16, kind="Internal").ap() for i in range(2)]
    Kd = [nc.dram_tensor("hy_K%d" % i, [128, NF, 512], BF16, kind="Internal").ap() for i in range(2)]
    Btd = nc.dram_tensor("hy_Bt", [128, NF, 512], BF16, kind="Internal").ap()
    z1_d = nc.dram_tensor("hy_z1", [S, 512], BF16, kind="Internal").ap()
    P.mark()
    Dt = P.sb([64, 64, 128], BF16, "Dt")
    Dinv = P.sb([128, 64, 64], BF16, "Dinv")
    F2a = P.sb([128, 128], BF16, "F2a")
    F2b = P.sb([128, 128], BF16, "F2b")
    Gm = P.sb([128, 128], BF16, "Gm")
    sgn = P.sb([128, 1], F32, "sgn")
    skipb = P.sb([128, 2, 512], F32, "skipb")
    P.add("pool", lambda e: e.dma_start(out=Dt, in_=I["hy_D0"].rearrange("a (b c) -> a b c", b=64)), W=["Dt"], dma="ht0")
    P.add("pool", lambda e: e.dma_start(out=F2a, in_=I["hy_F2a"]), W=["F2a"], dma="ht1")
    P.add("pool", lambda e: e.dma_start(out=F2b, in_=I["hy_F2b"]), W=["F2b"], dma="ht2")
    P.add("pool", lambda e: e.dma_start(out=Gm, in_=I["hy_G"]), W=["Gm"], dma="ht3")
    P.add("pool", lambda e: e.dma_start(out=Dinv, in_=I["hy_Dinv"].rearrange("a (b c) -> a b c", b=64)), W=["Dinv"], dma="ht4")
    P.add("sp", lambda e: e.dma_start(out=sgn, in_=I["hy_sgn"]), W=["sgn"], dma="c3")
    P.add("sp", lambda e: e.dma_start(out=skipb.rearrange("p a b -> p (a b)"), in_=I["hy_skip"].partition_broadcast(128)), W=["skipb"], dma="c4")

    P.mark()
    zT = P.sb([33, S], F32, "zT")
    g1T = P.sb([64, S], F32, "g1T")
    g2T = P.sb([64, S], F32, "g2T")
    hcols = P.sb([64, 4], F32, "hcols")
    w1 = P.sb([33, 64], F32, "w1")
    w2 = P.sb([64, 64], F32, "w2")
    w3 = P.sb([64, 2048], F32, "w3")
    b3r = P.sb([1, 2048], F32, "b3r")
    adec = P.sb([128, 2048], F32, "adec")
    nt01 = P.sb([128, NT], F32, "nt01")
    mpi = P.sb([128, 1], F32, "mpi")
    argt = P.sb([64, 512], F32, "argt")
    argm = P.sb([64, 512], F32, "argm")
    hfo = [P.sb([128, 2048], BF16, "hfo%d" % i) for i in range(2)]
    P.add("sp", lambda e: e.dma_start(out=zT, in_=I["hy_zT"]), W=["zT"], dma="hf0")
    P.add("sp", lambda e: e.dma_start(out=hcols, in_=I["hy_cols"]), W=["hcols"], dma="hf1")
    P.add("sp", lambda e: e.dma_start(out=w1, in_=I["hy_w1"]), W=["w1"], dma="hf2")
    P.add("sp", lambda e: e.dma_start(out=w2, in_=I["hy_w2"]), W=["w2"], dma="hf3")
    P.add("sp", lambda e: e.dma_start(out=w3, in_=I["hy_w3"]), W=["w3"], dma="hf4")
    P.add("sp", lambda e: e.dma_start(out=b3r, in_=I["hy_b3"]), W=["b3r"], dma="hf5")
    P.add("sp", lambda e: e.dma_start(out=adec, in_=I["hy_decay"].partition_broadcast(128)), W=["adec"], dma="hf6")
    P.add("sp", lambda e: e.dma_start(out=nt01, in_=I["hy_nt01"]), W=["nt01"], dma="hf7")
    P.add("pool", lambda e: e.memset(mpi, -math.pi), W=["mpi"])
    P.add("act", lambda e: e.activation(out=adec, in_=adec, func=AF.Abs), R=["adec"], W=["adec"])
    OFFS = math.pi + 16.0 * math.pi
    for (src, wt, kk, bcol, fcol, dst, nm) in ((zT, w1, 33, 0, 2, g1T, "g1T"), (g1T, w2, 64, 1, 3, g2T, "g2T")):
        for ch in range(8):
            pb_ = ch % 2
            P.add("pe", lambda e, src=src, wt=wt, kk=kk, ch=ch, pb_=pb_: e.matmul(ps[pb_][0:64, :], lhsT=wt[0:kk, :], rhs=src[0:kk, ch * 512:(ch + 1) * 512],
                                                                               start=True, stop=True), R=["zT", "g1T", "w1", "w2"], W=[psk[pb_]])
            P.add("dve", lambda e, pb_=pb_, bcol=bcol, fcol=fcol: e.tensor_scalar(out=argt, in0=ps[pb_][0:64, :], scalar1=hcols[:, bcol:bcol + 1],
                                                                                  scalar2=hcols[:, fcol:fcol + 1], op0=ALU.add, op1=ALU.mult),
                  R=[psk[pb_], "hcols"], W=["argt"])
            for _rep in range(2):
                P.add("dve", lambda e: e.tensor_scalar(out=argm, in0=argt, scalar1=math.pi, scalar2=None, op0=ALU.is_gt), R=["argt"], W=["argm"])
                P.add("dve", lambda e: e.scalar_tensor_tensor(out=argt, in0=argm, scalar=-2.0 * math.pi, in1=argt, op0=ALU.mult, op1=ALU.add),
                      R=["argm", "argt"], W=["argt"])
                P.add("dve", lambda e: e.tensor_scalar(out=argm, in0=argt, scalar1=-math.pi, scalar2=None, op0=ALU.is_lt), R=["argt"], W=["argm"])
                P.add("dve", lambda e: e.scalar_tensor_tensor(out=argt, in0=argm, scalar=2.0 * math.pi, in1=argt, op0=ALU.mult, op1=ALU.add),
                      R=["argm", "argt"], W=["argt"])
            P.add("act", lambda e, dst=dst, ch=ch: e.activation(out=dst[:, ch * 512:(ch + 1) * 512], in_=argt, func=AF.Sin),
                  R=["argt"], W=[nm])
    g2b = P.sb([64, S], BF16, "g2b")
    w3b = P.sb([64, 2048], BF16, "w3b")
    b3b = P.sb([1, 2048], BF16, "b3b")
    Etf = [P.sb([128, 2048], F32, "Etf%d" % i) for i in range(2)]
    P.add("act", lambda e: e.copy(out=g2b, in_=g2T), R=["g2T"], W=["g2b"])
    P.add("dve", lambda e: e.tensor_copy(out=w3b, in_=w3), R=["w3"], W=["w3b"])
    P.add("dve", lambda e: e.tensor_copy(out=b3b, in_=b3r), R=["b3r"], W=["b3b"])
    for i in range(NT):
        b = i % 2
        b0 = 4 * b
        for cg in range(4):
            pb_ = b0 + cg
            P.add("pe", lambda e, i=i, cg=cg, pb_=pb_: e.matmul(ps[pb_][:, :], lhsT=g2b[:, i * 128:(i + 1) * 128], rhs=w3b[:, cg * 512:(cg + 1) * 512],
                                                                 start=True, stop=False), R=["g2b", "w3b"], W=[psk[pb_]])
            P.add("pe", lambda e, cg=cg, pb_=pb_: e.matmul(ps[pb_][:, :], lhsT=onesb[0:1, 0:128], rhs=b3b[0:1, cg * 512:(cg + 1) * 512],
                                                            start=False, stop=True), R=["onesb", "b3b"], W=[psk[pb_]])
        P.add("act", lambda e, i=i, b=b: e.activation(out=Etf[b], in_=adec, func=AF.Exp, scale=nt01[:, i:i + 1]),
              R=["adec", "nt01"], W=["Etf%d" % b])
        for hf_ in range(2):
            P.add("dve", lambda e, hf_=hf_, b=b, b0=b0: e.tensor_tensor(out=hfo[b][:, hf_ * 1024:(hf_ + 1) * 1024],
                                                                      in0=psall[:, (b0 + 2 * hf_) * 512:(b0 + 2 * hf_ + 2) * 512],
                                                                      in1=Etf[b][:, hf_ * 1024:(hf_ + 1) * 1024], op=ALU.mult),
                  R=[psk[b0 + 2 * hf_], psk[b0 + 2 * hf_ + 1], "Etf%d" % b], W=["hfo%d" % b])
        if i == 0:
            for o in range(2):
                P.add("pool", lambda e, o=o: e.memset(hfo[0][0:1, o * 1024 + 512:o * 1024 + 1024], 0.0), R=[], W=["hfo0"])
        P.add("sp", lambda e, i=i, b=b: e.dma_start(out=Hd[i * 128:(i + 1) * 128, :], in_=hfo[b]), R=["hfo%d" % b], W=["Hd_%d" % i], dma="hfo%d" % b)
    P.barrier()
    P.release()
    if "hf" in dbg:
        thf = dbg_out("hf", [S, 2048])
        P.mark()
        tbf = P.sb([128, 2048], BF16, "tbf")
        tbf32 = P.sb([128, 2048], F32, "tbf32")
        for i in range(NT):
            P.add("sp", lambda e, i=i: e.dma_start(out=tbf, in_=Hd[i * 128:(i + 1) * 128, :]), R=["Hd_%d" % i], W=["tbf"], dma="dbg0")
            P.add("dve", lambda e: e.tensor_copy(out=tbf32, in_=tbf), R=["tbf"], W=["tbf32"])
            P.add("sp", lambda e, i=i: e.dma_start(out=thf[i * 128:(i + 1) * 128, :], in_=tbf32), R=["tbf32"], W=["dbgo"], dma="dbg1")
        P.barrier()
        P.release()

    ev = [0]
    HDK = ["Hd_%d" % i for i in range(NT)]
    Z1K = ["z1_d_%d" % i for i in range(8)]
    BD0K = ["Bd0_%d" % i for i in range(4)]
    BD1K = ["Bd1_%d" % i for i in range(4)]
    BTDK = ["Btd_%d" % i for i in range(8)]

    def evac(out, in_, Rk, Wk):
        ev[0] += 1
        if ev[0] % 2:
            P.add("act", lambda e: e.copy(out=out, in_=in_), R=Rk, W=Wk)
        else:
            P.add("dve", lambda e: e.tensor_copy(out=out, in_=in_), R=Rk, W=Wk)

    def stage1(src_view, cast, Bdst, bkey, srckeys):
        P.barrier()
        P.mark()
        xsb = [P.sb([64, 16, 512], BF16, "xsb%d" % i) for i in range(2)]
        Bsb = [P.sb([128, 16, 512], BF16, "Bsb%d" % i) for i in range(2)]
        def s1_load(ch):
            xb_ = ch % 2
            q = "pool" if cast else "sp"
            P.add(q, lambda e, ch=ch, xb_=xb_: e.dma_start(out=xsb[xb_], in_=src_view[:, ch * 16:(ch + 1) * 16, :]),
                  R=srckeys, W=["xsb%d" % xb_], dma="xsb%d" % xb_)

        s1_load(0)
        for ch in range(4):
            xb_ = ch % 2
            if ch + 1 < 4:
                s1_load(ch + 1)
            for g4 in range(4):
                b0 = 4 * (g4 % 2)
                for q4 in range(4):
                    s2l = g4 * 4 + q4
                    s2 = ch * 16 + s2l
                    P.add("pe", lambda e, s2=s2, s2l=s2l, xb_=xb_, pb_=b0 + q4: e.matmul(ps[pb_][:, :], lhsT=Dt[0:64, s2, :], rhs=xsb[xb_][0:64, s2l, :],
                                                                                        start=True, stop=True), R=["Dt", "xsb%d" % xb_], W=[psk[b0 + q4]])
                evac(Bsb[xb_][:, g4 * 4:(g4 + 1) * 4, :].rearrange("p a b -> p (a b)"), psall[:, b0 * 512:(b0 + 4) * 512],
                     [psk[b0 + k] for k in range(4)], ["Bsb%d_%d" % (xb_, g4)])
            P.add("sp", lambda e, ch=ch, xb_=xb_: e.dma_start(out=Bdst[:, ch * 16:(ch + 1) * 16, :], in_=Bsb[xb_]),
                  R=["Bsb%d_%d" % (xb_, k) for k in range(4)], W=["%s_%d" % (bkey, ch)], dma="Bsb%d" % xb_)
        P.barrier()
        P.release()

    def blocked(ap2d):
        return ap2d.rearrange("(s1 s2) c -> s1 s2 c", s2=64)

    for o in range(2):
        stage1(blocked(Hd[:, o * 1024:o * 1024 + 512]), False, Bd[0], "Bd0", HDK)
        stage1(blocked(Hd[:, o * 1024 + 512:o * 1024 + 1024]), False, Bd[1], "Bd1", HDK)
        P.mark()
        BTf = [P.sb([128, 8, 512], BF16, "BTf%d" % i) for i in range(2)]
        BTb = [P.sb([128, 8, 512], BF16, "BTb%d" % i) for i in range(2)]
        xfs = [P.sb([128, 1024], F32, "xfs%d" % i) for i in range(2)]
        kst = [P.sb([128, 1024], F32, "kst%d" % i) for i in range(2)]
        Kc = [P.sb([128, 8, 512], BF16, "Kc%d" % i) for i in range(2)]
        def f2_load(fc):
            cb_ = fc % 2
            for r in range(2):
                P.add("sp", lambda e, r=r, fc=fc, cb_=cb_: e.dma_start(
                    out=BTf[cb_][r * 64:(r + 1) * 64, :, :], in_=Bd[0][r * 64 + fc * 8:r * 64 + fc * 8 + 8, :, :].rearrange("f s c -> s f c")),
                    R=BD0K, W=["BTf%d" % cb_], dma="BTf%d_%d" % (cb_, r))
                P.add("sp", lambda e, r=r, fc=fc, cb_=cb_: e.dma_start(
                    out=BTb[cb_][r * 64:(r + 1) * 64, :, :], in_=Bd[1][r * 64 + fc * 8:r * 64 + fc * 8 + 8, :, :].rearrange("f s c -> s f c")),
                    R=BD1K, W=["BTb%d" % cb_], dma="BTb%d_%d" % (cb_, r))

        f2_load(0)
        for fc in range(8):
            cb_ = fc % 2
            if fc + 1 < 8:
                f2_load(fc + 1)
            for gq in range(4):
                t_ = gq % 2
                fa, fb = 2 * t_, 4 + 2 * t_
                for q2 in range(2):
                    f1l = gq * 2 + q2
                    P.add("pe", lambda e, f1l=f1l, cb_=cb_, pb_=fa + q2: e.matmul(ps[pb_][:, :], lhsT=F2a, rhs=BTf[cb_][:, f1l, :], start=True, stop=True),
                          R=["F2a", "BTf%d" % cb_], W=[psk[fa + q2]])
                    P.add("pe", lambda e, f1l=f1l, cb_=cb_, pb_=fb + q2: e.matmul(ps[pb_][:, :], lhsT=F2a, rhs=BTb[cb_][:, f1l, :], start=True, stop=True),
                          R=["F2a", "BTb%d" % cb_], W=[psk[fb + q2]])
                P.add("act", lambda e, fa=fa, t_=t_: e.copy(out=xfs[t_], in_=psall[:, fa * 512:(fa + 2) * 512]), R=[psk[fa], psk[fa + 1]], W=["xfs%d" % t_])
                P.add("dve", lambda e, fb=fb, t_=t_: e.scalar_tensor_tensor(out=kst[t_], in0=psall[:, fb * 512:(fb + 2) * 512], scalar=sgn[:, 0:1], in1=xfs[t_],
                                                                           op0=ALU.mult, op1=ALU.add),
                      R=[psk[fb], psk[fb + 1], "xfs%d" % t_, "sgn"], W=["kst%d" % t_])
                for q2 in range(2):
                    f1l = gq * 2 + q2
                    P.add("pool", lambda e, t_=t_, cb_=cb_, f1l=f1l, q2=q2, o=o: e.tensor_tensor(out=Kc[cb_][0:64, f1l, :], in0=kst[t_][0:64, q2 * 512:(q2 + 1) * 512],
                                                                                          in1=skipb[0:64, o, :], op=ALU.add),
                          R=["kst%d" % t_, "skipb"], W=["Kc%d_a%d" % (cb_, f1l)])
                P.add("act", lambda e, t_=t_, cb_=cb_, gq=gq: e.copy(out=Kc[cb_][64:128, gq * 2:gq * 2 + 2, :].rearrange("p a b -> p (a b)"), in_=kst[t_][64:128, :]),
                      R=["kst%d" % t_], W=["Kc%d_b%d" % (cb_, gq)])
            P.add("sp", lambda e, fc=fc, cb_=cb_, o=o: e.dma_start(out=Kd[o][:, fc * 8:(fc + 1) * 8, :], in_=Kc[cb_]),
                  R=["Kc%d_a%d" % (cb_, k) for k in range(8)] + ["Kc%d_b%d" % (cb_, k) for k in range(4)], W=["Kd%d_%d" % (o, fc)], dma="Kc%d" % cb_)
        P.barrier()
        P.release()
    if "kf" in dbg:
        tkf = dbg_out("kf", [2, 128, NF * 512])
        P.mark()
        tk16 = P.sb([128, 8, 512], BF16, "tk16")
        tk32 = P.sb([128, 8, 512], F32, "tk32")
        for o in range(2):
            for fc in range(8):
                P.add("sp", lambda e, o=o, fc=fc: e.dma_start(out=tk16, in_=Kd[o][:, fc * 8:(fc + 1) * 8, :]), R=["Kd%d_%d" % (o, fc)], W=["tk16"], dma="dbg0")
                P.add("dve", lambda e: e.tensor_copy(out=tk32, in_=tk16), R=["tk16"], W=["tk32"])
                P.add("sp", lambda e, o=o, fc=fc: e.dma_start(out=tkf[o][:, fc * 4096:(fc + 1) * 4096], in_=tk32.rearrange("p a b -> p (a b)")),
                      R=["tk32"], W=["dbgo"], dma="dbg1")
        P.barrier()
        P.release()

    P.add("pool", lambda e: e.dma_start(out=Dt, in_=I["hy_Dc"].rearrange("a (b c) -> a b c", b=64)), W=["Dt"], dma="ht0")
    for o in range(2):
        if o == 0:
            stage1(blocked(U_d[:, 1024:1536]), True, Bd[0], "Bd0", ["U_d"])
        else:
            stage1(blocked(z1_d), False, Bd[0], "Bd0", Z1K)
        P.mark()
        BT = [P.sb([128, 8, 512], BF16, "BT%d" % i) for i in range(2)]
        KA = [P.sb([128, 8, 512], BF16, "KA%d" % i) for i in range(2)]
        KB = [P.sb([128, 8, 512], BF16, "KB%d" % i) for i in range(2)]
        ta = [P.sb([128, 1024], F32, "ta%d" % i) for i in range(2)]
        tb2 = [P.sb([128, 1024], F32, "tb2%d" % i) for i in range(2)]
        Yc = [P.sb([128, 1024], BF16, "Yc%d" % i) for i in range(2)]
        Btsb = [P.sb([128, 8, 512], BF16, "Btsb%d" % i) for i in range(2)]
        def c2_load(fc):
            cb_ = fc % 2
            for r in range(2):
                P.add("sp", lambda e, r=r, fc=fc, cb_=cb_: e.dma_start(
                    out=BT[cb_][r * 64:(r + 1) * 64, :, :], in_=Bd[0][r * 64 + fc * 8:r * 64 + fc * 8 + 8, :, :].rearrange("f s c -> s f c")),
                    R=BD0K, W=["BT%d" % cb_], dma="BT%d_%d" % (cb_, r))
                P.add("sp", lambda e, r=r, fc=fc, cb_=cb_, o=o: e.dma_start(out=KA[cb_][r * 64:(r + 1) * 64, :, :], in_=Kd[o][0:64, fc * 8:(fc + 1) * 8, :]),
                      R=["Kd%d_%d" % (o, fc)], W=["KA%d" % cb_], dma="KA%d_%d" % (cb_, r))
                P.add("sp", lambda e, r=r, fc=fc, cb_=cb_, o=o: e.dma_start(out=KB[cb_][r * 64:(r + 1) * 64, :, :], in_=Kd[o][64:128, fc * 8:(fc + 1) * 8, :]),
                      R=["Kd%d_%d" % (o, fc)], W=["KB%d" % cb_], dma="KB%d_%d" % (cb_, r))

        c2_load(0)
        for fc in range(8):
            cb_ = fc % 2
            if fc + 1 < 8:
                c2_load(fc + 1)
            for gq in range(4):
                t_ = gq % 2
                fa, fb = 2 * t_, 4 + 2 * t_
                f0 = gq * 2
                for q2 in range(2):
                    f1l = f0 + q2
                    P.add("pe", lambda e, f1l=f1l, cb_=cb_, pb_=fa + q2: e.matmul(ps[pb_][:, :], lhsT=F2a, rhs=BT[cb_][:, f1l, :], start=True, stop=True),
                          R=["F2a", "BT%d" % cb_], W=[psk[fa + q2]])
                    P.add("pe", lambda e, f1l=f1l, cb_=cb_, pb_=fb + q2: e.matmul(ps[pb_][:, :], lhsT=F2b, rhs=BT[cb_][:, f1l, :], start=True, stop=True),
                          R=["F2b", "BT%d" % cb_], W=[psk[fb + q2]])
                P.add("dve", lambda e, fa=fa, t_=t_, cb_=cb_, f0=f0: e.tensor_tensor(out=ta[t_], in0=psall[:, fa * 512:(fa + 2) * 512],
                                                                                   in1=KA[cb_][:, f0:f0 + 2, :].rearrange("p a b -> p (a b)"), op=ALU.mult),
                      R=[psk[fa], psk[fa + 1], "KA%d" % cb_], W=["ta%d" % t_])
                P.add("dve", lambda e, fb=fb, t_=t_, cb_=cb_, f0=f0: e.tensor_tensor(out=tb2[t_], in0=psall[:, fb * 512:(fb + 2) * 512],
                                                                                   in1=KB[cb_][:, f0:f0 + 2, :].rearrange("p a b -> p (a b)"), op=ALU.mult),
                      R=[psk[fb], psk[fb + 1], "KB%d" % cb_], W=["tb2%d" % t_])
                P.add("pool", lambda e, t_=t_: e.tensor_tensor(out=Yc[t_], in0=ta[t_], in1=tb2[t_], op=ALU.add),
                      R=["ta%d" % t_, "tb2%d" % t_], W=["Yc%d" % t_])
                for q2 in range(2):
                    P.add("pe", lambda e, t_=t_, q2=q2, pb_=fa + q2: e.matmul(ps[pb_][:, :], lhsT=Gm, rhs=Yc[t_][:, q2 * 512:(q2 + 1) * 512], start=True, stop=True),
                          R=["Gm", "Yc%d" % t_], W=[psk[fa + q2]])
                P.add("act", lambda e, fa=fa, cb_=cb_, f0=f0: e.copy(out=Btsb[cb_][:, f0:f0 + 2, :].rearrange("p a b -> p (a b)"), in_=psall[:, fa * 512:(fa + 2) * 512]),
                      R=[psk[fa], psk[fa + 1]], W=["Btsb%d_%d" % (cb_, gq)])
            P.add("sp", lambda e, fc=fc, cb_=cb_: e.dma_start(out=Btd[:, fc * 8:(fc + 1) * 8, :], in_=Btsb[cb_]),
                  R=["Btsb%d_%d" % (cb_, k) for k in range(4)], W=["Btd_%d" % fc], dma="Btsb%d" % cb_)
        P.barrier()
        P.release()
        P.mark()
        BtT = [P.sb([128, 8, 512], BF16, "BtT%d" % i) for i in range(2)]
        gch = [P.sb([64, 8, 512], F32, "gch%d" % i) for i in range(2)]
        zo = [P.sb([64, 8, 512], BF16 if o == 0 else F32, "zo%d" % i) for i in range(2)]
        M = 64 if o == 0 else 32
        gcol = 0 if o == 0 else 512
        dst = blocked(z1_d) if o == 0 else blocked(hout_d)
        dkey = "z1_d" if o == 0 else "hout_d"
        def i1_load(tc):
            cb_ = tc % 2
            for r in range(2):
                P.add("sp", lambda e, r=r, tc=tc, cb_=cb_: e.dma_start(
                    out=BtT[cb_][r * 64:(r + 1) * 64, :, :], in_=Btd[r * 64 + tc * 8:r * 64 + tc * 8 + 8, :, :].rearrange("t f c -> f t c")),
                    R=BTDK, W=["BtT%d" % cb_], dma="BtT%d_%d" % (cb_, r))
            P.add("sp", lambda e, tc=tc, cb_=cb_, M=M, gcol=gcol: e.dma_start(
                out=gch[cb_][0:M, :, :], in_=blocked(U_d[0:M * 64, gcol:gcol + 512])[:, tc * 8:(tc + 1) * 8, :]),
                R=["U_d"], W=["gch%d" % cb_], dma="gch%d" % cb_)

        i1_load(0)
        for tc in range(8):
            cb_ = tc % 2
            if tc + 1 < 8:
                i1_load(tc + 1)
            for g4 in range(2):
                b0 = 4 * ((2 * tc + g4) % 2)
                for q4 in range(4):
                    t2l = g4 * 4 + q4
                    t2 = tc * 8 + t2l
                    P.add("pe", lambda e, t2=t2, t2l=t2l, cb_=cb_, pb_=b0 + q4, M=M: e.matmul(ps[pb_][0:M, :], lhsT=Dinv[:, t2, 0:M], rhs=BtT[cb_][:, t2l, :],
                                                                                             start=True, stop=True), R=["Dinv", "BtT%d" % cb_], W=[psk[b0 + q4]])
                P.add("dve", lambda e, g4=g4, cb_=cb_, b0=b0, M=M, zo=zo: e.tensor_tensor(out=zo[cb_][0:M, g4 * 4:(g4 + 1) * 4, :].rearrange("p a b -> p (a b)"),
                                                                                 in0=psall[0:M, b0 * 512:(b0 + 4) * 512],
                                                                                 in1=gch[cb_][0:M, g4 * 4:(g4 + 1) * 4, :].rearrange("p a b -> p (a b)"), op=ALU.mult),
                      R=[psk[b0 + k] for k in range(4)] + ["gch%d" % cb_], W=["zo%d_%d" % (cb_, g4)])
            P.add("sp", lambda e, tc=tc, cb_=cb_, M=M, dst=dst, zo=zo: e.dma_start(out=dst[0:M, tc * 8:(tc + 1) * 8, :], in_=zo[cb_][0:M, :, :]),
                  R=["zo%d_%d" % (cb_, k) for k in range(2)], W=["%s_%d" % (dkey, tc)], dma="zo%d" % cb_)
        P.barrier()
        P.release()
    P.barrier()
    P.release()
    if "z1" in dbg:
        tz1 = dbg_out("z1", [S, 512])
        P.mark()
        tz = P.sb([128, 512], F32, "tz")
        tzb = P.sb([128, 512], BF16, "tzb")
        for i in range(NT):
            P.add("sp", lambda e, i=i: e.dma_start(out=tzb, in_=z1_d[i * 128:(i + 1) * 128, :]), R=Z1K, W=["tzb"], dma="dbg0")
            P.add("dve", lambda e: e.tensor_copy(out=tz, in_=tzb), R=["tzb"], W=["tz"])
            P.add("sp", lambda e, i=i: e.dma_start(out=tz1[i * 128:(i + 1) * 128, :], in_=tz), R=["tz"], W=["dbgo"], dma="dbg1")
        P.barrier()
        P.release()
    if "h_out" in dbg:
        tho = dbg_out("h_out", [OWN, 512])
        P.mark()
        tz_ = P.sb([128, 512], F32, "tz_")
        for i in range(NTO):
            P.add("sp", lambda e, i=i: e.dma_start(out=tz_, in_=hout_d[i * 128:(i + 1) * 128, :]), R=["hout_d_%d" % k for k in range(8)], W=["tz_"], dma="dbg0")
            P.add("sp", lambda e, i=i: e.dma_start(out=tho[i * 128:(i + 1) * 128, :], in_=tz_), R=["tz_"], W=["dbgo"], dma="dbg1")
        P.barrier()
        P.release()


def hyena_tables(half):
    N = 8192
    f1 = np.arange(64, dtype=np.float64)
    s1 = np.arange(64, dtype=np.float64)
    s2 = np.arange(64, dtype=np.float64)
    th = 2 * np.pi * (f1[None, None, :] + 0.5) * (64 * s1[:, None, None] + s2[None, :, None]) / N
    D0 = np.concatenate([np.cos(th), -np.sin(th)], axis=2)
    perm = (np.arange(64) + 32 * half) % 64
    Dc = D0[perm]
    f2 = np.arange(64, dtype=np.float64)
    ph = 2 * np.pi * np.outer(s2, f2) / 64
    c, s_ = np.cos(ph), np.sin(ph)
    F2a = np.block([[c, -s_], [s_, c]])
    F2b = np.block([[s_, c], [-c, s_]])
    G = np.block([[c, s_], [-s_, c]])
    thi = 2 * np.pi * (f1[:, None, None] + 0.5) * (64 * s1[None, None, :] + s2[None, :, None]) / N
    Dinv0 = np.concatenate([np.cos(thi), -np.sin(thi)], axis=0) * (2.0 / N)
    Dinv = Dinv0[:, :, perm]
    sgn = np.ones((128, 1)); sgn[64:] = -1
    L = S
    t = np.arange(L, dtype=np.float32)
    t01 = t / np.float32(L)
    bands = np.linspace(1e-4, 15, 16, dtype=np.float32)
    ang = (np.float32(2.0 * math.pi) * t[:, None] * bands[None, :] / np.float32(L)).astype(np.float32)
    z = np.concatenate([t01[:, None], np.cos(ang), -np.sin(ang)], axis=-1).astype(np.float32)
    nt01 = (-t01).reshape(NT, 128).T
    f = np.float32
    return {
        "hy_D0": np.ascontiguousarray(D0.reshape(64, 64 * 128).astype(f)), "hy_Dc": np.ascontiguousarray(Dc.reshape(64, 64 * 128).astype(f)),
        "hy_F2a": np.ascontiguousarray(F2a.astype(f)), "hy_F2b": np.ascontiguousarray(F2b.astype(f)), "hy_G": np.ascontiguousarray(G.astype(f)),
        "hy_Dinv": np.ascontiguousarray(Dinv.reshape(128, 64 * 64).astype(f)), "hy_sgn": sgn.astype(f),
        "hy_zT": np.ascontiguousarray(z.T), "hy_nt01": np.ascontiguousarray(nt01.astype(f)),
    }


def host_inputs(inputs, core):
    b, half = divmod(core, 2)
    f32 = np.float32
    x = np.asarray(inputs["x"], dtype=f32)[b]
    own = slice(half * OWN, (half + 1) * OWN)
    oth = slice((1 - half) * OWN, (2 - half) * OWN)
    pos = np.concatenate([np.arange(S)[own], np.arange(S)[oth]])
    m = {}
    m["x_rot"] = np.ascontiguousarray(np.concatenate([x[own], x[oth]], axis=0))
    m["mem_b"] = np.ascontiguousarray(np.asarray(inputs["mem"], dtype=f32)[b])
    for k in ("mix_norm_g", "q_norm_g", "kv_norm_g", "hy_conv_b", "attn_out_g", "hy_out_g", "cross_norm_g",
              "mem_norm_g", "ffn_norm_g"):
        m[k] = np.ascontiguousarray(np.asarray(inputs[k], dtype=f32).reshape(1, -1))
    m["final_norm_g"] = np.ascontiguousarray(np.asarray(inputs["final_norm_g"], dtype=f32).reshape(1, -1))
    for k in ("w_in", "w_uq", "w_ukv", "hy_conv_w", "w_out", "w_mq", "w_mkv", "w_mo"):
        m[k] = np.ascontiguousarray(np.asarray(inputs[k], dtype=f32)[0])
    m["w_route"] = np.ascontiguousarray(np.concatenate([np.asarray(inputs["w_route_group"], f32)[0],
                                                        np.asarray(inputs["w_route_expert"], f32)[0]], axis=1))
    m["b_route"] = np.ascontiguousarray(np.concatenate([np.asarray(inputs["b_route_group"], f32)[0],
                                                        np.asarray(inputs["b_route_expert"], f32)[0]], axis=0).reshape(1, 36))
    m["w_gate"] = np.ascontiguousarray(np.asarray(inputs["w_gate"], f32)[0].reshape(32, D, 256))
    m["w_up"] = np.ascontiguousarray(np.asarray(inputs["w_up"], f32)[0].reshape(32, D, 256))
    m["w_down"] = np.ascontiguousarray(np.asarray(inputs["w_down"], f32)[0].reshape(32, 256, D))
    m["ident"] = np.eye(128, dtype=f32)
    inv = (10000.0 ** (-np.arange(16, dtype=np.float64) / 16)).astype(f32)
    ang = pos.astype(f32)[:, None] * inv[None, :]
    cs = np.concatenate([np.cos(ang), np.sin(ang)], axis=1).astype(f32)
    m["rope_cs"] = np.ascontiguousarray(cs.reshape(NT, 128, 32).transpose(1, 0, 2).reshape(128, NT * 32))
    m.update(hyena_tables(half))
    m["hy_cols"] = np.ascontiguousarray(np.stack([np.asarray(inputs["hy_b1"], f32)[0], np.asarray(inputs["hy_b2"], f32)[0],
                                                  np.asarray(inputs["hy_freq"], f32)[0, 0], np.asarray(inputs["hy_freq"], f32)[0, 1]], axis=1))
    for k in ("hy_w1", "hy_w2", "hy_w3"):
        m[k] = np.ascontiguousarray(np.asarray(inputs[k], f32)[0])
    m["hy_b3"] = np.ascontiguousarray(np.asarray(inputs["hy_b3"], f32).reshape(1, 2048))
    m["hy_decay"] = np.ascontiguousarray(np.asarray(inputs["hy_decay"], f32).reshape(1, 2048))
    m["hy_skip"] = np.ascontiguousarray(np.asarray(inputs["hy_skip"], f32).reshape(1, 1024))
    cw_ = np.asarray(inputs["hy_conv_w"], f32)[0]
    m["hy_conv_wT"] = np.ascontiguousarray(cw_.reshape(3, 12, 128).transpose(2, 1, 0).reshape(128, 36))
    m["hy_conv_bT"] = np.ascontiguousarray(np.asarray(inputs["hy_conv_b"], f32)[0].reshape(12, 128).T)
    hm = np.zeros((128, 2), f32)
    hm[:, 0] = half
    hm[:, 1] = 1 - half
    m["halfmask"] = hm
    return m


def kernel(**inputs):
    n = 8
    nc, _ = build()
    in_maps = [host_inputs(inputs, c) for c in range(n)]
    res = run_bass_kernel_spmd(nc, in_maps, core_ids=list(range(n)))
    out = np.zeros((4, S, D), np.float32)
    for c in range(n):
        b, half = divmod(c, 2)
        out[b, half * OWN:(half + 1) * OWN] = res.results[c]["out"]
    return out
```

```python
import math
import os
import contextlib
import numpy as np
import concourse.bass as bass
import concourse.mybir as mybir
from concourse.bass_utils import run_bass_kernel_spmd

F32 = mybir.dt.float32
BF16 = mybir.dt.bfloat16
AF = mybir.ActivationFunctionType
ALU = mybir.AluOpType
AX = mybir.AxisListType
ENGS = ("pe", "act", "dve", "pool", "sp")

D = 1024
S = 4096
OWN = 2048
NT = 32
NTO = 16
EPS = 1e-6
HD = 96
NH = 8


class Op:
    __slots__ = ("eng", "fn", "deps", "dma", "flag", "seq", "idx", "dmaval")

    def __init__(self, eng, fn, dma):
        self.eng = eng
        self.fn = fn
        self.deps = set()
        self.dma = dma
        self.flag = False
        self.seq = 0
        self.dmaval = 0


class Prog:
    ARENA_WORDS = 52000

    def __init__(self, nc):
        self.nc = nc
        self.ops = []
        self.lastw = {}
        self.readers = {}
        self.dma_count = {}
        self.sb_off = 0
        self.sb_marks = []
        self.arena = None
        self.dma_slots = {}

    def sb(self, shape, dtype, name=None):
        if self.arena is None:
            self.arena = self.nc.alloc_sbuf_tensor("arena", [128, self.ARENA_WORDS], F32)
        esz = 4 if dtype == F32 else 2
        nel = int(np.prod(shape[1:]))
        nwords = (nel * esz + 3) // 4
        nwords = (nwords + 15) // 16 * 16
        o = self.sb_off
        self.sb_off += nwords
        assert self.sb_off <= self.ARENA_WORDS, ("SBUF overflow", self.sb_off * 4, name)
        v = self.arena[0:shape[0], o:o + nwords]
        if esz == 2:
            v = v.bitcast(dtype)[:, 0:nel]
        else:
            v = v[:, 0:nel]
        if len(shape) > 2:
            names = " ".join("a%d" % i for i in range(len(shape) - 1))
            kw = {"a%d" % i: int(shape[i + 1]) for i in range(len(shape) - 1)}
            v = v.rearrange("p (%s) -> p %s" % (names, names), **kw)
        return v

    def mark(self):
        self.sb_marks.append(self.sb_off)

    def release(self):
        self.sb_off = self.sb_marks.pop()

    def add(self, eng, fn, R=(), W=(), dma=None):
        if dma is not None:
            slots = self.dma_slots.setdefault(eng, {"free": [], "n": 0, "map": {}})
            if dma not in slots["map"]:
                if slots["free"]:
                    slots["map"][dma] = slots["free"].pop()
                else:
                    slots["map"][dma] = slots["n"]
                    slots["n"] += 1
            dma = (eng, slots["map"][dma])
        op = Op(eng, fn, dma)
        op.idx = len(self.ops)
        if eng != "pe":
            psr = [r for r in R if isinstance(r, str) and r.startswith("ps") and r[2:].isdigit()]
            if psr:
                R = [r for r in R if r not in psr]
                W = list(W) + psr
        deps = set()
        for r in R:
            lw = self.lastw.get(r)
            if lw is not None:
                deps.add(lw)
        for w in W:
            lw = self.lastw.get(w)
            if lw is not None:
                deps.add(lw)
            for rd in self.readers.get(w, ()):
                deps.add(rd)
        if dma is not None:
            k = ("__dmasem", dma)
            lw = self.lastw.get(k)
            if lw is not None:
                deps.add(lw)
            self.lastw[k] = op
            self.dma_count[dma] = self.dma_count.get(dma, 0) + 1
            op.dmaval = 16 * self.dma_count[dma]
        deps.discard(op)
        for d in deps:
            if d.dma is None and d.eng == "pe" and eng == "pe" and dma is None:
                continue
            op.deps.add(d)
            d.flag = True
        for r in R:
            self.readers.setdefault(r, []).append(op)
        for w in W:
            self.lastw[w] = op
            self.readers[w] = []
        self.ops.append(op)
        return op

    def barrier(self):
        fr = {}
        dmas = set()
        allops = set(self.lastw.values())
        for v in self.readers.values():
            allops.update(v)
        for o in allops:
            if o.dma is not None:
                dmas.add(o)
            elif o.eng not in fr or fr[o.eng].idx < o.idx:
                fr[o.eng] = o
        for e in ENGS:
            op = Op(e, None, None)
            op.idx = len(self.ops)
            for d in list(fr.values()) + list(dmas):
                op.deps.add(d)
                d.flag = True
            self.ops.append(op)
        self.lastw = {}
        self.readers = {}
        for sl in self.dma_slots.values():
            sl["free"].extend(sl["map"].values())
            sl["map"].clear()

    def emit(self):
        nc = self.nc
        with contextlib.ExitStack() as st:
            esem = {e: st.enter_context(nc.semaphore("s_" + e)) for e in ENGS}
            dsem = {}
            for k in self.dma_count:
                dsem[k] = st.enter_context(nc.semaphore("d_%d" % len(dsem)))
            cnt = {e: 0 for e in ENGS}
            for op in self.ops:
                if op.dma is None and op.flag:
                    cnt[op.eng] += 1
                    op.seq = cnt[op.eng]
            byeng = {e: [o for o in self.ops if o.eng == e] for e in ENGS}
            if os.environ.get("KDEBUG"):
                print("sem counts", cnt, "ndma sems", len(dsem), "nops", {e: len(v) for e, v in byeng.items()})
            block = st.enter_context(nc.Block())

            def run(e, eng):
                waited = {}
                for op in byeng[e]:
                    need = {}
                    for d in op.deps:
                        if d.dma is not None:
                            s, v = dsem[d.dma], d.dmaval
                        else:
                            s, v = esem[d.eng], d.seq
                        key = id(s)
                        if waited.get(key, 0) >= v:
                            continue
                        if key not in need or need[key][1] < v:
                            need[key] = (s, v)
                    for key, (s, v) in need.items():
                        eng.wait_ge(s, v)
                        waited[key] = v
                    if op.fn is None:
                        continue
                    ins = op.fn(eng)
                    if op.dma is not None:
                        ins.then_inc(dsem[op.dma], 16)
                    elif op.flag:
                        ins.then_inc(esem[e], 1)

            @block.tensor
            def _(eng):
                run("pe", eng)

            @block.scalar
            def _(eng):
                run("act", eng)

            @block.vector
            def _(eng):
                run("dve", eng)

            @block.gpsimd
            def _(eng):
                run("pool", eng)

            @block.sync
            def _(eng):
                run("sp", eng)


INPUT_SHAPES = {
    "x_rot": [S, D], "mem_b": [256, D],
    "mix_norm_g": [1, D], "w_in": [D, 1952], "q_norm_g": [1, 256], "kv_norm_g": [1, 128],
    "w_uq": [256, 768], "w_ukv": [128, 1024], "hy_conv_w": [3, 1536], "hy_conv_b": [1, 1536],
    "attn_out_g": [1, 512], "hy_out_g": [1, 512], "w_out": [D, D],
    "cross_norm_g": [1, D], "mem_norm_g": [1, D], "w_mq": [D, D], "w_mkv": [D, 2 * D], "w_mo": [D, D],
    "ffn_norm_g": [1, D], "w_route": [D, 36], "b_route": [1, 36],
    "w_gate": [32, D, 256], "w_up": [32, D, 256], "w_down": [32, 256, D], "final_norm_g": [1, D],
    "ident": [128, 128], "rope_cs": [128, NT * 32], "halfmask": [128, 2],
    "hy_D0": [64, 64 * 128], "hy_Dc": [64, 64 * 128], "hy_F2a": [128, 128], "hy_F2b": [128, 128], "hy_G": [128, 128],
    "hy_Dinv": [128, 64 * 64], "hy_sgn": [128, 1], "hy_zT": [33, S], "hy_nt01": [128, NT],
    "hy_cols": [64, 4], "hy_w1": [33, 64], "hy_w2": [64, 64], "hy_w3": [64, 2048], "hy_b3": [1, 2048], "hy_decay": [1, 2048],
    "hy_skip": [1, 1024], "hy_conv_wT": [128, 36], "hy_conv_bT": [128, 12],
}


def build(stop=None, dbg=()):
    nc = bass.Bass("TRN2", target_bir_lowering=False)
    I = {k: nc.dram_tensor(k, v, F32, kind="ExternalInput").ap() for k, v in INPUT_SHAPES.items()}
    out_d = nc.dram_tensor("out", [OWN, D], F32, kind="ExternalOutput").ap()
    dbg_d = {}
    U_d = nc.dram_tensor("U_scr", [S, 1536], F32, kind="Internal").ap()
    hout_d = nc.dram_tensor("hout_scr", [OWN, 512], F32, kind="Internal").ap()
    combT_d = nc.dram_tensor("combT_scr", [32, OWN], F32, kind="Internal").ap()

    P = Prog(nc)
    psall = nc.alloc_psum_tensor("psall", [128, 4096], F32)
    ps = [psall[:, i * 512:(i + 1) * 512] for i in range(8)]
    psk = ["ps%d" % i for i in range(8)]

    def psb(i):
        return ps[i].bitcast(BF16)

    def dbg_out(name, shape):
        t = nc.dram_tensor("dbg_" + name, shape, F32, kind="ExternalOutput").ap()
        dbg_d[name] = t
        return t

    cnt = [0]

    def uid(s):
        cnt[0] += 1
        return "%s_%d" % (s, cnt[0])

    identf = P.sb([128, 128], F32, "identf")
    identb = P.sb([128, 128], BF16, "identb")
    halfm = P.sb([128, 2], F32, "halfm")
    st = P.sb([128, 8], F32, "st")
    junk = P.sb([128, 1024], F32, "junk")
    gb = P.sb([128, 1024], F32, "gb")
    onesb = P.sb([128, 128], BF16, "onesb")
    onesf = P.sb([128, 128], F32, "onesf")
    P.add("sp", lambda e: e.dma_start(out=identf, in_=I["ident"]), W=["identf"], dma="c0")
    P.add("sp", lambda e: e.dma_start(out=halfm, in_=I["halfmask"]), W=["halfm"], dma="c1")
    P.add("dve", lambda e: e.tensor_copy(out=identb, in_=identf), R=["identf"], W=["identb"])
    epst = P.sb([128, 1], F32, "epst")
    P.add("pool", lambda e: e.memset(epst, EPS), W=["epst"])
    P.add("pool", lambda e: e.memset(onesb, 1.0), W=["onesb"])
    P.add("pool", lambda e: e.memset(onesf, 1.0), W=["onesf"])

    def pipeline(n, stages):
        K_ = len(stages)
        for s_ in range(n + K_ - 1):
            for k_ in range(K_):
                i_ = s_ - k_
                if 0 <= i_ < n:
                    stages[k_](i_)

    def load_gain(name, n=D, key="gb"):
        P.add("sp", lambda e: e.dma_start(out=gb[:, 0:n], in_=I[name].partition_broadcast(128)), W=[key], dma="gain")

    st_tiles = {}
    for _n in ("st", "stq", "stk", "sta", "sth", "stm", "stx", "stf", "stg"):
        st_tiles[_n] = P.sb([128, 4], F32, "st_" + _n)

    def rms_norm(src, n, gview, out_bf, Rk, Wk, stk="st"):
        if stk not in st_tiles:
            st_tiles[stk] = P.sb([128, 4], F32, "st_" + stk)
        st = st_tiles[stk]
        P.add("act", lambda e: e.activation(out=junk[:, 0:n], in_=src, func=AF.Square, accum_out=st[:, 0:1]),
              R=Rk, W=["junk", stk + "0"])
        P.add("act", lambda e: e.activation(out=st[:, 2:3], in_=st[:, 0:1], func=AF.Sqrt, scale=1.0 / n, bias=epst[:, 0:1]),
              R=[stk + "0", "epst"], W=[stk + "2"])
        P.add("dve", lambda e: e.reciprocal(out=st[:, 3:4], in_=st[:, 2:3]), R=[stk + "2"], W=[stk + "3"])
        P.add("dve", lambda e: e.scalar_tensor_tensor(out=out_bf, in0=src, scalar=st[:, 3:4], in1=gview,
                                                      op0=ALU.mult, op1=ALU.mult),
              R=list(Rk) + [stk + "3", "gb"], W=Wk)

    P.mark()
    hqT = P.sb([128, 2, OWN], BF16, "hqT")
    hkvT = P.sb([128, S], BF16, "hkvT")
    krot = P.sb([128, NT, 32], F32, "krot")
    ropecs = P.sb([128, NT, 32], F32, "ropecs")
    P.add("sp", lambda e: e.dma_start(out=ropecs.rearrange("p a b -> p (a b)"), in_=I["rope_cs"]), W=["ropecs"], dma="c2")

    P.mark()
    hT = P.sb([128, 8, 2, OWN + 2], BF16, "hT")
    P.mark()
    xt = [P.sb([128, D], F32, "xt%d" % i) for i in range(4)]
    xn = [P.sb([128, D], BF16, "xn%d" % i) for i in range(4)]
    load_gain("mix_norm_g")
    def a1S0(i):
        b = i % 4
        P.add("sp", lambda e, i=i, b=b: e.dma_start(out=xt[b], in_=I["x_rot"][i * 128:(i + 1) * 128, :]),
              W=["xt%d" % b], dma="xt%d" % b)
        rms_norm(xt[b], D, gb, xn[b], ["xt%d" % b], ["xn%d" % b])

    def a1S1(i):
        b = i % 4
        pb = i % 4
        for k in range(8):
            P.add("pe", lambda e, k=k, b=b, pb=pb: e.transpose(out=psb(pb)[:, k * 128:(k + 1) * 128],
                                                                 in_=xn[b][:, k * 128:(k + 1) * 128], identity=identb),
                  R=["xn%d" % b, "identb"], W=[psk[pb]])

    def a1S2(i):
        pb = i % 4
        seg, j = divmod(i, NTO)
        dst = hT[:, :, seg, 1 + j * 128:1 + (j + 1) * 128]
        src = psb(pb).rearrange("p (k t) -> p k t", k=8)
        if i % 2 == 0:
            P.add("act", lambda e, dst=dst, src=src: e.copy(out=dst, in_=src), R=[psk[pb]], W=["hT"])
        else:
            P.add("dve", lambda e, dst=dst, src=src: e.tensor_copy(out=dst, in_=src), R=[psk[pb]], W=["hT"])

    pipeline(NT, [a1S0, a1S1, a1S2])
    for (ds, dc, ss_, sc, m) in ((0, 0, 1, OWN, 0), (0, OWN + 1, 1, 1, 1), (1, 0, 0, OWN, 1), (1, OWN + 1, 0, 1, 0)):
        P.add("dve", lambda e, ds=ds, dc=dc, ss_=ss_, sc=sc, m=m: e.tensor_scalar_mul(
            out=hT[:, :, ds, dc:dc + 1], in0=hT[:, :, ss_, sc:sc + 1], scalar1=halfm[:, m:m + 1]),
            R=["hT", "halfm"], W=["hT"])

    P.barrier()
    P.release()
    P.mark()
    w_mla = P.sb([128, 8, 416], BF16, "w_mla")
    P.add("pool", lambda e: e.dma_start(out=w_mla, in_=I["w_in"][:, 0:416].rearrange("(k p) n -> p k n", p=128)),
          W=["w_mla"], dma="w0")
    gq = P.sb([128, 256], F32, "gq")
    gkv = P.sb([128, 128], F32, "gkv")
    P.add("sp", lambda e: e.dma_start(out=gq, in_=I["q_norm_g"].partition_broadcast(128)), W=["gq"], dma="c3")
    P.add("sp", lambda e: e.dma_start(out=gkv, in_=I["kv_norm_g"].partition_broadcast(128)), W=["gkv"], dma="c4")
    hqn = [P.sb([128, 256], BF16, "hqn%d" % i) for i in range(3)]
    hkvn = [P.sb([128, 128], BF16, "hkvn%d" % i) for i in range(3)]
    tmp16 = P.sb([128, 4, 16], F32, "tmp16")
    def a2S0(i):
        seg, j = divmod(i, NTO)
        pb = i % 3
        for k in range(8):
            P.add("pe", lambda e, k=k, pb=pb, seg=seg, j=j: e.matmul(
                ps[pb][:, 0:416], lhsT=hT[:, k, seg, 1 + j * 128:1 + (j + 1) * 128], rhs=w_mla[:, k, :],
                start=(k == 0), stop=(k == 7)), R=["hT", "w_mla"], W=[psk[pb]])

    def a2S1(i):
        seg, j = divmod(i, NTO)
        pb = i % 3
        b = i % 3
        if seg == 0:
            rms_norm(ps[pb][:, 0:256], 256, gq, hqn[b], [psk[pb], "gq"], ["hqn%d" % b], stk="stq")
        rms_norm(ps[pb][:, 256:384], 128, gkv, hkvn[b], [psk[pb], "gkv"], ["hkvn%d" % b], stk="stk")
        x1 = ps[pb][:, 384:400]
        x2 = ps[pb][:, 400:416]
        c = ropecs[:, i, 0:16]
        s_ = ropecs[:, i, 16:32]
        P.add("dve", lambda e, x1=x1, c=c: e.tensor_tensor(out=tmp16[:, 0, :], in0=x1, in1=c, op=ALU.mult), R=[psk[pb], "ropecs"], W=["t16a"])
        P.add("dve", lambda e, x2=x2, s_=s_: e.tensor_tensor(out=tmp16[:, 1, :], in0=x2, in1=s_, op=ALU.mult), R=[psk[pb], "ropecs"], W=["t16b"])
        P.add("dve", lambda e, x1=x1, s_=s_: e.tensor_tensor(out=tmp16[:, 2, :], in0=x1, in1=s_, op=ALU.mult), R=[psk[pb], "ropecs"], W=["t16c"])
        P.add("dve", lambda e, x2=x2, c=c: e.tensor_tensor(out=tmp16[:, 3, :], in0=x2, in1=c, op=ALU.mult), R=[psk[pb], "ropecs"], W=["t16d"])
        P.add("dve", lambda e, i=i: e.tensor_tensor(out=krot[:, i, 0:16], in0=tmp16[:, 0, :], in1=tmp16[:, 1, :], op=ALU.subtract),
              R=["t16a", "t16b"], W=["krot"])
        P.add("dve", lambda e, i=i: e.tensor_tensor(out=krot[:, i, 16:32], in0=tmp16[:, 2, :], in1=tmp16[:, 3, :], op=ALU.add),
              R=["t16c", "t16d"], W=["krot"])

    def a2S2(i):
        seg, j = divmod(i, NTO)
        b = i % 3
        pt = 4 + i % 2
        if seg == 0:
            for k in range(2):
                P.add("pe", lambda e, k=k, b=b, pt=pt: e.transpose(out=psb(pt)[:, k * 128:(k + 1) * 128],
                                                                     in_=hqn[b][:, k * 128:(k + 1) * 128], identity=identb),
                      R=["hqn%d" % b, "identb"], W=[psk[pt]])
        P.add("pe", lambda e, b=b, pt=pt: e.transpose(out=psb(pt)[:, 256:384], in_=hkvn[b], identity=identb),
              R=["hkvn%d" % b, "identb"], W=[psk[pt]])

    def a2S3(i):
        seg, j = divmod(i, NTO)
        pt = 4 + i % 2
        if seg == 0:
            P.add("act", lambda e, pt=pt, j=j: e.copy(out=hqT[:, :, j * 128:(j + 1) * 128],
                                                       in_=psb(pt)[:, 0:256].rearrange("p (k t) -> p k t", k=2)),
                  R=[psk[pt]], W=["hqT"])
        P.add("act", lambda e, pt=pt, i=i: e.copy(out=hkvT[:, i * 128:(i + 1) * 128], in_=psb(pt)[:, 256:384]),
              R=[psk[pt]], W=["hkvT"])

    pipeline(NT, [a2S0, a2S1, a2S2, a2S3])

    P.barrier()
    P.release()
    w_hy = P.sb([128, 8, 512], BF16, "w_hy")
    cwT = P.sb([128, 12, 3], F32, "cwT")
    cbT = P.sb([128, 12], F32, "cbT")
    uT = [P.sb([128, 2, OWN + 2], F32, "uT0")] * 2
    tTc = P.sb([128, 4, 2, OWN], F32, "tTc")
    tmpP = P.sb([128, OWN], F32, "tmpP")
    uo = [P.sb([128, 512], F32, "uo%d" % i) for i in range(2)]
    P.add("sp", lambda e: e.dma_start(out=cwT.rearrange("p a b -> p (a b)"), in_=I["hy_conv_wT"]), W=["cwT"], dma="cw0")
    P.add("sp", lambda e: e.dma_start(out=cbT, in_=I["hy_conv_bT"]), W=["cbT"], dma="cw1")
    nev = 0
    for c3 in range(3):
        c0 = 416 + c3 * 512
        P.add("pool", lambda e, c0=c0: e.dma_start(out=w_hy, in_=I["w_in"][:, c0:c0 + 512].rearrange("(k p) n -> p k n", p=128)),
              W=["w_hy"], dma="w1")
        for c4 in range(4):
            ct = c3 * 4 + c4
            ub = 0
            u_ = uT[ub]
            for seg in range(2):
                for tc in range(4):
                    pb = 2 + (nev % 4)
                    for k in range(8):
                        P.add("pe", lambda e, k=k, pb=pb, seg=seg, tc=tc, c4=c4: e.matmul(
                            ps[pb][:, :], lhsT=w_hy[:, k, c4 * 128:(c4 + 1) * 128], rhs=hT[:, k, seg, 1 + tc * 512:1 + (tc + 1) * 512],
                            start=(k == 0), stop=(k == 7)), R=["hT", "w_hy"], W=[psk[pb]])
                    dst = u_[:, seg, 1 + tc * 512:1 + (tc + 1) * 512]
                    if nev % 2 == 0:
                        P.add("act", lambda e, pb=pb, dst=dst: e.copy(out=dst, in_=ps[pb][:, :]), R=[psk[pb]], W=["uT%d" % ub])
                    else:
                        P.add("dve", lambda e, pb=pb, dst=dst: e.tensor_copy(out=dst, in_=ps[pb][:, :]), R=[psk[pb]], W=["uT%d" % ub])
                    nev += 1
            for (ds, dc, ss_, sc, m) in ((0, 0, 1, OWN, 0), (0, OWN + 1, 1, 1, 1), (1, 0, 0, OWN, 1), (1, OWN + 1, 0, 1, 0)):
                P.add("dve", lambda e, u_=u_, ds=ds, dc=dc, ss_=ss_, sc=sc, m=m: e.tensor_scalar_mul(
                    out=u_[:, ds, dc:dc + 1], in0=u_[:, ss_, sc:sc + 1], scalar1=halfm[:, m:m + 1]),
                    R=["uT%d" % ub, "halfm"], W=["uT%d" % ub])
            for seg in range(2):
                t_ = tTc[:, c4, seg, :]
                P.add("act", lambda e, u_=u_, seg=seg, t_=t_, ct=ct: e.activation(out=t_, in_=u_[:, seg, 1:OWN + 1], func=AF.Identity,
                                                                               scale=cwT[:, ct, 1:2], bias=cbT[:, ct:ct + 1]),
                      R=["uT%d" % ub, "cwT", "cbT"], W=["tTc_%d_%d" % (c4, seg)])
                P.add("dve", lambda e, u_=u_, seg=seg, t_=t_, ct=ct: e.scalar_tensor_tensor(out=t_, in0=u_[:, seg, 0:OWN], scalar=cwT[:, ct, 0:1], in1=t_,
                                                                                         op0=ALU.mult, op1=ALU.add),
                      R=["uT%d" % ub, "cwT", "tTc_%d_%d" % (c4, seg)], W=["tTc_%d_%d" % (c4, seg)])
                P.add("act", lambda e, u_=u_, seg=seg, ct=ct: e.activation(out=tmpP, in_=u_[:, seg, 2:OWN + 2], func=AF.Copy, scale=cwT[:, ct, 2:3]),
                      R=["uT%d" % ub, "cwT"], W=["tmpP"])
                P.add("pool", lambda e, t_=t_: e.tensor_tensor(out=t_, in0=t_, in1=tmpP, op=ALU.add),
                      R=["tmpP", "tTc_%d_%d" % (c4, seg)], W=["tTc_%d_%d" % (c4, seg)])
        for i in range(NT):
            b = i % 2
            seg, j = divmod(i, NTO)
            pb = 6 + b
            for c4 in range(4):
                P.add("pe", lambda e, c4=c4, seg=seg, j=j, pb=pb: e.transpose(out=ps[pb][:, c4 * 128:(c4 + 1) * 128],
                                                                           in_=tTc[:, c4, seg, j * 128:(j + 1) * 128], identity=identf),
                      R=["tTc_%d_%d" % (c4, seg), "identf"], W=[psk[pb]])
            if b == 0:
                P.add("act", lambda e, pb=pb, b=b: e.copy(out=uo[b], in_=ps[pb][:, :]), R=[psk[pb]], W=["uo%d" % b])
            else:
                P.add("dve", lambda e, pb=pb, b=b: e.tensor_copy(out=uo[b], in_=ps[pb][:, :]), R=[psk[pb]], W=["uo%d" % b])
            P.add("sp", lambda e, i=i, b=b, c3=c3: e.dma_start(out=U_d[i * 128:(i + 1) * 128, c3 * 512:(c3 + 1) * 512], in_=uo[b]),
                  R=["uo%d" % b], W=["U_d"], dma="uo%d" % b)
    P.barrier()
    P.release()

    if stop == "A0":
        P.add("sp", None, R=[])
        P.emit()
        return nc, dbg_d
    if "uc" in dbg:
        tu = dbg_out("uc", [S, 1536])
        P.mark()
        tb = P.sb([128, 1536], F32, "dbgt")
        for i in range(NT):
            P.add("sp", lambda e, i=i: e.dma_start(out=tb, in_=U_d[i * 128:(i + 1) * 128, :]), R=["U_d"], W=["dbgt"], dma="dbg0")
            P.add("sp", lambda e, i=i: e.dma_start(out=tu[i * 128:(i + 1) * 128, :], in_=tb), R=["dbgt"], W=["dbgo"], dma="dbg1")
        P.barrier()
        P.release()

    if stop == "A":
        P.add("sp", None, R=["dbgo"])
        P.emit()
        return nc, dbg_d
    aout_d = nc.dram_tensor("aout_scr", [OWN, 512], F32, kind="Internal").ap()
    P.mark()
    G4 = 8
    KT = P.sb([128, G4, S], BF16, "KT")
    QT = P.sb([128, G4, OWN], BF16, "QT")
    Vaug = P.sb([128, NT, G4, 68], BF16, "Vaug")
    w_ukv = P.sb([128, 1024], BF16, "w_ukv")
    w_uq = P.sb([128, 2, 768], BF16, "w_uq")
    P.add("pool", lambda e: e.dma_start(out=w_ukv, in_=I["w_ukv"]), W=["w_ukv"], dma="w0")
    P.add("pool", lambda e: e.dma_start(out=w_uq, in_=I["w_uq"].rearrange("(k p) n -> p k n", p=128)), W=["w_uq"], dma="w1")
    Kaug = [P.sb([128, G4, 100], BF16, "Kaug%d" % i) for i in range(2)]
    Kaug2 = P.sb([128, G4, 100], BF16, "Kaug2")
    ksq2 = [P.sb([128, G4, 96], F32, "ksq2_%d" % i) for i in range(2)]
    ksq = ksq2[0]
    kn2 = P.sb([128, G4], F32, "kn2")
    kmax = P.sb([128, G4], F32, "kmax")
    kb = P.sb([128, 4], F32, "kb")
    qs2 = [P.sb([128, G4, 96], F32, "qs%d" % i) for i in range(2)]
    qn2 = [P.sb([128, G4], F32, "qn%d" % i) for i in range(2)]
    qt42 = [P.sb([128, 4, G4, 16], F32, "qt4_0")] * 2
    PT = [P.sb([128, 512], BF16, "PT%d" % i) for i in range(3)]
    oTs = P.sb([65, 512], F32, "oTs")
    rden = P.sb([128, 4], F32, "rden")
    astage = [P.sb([128, 4, 512], F32, "astage0")] * 2
    scale = HD ** -0.5
    it = 0
    for g in range(1):
        P.add("pool", lambda e: e.memset(Vaug.rearrange("p a b c -> p (a b c)"), 1.0), W=["Vaug"])
        for b in range(2):
            P.add("pool", lambda e, b=b: e.memset(Kaug[b].rearrange("p a b -> p (a b)"), 1.0), W=["Kaug%d" % b])
        P.add("pool", lambda e: e.memset(kmax, 0.0), W=["kmax"])
        ND = 3
        KaugN = [Kaug[0], Kaug[1], Kaug2]
        P.add("pool", lambda e: e.memset(Kaug2.rearrange("p a b -> p (a b)"), 1.0), W=["Kaug2"])

        def kS0(i):
            pbk = 2 * (i % 2)
            for hh in range(2):
                P.add("pe", lambda e, hh=hh, pbk=pbk, i=i: e.matmul(ps[pbk + hh][:, :], lhsT=hkvT[:, i * 128:(i + 1) * 128],
                                                                     rhs=w_ukv[:, hh * 512:(hh + 1) * 512], start=True, stop=True),
                      R=["hkvT", "w_ukv"], W=[psk[pbk + hh]])

        def kS1(i):
            pbk = 2 * (i % 2)
            kb_ = i % ND
            Ka = KaugN[kb_]
            v = psall[:, pbk * 512:(pbk + 2) * 512].rearrange("p (h c) -> p h c", h=8)
            P.add("act", lambda e, v=v, i=i: e.copy(out=Vaug[:, i, :, 0:64], in_=v[:, :, 64:128]), R=[psk[pbk], psk[pbk + 1]], W=["Vaug"])
            P.add("dve", lambda e, v=v, Ka=Ka: e.tensor_copy(out=Ka[:, :, 0:64], in_=v[:, :, 0:64]), R=[psk[pbk], psk[pbk + 1]], W=["Kaug%d" % kb_])
            P.add("pool", lambda e, Ka=Ka, i=i: e.tensor_copy(out=Ka[:, :, 64:96], in_=krot[:, i:i + 1, :].broadcast_to([128, G4, 32])),
                  R=["krot"], W=["Kaug%d" % kb_])
            P.add("dve", lambda e, Ka=Ka, kb_=kb_: e.tensor_tensor(out=ksq2[kb_ % 2], in0=Ka[:, :, 0:96], in1=Ka[:, :, 0:96], op=ALU.mult),
                  R=["Kaug%d" % kb_], W=["ksq2_%d" % (kb_ % 2)])
            P.add("dve", lambda e, kb_=kb_: e.tensor_reduce(out=kn2, in_=ksq2[kb_ % 2], axis=AX.X, op=ALU.add), R=["ksq2_%d" % (kb_ % 2)], W=["kn2"])
            P.add("dve", lambda e: e.tensor_tensor(out=kmax, in0=kmax, in1=kn2, op=ALU.max), R=["kn2", "kmax"], W=["kmax"])

        def kS2(i):
            kb_ = i % ND
            Ka = KaugN[kb_]
            pt = 4 + i % 2
            for h in range(G4):
                P.add("pe", lambda e, h=h, Ka=Ka, pt=pt: e.transpose(out=psb(pt)[0:97, h * 128:(h + 1) * 128], in_=Ka[:, h, 0:97], identity=identb),
                      R=["Kaug%d" % kb_, "identb"], W=[psk[pt]])
            P.add("act", lambda e, pt=pt, i=i: e.copy(out=KT[0:97, :, i * 128:(i + 1) * 128],
                                                       in_=psb(pt)[0:97, 0:G4 * 128].rearrange("p (h t) -> p h t", h=G4)),
                  R=[psk[pt]], W=["KT"])

        pipeline(NT, [kS0, kS1, kS2])
        P.add("dve", lambda e: e.tensor_reduce(out=kb[:, 1:2], in_=kmax, axis=AX.X, op=ALU.max), R=["kmax"], W=["kb1"])
        P.add("pe", lambda e: e.transpose(out=ps[6][0:1, 0:128], in_=kb[:, 1:2], identity=identf), R=["kb1", "identf"], W=[psk[6]])
        P.add("dve", lambda e: e.tensor_reduce(out=kb[0:1, 2:3], in_=ps[6][0:1, 0:128], axis=AX.X, op=ALU.max), R=[psk[6]], W=["kb2"])
        P.add("pe", lambda e: e.matmul(ps[7][:, 0:1], lhsT=onesf[0:1, 0:128], rhs=kb[0:1, 2:3], start=True, stop=True),
              R=["kb2", "onesf"], W=[psk[7]])
        P.add("act", lambda e: e.sqrt(out=kb[:, 0:1], in_=ps[7][:, 0:1]), R=[psk[7]], W=["kb0"])
        QaugN = KaugN

        def qS0(j):
            pa = 2 * (j % 2)
            for (pq, c0, ncol) in ((pa, 0, 480), (pa + 1, 480, 288)):
                for k in range(2):
                    P.add("pe", lambda e, pq=pq, c0=c0, ncol=ncol, k=k, j=j: e.matmul(
                        ps[pq][:, 0:ncol], lhsT=hqT[:, k, j * 128:(j + 1) * 128], rhs=w_uq[:, k, c0:c0 + ncol],
                        start=(k == 0), stop=(k == 1)), R=["hqT", "w_uq"], W=[psk[pq]])

        def qS1(j):
            pa = 2 * (j % 2)
            d_ = j % 2
            qb_ = j % ND
            Qa = QaugN[qb_]
            qs_ = qs2[d_]
            q4 = qt42[d_]
            P.add("act", lambda e, pa=pa, qs_=qs_: e.mul(out=qs_[:, 0:5, :], in_=ps[pa][:, 0:480].rearrange("p (h c) -> p h c", h=5), mul=scale),
                  R=[psk[pa]], W=["qs%d" % d_])
            P.add("act", lambda e, pa=pa, qs_=qs_: e.mul(out=qs_[:, 5:8, :], in_=ps[pa + 1][:, 0:288].rearrange("p (h c) -> p h c", h=3), mul=scale),
                  R=[psk[pa + 1]], W=["qs%d" % d_])
            c = ropecs[:, j:j + 1, 0:16].broadcast_to([128, G4, 16])
            s_ = ropecs[:, j:j + 1, 16:32].broadcast_to([128, G4, 16])
            x1 = qs_[:, :, 64:80]
            x2 = qs_[:, :, 80:96]
            P.add("dve", lambda e, x1=x1, c=c, q4=q4: e.tensor_tensor(out=q4[:, 0], in0=x1, in1=c, op=ALU.mult), R=["qs%d" % d_, "ropecs"], W=["qt4a"])
            P.add("dve", lambda e, x2=x2, s_=s_, q4=q4: e.tensor_tensor(out=q4[:, 1], in0=x2, in1=s_, op=ALU.mult), R=["qs%d" % d_, "ropecs"], W=["qt4b"])
            P.add("pool", lambda e, x1=x1, s_=s_, q4=q4: e.tensor_tensor(out=q4[:, 2], in0=x1, in1=s_, op=ALU.mult), R=["qs%d" % d_, "ropecs"], W=["qt4c"])
            P.add("pool", lambda e, x2=x2, c=c, q4=q4: e.tensor_tensor(out=q4[:, 3], in0=x2, in1=c, op=ALU.mult), R=["qs%d" % d_, "ropecs"], W=["qt4d"])
            P.add("dve", lambda e, qs_=qs_, q4=q4: e.tensor_tensor(out=qs_[:, :, 64:80], in0=q4[:, 0], in1=q4[:, 1], op=ALU.subtract),
                  R=["qt4a", "qt4b"], W=["qs%d" % d_])
            P.add("dve", lambda e, qs_=qs_, q4=q4: e.tensor_tensor(out=qs_[:, :, 80:96], in0=q4[:, 2], in1=q4[:, 3], op=ALU.add),
                  R=["qt4c", "qt4d"], W=["qs%d" % d_])
            P.add("dve", lambda e, qs_=qs_, d_=d_: e.tensor_tensor(out=ksq2[d_], in0=qs_, in1=qs_, op=ALU.mult), R=["qs%d" % d_], W=["ksq2_%d" % d_])
            P.add("dve", lambda e, d_=d_: e.tensor_reduce(out=qn2[d_], in_=ksq2[d_], axis=AX.X, op=ALU.add), R=["ksq2_%d" % d_], W=["qn%d" % d_])
            P.add("act", lambda e, d_=d_: e.sqrt(out=qn2[d_], in_=qn2[d_]), R=["qn%d" % d_], W=["qn%d" % d_])
            P.add("dve", lambda e, Qa=Qa, d_=d_: e.tensor_scalar(out=Qa[:, :, 96:97], in0=qn2[d_].rearrange("p (h o) -> p h o", o=1),
                                                                 scalar1=kb[:, 0:1], scalar2=-1.0, op0=ALU.mult, op1=ALU.mult),
                  R=["qn%d" % d_, "kb0"], W=["Kaug%d" % qb_])
            P.add("act", lambda e, Qa=Qa, qs_=qs_: e.copy(out=Qa[:, :, 0:96], in_=qs_), R=["qs%d" % d_], W=["Kaug%d" % qb_])

        def qS2(j):
            qb_ = j % ND
            Qa = QaugN[qb_]
            pt = 4 + j % 2
            for h in range(G4):
                P.add("pe", lambda e, h=h, Qa=Qa, pt=pt: e.transpose(out=psb(pt)[0:97, h * 128:(h + 1) * 128], in_=Qa[:, h, 0:97], identity=identb),
                      R=["Kaug%d" % qb_, "identb"], W=[psk[pt]])
            P.add("dve", lambda e, pt=pt, j=j: e.tensor_copy(out=QT[0:97, :, j * 128:(j + 1) * 128],
                                                             in_=psb(pt)[0:97, 0:G4 * 128].rearrange("p (h t) -> p h t", h=G4)),
                  R=[psk[pt]], W=["QT"])

        pipeline(NTO, [qS0, qS1, qS2])
        items = [(qc, h, kt) for qc in range(4) for h in range(G4) for kt in range(NT)]
        LA = 2

        def emit_scores(idx):
            qc, h, kt = items[idx]
            pb_ = idx % 3
            P.add("pe", lambda e, h=h, qc=qc, kt=kt, pb_=pb_: e.matmul(
                ps[pb_][:, :], lhsT=KT[0:97, h, kt * 128:(kt + 1) * 128], rhs=QT[0:97, h, qc * 512:(qc + 1) * 512],
                start=True, stop=True), R=["KT", "QT"], W=[psk[pb_]])

        def emit_epilogue(qc, h):
            po = 6 + h % 2
            sb_ = qc % 2
            P.add("dve", lambda e, po=po: e.tensor_copy(out=oTs, in_=ps[po][0:65, :]), R=[psk[po]], W=["oTs"])
            for t4 in range(4):
                pt = 3 + (t4 % 2)
                P.add("pe", lambda e, t4=t4, pt=pt: e.transpose(out=ps[pt][:, 0:65], in_=oTs[:, t4 * 128:(t4 + 1) * 128], identity=identf[0:65, 0:65]),
                      R=["oTs", "identf"], W=[psk[pt]])
                P.add("dve", lambda e, pt=pt, t4=t4: e.reciprocal(out=rden[:, t4:t4 + 1], in_=ps[pt][:, 64:65]), R=[psk[pt]], W=["rden%d" % t4])
                P.add("dve", lambda e, pt=pt, t4=t4, h=h, sb_=sb_: e.tensor_scalar_mul(
                    out=astage[sb_][:, t4, h * 64:(h + 1) * 64], in0=ps[pt][:, 0:64], scalar1=rden[:, t4:t4 + 1]),
                    R=[psk[pt], "rden%d" % t4], W=["astage0"])
            if h == G4 - 1:
                P.add("sp", lambda e, qc=qc, g=g, sb_=sb_: e.dma_start(
                    out=aout_d[qc * 512:(qc + 1) * 512, :].rearrange("(t p) c -> p t c", p=128), in_=astage[sb_]),
                    R=["astage0"], W=["aout_d"], dma="ast0")

        for idx in range(min(LA, len(items))):
            emit_scores(idx)
        pending = None
        for idx, (qc, h, kt) in enumerate(items):
            pb_ = idx % 3
            po = 6 + h % 2
            P.add("act", lambda e, pb_=pb_: e.activation(out=PT[pb_], in_=ps[pb_][:, :], func=AF.Exp),
                  R=[psk[pb_]], W=["PT%d" % pb_])
            if idx + LA < len(items):
                emit_scores(idx + LA)
            P.add("pe", lambda e, h=h, kt=kt, pb_=pb_, po=po: e.matmul(
                ps[po][0:65, :], lhsT=Vaug[:, kt, h, 0:65], rhs=PT[pb_], start=(kt == 0), stop=(kt == NT - 1)),
                R=["Vaug", "PT%d" % pb_], W=[psk[po]])
            if pending is not None and kt == 3:
                emit_epilogue(*pending)
                pending = None
            if kt == NT - 1:
                pending = (qc, h)
        if pending is not None:
            emit_epilogue(*pending)
    P.barrier()
    P.release()
    P.release()

    if "a_out" in dbg:
        ta = dbg_out("a_out", [OWN, 512])
        P.mark()
        tba = P.sb([128, NTO, 512], F32, "dbgt2")
        P.add("sp", lambda e: e.dma_start(out=tba, in_=aout_d.rearrange("(j p) c -> p j c", p=128)), R=["aout_d"], W=["dbgt2"], dma="dbg0")
        P.add("sp", lambda e: e.dma_start(out=ta.rearrange("(j p) c -> p j c", p=128), in_=tba), R=["dbgt2"], W=["dbgo"], dma="dbg1")
        P.barrier()
        P.release()

    if stop == "attn":
        P.add("sp", None, R=[])
        P.emit()
        return nc, dbg_d

    if "hout_in" in dbg:
        hin = nc.dram_tensor("dbg_hout_in", [OWN, 512], F32, kind="ExternalInput").ap()
        P.mark()
        tbh = P.sb([128, NTO, 512], F32, "tbh")
        P.add("sp", lambda e: e.dma_start(out=tbh, in_=hin.rearrange("(j p) c -> p j c", p=128)), W=["tbh"], dma="dbg0")
        P.add("sp", lambda e: e.dma_start(out=hout_d.rearrange("(j p) c -> p j c", p=128), in_=tbh), R=["tbh"], W=["hout_d"], dma="dbg1")
        P.barrier()
        P.release()
    else:
        hyena_phase(nc, P, I, ps, psk, psb, U_d, hout_d, identf, identb, onesb, onesf, halfm, dbg, dbg_out, psall)

    if stop == "C":
        P.add("sp", None, R=[])
        P.emit()
        return nc, dbg_d
    xres = P.sb([128, NTO, D], F32, "xres")
    P.add("sp", lambda e: e.dma_start(out=xres, in_=I["x_rot"][0:OWN, :].rearrange("(j p) c -> p j c", p=128)), W=["xres"], dma="xres")
    P.mark()
    w_out = P.sb([128, 8, D], BF16, "w_out")
    P.add("pool", lambda e: e.dma_start(out=w_out, in_=I["w_out"].rearrange("(k p) n -> p k n", p=128)), W=["w_out"], dma="w0")
    P.add("sp", lambda e: e.dma_start(out=gb[:, 0:512], in_=I["attn_out_g"].partition_broadcast(128)), W=["gb"], dma="gain")
    P.add("sp", lambda e: e.dma_start(out=gb[:, 512:1024], in_=I["hy_out_g"].partition_broadcast(128)), W=["gb"], dma="gain")
    ND_ = 3
    mixin = [P.sb([128, D], F32, "mixin%d" % i) for i in range(ND_)]
    mixbf = [P.sb([128, D], BF16, "mixbf%d" % i) for i in range(ND_)]
    mT = [P.sb([128, 8, 128], BF16, "mT%d" % i) for i in range(ND_)]

    def dS0(j):
        b = j % ND_
        P.add("sp", lambda e, j=j, b=b: e.dma_start(out=mixin[b][:, 0:512], in_=aout_d[j * 128:(j + 1) * 128, :]),
              R=["aout_d"], W=["mixin%d" % b], dma="mixa%d" % b)
        P.add("sp", lambda e, j=j, b=b: e.dma_start(out=mixin[b][:, 512:1024], in_=hout_d[j * 128:(j + 1) * 128, :]),
              R=["hout_d"] + ["hout_d_%d" % k for k in range(8)], W=["mixin%d" % b], dma="mixh%d" % b)

    def dS1(j):
        b = j % ND_
        rms_norm(mixin[b][:, 0:512], 512, gb[:, 0:512], mixbf[b][:, 0:512], ["mixin%d" % b], ["mixbfa%d" % b], stk="sta")
        rms_norm(mixin[b][:, 512:1024], 512, gb[:, 512:1024], mixbf[b][:, 512:1024], ["mixin%d" % b], ["mixbfh%d" % b], stk="sth")

    def dS2(j):
        b = j % ND_
        pt = j % 2
        for k in range(8):
            P.add("pe", lambda e, k=k, b=b, pt=pt: e.transpose(out=psb(pt)[:, k * 128:(k + 1) * 128], in_=mixbf[b][:, k * 128:(k + 1) * 128], identity=identb),
                  R=["mixbfa%d" % b, "mixbfh%d" % b, "identb"], W=[psk[pt]])

    def dS3(j):
        b = j % ND_
        pt = j % 2
        P.add("act", lambda e, b=b, pt=pt: e.copy(out=mT[b].rearrange("p k t -> p (k t)"), in_=psb(pt)), R=[psk[pt]], W=["mT%d" % b])

    def dS4(j):
        b = j % ND_
        for n in range(2):
            py = 2 + 2 * (j % 2) + n
            for k in range(8):
                P.add("pe", lambda e, k=k, b=b, n=n, py=py: e.matmul(ps[py][:, :], lhsT=mT[b][:, k, :], rhs=w_out[:, k, n * 512:(n + 1) * 512],
                                                                      start=(k == 0), stop=(k == 7)), R=["mT%d" % b, "w_out"], W=[psk[py]])
            P.add("dve", lambda e, j=j, n=n, py=py: e.tensor_tensor(out=xres[:, j, n * 512:(n + 1) * 512], in0=ps[py][:, :],
                                                                     in1=xres[:, j, n * 512:(n + 1) * 512], op=ALU.add),
                  R=[psk[py], "xres"], W=["xres"])

    pipeline(NTO, [dS0, dS1, dS2, dS3, dS4])
    P.barrier()
    P.release()
    if stop == "D":
        P.add("sp", None, R=[])
        P.emit()
        return nc, dbg_d
    if "x1" in dbg:
        tx1 = dbg_out("x1", [OWN, D])
        P.add("sp", lambda e: e.dma_start(out=tx1.rearrange("(j p) c -> p j c", p=128), in_=xres), R=["xres"], W=["dbgo"], dma="dbg1")
        P.barrier()

    P.mark()
    hmT = P.sb([128, 8, 256], BF16, "hmT")
    KmT = P.sb([128, 8, 256], BF16, "KmT")
    Vm = P.sb([128, 2, 4, 260], BF16, "Vm")
    ksqm = P.sb([128, 8, 256], BF16, "ksqm")
    kbx = P.sb([1, 8], F32, "kbx")
    P.mark()
    w_mkv = P.sb([128, 8, 2 * D], BF16, "w_mkv")
    P.add("pool", lambda e: e.dma_start(out=w_mkv, in_=I["w_mkv"].rearrange("(k p) n -> p k n", p=128)), W=["w_mkv"], dma="w0")
    load_gain("mem_norm_g")
    P.add("pool", lambda e: e.memset(Vm.rearrange("p a b c -> p (a b c)"), 1.0), W=["Vm"])
    memt = [P.sb([128, D], F32, "memt%d" % i) for i in range(2)]
    membf = [P.sb([128, D], BF16, "membf%d" % i) for i in range(2)]
    for mt in range(2):
        P.add("sp", lambda e, mt=mt: e.dma_start(out=memt[mt], in_=I["mem_b"][mt * 128:(mt + 1) * 128, :]), W=["memt%d" % mt], dma="memt%d" % mt)
        rms_norm(memt[mt], D, gb, membf[mt], ["memt%d" % mt], ["membf%d" % mt], stk="stm")
        for k in range(8):
            P.add("pe", lambda e, k=k, mt=mt: e.transpose(out=psb(mt)[:, k * 128:(k + 1) * 128], in_=membf[mt][:, k * 128:(k + 1) * 128], identity=identb),
                  R=["membf%d" % mt, "identb"], W=[psk[mt]])
        P.add("act", lambda e, mt=mt: e.copy(out=hmT[:, :, mt * 128:(mt + 1) * 128], in_=psb(mt).rearrange("p (k t) -> p k t", k=8)),
              R=[psk[mt]], W=["hmT"])
    for dt in range(8):
        pk = 2 + dt % 2
        for k in range(8):
            P.add("pe", lambda e, k=k, dt=dt, pk=pk: e.matmul(ps[pk][:, 0:256], lhsT=w_mkv[:, k, dt * 128:(dt + 1) * 128], rhs=hmT[:, k, :],
                                                               start=(k == 0), stop=(k == 7)), R=["w_mkv", "hmT"], W=[psk[pk]])
        P.add("act", lambda e, dt=dt, pk=pk: e.copy(out=KmT[:, dt, :], in_=ps[pk][:, 0:256]), R=[psk[pk]], W=["KmT"])
    for mt in range(2):
        for n in range(2):
            pv = 4 + n
            for k in range(8):
                P.add("pe", lambda e, k=k, mt=mt, n=n, pv=pv: e.matmul(ps[pv][:, :], lhsT=hmT[:, k, mt * 128:(mt + 1) * 128],
                                                                        rhs=w_mkv[:, k, D + n * 512:D + (n + 1) * 512],
                                                                        start=(k == 0), stop=(k == 7)), R=["w_mkv", "hmT"], W=[psk[pv]])
            P.add("dve", lambda e, mt=mt, n=n, pv=pv: e.tensor_copy(out=Vm[:, mt, 2 * n:2 * n + 2, 0:256],
                                                                    in_=ps[pv][:, :].rearrange("p (h c) -> p h c", h=2)),
                  R=[psk[pv]], W=["Vm"])
    P.add("dve", lambda e: e.tensor_tensor(out=ksqm, in0=KmT, in1=KmT, op=ALU.mult), R=["KmT"], W=["ksqm"])
    for hh in range(4):
        for dt in range(2):
            P.add("pe", lambda e, hh=hh, dt=dt: e.matmul(ps[6][0:1, 0:256], lhsT=onesb[:, 0:1], rhs=ksqm[:, 2 * hh + dt, :],
                                                          start=(dt == 0), stop=(dt == 1)), R=["ksqm", "onesb"], W=[psk[6]])
        P.add("dve", lambda e, hh=hh: e.tensor_reduce(out=kbx[0:1, hh:hh + 1], in_=ps[6][0:1, 0:256], axis=AX.X, op=ALU.max),
              R=[psk[6]], W=["kbx%d" % hh])
    P.add("dve", lambda e: e.tensor_reduce(out=kbx[0:1, 4:5], in_=kbx[0:1, 0:4], axis=AX.X, op=ALU.max),
          R=["kbx0", "kbx1", "kbx2", "kbx3"], W=["kbx4"])
    P.add("act", lambda e: e.sqrt(out=kbx[0:1, 5:6], in_=kbx[0:1, 4:5]), R=["kbx4"], W=["kbx5"])
    P.add("dve", lambda e: e.tensor_scalar_mul(out=kbx[0:1, 6:7], in0=kbx[0:1, 5:6], scalar1=-1.04), R=["kbx5"], W=["kbx6"])
    P.barrier()
    P.release()
    w_mq = P.sb([128, 8, D], BF16, "w_mq")
    w_mo = P.sb([128, 8, D], BF16, "w_mo")
    P.add("pool", lambda e: e.dma_start(out=w_mq, in_=I["w_mq"].rearrange("(k p) n -> p k n", p=128)), W=["w_mq"], dma="w0")
    P.add("pool", lambda e: e.dma_start(out=w_mo, in_=I["w_mo"].rearrange("(k p) n -> p k n", p=128)), W=["w_mo"], dma="w1")
    load_gain("cross_norm_g")
    hxbf = [P.sb([128, D], BF16, "hxbf%d" % i) for i in range(2)]
    hxT2 = [P.sb([128, 8, 512], BF16, "hxT0")] * 2
    qT2 = [P.sb([128, 8, 512], BF16, "qT%d" % i) for i in range(2)]
    qsqx2 = [P.sb([128, 8, 512], BF16, "qsqx0")] * 2
    negm2 = [P.sb([1, 4, 512], BF16, "negm%d" % i) for i in range(2)]
    qn4 = P.sb([1, 2048], F32, "qn4")
    PTm = [P.sb([128, 2, 512], BF16, "PTm%d" % i) for i in range(2)]
    rdn = [P.sb([1, 512], F32, "rdn%d" % i) for i in range(2)]
    rdb = [P.sb([128, 512], F32, "rdb%d" % i) for i in range(2)]
    oTx2 = [P.sb([128, 8, 512], BF16, "oTx%d" % i) for i in range(2)]
    def e_front(qc):
        pq_ = qc % 2
        def eS0(t4):
            j = qc * 4 + t4
            b = t4 % 2
            rms_norm(xres[:, j, :], D, gb, hxbf[b], ["xres"], ["hxbf%d" % b], stk="stx")

        def eS1(t4):
            b = t4 % 2
            pb = t4 % 2
            for k in range(8):
                P.add("pe", lambda e, k=k, b=b, pb=pb: e.transpose(out=psb(pb)[:, k * 128:(k + 1) * 128], in_=hxbf[b][:, k * 128:(k + 1) * 128], identity=identb),
                      R=["hxbf%d" % b, "identb"], W=[psk[pb]])

        def eS2(t4):
            pb = t4 % 2
            P.add("act", lambda e, pb=pb, t4=t4: e.copy(out=hxT2[pq_][:, :, t4 * 128:(t4 + 1) * 128], in_=psb(pb).rearrange("p (k t) -> p k t", k=8)),
                  R=[psk[pb]], W=["hxT0"])

        pipeline(4, [eS0, eS1, eS2])
        for dt in range(8):
            pq = 4 + dt % 2
            for k in range(8):
                P.add("pe", lambda e, k=k, dt=dt, pq=pq: e.matmul(ps[pq][:, :], lhsT=w_mq[:, k, dt * 128:(dt + 1) * 128], rhs=hxT2[pq_][:, k, :],
                                                                   start=(k == 0), stop=(k == 7)), R=["w_mq", "hxT0"], W=[psk[pq]])
            if dt % 2 == 0:
                P.add("act", lambda e, dt=dt, pq=pq: e.mul(out=qT2[pq_][:, dt, :], in_=ps[pq][:, :], mul=1.0 / 16.0), R=[psk[pq]], W=["qT%d_%d" % (pq_, dt)])
            else:
                P.add("dve", lambda e, dt=dt, pq=pq: e.tensor_scalar_mul(out=qT2[pq_][:, dt, :], in0=ps[pq][:, :], scalar1=1.0 / 16.0), R=[psk[pq]], W=["qT%d_%d" % (pq_, dt)])
        QTK = ["qT%d_%d" % (pq_, k) for k in range(8)]
        P.add("pool", lambda e: e.tensor_tensor(out=qsqx2[pq_], in0=qT2[pq_], in1=qT2[pq_], op=ALU.mult), R=QTK, W=["qsqx0"])
        for hh in range(4):
            for dt in range(2):
                P.add("pe", lambda e, hh=hh, dt=dt: e.matmul(ps[4 + hh][0:1, :], lhsT=onesb[:, 0:1], rhs=qsqx2[pq_][:, 2 * hh + dt, :],
                                                              start=(dt == 0), stop=(dt == 1)), R=["qsqx0", "onesb"], W=[psk[4 + hh]])
        P.add("act", lambda e: e.sqrt(out=qn4, in_=psall[0:1, 4 * 512:8 * 512]), R=[psk[4], psk[5], psk[6], psk[7]], W=["qn4"])
        P.add("dve", lambda e: e.tensor_scalar_mul(out=negm2[pq_].rearrange("p a b -> p (a b)"), in0=qn4, scalar1=kbx[0:1, 6:7]), R=["qn4", "kbx6"], W=["negm%d" % pq_])


    def e_heads(qc):
        pq_ = qc % 2
        QTK = ["qT%d_%d" % (pq_, k) for k in range(8)]
        def hA(hh):
            s0 = 2 * (hh % 2)
            for mt in range(2):
                for dt in range(2):
                    P.add("pe", lambda e, hh=hh, mt=mt, dt=dt, s0=s0: e.matmul(ps[s0 + mt][:, :], lhsT=KmT[:, 2 * hh + dt, mt * 128:(mt + 1) * 128],
                                                                                rhs=qT2[pq_][:, 2 * hh + dt, :], start=(dt == 0), stop=False),
                          R=["KmT"] + QTK, W=[psk[s0 + mt]])
                P.add("pe", lambda e, hh=hh, mt=mt, s0=s0: e.matmul(ps[s0 + mt][:, :], lhsT=onesb[0:1, 0:128], rhs=negm2[pq_][0:1, hh, :], start=False, stop=True),
                      R=["negm%d" % pq_, "onesb"], W=[psk[s0 + mt]])

        def hB(hh):
            s0 = 2 * (hh % 2)
            pb_ = hh % 2
            P.add("act", lambda e, s0=s0, pb_=pb_: e.activation(out=PTm[pb_].rearrange("p a b -> p (a b)"), in_=psall[:, s0 * 512:(s0 + 2) * 512], func=AF.Exp),
                  R=[psk[s0], psk[s0 + 1]], W=["PTm%d" % pb_])

        def hC(hh):
            if hh > 0:
                hD(hh - 1)
            pb_ = hh % 2
            for dv_ in range(2):
                for mt in range(2):
                    P.add("pe", lambda e, hh=hh, mt=mt, dv_=dv_, pb_=pb_: e.matmul(ps[4 + dv_][:, :], lhsT=Vm[:, mt, hh, dv_ * 128:(dv_ + 1) * 128], rhs=PTm[pb_][:, mt, :],
                                                                                   start=(mt == 0), stop=(mt == 1)), R=["Vm", "PTm%d" % pb_], W=[psk[4 + dv_]])
            for mt in range(2):
                P.add("pe", lambda e, hh=hh, mt=mt, pb_=pb_: e.matmul(ps[6][0:1, :], lhsT=Vm[:, mt, hh, 256:257], rhs=PTm[pb_][:, mt, :], start=(mt == 0), stop=(mt == 1)),
                      R=["Vm", "PTm%d" % pb_], W=[psk[6]])
            P.add("dve", lambda e, pb_=pb_: e.reciprocal(out=rdn[pb_], in_=ps[6][0:1, :]), R=[psk[6]], W=["rdn%d" % pb_])

        def hD(hh):
            pb_ = hh % 2
            P.add("pe", lambda e, pb_=pb_: e.matmul(ps[7][:, :], lhsT=onesf[0:1, 0:128], rhs=rdn[pb_], start=True, stop=True), R=["rdn%d" % pb_, "onesf"], W=[psk[7]])
            P.add("act", lambda e, pb_=pb_: e.copy(out=rdb[pb_], in_=ps[7][:, :]), R=[psk[7]], W=["rdb%d" % pb_])
            for dv_ in range(2):
                P.add("dve", lambda e, hh=hh, dv_=dv_, pb_=pb_: e.tensor_tensor(out=oTx2[pq_][:, 2 * hh + dv_, :], in0=ps[4 + dv_][:, :], in1=rdb[pb_], op=ALU.mult),
                      R=[psk[4 + dv_], "rdb%d" % pb_], W=["oTx%d_%d" % (pq_, 2 * hh + dv_)])

        pipeline(4, [hA, hB, hC])
        hD(3)

    def e_yproj(qc):
        pq_ = qc % 2
        OTK = ["oTx%d_%d" % (pq_, k) for k in range(8)]
        for t4 in range(4):
            j = qc * 4 + t4
            for n in range(2):
                py = (0, 1, 2, 3)[(t4 * 2 + n) % 4]
                for dt in range(8):
                    P.add("pe", lambda e, dt=dt, t4=t4, n=n, py=py: e.matmul(ps[py][:, :], lhsT=oTx2[pq_][:, dt, t4 * 128:(t4 + 1) * 128],
                                                                              rhs=w_mo[:, dt, n * 512:(n + 1) * 512], start=(dt == 0), stop=(dt == 7)),
                          R=OTK + ["w_mo"], W=[psk[py]])
                P.add("dve", lambda e, j=j, n=n, py=py: e.tensor_tensor(out=xres[:, j, n * 512:(n + 1) * 512], in0=ps[py][:, :],
                                                                         in1=xres[:, j, n * 512:(n + 1) * 512], op=ALU.add),
                      R=[psk[py], "xres"], W=["xres"])

    pipeline(4, [e_front, e_heads, e_yproj])
    P.barrier()
    P.release()
    if "x2" in dbg:
        tx2 = dbg_out("x2", [OWN, D])
        P.add("sp", lambda e: e.dma_start(out=tx2.rearrange("(j p) c -> p j c", p=128), in_=xres), R=["xres"], W=["dbgo"], dma="dbg1")
        P.barrier()
    if stop == "E":
        P.add("sp", None, R=[])
        P.emit()
        return nc, dbg_d

    P.mark()
    tT = P.sb([128, 8, OWN], BF16, "tT")
    combT = P.sb([32, OWN], F32, "combT")
    P.mark()
    load_gain("ffn_norm_g")
    w_r = P.sb([128, 8, 36], F32, "w_r")
    b_r = P.sb([128, 36], F32, "b_r")
    P.add("sp", lambda e: e.dma_start(out=w_r, in_=I["w_route"].rearrange("(k p) n -> p k n", p=128)), W=["w_r"], dma="c3")
    P.add("sp", lambda e: e.dma_start(out=b_r, in_=I["b_route"].partition_broadcast(128)), W=["b_r"], dma="c4")
    tnf = [P.sb([128, D], F32, "tnf%d" % i) for i in range(2)]
    tnb = [P.sb([128, D], BF16, "tnb%d" % i) for i in range(2)]
    tTf = P.sb([128, 8, 128], F32, "tTf")
    T_ = NTO
    lg = P.sb([128, T_, 36], F32, "lg")
    gmx = P.sb([128, T_], F32, "gmx")
    oh = P.sb([128, T_, 4], F32, "oh")
    ge = P.sb([128, T_, 4], F32, "ge")
    gsm = P.sb([128, T_], F32, "gsm")
    pg = P.sb([128, T_], F32, "pg")
    tmp48 = P.sb([128, T_, 4, 8], F32, "tmp48")
    ein = P.sb([128, T_, 8], F32, "ein")
    e2 = P.sb([128, T_, 8], F32, "e2")
    mk1 = P.sb([128, T_, 8], F32, "mk1")
    mk2 = P.sb([128, T_, 8], F32, "mk2")
    m1 = P.sb([128, T_], F32, "m1")
    m2 = P.sb([128, T_], F32, "m2")
    dd = P.sb([128, T_], F32, "dd")
    p1 = P.sb([128, T_], F32, "p1")
    p2 = P.sb([128, T_], F32, "p2")
    we = P.sb([128, T_, 8], F32, "we")
    we2 = P.sb([128, T_, 8], F32, "we2")
    comb = P.sb([128, T_, 4, 8], F32, "comb")
    seq = [0]

    def dv(fn, R, W):
        P.add("dve", fn, R=R, W=W)

    for j in range(NTO):
        b = j % 2
        rms_norm(xres[:, j, :], D, gb, tnf[b], ["xres"], ["tnf%d" % b], stk="stf")
        P.add("act", lambda e, b=b: e.copy(out=tnb[b], in_=tnf[b]), R=["tnf%d" % b], W=["tnb%d" % b])
        for k in range(8):
            P.add("pe", lambda e, k=k, b=b: e.transpose(out=psb(b)[:, k * 128:(k + 1) * 128], in_=tnb[b][:, k * 128:(k + 1) * 128], identity=identb),
                  R=["tnb%d" % b, "identb"], W=[psk[b]])
        P.add("act", lambda e, b=b, j=j: e.copy(out=tT[:, :, j * 128:(j + 1) * 128], in_=psb(b).rearrange("p (k t) -> p k t", k=8)),
              R=[psk[b]], W=["tT"])
        for k in range(8):
            pf = 2 + (k // 4)
            P.add("pe", lambda e, k=k, b=b, pf=pf: e.transpose(out=ps[pf][:, (k % 4) * 128:(k % 4 + 1) * 128], in_=tnf[b][:, k * 128:(k + 1) * 128], identity=identf),
                  R=["tnf%d" % b, "identf"], W=[psk[pf]])
        for hf in range(2):
            P.add("dve" if hf == 0 else "act", (lambda e, hf=hf: e.tensor_copy(out=tTf[:, hf * 4:(hf + 1) * 4, :], in_=ps[2 + hf][:, :].rearrange("p (k t) -> p k t", k=4)))
                  if hf == 0 else (lambda e, hf=hf: e.copy(out=tTf[:, hf * 4:(hf + 1) * 4, :], in_=ps[2 + hf][:, :].rearrange("p (k t) -> p k t", k=4))),
                  R=[psk[2 + hf]], W=["tTf%d" % hf])
        for k in range(8):
            P.add("pe", lambda e, k=k: e.matmul(ps[4][:, 0:36], lhsT=tTf[:, k, :], rhs=w_r[:, k, :], start=(k == 0), stop=(k == 7)),
                  R=["tTf0", "tTf1", "w_r"], W=[psk[4]])
        dv(lambda e, j=j: e.tensor_tensor(out=lg[:, j, :], in0=ps[4][:, 0:36], in1=b_r, op=ALU.add), [psk[4], "b_r"], ["lg"])

    def col(t, n):
        return t.rearrange("p (t o) -> p t o", o=1).broadcast_to([128, T_, n])

    gl = lg[:, :, 0:4]
    el = lg[:, :, 4:36].rearrange("p t (g e) -> p t g e", g=4)
    dv(lambda e: e.tensor_reduce(out=gmx, in_=gl, axis=AX.X, op=ALU.max), ["lg"], ["gmx"])
    dv(lambda e: e.tensor_tensor(out=oh, in0=gl, in1=col(gmx, 4), op=ALU.is_equal), ["lg", "gmx"], ["oh"])
    dv(lambda e: e.tensor_tensor(out=ge, in0=gl, in1=col(gmx, 4), op=ALU.subtract), ["lg", "gmx"], ["ge"])
    P.add("act", lambda e: e.activation(out=ge, in_=ge, func=AF.Exp), R=["ge"], W=["ge"])
    dv(lambda e: e.tensor_reduce(out=gsm, in_=ge, axis=AX.X, op=ALU.add), ["ge"], ["gsm"])
    dv(lambda e: e.reciprocal(out=pg, in_=gsm), ["gsm"], ["pg"])
    dv(lambda e: e.tensor_tensor(out=tmp48, in0=el, in1=oh.rearrange("p t (g o) -> p t g o", o=1).broadcast_to([128, T_, 4, 8]), op=ALU.mult),
       ["lg", "oh"], ["tmp48"])
    dv(lambda e: e.tensor_reduce(out=ein, in_=tmp48.rearrange("p t g e -> p t e g"), axis=AX.X, op=ALU.add), ["tmp48"], ["ein"])
    dv(lambda e: e.tensor_reduce(out=m1, in_=ein, axis=AX.X, op=ALU.max), ["ein"], ["m1"])
    dv(lambda e: e.tensor_tensor(out=mk1, in0=ein, in1=col(m1, 8), op=ALU.is_equal), ["ein", "m1"], ["mk1"])
    dv(lambda e: e.scalar_tensor_tensor(out=e2, in0=mk1, scalar=-1e30, in1=ein, op0=ALU.mult, op1=ALU.add), ["mk1", "ein"], ["e2"])
    dv(lambda e: e.tensor_reduce(out=m2, in_=e2, axis=AX.X, op=ALU.max), ["e2"], ["m2"])
    dv(lambda e: e.tensor_tensor(out=mk2, in0=e2, in1=col(m2, 8), op=ALU.is_equal), ["e2", "m2"], ["mk2"])
    dv(lambda e: e.tensor_tensor(out=dd, in0=m2, in1=m1, op=ALU.subtract), ["m1", "m2"], ["dd"])
    P.add("act", lambda e: e.activation(out=dd, in_=dd, func=AF.Exp), R=["dd"], W=["dd"])
    dv(lambda e: e.tensor_scalar_add(out=p1, in0=dd, scalar1=1.0), ["dd"], ["p1"])
    dv(lambda e: e.reciprocal(out=p1, in_=p1), ["p1"], ["p1"])
    dv(lambda e: e.tensor_tensor(out=p2, in0=dd, in1=p1, op=ALU.mult), ["dd", "p1"], ["p2"])
    dv(lambda e: e.tensor_tensor(out=p1, in0=p1, in1=pg, op=ALU.mult), ["p1", "pg"], ["p1"])
    dv(lambda e: e.tensor_tensor(out=p2, in0=p2, in1=pg, op=ALU.mult), ["p2", "pg"], ["p2"])
    dv(lambda e: e.tensor_tensor(out=we, in0=mk1, in1=col(p1, 8), op=ALU.mult), ["mk1", "p1"], ["we"])
    dv(lambda e: e.tensor_tensor(out=we2, in0=mk2, in1=col(p2, 8), op=ALU.mult), ["mk2", "p2"], ["we2"])
    dv(lambda e: e.tensor_tensor(out=we, in0=we, in1=we2, op=ALU.add), ["we", "we2"], ["we"])
    dv(lambda e: e.tensor_tensor(out=comb, in0=we.rearrange("p t (o e) -> p t o e", o=1).broadcast_to([128, T_, 4, 8]),
                                 in1=oh.rearrange("p t (g o) -> p t g o", o=1).broadcast_to([128, T_, 4, 8]), op=ALU.mult), ["we", "oh"], ["comb"])
    for j in range(NTO):
        pc_ = 5 + j % 2
        P.add("pe", lambda e, j=j, pc_=pc_: e.transpose(out=ps[pc_][0:32, 0:128], in_=comb[:, j, :, :].rearrange("p g e -> p (g e)"), identity=identf),
              R=["comb", "identf"], W=[psk[pc_]])
        dv(lambda e, j=j, pc_=pc_: e.tensor_copy(out=combT[:, j * 128:(j + 1) * 128], in_=ps[pc_][0:32, 0:128]), [psk[pc_]], ["combT"])
    P.add("sp", lambda e: e.dma_start(out=combT_d, in_=combT), R=["combT"], W=["combT_d"], dma="combT")
    if "comb" in dbg:
        tcb = dbg_out("comb", [32, OWN])
        P.add("sp", lambda e: e.dma_start(out=tcb, in_=combT), R=["combT"], W=["dbgo"], dma="dbg1")
    P.barrier()
    P.release()
    NSLOT = 4
    wg = [P.sb([128, 8, 256], BF16, "wg%d" % i) for i in range(NSLOT)]
    wu = [P.sb([128, 8, 256], BF16, "wu%d" % i) for i in range(NSLOT)]
    wd = [P.sb([128, 2, D], BF16, "wd%d" % i) for i in range(NSLOT)]
    CB = [P.sb([128, OWN], F32, "CB%d" % i) for i in range(2)]
    sa = [P.sb([128, 2, 512], F32, "sa%d" % i) for i in range(2)]
    sc = [P.sb([128, 2, 512], F32, "sc%d" % i) for i in range(2)]
    mTe = [P.sb([128, 2, 512], BF16, "mTe%d" % i) for i in range(2)]
    def load_expert(e_):
        sl = e_ % NSLOT
        P.add("pool", lambda e, e_=e_, sl=sl: e.dma_start(out=wg[sl], in_=I["w_gate"][e_].rearrange("(k p) n -> p k n", p=128)), W=["wg%d" % sl], dma="wg%d" % sl)
        P.add("pool", lambda e, e_=e_, sl=sl: e.dma_start(out=wu[sl], in_=I["w_up"][e_].rearrange("(k p) n -> p k n", p=128)), W=["wu%d" % sl], dma="wu%d" % sl)
        P.add("pool", lambda e, e_=e_, sl=sl: e.dma_start(out=wd[sl], in_=I["w_down"][e_].rearrange("(k p) n -> p k n", p=128)), W=["wd%d" % sl], dma="wd%d" % sl)

    for e_ in range(2):
        load_expert(e_)
    yb = 0
    for pr in range(16):
        for ee in range(2):
            if 2 * pr + 2 + ee < 32:
                load_expert(2 * pr + 2 + ee)
        for ee in range(2):
            e_ = 2 * pr + ee
            P.add("sp", lambda e, e_=e_, ee=ee: e.dma_start(out=CB[ee], in_=combT_d[e_:e_ + 1, :].partition_broadcast(128)),
                  R=["combT_d"], W=["CB%d" % ee], dma="CB%d" % ee)
        for c in range(4):
            for ee in range(2):
                e_ = 2 * pr + ee
                sl = e_ % NSLOT
                for f in range(2):
                    for k in range(8):
                        P.add("pe", lambda e, f=f, k=k, sl=sl, c=c: e.matmul(ps[f][:, :], lhsT=wg[sl][:, k, f * 128:(f + 1) * 128],
                                                                            rhs=tT[:, k, c * 512:(c + 1) * 512], start=(k == 0), stop=(k == 7)),
                              R=["wg%d" % sl, "tT"], W=[psk[f]])
                for f in range(2):
                    for k in range(8):
                        P.add("pe", lambda e, f=f, k=k, sl=sl, c=c: e.matmul(ps[2 + f][:, :], lhsT=wu[sl][:, k, f * 128:(f + 1) * 128],
                                                                            rhs=tT[:, k, c * 512:(c + 1) * 512], start=(k == 0), stop=(k == 7)),
                              R=["wu%d" % sl, "tT"], W=[psk[2 + f]])
                for f in range(2):
                    P.add("act", lambda e, f=f, ee=ee: e.activation(out=sa[ee][:, f, :], in_=ps[f][:, :], func=AF.Silu),
                          R=[psk[f]], W=["sa%d_%d" % (ee, f)])
                    P.add("pool", lambda e, f=f, ee=ee, c=c: e.tensor_tensor(out=sc[ee][:, f, :], in0=sa[ee][:, f, :],
                                                                              in1=CB[ee][:, c * 512:(c + 1) * 512], op=ALU.mult),
                          R=["sa%d_%d" % (ee, f), "CB%d" % ee], W=["sc%d_%d" % (ee, f)])
                    P.add("dve", lambda e, f=f, ee=ee: e.tensor_tensor(out=mTe[ee][:, f, :], in0=ps[2 + f][:, :], in1=sc[ee][:, f, :], op=ALU.mult),
                          R=[psk[2 + f], "sc%d_%d" % (ee, f)], W=["mTe%d" % ee])
            for t4 in range(4):
                j = c * 4 + t4
                for n in range(2):
                    py = 4 + (yb % 4)
                    yb += 1
                    cnt_mm = 0
                    for ee in range(2):
                        sl = (2 * pr + ee) % NSLOT
                        for f in range(2):
                            P.add("pe", lambda e, ee=ee, f=f, sl=sl, t4=t4, n=n, py=py, cnt_mm=cnt_mm: e.matmul(
                                ps[py][:, :], lhsT=mTe[ee][:, f, t4 * 128:(t4 + 1) * 128], rhs=wd[sl][:, f, n * 512:(n + 1) * 512],
                                start=(cnt_mm == 0), stop=(cnt_mm == 3)), R=["mTe%d" % ee, "wd%d" % sl], W=[psk[py]])
                            cnt_mm += 1
                    P.add("dve", lambda e, j=j, n=n, py=py: e.tensor_tensor(out=xres[:, j, n * 512:(n + 1) * 512], in0=ps[py][:, :],
                                                                             in1=xres[:, j, n * 512:(n + 1) * 512], op=ALU.add),
                          R=[psk[py], "xres"], W=["xres"])
    P.barrier()
    P.release()
    if "x3" in dbg:
        tx3 = dbg_out("x3", [OWN, D])
        P.add("sp", lambda e: e.dma_start(out=tx3.rearrange("(j p) c -> p j c", p=128), in_=xres), R=["xres"], W=["dbgo"], dma="dbg1")
        P.barrier()

    load_gain("final_norm_g")
    xo = [P.sb([128, D], F32, "xo%d" % i) for i in range(2)]
    for j in range(NTO):
        b = j % 2
        rms_norm(xres[:, j, :], D, gb, xo[b], ["xres"], ["xo%d" % b], stk="stg")
        P.add("sp", lambda e, j=j, b=b: e.dma_start(out=out_d[j * 128:(j + 1) * 128, :], in_=xo[b]),
              R=["xo%d" % b], W=["out_d%d" % b], dma="xo%d" % b)
    P.add("sp", None, R=["out_d0", "out_d1", "dbgo"])
    P.emit()
    return nc, dbg_d


def hyena_phase(nc, P, I, ps, psk, psb, U_d, hout_d, identf, identb, onesb, onesf, halfm, dbg, dbg_out, psall):
    NF = 64
    Hd = nc.dram_tensor("hy_H", [S, 2048], BF16, kind="Internal").ap()
    Bd = [nc.dram_tensor("hy_B%d" % i, [128, NF, 512], BF16, kind="Internal").ap() for i in range(2)]
    Kd = [nc.dram_tensor("hy_K%d" % i, [128, NF, 512], BF16, kind="Internal").ap() for i in range(2)]
    Btd = nc.dram_tensor("hy_Bt", [128, NF, 512], BF16, kind="Internal").ap()
    z1_d = nc.dram_tensor("hy_z1", [S, 512], BF16, kind="Internal").ap()
    P.mark()
    Dt = P.sb([64, 64, 128], BF16, "Dt")
    Dinv = P.sb([128, 64, 64], BF16, "Dinv")
    F2a = P.sb([128, 128], BF16, "F2a")
    F2b = P.sb([128, 128], BF16, "F2b")
    Gm = P.sb([128, 128], BF16, "Gm")
    sgn = P.sb([128, 1], F32, "sgn")
    skipb = P.sb([128, 2, 512], F32, "skipb")
    P.add("pool", lambda e: e.dma_start(out=Dt, in_=I["hy_D0"].rearrange("a (b c) -> a b c", b=64)), W=["Dt"], dma="ht0")
    P.add("pool", lambda e: e.dma_start(out=F2a, in_=I["hy_F2a"]), W=["F2a"], dma="ht1")
    P.add("pool", lambda e: e.dma_start(out=F2b, in_=I["hy_F2b"]), W=["F2b"], dma="ht2")
    P.add("pool", lambda e: e.dma_start(out=Gm, in_=I["hy_G"]), W=["Gm"], dma="ht3")
    P.add("pool", lambda e: e.dma_start(out=Dinv, in_=I["hy_Dinv"].rearrange("a (b c) -> a b c", b=64)), W=["Dinv"], dma="ht4")
    P.add("sp", lambda e: e.dma_start(out=sgn, in_=I["hy_sgn"]), W=["sgn"], dma="c3")
    P.add("sp", lambda e: e.dma_start(out=skipb.rearrange("p a b -> p (a b)"), in_=I["hy_skip"].partition_broadcast(128)), W=["skipb"], dma="c4")

    P.mark()
    zT = P.sb([33, S], F32, "zT")
    g1T = P.sb([64, S], F32, "g1T")
    g2T = P.sb([64, S], F32, "g2T")
    hcols = P.sb([64, 4], F32, "hcols")
    w1 = P.sb([33, 64], F32, "w1")
    w2 = P.sb([64, 64], F32, "w2")
    w3 = P.sb([64, 2048], F32, "w3")
    b3r = P.sb([1, 2048], F32, "b3r")
    adec = P.sb([128, 2048], F32, "adec")
    nt01 = P.sb([128, NT], F32, "nt01")
    mpi = P.sb([128, 1], F32, "mpi")
    argt = P.sb([64, 512], F32, "argt")
    argm = P.sb([64, 512], F32, "argm")
    hfo = [P.sb([128, 2048], BF16, "hfo%d" % i) for i in range(2)]
    P.add("sp", lambda e: e.dma_start(out=zT, in_=I["hy_zT"]), W=["zT"], dma="hf0")
    P.add("sp", lambda e: e.dma_start(out=hcols, in_=I["hy_cols"]), W=["hcols"], dma="hf1")
    P.add("sp", lambda e: e.dma_start(out=w1, in_=I["hy_w1"]), W=["w1"], dma="hf2")
    P.add("sp", lambda e: e.dma_start(out=w2, in_=I["hy_w2"]), W=["w2"], dma="hf3")
    P.add("sp", lambda e: e.dma_start(out=w3, in_=I["hy_w3"]), W=["w3"], dma="hf4")
    P.add("sp", lambda e: e.dma_start(out=b3r, in_=I["hy_b3"]), W=["b3r"], dma="hf5")
    P.add("sp", lambda e: e.dma_start(out=adec, in_=I["hy_decay"].partition_broadcast(128)), W=["adec"], dma="hf6")
    P.add("sp", lambda e: e.dma_start(out=nt01, in_=I["hy_nt01"]), W=["nt01"], dma="hf7")
    P.add("pool", lambda e: e.memset(mpi, -math.pi), W=["mpi"])
    P.add("act", lambda e: e.activation(out=adec, in_=adec, func=AF.Abs), R=["adec"], W=["adec"])
    OFFS = math.pi + 16.0 * math.pi
    for (src, wt, kk, bcol, fcol, dst, nm) in ((zT, w1, 33, 0, 2, g1T, "g1T"), (g1T, w2, 64, 1, 3, g2T, "g2T")):
        for ch in range(8):
            pb_ = ch % 2
            P.add("pe", lambda e, src=src, wt=wt, kk=kk, ch=ch, pb_=pb_: e.matmul(ps[pb_][0:64, :], lhsT=wt[0:kk, :], rhs=src[0:kk, ch * 512:(ch + 1) * 512],
                                                                               start=True, stop=True), R=["zT", "g1T", "w1", "w2"], W=[psk[pb_]])
            P.add("dve", lambda e, pb_=pb_, bcol=bcol, fcol=fcol: e.tensor_scalar(out=argt, in0=ps[pb_][0:64, :], scalar1=hcols[:, bcol:bcol + 1],
                                                                                  scalar2=hcols[:, fcol:fcol + 1], op0=ALU.add, op1=ALU.mult),
                  R=[psk[pb_], "hcols"], W=["argt"])
            for _rep in range(2):
                P.add("dve", lambda e: e.tensor_scalar(out=argm, in0=argt, scalar1=math.pi, scalar2=None, op0=ALU.is_gt), R=["argt"], W=["argm"])
                P.add("dve", lambda e: e.scalar_tensor_tensor(out=argt, in0=argm, scalar=-2.0 * math.pi, in1=argt, op0=ALU.mult, op1=ALU.add),
                      R=["argm", "argt"], W=["argt"])
                P.add("dve", lambda e: e.tensor_scalar(out=argm, in0=argt, scalar1=-math.pi, scalar2=None, op0=ALU.is_lt), R=["argt"], W=["argm"])
                P.add("dve", lambda e: e.scalar_tensor_tensor(out=argt, in0=argm, scalar=2.0 * math.pi, in1=argt, op0=ALU.mult, op1=ALU.add),
                      R=["argm", "argt"], W=["argt"])
            P.add("act", lambda e, dst=dst, ch=ch: e.activation(out=dst[:, ch * 512:(ch + 1) * 512], in_=argt, func=AF.Sin),
                  R=["argt"], W=[nm])
    g2b = P.sb([64, S], BF16, "g2b")
    w3b = P.sb([64, 2048], BF16, "w3b")
    b3b = P.sb([1, 2048], BF16, "b3b")
    Etf = [P.sb([128, 2048], F32, "Etf%d" % i) for i in range(2)]
    P.add("act", lambda e: e.copy(out=g2b, in_=g2T), R=["g2T"], W=["g2b"])
    P.add("dve", lambda e: e.tensor_copy(out=w3b, in_=w3), R=["w3"], W=["w3b"])
    P.add("dve", lambda e: e.tensor_copy(out=b3b, in_=b3r), R=["b3r"], W=["b3b"])
    for i in range(NT):
        b = i % 2
        b0 = 4 * b
        for cg in range(4):
            pb_ = b0 + cg
            P.add("pe", lambda e, i=i, cg=cg, pb_=pb_: e.matmul(ps[pb_][:, :], lhsT=g2b[:, i * 128:(i + 1) * 128], rhs=w3b[:, cg * 512:(cg + 1) * 512],
                                                                 start=True, stop=False), R=["g2b", "w3b"], W=[psk[pb_]])
            P.add("pe", lambda e, cg=cg, pb_=pb_: e.matmul(ps[pb_][:, :], lhsT=onesb[0:1, 0:128], rhs=b3b[0:1, cg * 512:(cg + 1) * 512],
                                                            start=False, stop=True), R=["onesb", "b3b"], W=[psk[pb_]])
        P.add("act", lambda e, i=i, b=b: e.activation(out=Etf[b], in_=adec, func=AF.Exp, scale=nt01[:, i:i + 1]),
              R=["adec", "nt01"], W=["Etf%d" % b])
        for hf_ in range(2):
            P.add("dve", lambda e, hf_=hf_, b=b, b0=b0: e.tensor_tensor(out=hfo[b][:, hf_ * 1024:(hf_ + 1) * 1024],
                                                                      in0=psall[:, (b0 + 2 * hf_) * 512:(b0 + 2 * hf_ + 2) * 512],
                                                                      in1=Etf[b][:, hf_ * 1024:(hf_ + 1) * 1024], op=ALU.mult),
                  R=[psk[b0 + 2 * hf_], psk[b0 + 2 * hf_ + 1], "Etf%d" % b], W=["hfo%d" % b])
        if i == 0:
            for o in range(2):
                P.add("pool", lambda e, o=o: e.memset(hfo[0][0:1, o * 1024 + 512:o * 1024 + 1024], 0.0), R=[], W=["hfo0"])
        P.add("sp", lambda e, i=i, b=b: e.dma_start(out=Hd[i * 128:(i + 1) * 128, :], in_=hfo[b]), R=["hfo%d" % b], W=["Hd_%d" % i], dma="hfo%d" % b)
    P.barrier()
    P.release()
    if "hf" in dbg:
        thf = dbg_out("hf", [S, 2048])
        P.mark()
        tbf = P.sb([128, 2048], BF16, "tbf")
        tbf32 = P.sb([128, 2048], F32, "tbf32")
        for i in range(NT):
            P.add("sp", lambda e, i=i: e.dma_start(out=tbf, in_=Hd[i * 128:(i + 1) * 128, :]), R=["Hd_%d" % i], W=["tbf"], dma="dbg0")
            P.add("dve", lambda e: e.tensor_copy(out=tbf32, in_=tbf), R=["tbf"], W=["tbf32"])
            P.add("sp", lambda e, i=i: e.dma_start(out=thf[i * 128:(i + 1) * 128, :], in_=tbf32), R=["tbf32"], W=["dbgo"], dma="dbg1")
        P.barrier()
        P.release()

    ev = [0]
    HDK = ["Hd_%d" % i for i in range(NT)]
    Z1K = ["z1_d_%d" % i for i in range(8)]
    BD0K = ["Bd0_%d" % i for i in range(4)]
    BD1K = ["Bd1_%d" % i for i in range(4)]
    BTDK = ["Btd_%d" % i for i in range(8)]

    def evac(out, in_, Rk, Wk):
        ev[0] += 1
        if ev[0] % 2:
            P.add("act", lambda e: e.copy(out=out, in_=in_), R=Rk, W=Wk)
        else:
            P.add("dve", lambda e: e.tensor_copy(out=out, in_=in_), R=Rk, W=Wk)

    def stage1(src_view, cast, Bdst, bkey, srckeys):
        P.barrier()
        P.mark()
        xsb = [P.sb([64, 16, 512], BF16, "xsb%d" % i) for i in range(2)]
        Bsb = [P.sb([128, 16, 512], BF16, "Bsb%d" % i) for i in range(2)]
        def s1_load(ch):
            xb_ = ch % 2
            q = "pool" if cast else "sp"
            P.add(q, lambda e, ch=ch, xb_=xb_: e.dma_start(out=xsb[xb_], in_=src_view[:, ch * 16:(ch + 1) * 16, :]),
                  R=srckeys, W=["xsb%d" % xb_], dma="xsb%d" % xb_)

        s1_load(0)
        for ch in range(4):
            xb_ = ch % 2
            if ch + 1 < 4:
                s1_load(ch + 1)
            for g4 in range(4):
                b0 = 4 * (g4 % 2)
                for q4 in range(4):
                    s2l = g4 * 4 + q4
                    s2 = ch * 16 + s2l
                    P.add("pe", lambda e, s2=s2, s2l=s2l, xb_=xb_, pb_=b0 + q4: e.matmul(ps[pb_][:, :], lhsT=Dt[0:64, s2, :], rhs=xsb[xb_][0:64, s2l, :],
                                                                                        start=True, stop=True), R=["Dt", "xsb%d" % xb_], W=[psk[b0 + q4]])
                evac(Bsb[xb_][:, g4 * 4:(g4 + 1) * 4, :].rearrange("p a b -> p (a b)"), psall[:, b0 * 512:(b0 + 4) * 512],
                     [psk[b0 + k] for k in range(4)], ["Bsb%d_%d" % (xb_, g4)])
            P.add("act", lambda e, ch=ch, xb_=xb_: e.dma_start(out=Bdst[:, ch * 16:(ch + 1) * 16, :], in_=Bsb[xb_]),
                  R=["Bsb%d_%d" % (xb_, k) for k in range(4)], W=["%s_%d" % (bkey, ch)], dma="Bsb%d" % xb_)
        P.barrier()
        P.release()

    def blocked(ap2d):
        return ap2d.rearrange("(s1 s2) c -> s1 s2 c", s2=64)

    for o in range(2):
        stage1(blocked(Hd[:, o * 1024:o * 1024 + 512]), False, Bd[0], "Bd0", HDK)
        stage1(blocked(Hd[:, o * 1024 + 512:o * 1024 + 1024]), False, Bd[1], "Bd1", HDK)
        P.mark()
        BTf = [P.sb([128, 8, 512], BF16, "BTf%d" % i) for i in range(2)]
        BTb = [P.sb([128, 8, 512], BF16, "BTb%d" % i) for i in range(2)]
        xfs = [P.sb([128, 1024], F32, "xfs%d" % i) for i in range(2)]
        kst = [P.sb([128, 1024], F32, "kst%d" % i) for i in range(2)]
        Kc = [P.sb([128, 8, 512], BF16, "Kc%d" % i) for i in range(2)]
        def f2_load(fc):
            cb_ = fc % 2
            for r in range(2):
                P.add("sp", lambda e, r=r, fc=fc, cb_=cb_: e.dma_start(
                    out=BTf[cb_][r * 64:(r + 1) * 64, :, :], in_=Bd[0][r * 64 + fc * 8:r * 64 + fc * 8 + 8, :, :].rearrange("f s c -> s f c")),
                    R=BD0K, W=["BTf%d" % cb_], dma="BTf%d_%d" % (cb_, r))
                P.add("sp", lambda e, r=r, fc=fc, cb_=cb_: e.dma_start(
                    out=BTb[cb_][r * 64:(r + 1) * 64, :, :], in_=Bd[1][r * 64 + fc * 8:r * 64 + fc * 8 + 8, :, :].rearrange("f s c -> s f c")),
                    R=BD1K, W=["BTb%d" % cb_], dma="BTb%d_%d" % (cb_, r))

        f2_load(0)
        for fc in range(8):
            cb_ = fc % 2
            if fc + 1 < 8:
                f2_load(fc + 1)
            for gq in range(4):
                t_ = gq % 2
                fa, fb = 2 * t_, 4 + 2 * t_
                for q2 in range(2):
                    f1l = gq * 2 + q2
                    P.add("pe", lambda e, f1l=f1l, cb_=cb_, pb_=fa + q2: e.matmul(ps[pb_][:, :], lhsT=F2a, rhs=BTf[cb_][:, f1l, :], start=True, stop=True),
                          R=["F2a", "BTf%d" % cb_], W=[psk[fa + q2]])
                    P.add("pe", lambda e, f1l=f1l, cb_=cb_, pb_=fb + q2: e.matmul(ps[pb_][:, :], lhsT=F2a, rhs=BTb[cb_][:, f1l, :], start=True, stop=True),
                          R=["F2a", "BTb%d" % cb_], W=[psk[fb + q2]])
                P.add("act", lambda e, fa=fa, t_=t_: e.copy(out=xfs[t_], in_=psall[:, fa * 512:(fa + 2) * 512]), R=[psk[fa], psk[fa + 1]], W=["xfs%d" % t_])
                P.add("dve", lambda e, fb=fb, t_=t_: e.scalar_tensor_tensor(out=kst[t_], in0=psall[:, fb * 512:(fb + 2) * 512], scalar=sgn[:, 0:1], in1=xfs[t_],
                                                                           op0=ALU.mult, op1=ALU.add),
                      R=[psk[fb], psk[fb + 1], "xfs%d" % t_, "sgn"], W=["kst%d" % t_])
                for q2 in range(2):
                    f1l = gq * 2 + q2
                    P.add("pool", lambda e, t_=t_, cb_=cb_, f1l=f1l, q2=q2, o=o: e.tensor_tensor(out=Kc[cb_][0:64, f1l, :], in0=kst[t_][0:64, q2 * 512:(q2 + 1) * 512],
                                                                                          in1=skipb[0:64, o, :], op=ALU.add),
                          R=["kst%d" % t_, "skipb"], W=["Kc%d_a%d" % (cb_, f1l)])
                P.add("act", lambda e, t_=t_, cb_=cb_, gq=gq: e.copy(out=Kc[cb_][64:128, gq * 2:gq * 2 + 2, :].rearrange("p a b -> p (a b)"), in_=kst[t_][64:128, :]),
                      R=["kst%d" % t_], W=["Kc%d_b%d" % (cb_, gq)])
            P.add("act", lambda e, fc=fc, cb_=cb_, o=o: e.dma_start(out=Kd[o][:, fc * 8:(fc + 1) * 8, :], in_=Kc[cb_]),
                  R=["Kc%d_a%d" % (cb_, k) for k in range(8)] + ["Kc%d_b%d" % (cb_, k) for k in range(4)], W=["Kd%d_%d" % (o, fc)], dma="Kc%d" % cb_)
        P.barrier()
        P.release()
    if "kf" in dbg:
        tkf = dbg_out("kf", [2, 128, NF * 512])
        P.mark()
        tk16 = P.sb([128, 8, 512], BF16, "tk16")
        tk32 = P.sb([128, 8, 512], F32, "tk32")
        for o in range(2):
            for fc in range(8):
                P.add("sp", lambda e, o=o, fc=fc: e.dma_start(out=tk16, in_=Kd[o][:, fc * 8:(fc + 1) * 8, :]), R=["Kd%d_%d" % (o, fc)], W=["tk16"], dma="dbg0")
                P.add("dve", lambda e: e.tensor_copy(out=tk32, in_=tk16), R=["tk16"], W=["tk32"])
                P.add("sp", lambda e, o=o, fc=fc: e.dma_start(out=tkf[o][:, fc * 4096:(fc + 1) * 4096], in_=tk32.rearrange("p a b -> p (a b)")),
                      R=["tk32"], W=["dbgo"], dma="dbg1")
        P.barrier()
        P.release()

    P.add("pool", lambda e: e.dma_start(out=Dt, in_=I["hy_Dc"].rearrange("a (b c) -> a b c", b=64)), W=["Dt"], dma="ht0")
    for o in range(2):
        if o == 0:
            stage1(blocked(U_d[:, 1024:1536]), True, Bd[0], "Bd0", ["U_d"])
        else:
            stage1(blocked(z1_d), False, Bd[0], "Bd0", Z1K)
        P.mark()
        BT = [P.sb([128, 8, 512], BF16, "BT%d" % i) for i in range(2)]
        KA = [P.sb([128, 8, 512], BF16, "KA%d" % i) for i in range(2)]
        KB = [P.sb([128, 8, 512], BF16, "KB%d" % i) for i in range(2)]
        ta = [P.sb([128, 1024], F32, "ta%d" % i) for i in range(2)]
        tb2 = [P.sb([128, 1024], F32, "tb2%d" % i) for i in range(2)]
        Yc = [P.sb([128, 1024], BF16, "Yc%d" % i) for i in range(2)]
        Btsb = [P.sb([128, 8, 512], BF16, "Btsb%d" % i) for i in range(2)]
        def c2_load(fc):
            cb_ = fc % 2
            for r in range(2):
                P.add("sp", lambda e, r=r, fc=fc, cb_=cb_: e.dma_start(
                    out=BT[cb_][r * 64:(r + 1) * 64, :, :], in_=Bd[0][r * 64 + fc * 8:r * 64 + fc * 8 + 8, :, :].rearrange("f s c -> s f c")),
                    R=BD0K, W=["BT%d" % cb_], dma="BT%d_%d" % (cb_, r))
                P.add("sp", lambda e, r=r, fc=fc, cb_=cb_, o=o: e.dma_start(out=KA[cb_][r * 64:(r + 1) * 64, :, :], in_=Kd[o][0:64, fc * 8:(fc + 1) * 8, :]),
                      R=["Kd%d_%d" % (o, fc)], W=["KA%d" % cb_], dma="KA%d_%d" % (cb_, r))
                P.add("sp", lambda e, r=r, fc=fc, cb_=cb_, o=o: e.dma_start(out=KB[cb_][r * 64:(r + 1) * 64, :, :], in_=Kd[o][64:128, fc * 8:(fc + 1) * 8, :]),
                      R=["Kd%d_%d" % (o, fc)], W=["KB%d" % cb_], dma="KB%d_%d" % (cb_, r))

        c2_load(0)
        for fc in range(8):
            cb_ = fc % 2
            if fc + 1 < 8:
                c2_load(fc + 1)
            for gq in range(4):
                t_ = gq % 2
                fa, fb = 2 * t_, 4 + 2 * t_
                f0 = gq * 2
                for q2 in range(2):
                    f1l = f0 + q2
                    P.add("pe", lambda e, f1l=f1l, cb_=cb_, pb_=fa + q2: e.matmul(ps[pb_][:, :], lhsT=F2a, rhs=BT[cb_][:, f1l, :], start=True, stop=True),
                          R=["F2a", "BT%d" % cb_], W=[psk[fa + q2]])
                    P.add("pe", lambda e, f1l=f1l, cb_=cb_, pb_=fb + q2: e.matmul(ps[pb_][:, :], lhsT=F2b, rhs=BT[cb_][:, f1l, :], start=True, stop=True),
                          R=["F2b", "BT%d" % cb_], W=[psk[fb + q2]])
                P.add("dve", lambda e, fa=fa, t_=t_, cb_=cb_, f0=f0: e.tensor_tensor(out=ta[t_], in0=psall[:, fa * 512:(fa + 2) * 512],
                                                                                   in1=KA[cb_][:, f0:f0 + 2, :].rearrange("p a b -> p (a b)"), op=ALU.mult),
                      R=[psk[fa], psk[fa + 1], "KA%d" % cb_], W=["ta%d" % t_])
                P.add("dve", lambda e, fb=fb, t_=t_, cb_=cb_, f0=f0: e.tensor_tensor(out=tb2[t_], in0=psall[:, fb * 512:(fb + 2) * 512],
                                                                                   in1=KB[cb_][:, f0:f0 + 2, :].rearrange("p a b -> p (a b)"), op=ALU.mult),
                      R=[psk[fb], psk[fb + 1], "KB%d" % cb_], W=["tb2%d" % t_])
                P.add("pool", lambda e, t_=t_: e.tensor_tensor(out=Yc[t_], in0=ta[t_], in1=tb2[t_], op=ALU.add),
                      R=["ta%d" % t_, "tb2%d" % t_], W=["Yc%d" % t_])
                for q2 in range(2):
                    P.add("pe", lambda e, t_=t_, q2=q2, pb_=fa + q2: e.matmul(ps[pb_][:, :], lhsT=Gm, rhs=Yc[t_][:, q2 * 512:(q2 + 1) * 512], start=True, stop=True),
                          R=["Gm", "Yc%d" % t_], W=[psk[fa + q2]])
                P.add("act", lambda e, fa=fa, cb_=cb_, f0=f0: e.copy(out=Btsb[cb_][:, f0:f0 + 2, :].rearrange("p a b -> p (a b)"), in_=psall[:, fa * 512:(fa + 2) * 512]),
                      R=[psk[fa], psk[fa + 1]], W=["Btsb%d_%d" % (cb_, gq)])
            P.add("act", lambda e, fc=fc, cb_=cb_: e.dma_start(out=Btd[:, fc * 8:(fc + 1) * 8, :], in_=Btsb[cb_]),
                  R=["Btsb%d_%d" % (cb_, k) for k in range(4)], W=["Btd_%d" % fc], dma="Btsb%d" % cb_)
        P.barrier()
        P.release()
        P.mark()
        BtT = [P.sb([128, 8, 512], BF16, "BtT%d" % i) for i in range(2)]
        gch = [P.sb([64, 8, 512], F32, "gch%d" % i) for i in range(2)]
        zo = [P.sb([64, 8, 512], BF16 if o == 0 else F32, "zo%d" % i) for i in range(2)]
        M = 64 if o == 0 else 32
        gcol = 0 if o == 0 else 512
        dst = blocked(z1_d) if o == 0 else blocked(hout_d)
        dkey = "z1_d" if o == 0 else "hout_d"
        def i1_load(tc):
            cb_ = tc % 2
            for r in range(2):
                P.add("sp", lambda e, r=r, tc=tc, cb_=cb_: e.dma_start(
                    out=BtT[cb_][r * 64:(r + 1) * 64, :, :], in_=Btd[r * 64 + tc * 8:r * 64 + tc * 8 + 8, :, :].rearrange("t f c -> f t c")),
                    R=BTDK, W=["BtT%d" % cb_], dma="BtT%d_%d" % (cb_, r))
            P.add("sp", lambda e, tc=tc, cb_=cb_, M=M, gcol=gcol: e.dma_start(
                out=gch[cb_][0:M, :, :], in_=blocked(U_d[0:M * 64, gcol:gcol + 512])[:, tc * 8:(tc + 1) * 8, :]),
                R=["U_d"], W=["gch%d" % cb_], dma="gch%d" % cb_)

        i1_load(0)
        for tc in range(8):
            cb_ = tc % 2
            if tc + 1 < 8:
                i1_load(tc + 1)
            for g4 in range(2):
                b0 = 4 * ((2 * tc + g4) % 2)
                for q4 in range(4):
                    t2l = g4 * 4 + q4
                    t2 = tc * 8 + t2l
                    P.add("pe", lambda e, t2=t2, t2l=t2l, cb_=cb_, pb_=b0 + q4, M=M: e.matmul(ps[pb_][0:M, :], lhsT=Dinv[:, t2, 0:M], rhs=BtT[cb_][:, t2l, :],
                                                                                             start=True, stop=True), R=["Dinv", "BtT%d" % cb_], W=[psk[b0 + q4]])
                P.add("dve", lambda e, g4=g4, cb_=cb_, b0=b0, M=M, zo=zo: e.tensor_tensor(out=zo[cb_][0:M, g4 * 4:(g4 + 1) * 4, :].rearrange("p a b -> p (a b)"),
                                                                                 in0=psall[0:M, b0 * 512:(b0 + 4) * 512],
                                                                                 in1=gch[cb_][0:M, g4 * 4:(g4 + 1) * 4, :].rearrange("p a b -> p (a b)"), op=ALU.mult),
                      R=[psk[b0 + k] for k in range(4)] + ["gch%d" % cb_], W=["zo%d_%d" % (cb_, g4)])
            P.add("act", lambda e, tc=tc, cb_=cb_, M=M, dst=dst, zo=zo: e.dma_start(out=dst[0:M, tc * 8:(tc + 1) * 8, :], in_=zo[cb_][0:M, :, :]),
                  R=["zo%d_%d" % (cb_, k) for k in range(2)], W=["%s_%d" % (dkey, tc)], dma="zo%d" % cb_)
        P.barrier()
        P.release()
    P.barrier()
    P.release()
    if "z1" in dbg:
        tz1 = dbg_out("z1", [S, 512])
        P.mark()
        tz = P.sb([128, 512], F32, "tz")
        tzb = P.sb([128, 512], BF16, "tzb")
        for i in range(NT):
            P.add("sp", lambda e, i=i: e.dma_start(out=tzb, in_=z1_d[i * 128:(i + 1) * 128, :]), R=Z1K, W=["tzb"], dma="dbg0")
            P.add("dve", lambda e: e.tensor_copy(out=tz, in_=tzb), R=["tzb"], W=["tz"])
            P.add("sp", lambda e, i=i: e.dma_start(out=tz1[i * 128:(i + 1) * 128, :], in_=tz), R=["tz"], W=["dbgo"], dma="dbg1")
        P.barrier()
        P.release()
    if "h_out" in dbg:
        tho = dbg_out("h_out", [OWN, 512])
        P.mark()
        tz_ = P.sb([128, 512], F32, "tz_")
        for i in range(NTO):
            P.add("sp", lambda e, i=i: e.dma_start(out=tz_, in_=hout_d[i * 128:(i + 1) * 128, :]), R=["hout_d_%d" % k for k in range(8)], W=["tz_"], dma="dbg0")
            P.add("sp", lambda e, i=i: e.dma_start(out=tho[i * 128:(i + 1) * 128, :], in_=tz_), R=["tz_"], W=["dbgo"], dma="dbg1")
        P.barrier()
        P.release()


def hyena_tables(half):
    N = 8192
    f1 = np.arange(64, dtype=np.float64)
    s1 = np.arange(64, dtype=np.float64)
    s2 = np.arange(64, dtype=np.float64)
    th = 2 * np.pi * (f1[None, None, :] + 0.5) * (64 * s1[:, None, None] + s2[None, :, None]) / N
    D0 = np.concatenate([np.cos(th), -np.sin(th)], axis=2)
    perm = (np.arange(64) + 32 * half) % 64
    Dc = D0[perm]
    f2 = np.arange(64, dtype=np.float64)
    ph = 2 * np.pi * np.outer(s2, f2) / 64
    c, s_ = np.cos(ph), np.sin(ph)
    F2a = np.block([[c, -s_], [s_, c]])
    F2b = np.block([[s_, c], [-c, s_]])
    G = np.block([[c, s_], [-s_, c]])
    thi = 2 * np.pi * (f1[:, None, None] + 0.5) * (64 * s1[None, None, :] + s2[None, :, None]) / N
    Dinv0 = np.concatenate([np.cos(thi), -np.sin(thi)], axis=0) * (2.0 / N)
    Dinv = Dinv0[:, :, perm]
    sgn = np.ones((128, 1)); sgn[64:] = -1
    L = S
    t = np.arange(L, dtype=np.float32)
    t01 = t / np.float32(L)
    bands = np.linspace(1e-4, 15, 16, dtype=np.float32)
    ang = (np.float32(2.0 * math.pi) * t[:, None] * bands[None, :] / np.float32(L)).astype(np.float32)
    z = np.concatenate([t01[:, None], np.cos(ang), -np.sin(ang)], axis=-1).astype(np.float32)
    nt01 = (-t01).reshape(NT, 128).T
    f = np.float32
    return {
        "hy_D0": np.ascontiguousarray(D0.reshape(64, 64 * 128).astype(f)), "hy_Dc": np.ascontiguousarray(Dc.reshape(64, 64 * 128).astype(f)),
        "hy_F2a": np.ascontiguousarray(F2a.astype(f)), "hy_F2b": np.ascontiguousarray(F2b.astype(f)), "hy_G": np.ascontiguousarray(G.astype(f)),
        "hy_Dinv": np.ascontiguousarray(Dinv.reshape(128, 64 * 64).astype(f)), "hy_sgn": sgn.astype(f),
        "hy_zT": np.ascontiguousarray(z.T), "hy_nt01": np.ascontiguousarray(nt01.astype(f)),
    }


def host_inputs(inputs, core):
    b, half = divmod(core, 2)
    f32 = np.float32
    x = np.asarray(inputs["x"], dtype=f32)[b]
    own = slice(half * OWN, (half + 1) * OWN)
    oth = slice((1 - half) * OWN, (2 - half) * OWN)
    pos = np.concatenate([np.arange(S)[own], np.arange(S)[oth]])
    m = {}
    m["x_rot"] = np.ascontiguousarray(np.concatenate([x[own], x[oth]], axis=0))
    m["mem_b"] = np.ascontiguousarray(np.asarray(inputs["mem"], dtype=f32)[b])
    for k in ("mix_norm_g", "q_norm_g", "kv_norm_g", "hy_conv_b", "attn_out_g", "hy_out_g", "cross_norm_g",
              "mem_norm_g", "ffn_norm_g"):
        m[k] = np.ascontiguousarray(np.asarray(inputs[k], dtype=f32).reshape(1, -1))
    m["final_norm_g"] = np.ascontiguousarray(np.asarray(inputs["final_norm_g"], dtype=f32).reshape(1, -1))
    for k in ("w_in", "w_uq", "w_ukv", "hy_conv_w", "w_out", "w_mq", "w_mkv", "w_mo"):
        m[k] = np.ascontiguousarray(np.asarray(inputs[k], dtype=f32)[0])
    m["w_route"] = np.ascontiguousarray(np.concatenate([np.asarray(inputs["w_route_group"], f32)[0],
                                                        np.asarray(inputs["w_route_expert"], f32)[0]], axis=1))
    m["b_route"] = np.ascontiguousarray(np.concatenate([np.asarray(inputs["b_route_group"], f32)[0],
                                                        np.asarray(inputs["b_route_expert"], f32)[0]], axis=0).reshape(1, 36))
    m["w_gate"] = np.ascontiguousarray(np.asarray(inputs["w_gate"], f32)[0].reshape(32, D, 256))
    m["w_up"] = np.ascontiguousarray(np.asarray(inputs["w_up"], f32)[0].reshape(32, D, 256))
    m["w_down"] = np.ascontiguousarray(np.asarray(inputs["w_down"], f32)[0].reshape(32, 256, D))
    m["ident"] = np.eye(128, dtype=f32)
    inv = (10000.0 ** (-np.arange(16, dtype=np.float64) / 16)).astype(f32)
    ang = pos.astype(f32)[:, None] * inv[None, :]
    cs = np.concatenate([np.cos(ang), np.sin(ang)], axis=1).astype(f32)
    m["rope_cs"] = np.ascontiguousarray(cs.reshape(NT, 128, 32).transpose(1, 0, 2).reshape(128, NT * 32))
    m.update(hyena_tables(half))
    m["hy_cols"] = np.ascontiguousarray(np.stack([np.asarray(inputs["hy_b1"], f32)[0], np.asarray(inputs["hy_b2"], f32)[0],
                                                  np.asarray(inputs["hy_freq"], f32)[0, 0], np.asarray(inputs["hy_freq"], f32)[0, 1]], axis=1))
    for k in ("hy_w1", "hy_w2", "hy_w3"):
        m[k] = np.ascontiguousarray(np.asarray(inputs[k], f32)[0])
    m["hy_b3"] = np.ascontiguousarray(np.asarray(inputs["hy_b3"], f32).reshape(1, 2048))
    m["hy_decay"] = np.ascontiguousarray(np.asarray(inputs["hy_decay"], f32).reshape(1, 2048))
    m["hy_skip"] = np.ascontiguousarray(np.asarray(inputs["hy_skip"], f32).reshape(1, 1024))
    cw_ = np.asarray(inputs["hy_conv_w"], f32)[0]
    m["hy_conv_wT"] = np.ascontiguousarray(cw_.reshape(3, 12, 128).transpose(2, 1, 0).reshape(128, 36))
    m["hy_conv_bT"] = np.ascontiguousarray(np.asarray(inputs["hy_conv_b"], f32)[0].reshape(12, 128).T)
    hm = np.zeros((128, 2), f32)
    hm[:, 0] = half
    hm[:, 1] = 1 - half
    m["halfmask"] = hm
    return m


def kernel(**inputs):
    n = 8
    nc, _ = build()
    in_maps = [host_inputs(inputs, c) for c in range(n)]
    res = run_bass_kernel_spmd(nc, in_maps, core_ids=list(range(n)))
    out = np.zeros((4, S, D), np.float32)
    for c in range(n):
        b, half = divmod(c, 2)
        out[b, half * OWN:(half + 1) * OWN] = res.results[c]["out"]
    return out
```
